# Optimizing a Trainium2 kernel written in Bass

```python
import math
import jax, jax.numpy as jnp
from jax import lax
import numpy as np

D_MODEL = 1024
BATCH = 4
SEQ = 4096
DEPTH = 1

CONV_WIDTH = 512
CONV_KERNEL = 31
N_HEADS = 8
HEAD_DIM = 64
ATTN_WIDTH = N_HEADS * HEAD_DIM
IDX_HEADS = 8
IDX_DIM = 64
TOPK_MAX = 256
Q_BLOCK = 128
ROPE_THETA = 10000.0
N_GROUPS = 4
EXPERTS_PER_GROUP = 4
N_EXPERTS = N_GROUPS * EXPERTS_PER_GROUP
TOP_K_EXPERTS = 2
D_EXPERT = 256
EPS = 1e-6
IN_SIZES = (2 * CONV_WIDTH, ATTN_WIDTH, ATTN_WIDTH, ATTN_WIDTH,
            IDX_HEADS * IDX_DIM, IDX_DIM, IDX_HEADS, D_MODEL, D_MODEL)
IN_COLS = sum(IN_SIZES)

kernel_name = "hybrid_conv_dsa_hmoe_block"


def rms_norm(x, g):
    xf = x.astype(jnp.float32)
    y = xf * lax.rsqrt(jnp.mean(xf * xf, axis=-1, keepdims=True) + EPS)
    return (y * g.astype(jnp.float32)).astype(x.dtype)


def rope_tables(positions, dim, dtype):
    inv = 1.0 / (ROPE_THETA ** (jnp.arange(0, dim, 2, dtype=jnp.float32) / dim))
    ang = positions.astype(jnp.float32)[..., None] * inv
    return jnp.cos(ang).astype(dtype), jnp.sin(ang).astype(dtype)


def apply_rope(x, cos, sin):
    x1, x2 = jnp.split(x, 2, axis=-1)
    return jnp.concatenate([x1 * cos - x2 * sin, x2 * cos + x1 * sin], axis=-1)


def conformer_conv(u, w_dw, b_dw, ln_g, ln_b, w_out):
    a, b = jnp.split(u, 2, axis=-1)
    g = a * jax.nn.sigmoid(b)
    c = lax.conv_general_dilated(g, w_dw.astype(g.dtype), window_strides=(1,),
                                 padding=[(CONV_KERNEL - 1, 0)],
                                 dimension_numbers=('NWC', 'WIO', 'NWC'),
                                 feature_group_count=CONV_WIDTH) + b_dw
    cf = c.astype(jnp.float32)
    mu = jnp.mean(cf, axis=-1, keepdims=True)
    var = jnp.mean(jnp.square(cf - mu), axis=-1, keepdims=True)
    n = ((cf - mu) * lax.rsqrt(var + EPS) * ln_g + ln_b).astype(u.dtype)
    return jax.nn.silu(n) @ w_out


def dsa_attention(q, k, v, qi, ki, wi):
    B, S = q.shape[0], q.shape[1]
    n_blocks = S // Q_BLOCK
    topk = min(TOPK_MAX, S // 4)
    key_pos = jnp.arange(S)
    scale = HEAD_DIM ** -0.5

    def block(i):
        start = i * Q_BLOCK
        qb = lax.dynamic_slice_in_dim(q, start, Q_BLOCK, axis=1)
        qib = lax.dynamic_slice_in_dim(qi, start, Q_BLOCK, axis=1)
        wib = lax.dynamic_slice_in_dim(wi, start, Q_BLOCK, axis=1)
        t = start + jnp.arange(Q_BLOCK)
        causal = key_pos[None, :] <= t[:, None]
        rel = jax.nn.relu(jnp.einsum('bqhd,bsd->bqhs', qib, ki))
        scores = jnp.einsum('bqhs,bqh->bqs', rel, wib).astype(jnp.float32)
        scores = jnp.where(causal[None], scores, -jnp.inf)
        _, idx = lax.top_k(scores, topk)
        valid = idx <= t[None, :, None]
        k_sel = jax.vmap(lambda kb, ib: kb[ib])(k, idx)
        v_sel = jax.vmap(lambda vb, ib: vb[ib])(v, idx)
        logits = jnp.einsum('bqhd,bqkhd->bqhk', qb, k_sel).astype(jnp.float32) * scale
        logits = jnp.where(valid[:, :, None, :], logits, -jnp.inf)
        p = jax.nn.softmax(logits, axis=-1).astype(v.dtype)
        return jnp.einsum('bqhk,bqkhd->bqhd', p, v_sel)

    out = lax.map(block, jnp.arange(n_blocks))
    return jnp.transpose(out, (1, 0, 2, 3, 4)).reshape(B, S, N_HEADS * HEAD_DIM)


def hier_moe(h, w_rg, b_rg, w_re, b_re, w_gate, w_up, w_down):
    B, S, D = h.shape
    hf = h.reshape(-1, D)
    T = hf.shape[0]
    g_prob = jax.nn.softmax((hf @ w_rg + b_rg).astype(jnp.float32), axis=-1)
    p_g, gi = lax.top_k(g_prob, 1)
    e_logits = (jnp.einsum('td,dge->tge', hf, w_re) + b_re).astype(jnp.float32)
    e_sel = jnp.take_along_axis(e_logits, gi[:, :, None], axis=1)[:, 0]
    p_e, ei = lax.top_k(jax.nn.softmax(e_sel, axis=-1), TOP_K_EXPERTS)
    p_e = p_e / jnp.sum(p_e, axis=-1, keepdims=True)
    comb = p_g * p_e
    eid = gi * EXPERTS_PER_GROUP + ei
    gate_w = jnp.zeros((T, N_EXPERTS), jnp.float32).at[jnp.arange(T)[:, None], eid].add(comb)
    gate_w = gate_w.astype(h.dtype)
    hid = jax.nn.silu(jnp.einsum('td,edf->tef', hf, w_gate)) * jnp.einsum('td,edf->tef', hf, w_up)
    hid = hid * gate_w[:, :, None]
    y = jnp.einsum('tef,efd->td', hid, w_down)
    return y.reshape(B, S, D)


def setup_inputs(seed: int = 0) -> dict:
    key = jax.random.key(seed)
    ks = jax.random.split(key, 24)
    f32 = jnp.float32
    nrm = lambda k, shape, fan_in: jax.random.normal(k, shape, f32) * (fan_in ** -0.5)
    L = DEPTH
    return {
        "x": jax.random.normal(ks[0], (BATCH, SEQ, D_MODEL), f32),
        "positions": jnp.broadcast_to(jnp.arange(SEQ, dtype=jnp.int32), (BATCH, SEQ)),
        "g_mix": 1.0 + 0.02 * jax.random.normal(ks[1], (L, D_MODEL), f32),
        "w_in": nrm(ks[2], (L, D_MODEL, IN_COLS), D_MODEL),
        "w_dw": nrm(ks[3], (L, CONV_KERNEL, 1, CONV_WIDTH), CONV_KERNEL),
        "b_dw": 0.02 * jax.random.normal(ks[4], (L, CONV_WIDTH), f32),
        "ln_g": 1.0 + 0.02 * jax.random.normal(ks[5], (L, CONV_WIDTH), f32),
        "ln_b": 0.02 * jax.random.normal(ks[6], (L, CONV_WIDTH), f32),
        "w_conv_out": nrm(ks[7], (L, CONV_WIDTH, D_MODEL), CONV_WIDTH),
        "w_attn_out": nrm(ks[8], (L, ATTN_WIDTH, D_MODEL), ATTN_WIDTH),
        "w_o": nrm(ks[9], (L, D_MODEL, D_MODEL), D_MODEL),
        "g_ffn": 1.0 + 0.02 * jax.random.normal(ks[10], (L, D_MODEL), f32),
        "w_rg": nrm(ks[11], (L, D_MODEL, N_GROUPS), D_MODEL),
        "b_rg": 0.01 * jax.random.normal(ks[12], (L, N_GROUPS), f32),
        "w_re": nrm(ks[13], (L, D_MODEL, N_GROUPS, EXPERTS_PER_GROUP), D_MODEL),
        "b_re": 0.01 * jax.random.normal(ks[14], (L, N_GROUPS, EXPERTS_PER_GROUP), f32),
        "w_gate": nrm(ks[15], (L, N_EXPERTS, D_MODEL, D_EXPERT), D_MODEL),
        "w_up": nrm(ks[16], (L, N_EXPERTS, D_MODEL, D_EXPERT), D_MODEL),
        "w_down": nrm(ks[17], (L, N_EXPERTS, D_EXPERT, D_MODEL), D_EXPERT),
        "g_final": 1.0 + 0.02 * jax.random.normal(ks[18], (D_MODEL,), f32),
    }


def reference(x, positions, g_mix, w_in, w_dw, b_dw, ln_g, ln_b, w_conv_out, w_attn_out,
              w_o, g_ffn, w_rg, b_rg, w_re, b_re, w_gate, w_up, w_down, g_final):
    B, S, _ = x.shape
    cos, sin = rope_tables(positions, HEAD_DIM, x.dtype)
    cos_h, sin_h = cos[:, :, None, :], sin[:, :, None, :]
    idx_scale = (IDX_HEADS ** -0.5) * (IDX_DIM ** -0.5)
    split_pts = np.cumsum(IN_SIZES)[:-1].tolist()
    for l in range(DEPTH):
        h = rms_norm(x, g_mix[l])
        z = h @ w_in[l]
        u_conv, q, k, v, qi, ki, wi, gc, ga = jnp.split(z, split_pts, axis=-1)
        q = apply_rope(q.reshape(B, S, N_HEADS, HEAD_DIM), cos_h, sin_h)
        k = apply_rope(k.reshape(B, S, N_HEADS, HEAD_DIM), cos_h, sin_h)
        v = v.reshape(B, S, N_HEADS, HEAD_DIM)
        qi = apply_rope(qi.reshape(B, S, IDX_HEADS, IDX_DIM), cos_h, sin_h)
        ki = apply_rope(ki, cos, sin)
        wi = wi * idx_scale
        y_conv = conformer_conv(u_conv, w_dw[l], b_dw[l], ln_g[l], ln_b[l], w_conv_out[l])
        y_attn = dsa_attention(q, k, v, qi, ki, wi) @ w_attn_out[l]
        m = jax.nn.sigmoid(gc) * y_conv + jax.nn.sigmoid(ga) * y_attn
        x = x + m @ w_o[l]
        h2 = rms_norm(x, g_ffn[l])
        x = x + hier_moe(h2, w_rg[l], b_rg[l], w_re[l], b_re[l], w_gate[l], w_up[l], w_down[l])
    return rms_norm(x, g_final)
```

```python
from contextlib import ExitStack
import numpy as np
import concourse.bass as bass
import concourse.mybir as mybir
from concourse.bass_utils import run_bass_kernel_spmd

F32 = mybir.dt.float32
BF16 = mybir.dt.bfloat16
I32 = mybir.dt.int32
ALU = mybir.AluOpType
AF = mybir.ActivationFunctionType
AX = mybir.AxisListType

D = 1024
S = 4096
NB = 32
NS = 16
EPS = 1e-6
NIT1 = 10
NIT2 = 10
NIT = 18
TOPK = 256
NEG = -1.0e30
C_U, C_Q, C_K, C_V, C_QI, C_KI, C_WI, C_GC, C_GA = 0, 1024, 1536, 2048, 2560, 3072, 3136, 3144, 4168
IDX_SCALE = float((8 ** -0.5) * (64 ** -0.5))
ATT_SCALE = float(64 ** -0.5)


class Buf:
    __slots__ = ("name", "t", "lw", "rd", "dsem", "dcnt")

    def __init__(self, name, t=None):
        self.name = name
        self.t = t
        self.lw = None
        self.rd = {}
        self.dsem = None
        self.dcnt = 0

    def __getitem__(self, key):
        return self.t[key]


class Eng:
    def __init__(self, k, name, eng):
        self.name = name
        self.eng = eng
        self.sem = k.new_sem("e_" + name)
        self.cnt = 0
        self.seen = {}

    def wait_tok(self, tok):
        if tok is None:
            return
        sem, val, _ = tok
        key = id(sem)
        if self.seen.get(key, 0) >= val:
            return
        self.eng.wait_ge(sem, val)
        self.seen[key] = val


class K:
    def __init__(self, nc, stack):
        self.nc = nc
        self.stack = stack
        self.pe = Eng(self, "pe", nc.tensor)
        self.act = Eng(self, "act", nc.scalar)
        self.dve = Eng(self, "dve", nc.vector)
        self.pool = Eng(self, "pool", nc.gpsimd)
        self.sp = Eng(self, "sp", nc.sync)
        self.engs = [self.pe, self.act, self.dve, self.pool, self.sp]
        self.dma_bufs = []
        self.nins = 0
        self.banks = []
        self.bank_i = 0
        self.pinned = set()

    def new_sem(self, name):
        return self.stack.enter_context(self.nc.semaphore(name))

    def sb(self, name, shape, dt, stack=None):
        t = (stack or self.stack).enter_context(self.nc.sbuf_tensor("s_" + name, list(shape), dt))
        return Buf(name, t)

    def mk_banks(self):
        for i in range(8):
            t = self.stack.enter_context(self.nc.psum_tensor("bank%d" % i, [128, 512], F32))
            self.banks.append(Buf("bank%d" % i, t))

    def bank(self, pin=False):
        for _ in range(16):
            b = self.banks[self.bank_i % 8]
            self.bank_i += 1
            if b.name not in self.pinned:
                if pin:
                    self.pinned.add(b.name)
                return b
        raise RuntimeError("no psum bank")

    def unpin(self, b):
        self.pinned.discard(b.name)

    def _deps(self, e, reads, writes):
        for b in reads:
            if b.lw is not None:
                e.wait_tok(b.lw)
        for b in writes:
            if b.lw is not None and b.lw[2] != e.name:
                e.wait_tok(b.lw)
            for en, tok in b.rd.items():
                if en != e.name:
                    e.wait_tok(tok)

    def _commit(self, tok, reads, writes):
        for b in reads:
            b.rd[tok[2]] = tok
        for b in writes:
            b.lw = tok
            b.rd = {}

    def op(self, e, fn, reads=(), writes=()):
        self._deps(e, reads, writes)
        ins = fn(e.eng)
        e.cnt += 1
        ins.then_inc(e.sem, 1)
        tok = (e.sem, e.cnt, e.name)
        self._commit(tok, reads, writes)
        self.nins += 1
        return tok

    def dma(self, e, out, in_, reads=(), writes=(), sembuf=None):
        self._deps(e, reads, writes)
        sb_ = sembuf if sembuf is not None else (writes[0] if writes else reads[0])
        if sb_.dsem is None:
            sb_.dsem = self.new_sem("d_" + sb_.name)
            self.dma_bufs.append(sb_)
        ins = e.eng.dma_start(out=out, in_=in_)
        sb_.dcnt += 16
        ins.then_inc(sb_.dsem, 16)
        tok = (sb_.dsem, sb_.dcnt, "dma_" + sb_.name)
        self._commit(tok, reads, writes)
        self.nins += 1
        return tok

    def barrier(self):
        toks = [(e.sem, e.cnt, e.name) for e in self.engs if e.cnt > 0]
        toks += [(b.dsem, b.dcnt, "dma") for b in self.dma_bufs]
        for e in self.engs:
            for t in toks:
                if t[2] != e.name:
                    e.wait_tok(t)


def bf(ap):
    return ap.bitcast(BF16)


def build_nc(phases=("A1", "A2", "B", "C"), debug=False, nslots=NS, bstop=9):
    nc = bass.Bass("TRN2", target_bir_lowering=False)

    def din(name, shape, dt=F32):
        return nc.dram_tensor(name, list(shape), dt, kind="ExternalInput").ap()

    def dscr(name, shape, dt):
        return nc.dram_tensor(name, list(shape), dt, kind="Internal").ap()

    x_full = din("x_full", [S, D])
    x_own = din("x_own", [NS * 128, D])
    x_halo = din("x_halo", [NS * 32, D])
    pos_full = din("pos_full", [128, NB], I32)
    pos_own = din("pos_own", [128, NS], I32)
    invf = din("invf", [128, 32])
    cmask_d = din("cmask", [128, NS, 256])
    ident_d = din("ident", [128, 128])
    w_in = din("w_in", [D, 5192])
    wdw_d = din("wdw", [128, 4, 31])
    bdw_d = din("bdw", [128, 4])
    lng_d = din("lng", [128, 4])
    lnb_d = din("lnb", [128, 4])
    gmix_d = din("gmix", [128, 8])
    gffn_d = din("gffn", [128, 8])
    gfin_d = din("gfin", [128, D])
    br_d = din("b_r", [128, 20])
    wr_d = din("w_r", [D, 20])
    wco_d = din("w_conv_out", [512, D])
    wao_d = din("w_attn_out", [512, D])
    wo_d = din("w_o", [D, D])
    wg_d = din("w_gate", [16, D, 256])
    wu_d = din("w_up", [16, D, 256])
    wd_d = din("w_down", [16, 256, D])
    out_d = nc.dram_tensor("out", [NS * 128, D], F32, kind="ExternalOutput").ap()

    qT_d = dscr("qT_d", [NS, 128, 512], BF16)
    qiT_d = dscr("qiT_d", [NS, 128, 512], BF16)
    mc_d = dscr("mc_d", [NS, 128, 1024], BF16)
    sga_d = dscr("sga_d", [NS, 128, 1024], BF16)
    x1_d = dscr("x1_d", [NS, 128, D], F32)
    h2T_d = dscr("h2T_d", [NS, 128, 1024], BF16)
    dbg = {}
    if debug:
        dbg["kT"] = nc.dram_tensor("dbg_kT", [128, 4, S], BF16, kind="ExternalOutput").ap()
        dbg["v"] = nc.dram_tensor("dbg_v", [128, NB, 8 * 66], BF16, kind="ExternalOutput").ap()
        dbg["kiT"] = nc.dram_tensor("dbg_kiT", [128, S], BF16, kind="ExternalOutput").ap()
        dbg["qT"] = nc.dram_tensor("dbg_qT", [NS, 128, 512], BF16, kind="ExternalOutput").ap()
        dbg["qiT"] = nc.dram_tensor("dbg_qiT", [NS, 128, 512], BF16, kind="ExternalOutput").ap()
        dbg["mc"] = nc.dram_tensor("dbg_mc", [NS, 128, 1024], BF16, kind="ExternalOutput").ap()
        dbg["sga"] = nc.dram_tensor("dbg_sga", [NS, 128, 1024], BF16, kind="ExternalOutput").ap()
        dbg["wi"] = nc.dram_tensor("dbg_wi", [128, NS * 8], F32, kind="ExternalOutput").ap()
        dbg["x1"] = nc.dram_tensor("dbg_x1", [NS, 128, D], F32, kind="ExternalOutput").ap()
        dbg["gw"] = nc.dram_tensor("dbg_gw", [128, NS * 16], F32, kind="ExternalOutput").ap()
        dbg["sc"] = nc.dram_tensor("dbg_sc", [NS, 128, S], F32, kind="ExternalOutput").ap()
        dbg["thr"] = nc.dram_tensor("dbg_thr", [NS, 128, 1], F32, kind="ExternalOutput").ap()
        dbg["attn"] = nc.dram_tensor("dbg_attn", [NS, 128, 512], F32, kind="ExternalOutput").ap()

    with ExitStack() as st:
        k = K(nc, st)
        pe, act, dve, pool, sp = k.pe, k.act, k.dve, k.pool, k.sp
        k.mk_banks()

        ident = k.sb("ident", [128, 128], BF16)
        identf = k.sb("identf", [128, 128], F32)
        wi_all = k.sb("wi_all", [128, NS * 8], F32)
        gw_all = k.sb("gw_all", [128, NS * 16], F32)
        gmix = k.sb("gmix", [128, 8], F32)
        gffn = k.sb("gffn", [128, 8], F32)
        ssL = [k.sb("ss%d" % i, [128, 1], F32) for i in range(2)]
        rsL = [k.sb("rs%d" % i, [128, 1], F32) for i in range(2)]
        hbL = [k.sb("hb%d" % i, [128, D], BF16) for i in range(2)]
        nrm_i = [0]

        k.dma(sp, identf[:], ident_d, writes=[identf])
        k.dma(pool, ident[:], ident_d, writes=[ident])
        k.dma(sp, gmix[:], gmix_d, writes=[gmix])
        k.dma(sp, gffn[:], gffn_d, writes=[gffn])

        def rope_tables(pos_d, n, cosT, sinT, stk):
            pi_ = k.sb("pos_i%d" % n, [128, n], I32, stk)
            pf = k.sb("pos_f%d" % n, [128, n], F32, stk)
            iv = k.sb("invf%d" % n, [128, 32], F32, stk)
            ang = k.sb("ang%d" % n, [128, n, 32], F32, stk)
            tmp = k.sb("angt%d" % n, [128, n, 32], F32, stk)
            k.dma(sp, pi_[:], pos_d, writes=[pi_])
            k.dma(sp, iv[:], invf, writes=[iv])
            k.op(dve, lambda e: e.tensor_copy(pf[:], pi_[:]), reads=[pi_], writes=[pf])
            k.op(dve, lambda e: e.tensor_tensor(ang[:], pf[:].unsqueeze(2).to_broadcast([128, n, 32]),
                                                iv[:].unsqueeze(1).to_broadcast([128, n, 32]), ALU.mult),
                 reads=[pf, iv], writes=[ang])
            ki_ = k.sb("angk%d" % n, [128, n, 32], I32, stk)
            kf_ = k.sb("angf%d" % n, [128, n, 32], F32, stk)
            two_pi = float(2 * np.pi)
            C1 = 6.28125
            C2 = float(2 * np.pi - 6.28125)
            PI_SAFE = 3.1415925

            def sin_of(dst, shift):
                if shift != 0.0:
                    k.op(dve, lambda e: e.tensor_scalar(tmp[:], ang[:], shift, None, ALU.add), reads=[ang], writes=[tmp])
                    src = tmp
                else:
                    src = ang
                k.op(dve, lambda e: e.tensor_scalar(kf_[:], src[:], 1.0 / two_pi, None, ALU.mult),
                     reads=[src], writes=[kf_])
                k.op(dve, lambda e: e.tensor_copy(ki_[:], kf_[:]), reads=[kf_], writes=[ki_])
                k.op(dve, lambda e: e.tensor_copy(kf_[:], ki_[:]), reads=[ki_], writes=[kf_])
                k.op(dve, lambda e: e.scalar_tensor_tensor(tmp[:], kf_[:], -C1, src[:], ALU.mult, ALU.add),
                     reads=[kf_, src], writes=[tmp])
                k.op(dve, lambda e: e.scalar_tensor_tensor(tmp[:], kf_[:], -C2, tmp[:], ALU.mult, ALU.add),
                     reads=[kf_, tmp], writes=[tmp])
                k.op(dve, lambda e: e.tensor_scalar(kf_[:], tmp[:], float(np.pi), -two_pi, ALU.is_gt, ALU.mult),
                     reads=[tmp], writes=[kf_])
                k.op(dve, lambda e: e.tensor_tensor(tmp[:], tmp[:], kf_[:], ALU.add), reads=[tmp, kf_], writes=[tmp])
                k.op(dve, lambda e: e.tensor_scalar(kf_[:], tmp[:], -float(np.pi), two_pi, ALU.is_lt, ALU.mult),
                     reads=[tmp], writes=[kf_])
                k.op(dve, lambda e: e.tensor_tensor(tmp[:], tmp[:], kf_[:], ALU.add), reads=[tmp, kf_], writes=[tmp])
                k.op(dve, lambda e: e.tensor_scalar(tmp[:], tmp[:], -PI_SAFE, PI_SAFE, ALU.max, ALU.min),
                     reads=[tmp], writes=[tmp])
                k.op(act, lambda e: e.activation(dst[:], tmp[:], AF.Sin), reads=[tmp], writes=[dst])

            sin_of(sinT, 0.0)
            sin_of(cosT, float(0.5 * np.pi))

        hbN = 4
        hbX = [k.sb("hbx%d" % i, [128, D], BF16) for i in range(hbN - 2)]

        def norm_pre(xt, n):
            hbs = hbL + hbX
            ss, rs, hb = ssL[nrm_i[0] % 2], rsL[nrm_i[0] % 2], hbs[nrm_i[0] % hbN]
            junk = hb
            nrm_i[0] += 1
            k.op(dve, lambda e: e.memset(ss[0:n, :], 0.0), writes=[ss])
            k.op(act, lambda e: e.activation(junk[0:n, :], xt[0:n, :], AF.Square, accum_out=ss[0:n, 0:1]),
                 reads=[xt, ss], writes=[junk, ss])
            k.op(act, lambda e: e.activation(rs[0:n, :], ss[0:n, :], AF.Sqrt, bias=EPS, scale=1.0 / D),
                 reads=[ss], writes=[rs])
            k.op(dve, lambda e: e.reciprocal(rs[0:n, :], rs[0:n, :]), reads=[rs], writes=[rs])
            k.op(act, lambda e: e.activation(hb[0:n, :], xt[0:n, :], AF.Copy, scale=rs[0:n, 0:1]),
                 reads=[xt, rs], writes=[hb])
            return hb

        def norm_post(hb, n, gT, hT, hT_ap=None):
            if hT_ap is None:
                hT_ap = hT[:, :, 0:n]
            tp = k.bank()
            tpv = bf(tp[:])[:, 0:8 * 128].rearrange("p (a b) -> p a b", a=8)
            for kc in range(8):
                k.op(pe, lambda e, kc=kc: e.transpose(tpv[:, kc, 0:n], hb[0:n, kc * 128:(kc + 1) * 128],
                                                      ident[0:n, 0:n]),
                     reads=[hb, ident], writes=[tp])
            k.op(dve, lambda e: e.tensor_tensor(hT_ap, tpv[:, :, 0:n],
                                                gT[:].unsqueeze(2).to_broadcast([128, 8, n]), ALU.mult),
                 reads=[tp, gT], writes=[hT])

        def norm_T(xt, n, gT, hT, hT_ap=None):
            hb = norm_pre(xt, n)
            norm_post(hb, n, gT, hT, hT_ap)

        def proj_tok(hT, w, c0, ncols):
            pb = k.bank()
            for kc in range(8):
                k.op(pe, lambda e, kc=kc: e.matmul(pb[:, 0:ncols], hT[:, kc, :], w[:, kc, c0:c0 + ncols],
                                                   start=(kc == 0), stop=(kc == 7)),
                     reads=[hT, w], writes=[pb])
            return pb

        def rope(pb, nh, cosT, sinT, ti, outb, tmpA, tmpB):
            pv = pb[:, 0:nh * 64].rearrange("p (h d) -> p h d", h=nh)
            ov = outb[:, 0:nh * 64].rearrange("p (h d) -> p h d", h=nh)
            av = tmpA[:, 0:nh * 64].rearrange("p (h d) -> p h d", h=nh)
            bv = tmpB[:, 0:nh * 64].rearrange("p (h d) -> p h d", h=nh)
            cb = cosT[:, ti, :].unsqueeze(1).to_broadcast([128, nh, 32])
            sb_ = sinT[:, ti, :].unsqueeze(1).to_broadcast([128, nh, 32])
            k.op(dve, lambda e: e.tensor_tensor(av[:, :, 0:32], pv[:, :, 0:32], cb, ALU.mult),
                 reads=[pb, cosT], writes=[tmpA])
            k.op(dve, lambda e: e.tensor_tensor(av[:, :, 32:64], pv[:, :, 32:64], cb, ALU.mult),
                 reads=[pb, cosT], writes=[tmpA])
            k.op(dve, lambda e: e.tensor_tensor(bv[:, :, 0:32], pv[:, :, 32:64], sb_, ALU.mult),
                 reads=[pb, sinT], writes=[tmpB])
            k.op(dve, lambda e: e.tensor_tensor(bv[:, :, 32:64], pv[:, :, 0:32], sb_, ALU.mult),
                 reads=[pb, sinT], writes=[tmpB])
            k.op(dve, lambda e: e.tensor_tensor(ov[:, :, 0:32], av[:, :, 0:32], bv[:, :, 0:32], ALU.subtract),
                 reads=[tmpA, tmpB], writes=[outb])
            k.op(dve, lambda e: e.tensor_tensor(ov[:, :, 32:64], av[:, :, 32:64], bv[:, :, 32:64], ALU.add),
                 reads=[tmpA, tmpB], writes=[outb])

        w_in_v = w_in.rearrange("(k p) c -> p k c", p=128)
        def phase_A1():
            with ExitStack() as s1:
                cosF = k.sb("cosF", [128, NB, 32], F32, s1)
                sinF = k.sb("sinF", [128, NB, 32], F32, s1)
                rope_tables(pos_full, NB, cosF, sinF, s1)
                wA = k.sb("wA1", [128, 8, 1088], BF16, s1)
                for kc in range(8):
                    k.dma(pool, wA[:, kc, 0:1024], w_in_v[:, kc, C_K:C_K + 1024], writes=[wA])
                    k.dma(pool, wA[:, kc, 1024:1088], w_in_v[:, kc, C_KI:C_KI + 64], writes=[wA])
                xt2 = [k.sb("xtA%d" % i, [128, D], F32, s1) for i in range(2)]
                hT2 = [k.sb("hTA%d" % i, [128, 8, 128], BF16, s1) for i in range(2)]
                krL = [k.sb("kr%d" % i, [128, 512], BF16, s1) for i in range(2)]
                kirL = [k.sb("kir%d" % i, [128, 128], BF16, s1) for i in range(2)]
                tAL = [k.sb("tA%d" % i, [128, 512], F32, s1) for i in range(2)]
                tBL = [k.sb("tB%d" % i, [128, 512], F32, s1) for i in range(2)]
                tCL = [k.sb("tC%d" % i, [128, 64], F32, s1) for i in range(2)]
                tDL = [k.sb("tD%d" % i, [128, 64], F32, s1) for i in range(2)]
                hbs1 = {}

                def pre1(ti):
                    k.dma(sp, xt2[ti % 2][:], x_full[ti * 128:(ti + 1) * 128, :], writes=[xt2[ti % 2]])
                    hbs1[ti] = norm_pre(xt2[ti % 2], 128)

                def post1(ti):
                    norm_post(hbs1.pop(ti), 128, gmix, hT2[ti % 2])

                pre1(0)
                post1(0)
                pre1(1)
                for ti in range(NB):
                    if ti + 1 < NB:
                        post1(ti + 1)
                    hT = hT2[ti % 2]
                    kr, kir, tA, tB = krL[ti % 2], kirL[ti % 2], tAL[ti % 2], tBL[ti % 2]
                    tC, tD = tCL[ti % 2], tDL[ti % 2]
                    pk = proj_tok(hT, wA, 0, 512)
                    pv = proj_tok(hT, wA, 512, 512)
                    pki = proj_tok(hT, wA, 1024, 64)
                    if ti + 2 < NB:
                        pre1(ti + 2)
                    rope(pk, 8, cosF, sinF, ti, kr, tA, tB)
                    Vv = V[:, ti, :].rearrange("p (h d) -> p h d", h=8)
                    k.op(act, lambda e: e.activation(Vv[:, :, 0:64],
                                                     pv[:, 0:512].rearrange("p (h d) -> p h d", h=8), AF.Copy),
                         reads=[pv], writes=[V])
                    rope(pki, 1, cosF, sinF, ti, kir, tC, tD)
                    k.op(dve, lambda e, kir=kir: e.tensor_copy(kir[:, 64:128], kir[:, 0:64]), reads=[kir], writes=[kir])
                    tb = k.bank()
                    tbv = bf(tb[:])
                    for pr in range(4):
                        k.op(pe, lambda e, pr=pr: e.transpose(tbv[:, pr * 128:(pr + 1) * 128],
                                                              kr[:, pr * 128:(pr + 1) * 128], ident[:]),
                             reads=[kr, ident], writes=[tb])
                    k.op(pe, lambda e: e.transpose(tbv[:, 512:640], kir[:], ident[:]), reads=[kir, ident], writes=[tb])
                    k.op(act, lambda e: e.activation(KT[:, :, ti * 128:(ti + 1) * 128],
                                                     tbv[:, 0:512].rearrange("p (a b) -> p a b", a=4), AF.Copy),
                         reads=[tb], writes=[KT])
                    k.op(act, lambda e: e.activation(kiT[:, ti * 128:(ti + 1) * 128], tbv[:, 512:640], AF.Copy),
                         reads=[tb], writes=[kiT])
                k.barrier()
        def dbg_A1():
            for pr in range(4):
                k.dma(sp, dbg["kT"][:, pr, :], KT[:, pr, :], reads=[KT])
            for t8 in range(0, NB, 4):
                k.dma(sp, dbg["v"][:, t8:t8 + 4, :], V[:, t8:t8 + 4, :], reads=[V])
            k.dma(sp, dbg["kiT"], kiT[:], reads=[kiT])

        def phase_A2():
            with ExitStack() as s2:
                cosO = k.sb("cosO", [128, NS, 32], F32, s2)
                sinO = k.sb("sinO", [128, NS, 32], F32, s2)
                rope_tables(pos_own, NS, cosO, sinO, s2)
                WU, WQ, WQI, WWI, WGC, WGA = 0, 1024, 1536, 2048, 2056, 3080
                wB = k.sb("wB", [128, 8, 4104], BF16, s2)
                for kc in range(8):
                    k.dma(pool, wB[:, kc, WU:WU + 1024], w_in_v[:, kc, C_U:C_U + 1024], writes=[wB])
                    k.dma(pool, wB[:, kc, WQ:WQ + 512], w_in_v[:, kc, C_Q:C_Q + 512], writes=[wB])
                    k.dma(pool, wB[:, kc, WQI:WQI + 512], w_in_v[:, kc, C_QI:C_QI + 512], writes=[wB])
                    k.dma(pool, wB[:, kc, WWI:WWI + 8], w_in_v[:, kc, C_WI:C_WI + 8], writes=[wB])
                    k.dma(pool, wB[:, kc, WGC:WGC + 2048], w_in_v[:, kc, C_GC:C_GC + 2048], writes=[wB])
                wco = k.sb("wco", [128, 4, D], BF16, s2)
                for kc in range(4):
                    k.dma(pool, wco[:, kc, :], wco_d[kc * 128:(kc + 1) * 128, :], writes=[wco])
                wdw = k.sb("wdw", [128, 4, 31], F32, s2)
                bdw = k.sb("bdw", [128, 4], F32, s2)
                lng = k.sb("lng", [128, 4], F32, s2)
                lnb = k.sb("lnb", [128, 4], F32, s2)
                k.dma(sp, wdw[:], wdw_d, writes=[wdw])
                k.dma(sp, bdw[:], bdw_d, writes=[bdw])
                k.dma(sp, lng[:], lng_d, writes=[lng])
                k.dma(sp, lnb[:], lnb_d, writes=[lnb])
                Dg = k.sb("Dg", [128, 124, 128], BF16, s2)
                for c in range(4):
                    for j in range(31):
                        k.op(dve, lambda e, c=c, j=j: e.tensor_scalar(Dg[:, c * 31 + j, :], identf[:],
                                                                       wdw[:, c, j:j + 1], None, ALU.mult),
                             reads=[identf, wdw], writes=[Dg])
                ones = k.sb("ones", [128, 128], F32, s2)
                k.op(pool, lambda e: e.memset(ones[:], 1.0), writes=[ones])
                xt2 = [k.sb("xtB%d" % i, [128, D], F32, s2) for i in range(2)]
                xh2 = [k.sb("xhB%d" % i, [32, D], F32, s2) for i in range(2)]
                hT_L = [k.sb("hTB_%d" % i_, [128, 8, 128], BF16, s2) for i_ in range(2)]
                hTh_L = [k.sb("hThB_%d" % i_, [128, 8, 32], BF16, s2) for i_ in range(2)]
                qr_L = [k.sb("qr_%d" % i_, [128, 512], BF16, s2) for i_ in range(2)]
                qir_L = [k.sb("qir_%d" % i_, [128, 512], BF16, s2) for i_ in range(2)]
                tA_L = [k.sb("tA2_%d" % i_, [128, 512], F32, s2) for i_ in range(2)]
                tB_L = [k.sb("tB2_%d" % i_, [128, 512], F32, s2) for i_ in range(2)]
                qT_s_L = [k.sb("qT_s_%d" % i_, [128, 512], BF16, s2) for i_ in range(2)]
                qiT_s_L = [k.sb("qiT_s_%d" % i_, [128, 512], BF16, s2) for i_ in range(2)]
                sg_L = [k.sb("sgl_%d" % i_, [128, 160], F32, s2) for i_ in range(2)]
                gT_L = [k.sb("gT_%d" % i_, [128, 4, 160], BF16, s2) for i_ in range(2)]
                csb_L = [k.sb("csb_%d" % i_, [128, 4, 128], F32, s2) for i_ in range(2)]
                csq_L = [k.sb("csq_%d" % i_, [128, 4, 128], F32, s2) for i_ in range(2)]
                mean_L = [k.sb("mean_%d" % i_, [128, 128], F32, s2) for i_ in range(2)]
                msq_L = [k.sb("msq_%d" % i_, [128, 128], F32, s2) for i_ in range(2)]
                rstd_L = [k.sb("rstd_%d" % i_, [128, 128], F32, s2) for i_ in range(2)]
                nrm_L = [k.sb("nrm_%d" % i_, [128, 4, 128], F32, s2) for i_ in range(2)]
                snT_L = [k.sb("snT_%d" % i_, [128, 4, 128], BF16, s2) for i_ in range(2)]
                sgc_L = [k.sb("sgc_%d" % i_, [128, 8, 128], F32, s2) for i_ in range(2)]
                mc_s_L = [k.sb("mc_s_%d" % i_, [128, 8, 128], BF16, s2) for i_ in range(2)]
                sga_s_L = [k.sb("sga_s_%d" % i_, [128, 8, 128], BF16, s2) for i_ in range(2)]
                qTd_b = Buf("qTd_b")
                mcd_b = Buf("mcd_b")

                hbs2 = {}

                def pre2(j):
                    k.dma(sp, xt2[j % 2][:], x_own[j * 128:(j + 1) * 128, :], writes=[xt2[j % 2]])
                    k.dma(sp, xh2[j % 2][:], x_halo[j * 32:(j + 1) * 32, :], writes=[xh2[j % 2]])
                    hbs2[j] = (norm_pre(xh2[j % 2], 32), norm_pre(xt2[j % 2], 128))

                def post2(j):
                    a_, b_ = hbs2.pop(j)
                    norm_post(a_, 32, gmix, hTh_L[j % 2])
                    norm_post(b_, 128, gmix, hT_L[j % 2])

                for j in range(nslots):
                    xt = xt2[j % 2]
                    xh = xh2[j % 2]
                    hT, hTh, qr, qir, tA, tB, qT_s, qiT_s, sg, gT, csb, csq, mean, msq, rstd, nrm, snT, sgc, mc_s, sga_s = hT_L[j % 2], hTh_L[j % 2], qr_L[j % 2], qir_L[j % 2], tA_L[j % 2], tB_L[j % 2], qT_s_L[j % 2], qiT_s_L[j % 2], sg_L[j % 2], gT_L[j % 2], csb_L[j % 2], csq_L[j % 2], mean_L[j % 2], msq_L[j % 2], rstd_L[j % 2], nrm_L[j % 2], snT_L[j % 2], sgc_L[j % 2], mc_s_L[j % 2], sga_s_L[j % 2]
                    if j == 0:
                        pre2(0)
                        post2(0)
                        if nslots > 1:
                            pre2(1)
                    if j + 1 < nslots:
                        post2(j + 1)
                    pq = proj_tok(hT, wB, WQ, 512)
                    rope(pq, 8, cosO, sinO, j, qr, tA, tB)
                    pqi = proj_tok(hT, wB, WQI, 512)
                    rope(pqi, 8, cosO, sinO, j, qir, tA, tB)
                    pw = proj_tok(hT, wB, WWI, 8)
                    k.op(act, lambda e: e.activation(wi_all[:, j * 8:(j + 1) * 8], pw[:, 0:8], AF.Copy,
                                                     scale=IDX_SCALE), reads=[pw], writes=[wi_all])
                    for src, dstb, dd in ((qr, qT_s, qT_d), (qir, qiT_s, qiT_d)):
                        tb = k.bank()
                        tbv = bf(tb[:])
                        for pr in range(4):
                            k.op(pe, lambda e, pr=pr, src=src, tbv=tbv: e.transpose(
                                tbv[:, pr * 128:(pr + 1) * 128], src[:, pr * 128:(pr + 1) * 128], ident[:]),
                                 reads=[src, ident], writes=[tb])
                        k.op(act, lambda e, dstb=dstb, tbv=tbv: e.activation(dstb[:], tbv[:, 0:512], AF.Copy),
                             reads=[tb], writes=[dstb])
                        k.dma(sp, dd[j], dstb[:], reads=[dstb], writes=[qTd_b], sembuf=dstb)
                    for c in range(4):
                        pu = k.bank()
                        puv = pu[:, 0:320].rearrange("p (a b) -> p a b", a=2)
                        for half, col0 in ((0, WU + c * 128), (1, WU + 512 + c * 128)):
                            for kc in range(8):
                                k.op(pe, lambda e, kc=kc, half=half, col0=col0, puv=puv: e.matmul(
                                    puv[:, half, 0:32], wB[:, kc, col0:col0 + 128], hTh[:, kc, :],
                                    start=(kc == 0), stop=(kc == 7)), reads=[wB, hTh], writes=[pu])
                            for kc in range(8):
                                k.op(pe, lambda e, kc=kc, half=half, col0=col0, puv=puv: e.matmul(
                                    puv[:, half, 32:160], wB[:, kc, col0:col0 + 128], hT[:, kc, :],
                                    start=(kc == 0), stop=(kc == 7)), reads=[wB, hT], writes=[pu])
                        k.op(act, lambda e, puv=puv: e.activation(sg[:], puv[:, 1, :], AF.Sigmoid),
                             reads=[pu], writes=[sg])
                        k.op(dve, lambda e, puv=puv, c=c: e.tensor_tensor(gT[:, c, :], puv[:, 0, :], sg[:], ALU.mult),
                             reads=[pu, sg], writes=[gT])
                    if j + 2 < nslots:
                        pre2(j + 2)
                    pc = k.bank()
                    pcv = pc[:, 0:512].rearrange("p (a b) -> p a b", a=4)
                    for c in range(4):
                        for jj in range(31):
                            k.op(pe, lambda e, c=c, jj=jj: e.matmul(pcv[:, c, :], Dg[:, c * 31 + jj, :],
                                                                    gT[:, c, 2 + jj:2 + jj + 128],
                                                                    start=(jj == 0), stop=(jj == 30)),
                                 reads=[Dg, gT], writes=[pc])
                    k.op(dve, lambda e: e.tensor_tensor(csb[:], pcv, bdw[:].unsqueeze(2).to_broadcast([128, 4, 128]),
                                                        ALU.add), reads=[pc, bdw], writes=[csb])
                    k.op(act, lambda e: e.activation(csq[:], csb[:], AF.Square), reads=[csb], writes=[csq])
                    pst = k.bank()
                    for c in range(4):
                        k.op(pe, lambda e, c=c: e.matmul(pst[:, 0:128], ones[:], csb[:, c, :], start=(c == 0),
                                                         stop=(c == 3)), reads=[ones, csb], writes=[pst])
                    for c in range(4):
                        k.op(pe, lambda e, c=c: e.matmul(pst[:, 128:256], ones[:], csq[:, c, :], start=(c == 0),
                                                         stop=(c == 3)), reads=[ones, csq], writes=[pst])
                    k.op(act, lambda e: e.activation(mean[:], pst[:, 0:128], AF.Copy, scale=1.0 / 512),
                         reads=[pst], writes=[mean])
                    k.op(dve, lambda e: e.tensor_tensor(msq[:], mean[:], mean[:], ALU.mult), reads=[mean], writes=[msq])
                    k.op(dve, lambda e: e.scalar_tensor_tensor(rstd[:], pst[:, 128:256], 1.0 / 512, msq[:],
                                                               ALU.mult, ALU.subtract),
                         reads=[pst, msq], writes=[rstd])
                    k.op(act, lambda e: e.activation(rstd[:], rstd[:], AF.Sqrt, bias=EPS, scale=1.0),
                         reads=[rstd], writes=[rstd])
                    k.op(dve, lambda e: e.reciprocal(rstd[:], rstd[:]), reads=[rstd], writes=[rstd])
                    k.op(dve, lambda e: e.tensor_tensor(nrm[:], csb[:], mean[:].unsqueeze(1).to_broadcast([128, 4, 128]),
                                                        ALU.subtract), reads=[csb, mean], writes=[nrm])
                    k.op(dve, lambda e: e.tensor_tensor(nrm[:], nrm[:], rstd[:].unsqueeze(1).to_broadcast([128, 4, 128]),
                                                        ALU.mult), reads=[nrm, rstd], writes=[nrm])
                    for c in range(4):
                        k.op(act, lambda e, c=c: e.activation(snT[:, c, :], nrm[:, c, :], AF.Silu,
                                                              bias=lnb[:, c:c + 1], scale=lng[:, c:c + 1]),
                             reads=[nrm, lnb, lng], writes=[snT])
                    for gi, (wcol, dst) in enumerate(((WGC, None), (WGA, sga_s))):
                        for hb_ in range(2):
                            pg = k.bank()
                            pgv = pg[:, 0:512].rearrange("p (a b) -> p a b", a=4)
                            for m in range(4):
                                col0 = wcol + (hb_ * 4 + m) * 128
                                for kc in range(8):
                                    k.op(pe, lambda e, kc=kc, m=m, col0=col0, pgv=pgv: e.matmul(
                                        pgv[:, m, :], wB[:, kc, col0:col0 + 128], hT[:, kc, :],
                                        start=(kc == 0), stop=(kc == 7)), reads=[wB, hT], writes=[pg])
                            tgt = sgc if gi == 0 else sga_s
                            k.op(act, lambda e, pgv=pgv, tgt=tgt, hb_=hb_: e.activation(
                                tgt[:, hb_ * 4:(hb_ + 1) * 4, :], pgv, AF.Sigmoid), reads=[pg], writes=[tgt])
                    for hb_ in range(2):
                        py = k.bank()
                        pyv = py[:, 0:512].rearrange("p (a b) -> p a b", a=4)
                        for m in range(4):
                            mm = hb_ * 4 + m
                            for kc in range(4):
                                k.op(pe, lambda e, kc=kc, m=m, mm=mm, pyv=pyv: e.matmul(
                                    pyv[:, m, :], wco[:, kc, mm * 128:(mm + 1) * 128], snT[:, kc, :],
                                    start=(kc == 0), stop=(kc == 3)), reads=[wco, snT], writes=[py])
                        k.op(dve, lambda e, pyv=pyv, hb_=hb_: e.tensor_tensor(
                            mc_s[:, hb_ * 4:(hb_ + 1) * 4, :], pyv, sgc[:, hb_ * 4:(hb_ + 1) * 4, :], ALU.mult),
                             reads=[py, sgc], writes=[mc_s])
                    k.dma(sp, mc_d[j], mc_s[:].rearrange("p a b -> p (a b)"), reads=[mc_s], writes=[mcd_b], sembuf=mc_s)
                    k.dma(sp, sga_d[j], sga_s[:].rearrange("p a b -> p (a b)"), reads=[sga_s], writes=[mcd_b],
                          sembuf=sga_s)
                k.barrier()
        def dbg_A2():
            k.dma(sp, dbg["wi"], wi_all[:], reads=[wi_all])
            for nm, src in (("qT", qT_d), ("qiT", qiT_d), ("mc", mc_d), ("sga", sga_d)):
                b_ = Buf("dbgc_" + nm)
                k.dma(sp, dbg[nm], src, writes=[b_])
            k.barrier()

        def phase_B():
            with ExitStack() as s3:
                cm2 = [k.sb("cm%d" % i, [128, 256], F32, s3) for i in range(2)]
                pw2 = k.sb("pw2", [128, NIT + 2], F32, s3)
                for it in range(NIT + 2):
                    k.op(dve, lambda e, it=it: e.memset(pw2[:, it:it + 1], float(2.0 ** -(it + 1))), writes=[pw2])
                qT2 = [k.sb("qTb%d" % i, [128, 4, 256], BF16, s3) for i in range(2)]
                for b_ in qT2:
                    k.op(dve, lambda e: e.memset(b_[:], 0.0), writes=[b_])
                qiT2 = [k.sb("qiTb%d" % i, [128, 512], BF16, s3) for i in range(2)]
                scL = [k.sb("sc%d" % i, [128, S], F32, s3) for i in range(2)]
                scbL = [k.sb("scb%d" % i, [128, S], BF16, s3) for i in range(2)]
                Qb = k.sb("Qbtab", [128, NIT + 2], F32, s3)
                Qb2 = k.sb("Qb2tab", [128, NIT + 2], F32, s3)
                s16 = k.sb("s16", [128, 16], F32, s3)
                AL = [k.sb("Ab%d" % i, [128, 512], BF16, s3) for i in range(4)]
                wabs = k.sb("wabs", [128, 8], F32, s3)
                sgn = k.sb("sgn", [128, 8], F32, s3)
                sgd = k.sb("sgd", [128, 8, 128], BF16, s3)
                msk = k.sb("msk", [128, S], BF16, s3)
                Mb = k.sb("Mb", [128, NB, 128], BF16, s3)
                st8 = k.sb("st8", [128, 8], F32, s3)
                Q = k.sb("Qtab", [128, NIT + 2], F32, s3)
                Q2 = k.sb("Q2tab", [128, NIT + 2], F32, s3)
                E2 = [k.sb("Eb%d" % i, [128, 4, 128], BF16, s3) for i in range(3)]
                rden = k.sb("rden", [128, 8], F32, s3)
                Osb = k.sb("Osb", [128, 2, 4, 66], F32, s3)
                thr = st8[:, 6:7]
                ai = [0]
                MBIG = 30000.0
                if debug:
                    attnf = k.sb("attnf", [128, 512], F32, s3)

                def loads_a(j):
                    p = j % 2
                    k.dma(sp, qiT2[p][:], qiT_d[j], writes=[qiT2[p]])
                    k.dma(sp, cm2[p][:], cmask_d[:, j, :], writes=[cm2[p]])

                def loads_b(j):
                    p = j % 2
                    k.dma(sp, qT2[p][0:64, :, 0:128], qT_d[j][0:64, :].rearrange("p (a b) -> p a b", a=4), writes=[qT2[p]])
                    k.dma(sp, qT2[p][64:128, :, 128:256], qT_d[j][64:128, :].rearrange("p (a b) -> p a b", a=4),
                          writes=[qT2[p]])

                def prep(j):
                    wsl = wi_all[:, j * 8:(j + 1) * 8]
                    k.op(dve, lambda e: e.tensor_scalar(sgn[:], wsl, 0.0, 2.0, ALU.is_ge, ALU.mult), reads=[wi_all], writes=[sgn])
                    k.op(dve, lambda e: e.tensor_scalar(sgn[:], sgn[:], -1.0, None, ALU.add), reads=[sgn], writes=[sgn])
                    k.op(dve, lambda e: e.tensor_tensor(wabs[:], wsl, sgn[:], ALU.mult), reads=[wi_all, sgn], writes=[wabs])
                    k.op(dve, lambda e: e.tensor_tensor(sgd[:], ident[:].unsqueeze(1).to_broadcast([128, 8, 128]),
                                                        sgn[:].unsqueeze(2).to_broadcast([128, 8, 128]), ALU.mult),
                         reads=[ident, sgn], writes=[sgd])

                def indexer(j):
                    sc = scL[j % 2]
                    scb = scbL[j % 2]
                    qiTs = qiT2[j % 2]
                    L = 256 * (j + 1)
                    nch = (L + 511) // 512
                    for cc in range(nch):
                        c0 = cc * 512
                        W = min(512, L - c0)
                        pacc = k.bank(pin=True)
                        pbs = {}

                        def qi_mm(h):
                            pb = k.bank()
                            pp = (h % 2) * 64
                            k.op(pe, lambda e: e.matmul(pb[:, 0:W], qiTs[pp:pp + 64, (h // 2) * 128:(h // 2 + 1) * 128],
                                                        kiT[pp:pp + 64, c0:c0 + W], start=True, stop=True),
                                 reads=[qiTs, kiT], writes=[pb])
                            pbs[h] = pb

                        qi_mm(0)
                        qi_mm(1)
                        qi_mm(2)
                        for h in range(8):
                            if h + 3 < 8:
                                qi_mm(h + 3)
                            pb = pbs[h]
                            A = AL[ai[0] % 4]
                            ai[0] += 1
                            k.op(act, lambda e: e.activation(A[:, 0:W], pb[:, 0:W], AF.Relu, scale=wabs[:, h:h + 1]),
                                 reads=[pb, wabs], writes=[A])
                            k.op(pe, lambda e: e.matmul(pacc[:, 0:W], sgd[:, h, :], A[:, 0:W], start=(h == 0), stop=(h == 7)),
                                 reads=[sgd, A], writes=[pacc])
                        k.op(act, lambda e: e.activation(sc[:, c0:c0 + W], pacc[:, 0:W], AF.Copy), reads=[pacc], writes=[sc])
                        k.op(act, lambda e: e.activation(scb[:, c0:c0 + W], pacc[:, 0:W], AF.Copy), reads=[pacc], writes=[scb])
                        k.unpin(pacc)

                def thresh(j):
                    sc = scL[j % 2]
                    L = 256 * (j + 1)
                    cmask = cm2[j % 2]
                    if j == 0:
                        k.op(dve, lambda e: e.tensor_tensor(sc[:, L - 256:L], sc[:, L - 256:L], cmask[:], ALU.add),
                             reads=[sc, cmask], writes=[sc])
                        k.op(dve, lambda e: e.memset(thr, -1.0e29), writes=[st8])
                        return
                    k.op(dve, lambda e: e.tensor_reduce(st8[:, 0:1], sc[:, 0:L], AX.X, ALU.max), reads=[sc], writes=[st8])
                    k.op(dve, lambda e: e.tensor_reduce(st8[:, 1:2], sc[:, 0:L], AX.X, ALU.min), reads=[sc], writes=[st8])
                    k.op(dve, lambda e: e.tensor_tensor(sc[:, L - 256:L], sc[:, L - 256:L], cmask[:], ALU.add),
                         reads=[sc, cmask], writes=[sc])
                    k.op(dve, lambda e: e.tensor_tensor(st8[:, 2:3], st8[:, 0:1], st8[:, 1:2], ALU.subtract),
                         reads=[st8], writes=[st8])
                    k.op(dve, lambda e: e.tensor_scalar(st8[:, 5:6], st8[:, 2:3], float(2.0 ** -10), None, ALU.mult),
                         reads=[st8], writes=[st8])
                    k.op(dve, lambda e: e.tensor_scalar(st8[:, 2:3], st8[:, 2:3], float(1.0 + 2.0 ** -9), 1e-30,
                                                        ALU.mult, ALU.add), reads=[st8], writes=[st8])
                    k.op(dve, lambda e: e.tensor_tensor(st8[:, 7:8], st8[:, 1:2], st8[:, 5:6], ALU.subtract),
                         reads=[st8], writes=[st8])
                    k.op(dve, lambda e: e.tensor_scalar(Q[:], pw2[:], st8[:, 2:3], None, ALU.mult),
                         reads=[pw2, st8], writes=[Q])
                    k.op(dve, lambda e: e.tensor_scalar(Q2[:], Q[:], 2.0, None, ALU.mult), reads=[Q], writes=[Q2])
                    k.op(dve, lambda e: e.tensor_tensor(st8[:, 3:4], st8[:, 7:8], Q[:, 0:1], ALU.add),
                         reads=[st8, Q], writes=[st8])
                    scb = scbL[j % 2]
                    k.op(dve, lambda e: e.tensor_copy(scb[:, L - 256:L], sc[:, L - 256:L]), reads=[sc], writes=[scb])

                    def bisect(src, Qt, Q2t, nit):
                        for it in range(nit):
                            k.op(dve, lambda e: e.memset(st8[:, 4:5], 0.0), writes=[st8])
                            k.op(dve, lambda e: e.tensor_scalar(msk[:, 0:L], src[:, 0:L], st8[:, 3:4], 0.0,
                                                                ALU.is_ge, ALU.add, accum_out=st8[:, 4:5]),
                                 reads=[src, st8], writes=[st8, msk])
                            k.op(dve, lambda e: e.tensor_scalar(st8[:, 5:6], st8[:, 4:5], TOPK - 0.5,
                                                                Q2t[:, it + 1:it + 2], ALU.is_ge, ALU.mult),
                                 reads=[st8, Q2t], writes=[st8])
                            k.op(dve, lambda e: e.scalar_tensor_tensor(st8[:, 3:4], st8[:, 3:4], Qt[:, it + 1:it + 2],
                                                                       st8[:, 5:6], ALU.subtract, ALU.add),
                                 reads=[st8, Qt], writes=[st8])

                    bisect(scb, Q, Q2, NIT1)
                    k.op(dve, lambda e: e.tensor_tensor(s16[:, 0:1], st8[:, 3:4], Q[:, NIT1:NIT1 + 1], ALU.subtract),
                         reads=[st8, Q], writes=[s16])
                    k.op(dve, lambda e: e.tensor_tensor(s16[:, 1:2], s16[:, 0:1], Q2[:, NIT1:NIT1 + 1], ALU.add),
                         reads=[s16, Q2], writes=[s16])
                    k.op(dve, lambda e: e.tensor_scalar(s16[:, 2:4], s16[:, 0:2], -1.0, None, ALU.mult),
                         reads=[s16], writes=[s16])
                    k.op(dve, lambda e: e.tensor_reduce(s16[:, 4:5], s16[:, 0:4], AX.X, ALU.max), reads=[s16], writes=[s16])
                    k.op(dve, lambda e: e.tensor_scalar(s16[:, 5:6], s16[:, 4:5], float(1.01 * 2.0 ** -8), 1e-30,
                                                        ALU.mult, ALU.add), reads=[s16], writes=[s16])
                    k.op(dve, lambda e: e.tensor_tensor(s16[:, 6:7], s16[:, 0:1], s16[:, 5:6], ALU.subtract),
                         reads=[s16], writes=[s16])
                    k.op(dve, lambda e: e.scalar_tensor_tensor(s16[:, 7:8], s16[:, 5:6], 2.0, Q2[:, NIT1:NIT1 + 1],
                                                               ALU.mult, ALU.add), reads=[s16, Q2], writes=[s16])
                    k.op(dve, lambda e: e.tensor_scalar(Qb[:], pw2[:], s16[:, 7:8], None, ALU.mult),
                         reads=[pw2, s16], writes=[Qb])
                    k.op(dve, lambda e: e.tensor_scalar(Qb2[:], Qb[:], 2.0, None, ALU.mult), reads=[Qb], writes=[Qb2])
                    k.op(dve, lambda e: e.tensor_tensor(st8[:, 3:4], s16[:, 6:7], Qb[:, 0:1], ALU.add),
                         reads=[s16, Qb], writes=[st8])
                    bisect(sc, Qb, Qb2, NIT2)
                    k.op(dve, lambda e: e.tensor_tensor(thr, st8[:, 3:4], Qb[:, NIT2:NIT2 + 1], ALU.subtract),
                         reads=[st8, Qb], writes=[st8])

                def mask(j):
                    sc = scL[j % 2]
                    L = 256 * (j + 1)
                    nkb = 2 * (j + 1)
                    if debug:
                        b_ = Buf("dbg_sc%d" % j)
                        for c0_ in range(0, L, 1024):
                            c1_ = min(L, c0_ + 1024)
                            k.dma(sp, dbg["sc"][j][:, c0_:c1_], sc[:, c0_:c1_], reads=[sc], writes=[b_], sembuf=b_)
                        k.dma(sp, dbg["thr"][j], thr, reads=[st8], writes=[b_], sembuf=b_)
                    for g0 in range(0, nkb, 8):
                        gn = min(8, nkb - g0)
                        k.op(dve, lambda e: e.tensor_scalar(msk[:, g0 * 128:(g0 + gn) * 128],
                                                            sc[:, g0 * 128:(g0 + gn) * 128], thr, None, ALU.is_ge),
                             reads=[sc, st8], writes=[msk])
                        tb = k.bank()
                        tbv = bf(tb[:])
                        for q_ in range(gn):
                            k.op(pe, lambda e: e.transpose(tbv[:, q_ * 128:(q_ + 1) * 128],
                                                           msk[:, (g0 + q_) * 128:(g0 + q_ + 1) * 128], ident[:]),
                                 reads=[msk, ident], writes=[tb])
                        k.op(act, lambda e: e.activation(Mb[:, g0:g0 + gn, :],
                                                         tbv[:, 0:gn * 128].rearrange("p (a b) -> p a b", a=gn),
                                                         AF.Identity, bias=-MBIG, scale=MBIG),
                             reads=[tb], writes=[Mb])

                def attention(j):
                    qTs = qT2[j % 2]
                    nkb = 2 * (j + 1)
                    po = [k.bank(pin=True), k.bank(pin=True)]
                    pov = [b_[:, 0:512].rearrange("p (h d) -> p h d", h=4) for b_ in po]
                    units = [(kb, hg) for kb in range(nkb) for hg in range(2)]

                    def qk(u):
                        kb, hg = units[u]
                        pl = k.bank()
                        plv = pl[:, 0:512].rearrange("p (a b) -> p a b", a=4)
                        k.op(pe, lambda e: e.matmul(plv, ident[:], Mb[:, kb, :].unsqueeze(1).to_broadcast([128, 4, 128]),
                                                    start=True, stop=False), reads=[ident, Mb], writes=[pl])
                        for pi in range(2):
                            pr = hg * 2 + pi
                            k.op(pe, lambda e: e.matmul(pl[:, pi * 256:(pi + 1) * 256], KT[:, pr, kb * 128:(kb + 1) * 128],
                                                        qTs[:, pr, :], start=False, stop=(pi == 1)),
                                 reads=[KT, qTs], writes=[pl])
                        return pl, plv

                    LA = 2
                    pend = [qk(u) for u in range(min(LA, len(units)))]
                    for u in range(len(units)):
                        if u + LA < len(units):
                            pend.append(qk(u + LA))
                        kb, hg = units[u]
                        pl, plv = pend.pop(0)
                        E = E2[u % 3]
                        k.op(act, lambda e: e.activation(E[:], plv, AF.Exp, scale=ATT_SCALE), reads=[pl], writes=[E])
                        for hh in range(4):
                            h = hg * 4 + hh
                            k.op(pe, lambda e: e.matmul(pov[hg][:, hh, 0:66], E[:, hh, :], V[:, kb, h * 66:(h + 1) * 66],
                                                        start=(kb == 0 and hh == 0), stop=(kb == nkb - 1 and hh == 3)),
                                 reads=[E, V], writes=[po[hg]])
                    for hg in range(2):
                        k.op(act, lambda e: e.activation(Osb[:, hg, :, :], pov[hg][:, :, 0:66], AF.Copy),
                             reads=[po[hg]], writes=[Osb])
                        k.unpin(po[hg])

                def finalize(j):
                    for hg in range(2):
                        k.op(dve, lambda e: e.reciprocal(rden[:, hg * 4:(hg + 1) * 4].unsqueeze(2), Osb[:, hg, :, 64:65]),
                             reads=[Osb], writes=[rden])
                        k.op(dve, lambda e: e.tensor_tensor(
                            attn_all[:, j, hg * 256:(hg + 1) * 256].rearrange("p (h d) -> p h d", h=4), Osb[:, hg, :, 0:64],
                            rden[:, hg * 4:(hg + 1) * 4].unsqueeze(2).to_broadcast([128, 4, 64]), ALU.mult),
                             reads=[Osb, rden], writes=[attn_all])
                    if debug:
                        k.op(dve, lambda e: e.tensor_copy(attnf[:], attn_all[:, j, :]), reads=[attn_all], writes=[attnf])
                        k.dma(sp, dbg["attn"][j], attnf[:], reads=[attnf])

                loads_a(0)
                loads_b(0)
                if nslots > 1:
                    loads_a(1)
                    loads_b(1)
                prep(0)
                indexer(0)
                thresh(0)
                mask(0)
                if nslots > 1:
                    prep(1)
                    indexer(1)
                for i in range(nslots):
                    if i + 2 < nslots:
                        prep(i + 2)
                    if i + 1 < nslots:
                        thresh(i + 1)
                    attention(i)
                    if i + 2 < nslots:
                        loads_a(i + 2)
                        indexer(i + 2)
                    if i + 1 < nslots:
                        mask(i + 1)
                    finalize(i)
                    if i + 2 < nslots:
                        loads_b(i + 2)
                k.barrier()

            with ExitStack() as s3:
                wao = k.sb("wao", [128, 4, D], BF16, s3)
                wo = k.sb("wo", [128, 8, D], BF16, s3)
                wr = k.sb("wr", [128, 8, 20], BF16, s3)
                br = k.sb("br", [128, 20], F32, s3)
                for kc in range(4):
                    k.dma(pool, wao[:, kc, :], wao_d[kc * 128:(kc + 1) * 128, :], writes=[wao])
                for kc in range(8):
                    k.dma(pool, wo[:, kc, :], wo_d[kc * 128:(kc + 1) * 128, :], writes=[wo])
                k.dma(pool, wr[:], wr_d.rearrange("(k p) c -> p k c", p=128), writes=[wr])
                k.dma(sp, br[:], br_d, writes=[br])
                mc2 = [k.sb("mcb%d" % i, [128, 8, 128], BF16, s3) for i in range(2)]
                sga2 = [k.sb("sgab%d" % i, [128, 8, 128], BF16, s3) for i in range(2)]
                xoL = [k.sb("xob%d" % i, [128, D], F32, s3) for i in range(2)]
                h2L = [k.sb("h2s%d" % i, [128, 8, 128], BF16, s3) for i in range(2)]
                h2d_b = Buf("h2d_b")
                attnTL = [k.sb("attnT%d" % i, [128, 4, 128], BF16, s3) for i in range(2)]
                mTL = [k.sb("mT%d" % i, [128, 8, 128], BF16, s3) for i in range(2)]
                x1L = [k.sb("x1_%d" % i, [128, D], F32, s3) for i in range(2)]
                lg = k.sb("lg", [128, 20], F32, s3)
                r8 = k.sb("r8", [128, 16], F32, s3)
                ohg = k.sb("ohg", [128, 4], F32, s3)
                t16 = k.sb("t16", [128, 16], F32, s3)
                esel = k.sb("esel", [128, 4], F32, s3)
                oh1 = k.sb("oh1", [128, 4], F32, s3)
                oh2 = k.sb("oh2", [128, 4], F32, s3)
                em = k.sb("em", [128, 4], F32, s3)
                we = k.sb("we", [128, 4], F32, s3)
                x1d_b = Buf("x1d_b")

                def loads_c(j):
                    p = j % 2
                    k.dma(sp, mc2[p][:].rearrange("p a b -> p (a b)"), mc_d[j], writes=[mc2[p]])
                    k.dma(sp, sga2[p][:].rearrange("p a b -> p (a b)"), sga_d[j], writes=[sga2[p]])
                    k.dma(sp, xoL[p][:], x_own[j * 128:(j + 1) * 128, :], writes=[xoL[p]])

                def tail(j):
                    mcs, sgas = mc2[j % 2], sga2[j % 2]
                    xo, x1, mT, attnT = xoL[j % 2], x1L[j % 2], mTL[j % 2], attnTL[j % 2]
                    tb = k.bank()
                    tbv = bf(tb[:])
                    for q_ in range(4):
                        k.op(pe, lambda e: e.transpose(tbv[:, q_ * 128:(q_ + 1) * 128],
                                                       attn_all[:, j, q_ * 128:(q_ + 1) * 128], ident[:]),
                             reads=[attn_all, ident], writes=[tb])
                    k.op(act, lambda e: e.activation(attnT[:], tbv[:, 0:512].rearrange("p (a b) -> p a b", a=4), AF.Copy),
                         reads=[tb], writes=[attnT])
                    x1v = x1[:].rearrange("p (a b) -> p a b", a=8)
                    for hb_ in range(2):
                        py = k.bank()
                        pyv = py[:, 0:512].rearrange("p (a b) -> p a b", a=4)
                        for m in range(4):
                            mm = hb_ * 4 + m
                            for kc in range(4):
                                k.op(pe, lambda e: e.matmul(pyv[:, m, :], wao[:, kc, mm * 128:(mm + 1) * 128], attnT[:, kc, :],
                                                            start=(kc == 0), stop=(kc == 3)), reads=[wao, attnT], writes=[py])
                        k.op(dve, lambda e: e.tensor_tensor(x1v[:, hb_ * 4:(hb_ + 1) * 4, :], pyv,
                                                            sgas[:, hb_ * 4:(hb_ + 1) * 4, :], ALU.mult),
                             reads=[py, sgas], writes=[x1])
                    k.op(dve, lambda e: e.tensor_tensor(mT[:], x1v, mcs[:], ALU.add), reads=[x1, mcs], writes=[mT])
                    for half in range(2):
                        px = k.bank()
                        for kc in range(8):
                            k.op(pe, lambda e: e.matmul(px[:, 0:512], mT[:, kc, :], wo[:, kc, half * 512:(half + 1) * 512],
                                                        start=(kc == 0), stop=(kc == 7)), reads=[mT, wo], writes=[px])
                        k.op(dve, lambda e: e.tensor_tensor(x1[:, half * 512:(half + 1) * 512], px[:, 0:512],
                                                            xo[:, half * 512:(half + 1) * 512], ALU.add),
                             reads=[px, xo], writes=[x1])
                    k.dma(sp, x1_d[j], x1[:], reads=[x1], writes=[x1d_b], sembuf=x1)
                    if debug:
                        k.dma(sp, dbg["x1"][j], x1[:], reads=[x1], sembuf=x1)
                    hbsT[j] = norm_pre(x1, 128)

                def tailB(j):
                    h2s = h2L[j % 2]
                    norm_post(hbsT.pop(j), 128, gffn, h2s)
                    k.dma(sp, h2T_d[j], h2s[:].rearrange("p a b -> p (a b)"), reads=[h2s], writes=[h2d_b], sembuf=h2s)
                    pr_ = k.bank()
                    for kc in range(8):
                        k.op(pe, lambda e: e.matmul(pr_[:, 0:20], h2s[:, kc, :], wr[:, kc, :],
                                                    start=(kc == 0), stop=(kc == 7)), reads=[h2s, wr], writes=[pr_])
                    k.op(dve, lambda e: e.tensor_tensor(lg[:], pr_[:, 0:20], br[:], ALU.add), reads=[pr_, br], writes=[lg])
                    k.op(dve, lambda e: e.tensor_reduce(r8[:, 0:1], lg[:, 0:4], AX.X, ALU.max), reads=[lg], writes=[r8])
                    k.op(dve, lambda e: e.tensor_scalar(r8[:, 1:2], r8[:, 0:1], -1.0, None, ALU.mult), reads=[r8], writes=[r8])
                    k.op(dve, lambda e: e.memset(r8[:, 2:3], 0.0), writes=[r8])
                    k.op(act, lambda e: e.activation(t16[:, 0:4], lg[:, 0:4], AF.Exp, bias=r8[:, 1:2], scale=1.0,
                                                     accum_out=r8[:, 2:3]), reads=[lg, r8], writes=[t16, r8])
                    k.op(dve, lambda e: e.reciprocal(r8[:, 3:4], r8[:, 2:3]), reads=[r8], writes=[r8])
                    k.op(dve, lambda e: e.tensor_scalar(ohg[:], lg[:, 0:4], r8[:, 0:1], r8[:, 3:4], ALU.is_ge, ALU.mult),
                         reads=[lg, r8], writes=[ohg])
                    k.op(dve, lambda e: e.tensor_scalar(oh1[:], lg[:, 0:4], r8[:, 0:1], None, ALU.is_ge),
                         reads=[lg, r8], writes=[oh1])
                    k.op(dve, lambda e: e.tensor_tensor(t16[:].rearrange("p (g e) -> p g e", g=4),
                                                        lg[:, 4:20].rearrange("p (g e) -> p g e", g=4),
                                                        oh1[:].unsqueeze(2).to_broadcast([128, 4, 4]), ALU.mult),
                         reads=[lg, oh1], writes=[t16])
                    k.op(dve, lambda e: e.tensor_reduce(esel[:], t16[:].rearrange("p (g e) -> p e g", g=4), AX.X, ALU.add),
                         reads=[t16], writes=[esel])
                    k.op(dve, lambda e: e.tensor_reduce(r8[:, 4:5], esel[:], AX.X, ALU.max), reads=[esel], writes=[r8])
                    k.op(dve, lambda e: e.tensor_scalar(oh1[:], esel[:], r8[:, 4:5], None, ALU.is_ge),
                         reads=[esel, r8], writes=[oh1])
                    k.op(dve, lambda e: e.scalar_tensor_tensor(em[:], oh1[:], NEG, esel[:], ALU.mult, ALU.add),
                         reads=[oh1, esel], writes=[em])
                    k.op(dve, lambda e: e.tensor_reduce(r8[:, 5:6], em[:], AX.X, ALU.max), reads=[em], writes=[r8])
                    k.op(dve, lambda e: e.tensor_scalar(oh2[:], em[:], r8[:, 5:6], None, ALU.is_ge),
                         reads=[em, r8], writes=[oh2])
                    k.op(dve, lambda e: e.tensor_tensor(r8[:, 6:7], r8[:, 4:5], r8[:, 5:6], ALU.subtract),
                         reads=[r8], writes=[r8])
                    k.op(act, lambda e: e.activation(r8[:, 7:8], r8[:, 6:7], AF.Sigmoid), reads=[r8], writes=[r8])
                    k.op(dve, lambda e: e.tensor_scalar(r8[:, 8:9], r8[:, 7:8], -1.0, 1.0, ALU.mult, ALU.add),
                         reads=[r8], writes=[r8])
                    k.op(dve, lambda e: e.tensor_scalar(we[:], oh1[:], r8[:, 7:8], None, ALU.mult), reads=[oh1, r8], writes=[we])
                    k.op(dve, lambda e: e.scalar_tensor_tensor(we[:], oh2[:], r8[:, 8:9], we[:], ALU.mult, ALU.add),
                         reads=[oh2, r8, we], writes=[we])
                    k.op(dve, lambda e: e.tensor_tensor(gw_all[:, j * 16:(j + 1) * 16].rearrange("p (g e) -> p g e", g=4),
                                                        ohg[:].unsqueeze(2).to_broadcast([128, 4, 4]),
                                                        we[:].unsqueeze(1).to_broadcast([128, 4, 4]), ALU.mult),
                         reads=[ohg, we], writes=[gw_all])

                hbsT = {}
                loads_c(0)
                if nslots > 1:
                    loads_c(1)
                tail(0)
                for j in range(nslots):
                    if j + 1 < nslots:
                        tail(j + 1)
                    tailB(j)
                    if j + 2 < nslots:
                        loads_c(j + 2)
                k.barrier()
        def phase_C():
            with ExitStack() as s4:
                acc = k.sb("acc", [128, NS, D], F32, s4)
                h2T = k.sb("h2T", [128, 8, NS * 128], BF16, s4)
                for j in range(nslots):
                    k.dma(sp, h2T[:, :, j * 128:(j + 1) * 128], h2T_d[j].rearrange("p (a b) -> p a b", a=8), writes=[h2T])
                for j in range(nslots):
                    k.dma(sp, acc[:, j, :], x1_d[j], writes=[acc])
                gfin = k.sb("gfin", [128, D], F32, s4)
                k.dma(sp, gfin[:], gfin_d, writes=[gfin])
                wg2 = [k.sb("wg%d" % i, [128, 8, 256], BF16, s4) for i in range(2)]
                wu2 = [k.sb("wu%d" % i, [128, 8, 256], BF16, s4) for i in range(2)]
                wd2 = [k.sb("wd%d" % i, [128, 2, D], BF16, s4) for i in range(2)]
                sgl2 = [k.sb("sgm%d" % i, [128, 512], F32, s4) for i in range(2)]
                hid2 = [k.sb("hid%d" % i, [128, 2, 512], BF16, s4) for i in range(2)]
                ot2 = [k.sb("ot%d" % i, [128, D], F32, s4) for i in range(2)]

                def wload(ex):
                    p = ex % 2
                    k.dma(pool, wg2[p][:], wg_d[ex].rearrange("(k p) c -> p k c", p=128), writes=[wg2[p]])
                    k.dma(pool, wu2[p][:], wu_d[ex].rearrange("(k p) c -> p k c", p=128), writes=[wu2[p]])
                    k.dma(pool, wd2[p][:], wd_d[ex].rearrange("(k p) c -> p k c", p=128), writes=[wd2[p]])

                ngrp = (nslots + 3) // 4
                wload(0)
                hi = 0
                for ex in range(16):
                    if ex + 1 < 16:
                        wload(ex + 1)
                    p = ex % 2
                    wg, wu, wd = wg2[p], wu2[p], wd2[p]
                    for tg in range(ngrp):
                        ns_ = min(4, nslots - tg * 4)
                        N = ns_ * 128
                        hid = hid2[hi % 2]
                        hi += 1
                        for fc in range(2):
                            pg = k.bank()
                            pu = k.bank()
                            for kc in range(8):
                                k.op(pe, lambda e, kc=kc, fc=fc, pg=pg: e.matmul(
                                    pg[:, 0:N], wg[:, kc, fc * 128:(fc + 1) * 128], h2T[:, kc, tg * 512:tg * 512 + N],
                                    start=(kc == 0), stop=(kc == 7)), reads=[wg, h2T], writes=[pg])
                            for kc in range(8):
                                k.op(pe, lambda e, kc=kc, fc=fc, pu=pu: e.matmul(
                                    pu[:, 0:N], wu[:, kc, fc * 128:(fc + 1) * 128], h2T[:, kc, tg * 512:tg * 512 + N],
                                    start=(kc == 0), stop=(kc == 7)), reads=[wu, h2T], writes=[pu])
                            sgl = sgl2[fc]
                            k.op(act, lambda e, pg=pg, sgl=sgl: e.activation(sgl[:, 0:N], pg[:, 0:N], AF.Silu),
                                 reads=[pg], writes=[sgl])
                            k.op(dve, lambda e, pu=pu, sgl=sgl, fc=fc, hid=hid: e.tensor_tensor(
                                hid[:, fc, 0:N], pu[:, 0:N], sgl[:, 0:N], ALU.mult), reads=[pu, sgl], writes=[hid])
                        for s_ in range(ns_):
                            j = tg * 4 + s_
                            for half in range(2):
                                py = k.bank()
                                for fc in range(2):
                                    k.op(pe, lambda e, fc=fc, half=half, py=py, s_=s_, hid=hid: e.matmul(
                                        py[:, 0:512], hid[:, fc, s_ * 128:(s_ + 1) * 128],
                                        wd[:, fc, half * 512:(half + 1) * 512], start=(fc == 0), stop=(fc == 1)),
                                         reads=[hid, wd], writes=[py])
                                k.op(dve, lambda e, py=py, j=j, half=half, ex=ex: e.scalar_tensor_tensor(
                                    acc[:, j, half * 512:(half + 1) * 512], py[:, 0:512],
                                    gw_all[:, j * 16 + ex:j * 16 + ex + 1], acc[:, j, half * 512:(half + 1) * 512],
                                    ALU.mult, ALU.add), reads=[py, gw_all, acc], writes=[acc])
                for j in range(nslots):
                    ot = ot2[j % 2]
                    ss, rs, junk = ssL[j % 2], rsL[j % 2], hbL[j % 2]
                    k.op(dve, lambda e, ss=ss: e.memset(ss[:], 0.0), writes=[ss])
                    k.op(act, lambda e, j=j, junk=junk, ss=ss: e.activation(junk[:], acc[:, j, :], AF.Square, accum_out=ss[:, 0:1]),
                         reads=[acc, ss], writes=[junk, ss])
                    k.op(act, lambda e, rs=rs, ss=ss: e.activation(rs[:], ss[:], AF.Sqrt, bias=EPS, scale=1.0 / D), reads=[ss], writes=[rs])
                    k.op(dve, lambda e, rs=rs: e.reciprocal(rs[:], rs[:]), reads=[rs], writes=[rs])
                    k.op(dve, lambda e, j=j, ot=ot, rs=rs: e.scalar_tensor_tensor(ot[:], acc[:, j, :], rs[:, 0:1], gfin[:],
                                                                          ALU.mult, ALU.mult),
                         reads=[acc, rs, gfin], writes=[ot])
                    k.dma(sp, out_d[j * 128:(j + 1) * 128, :], ot[:], reads=[ot], sembuf=ot)
                k.barrier()
        if "A2" in phases:
            phase_A2()
            if debug:
                dbg_A2()
        with ExitStack() as skv:
            KT = k.sb("KT", [128, 4, S], BF16, skv)
            V = k.sb("V", [128, NB, 8 * 66], BF16, skv)
            kiT = k.sb("kiT", [128, S], BF16, skv)
            k.op(pool, lambda e: e.memset(V[:], 1.0), writes=[V])
            if "A1" in phases:
                phase_A1()
                if debug:
                    dbg_A1()
            attn_all = k.sb("attn_all", [128, NS, 512], BF16, skv)
            if "B" in phases:
                phase_B()
                if debug:
                    k.dma(sp, dbg["gw"], gw_all[:], reads=[gw_all])
            k.barrier()
        if "C" in phases:
            phase_C()
        k.barrier()
        build_nc.nins = k.nins
    return nc


def own_blocks(r):
    return [2 * j + ((j % 2) ^ r) for j in range(NS)]


def prep_core(inp, c):
    b, r = c // 2, c % 2
    x = np.asarray(inp["x"], dtype=np.float32)
    pos = np.asarray(inp["positions"]).astype(np.int32)
    blocks = own_blocks(r)
    xb = x[b]
    x_own = np.concatenate([xb[i * 128:(i + 1) * 128] for i in blocks], axis=0)
    x_halo = np.zeros((NS * 32, D), np.float32)
    for j, i in enumerate(blocks):
        if i > 0:
            x_halo[j * 32:(j + 1) * 32] = xb[i * 128 - 32:i * 128]
    pos_full = np.ascontiguousarray(pos[b].reshape(NB, 128).T)
    pos_own = np.ascontiguousarray(np.stack([pos[b][i * 128:(i + 1) * 128] for i in blocks], axis=1))
    invf = (1.0 / (np.float32(10000.0) ** (np.arange(0, 64, 2, dtype=np.float32) / np.float32(64)))).astype(np.float32)
    invf = np.ascontiguousarray(np.broadcast_to(invf[None, :], (128, 32)))
    cmask = np.zeros((128, NS, 256), np.float32)
    for j, i in enumerate(blocks):
        qidx = i * 128 + np.arange(128)[:, None]
        kidx = 256 * j + np.arange(256)[None, :]
        cmask[:, j, :] = np.where(kidx > qidx, np.float32(NEG), np.float32(0.0))

    def fm(v, nchunk):
        return np.ascontiguousarray(np.asarray(v, np.float32).reshape(nchunk, 128).T)

    w_dw = np.asarray(inp["w_dw"], np.float32)[0, :, 0, :]
    wdw = np.ascontiguousarray(w_dw.T.reshape(4, 128, 31).transpose(1, 0, 2))
    w_r = np.concatenate([np.asarray(inp["w_rg"], np.float32)[0],
                          np.asarray(inp["w_re"], np.float32)[0].reshape(D, 16)], axis=1)
    b_r = np.concatenate([np.asarray(inp["b_rg"], np.float32)[0], np.asarray(inp["b_re"], np.float32)[0].reshape(16)])
    m = {
        "x_full": np.ascontiguousarray(xb), "x_own": x_own, "x_halo": x_halo,
        "pos_full": pos_full, "pos_own": pos_own, "invf": invf, "cmask": cmask,
        "ident": np.eye(128, dtype=np.float32),
        "w_in": np.ascontiguousarray(np.asarray(inp["w_in"], np.float32)[0]),
        "wdw": wdw, "bdw": fm(inp["b_dw"][0], 4), "lng": fm(inp["ln_g"][0], 4), "lnb": fm(inp["ln_b"][0], 4),
        "gmix": fm(inp["g_mix"][0], 8), "gffn": fm(inp["g_ffn"][0], 8),
        "gfin": np.ascontiguousarray(np.broadcast_to(np.asarray(inp["g_final"], np.float32)[None, :], (128, D))),
        "b_r": np.ascontiguousarray(np.broadcast_to(b_r[None, :], (128, 20))),
        "w_r": np.ascontiguousarray(w_r),
        "w_conv_out": np.ascontiguousarray(np.asarray(inp["w_conv_out"], np.float32)[0]),
        "w_attn_out": np.ascontiguousarray(np.asarray(inp["w_attn_out"], np.float32)[0]),
        "w_o": np.ascontiguousarray(np.asarray(inp["w_o"], np.float32)[0]),
        "w_gate": np.ascontiguousarray(np.asarray(inp["w_gate"], np.float32)[0]),
        "w_up": np.ascontiguousarray(np.asarray(inp["w_up"], np.float32)[0]),
        "w_down": np.ascontiguousarray(np.asarray(inp["w_down"], np.float32)[0]),
    }
    return m


_NC_CACHE = {}


def kernel(**inputs):
    if "nc" not in _NC_CACHE:
        _NC_CACHE["nc"] = build_nc()
    nc = _NC_CACHE["nc"]
    in_maps = [prep_core(inputs, c) for c in range(8)]
    res = run_bass_kernel_spmd(nc, in_maps, core_ids=list(range(8)))
    out = np.empty((4, S, D), np.float32)
    for c in range(8):
        b, r = c // 2, c % 2
        o = np.asarray(res.results[c]["out"], dtype=np.float32)
        for j, i in enumerate(own_blocks(r)):
            out[b, i * 128:(i + 1) * 128] = o[j * 128:(j + 1) * 128]
    return out
```

```python
from contextlib import ExitStack
import numpy as np
import concourse.bass as bass
import concourse.mybir as mybir
from concourse.bass_utils import run_bass_kernel_spmd

F32 = mybir.dt.float32
BF16 = mybir.dt.bfloat16
I32 = mybir.dt.int32
ALU = mybir.AluOpType
AF = mybir.ActivationFunctionType
AX = mybir.AxisListType

D = 1024
S = 4096
NB = 32
NS = 16
EPS = 1e-6
NIT = 14
TOPK = 256
NEG = -1.0e30
C_U, C_Q, C_K, C_V, C_QI, C_KI, C_WI, C_GC, C_GA = 0, 1024, 1536, 2048, 2560, 3072, 3136, 3144, 4168
IDX_SCALE = float((8 ** -0.5) * (64 ** -0.5))
ATT_SCALE = float(64 ** -0.5)


class Buf:
    __slots__ = ("name", "t", "lw", "rd", "dsem", "dcnt")

    def __init__(self, name, t=None):
        self.name = name
        self.t = t
        self.lw = None
        self.rd = {}
        self.dsem = None
        self.dcnt = 0

    def __getitem__(self, key):
        return self.t[key]


class Eng:
    def __init__(self, k, name, eng):
        self.name = name
        self.eng = eng
        self.sem = k.new_sem("e_" + name)
        self.cnt = 0
        self.seen = {}

    def wait_tok(self, tok):
        if tok is None:
            return
        sem, val, _ = tok
        key = id(sem)
        if self.seen.get(key, 0) >= val:
            return
        self.eng.wait_ge(sem, val)
        self.seen[key] = val


class K:
    def __init__(self, nc, stack):
        self.nc = nc
        self.stack = stack
        self.pe = Eng(self, "pe", nc.tensor)
        self.act = Eng(self, "act", nc.scalar)
        self.dve = Eng(self, "dve", nc.vector)
        self.pool = Eng(self, "pool", nc.gpsimd)
        self.sp = Eng(self, "sp", nc.sync)
        self.engs = [self.pe, self.act, self.dve, self.pool, self.sp]
        self.dma_bufs = []
        self.nins = 0
        self.banks = []
        self.bank_i = 0
        self.pinned = set()

    def new_sem(self, name):
        return self.stack.enter_context(self.nc.semaphore(name))

    def sb(self, name, shape, dt, stack=None):
        t = (stack or self.stack).enter_context(self.nc.sbuf_tensor("s_" + name, list(shape), dt))
        return Buf(name, t)

    def mk_banks(self):
        for i in range(8):
            t = self.stack.enter_context(self.nc.psum_tensor("bank%d" % i, [128, 512], F32))
            self.banks.append(Buf("bank%d" % i, t))

    def bank(self, pin=False):
        for _ in range(16):
            b = self.banks[self.bank_i % 8]
            self.bank_i += 1
            if b.name not in self.pinned:
                if pin:
                    self.pinned.add(b.name)
                return b
        raise RuntimeError("no psum bank")

    def unpin(self, b):
        self.pinned.discard(b.name)

    def _deps(self, e, reads, writes):
        for b in reads:
            if b.lw is not None:
                e.wait_tok(b.lw)
        for b in writes:
            if b.lw is not None and b.lw[2] != e.name:
                e.wait_tok(b.lw)
            for en, tok in b.rd.items():
                if en != e.name:
                    e.wait_tok(tok)

    def _commit(self, tok, reads, writes):
        for b in reads:
            b.rd[tok[2]] = tok
        for b in writes:
            b.lw = tok
            b.rd = {}

    def op(self, e, fn, reads=(), writes=()):
        self._deps(e, reads, writes)
        ins = fn(e.eng)
        e.cnt += 1
        ins.then_inc(e.sem, 1)
        tok = (e.sem, e.cnt, e.name)
        self._commit(tok, reads, writes)
        self.nins += 1
        return tok

    def dma(self, e, out, in_, reads=(), writes=(), sembuf=None):
        self._deps(e, reads, writes)
        sb_ = sembuf if sembuf is not None else (writes[0] if writes else reads[0])
        if sb_.dsem is None:
            sb_.dsem = self.new_sem("d_" + sb_.name)
            self.dma_bufs.append(sb_)
        ins = e.eng.dma_start(out=out, in_=in_)
        sb_.dcnt += 16
        ins.then_inc(sb_.dsem, 16)
        tok = (sb_.dsem, sb_.dcnt, "dma_" + sb_.name)
        self._commit(tok, reads, writes)
        self.nins += 1
        return tok

    def barrier(self):
        toks = [(e.sem, e.cnt, e.name) for e in self.engs if e.cnt > 0]
        toks += [(b.dsem, b.dcnt, "dma") for b in self.dma_bufs]
        for e in self.engs:
            for t in toks:
                if t[2] != e.name:
                    e.wait_tok(t)


def bf(ap):
    return ap.bitcast(BF16)


def build_nc(phases=("A1", "A2", "B", "C"), debug=False, nslots=NS, bstop=9):
    nc = bass.Bass("TRN2", target_bir_lowering=False)

    def din(name, shape, dt=F32):
        return nc.dram_tensor(name, list(shape), dt, kind="ExternalInput").ap()

    def dscr(name, shape, dt):
        return nc.dram_tensor(name, list(shape), dt, kind="Internal").ap()

    x_full = din("x_full", [S, D])
    x_own = din("x_own", [NS * 128, D])
    x_halo = din("x_halo", [NS * 32, D])
    pos_full = din("pos_full", [128, NB], I32)
    pos_own = din("pos_own", [128, NS], I32)
    invf = din("invf", [128, 32])
    cmask_d = din("cmask", [128, NS, 256])
    ident_d = din("ident", [128, 128])
    w_in = din("w_in", [D, 5192])
    wdw_d = din("wdw", [128, 4, 31])
    bdw_d = din("bdw", [128, 4])
    lng_d = din("lng", [128, 4])
    lnb_d = din("lnb", [128, 4])
    gmix_d = din("gmix", [128, 8])
    gffn_d = din("gffn", [128, 8])
    gfin_d = din("gfin", [128, D])
    br_d = din("b_r", [128, 20])
    wr_d = din("w_r", [D, 20])
    wco_d = din("w_conv_out", [512, D])
    wao_d = din("w_attn_out", [512, D])
    wo_d = din("w_o", [D, D])
    wg_d = din("w_gate", [16, D, 256])
    wu_d = din("w_up", [16, D, 256])
    wd_d = din("w_down", [16, 256, D])
    out_d = nc.dram_tensor("out", [NS * 128, D], F32, kind="ExternalOutput").ap()

    qT_d = dscr("qT_d", [NS, 128, 512], BF16)
    qiT_d = dscr("qiT_d", [NS, 128, 512], BF16)
    mc_d = dscr("mc_d", [NS, 128, 1024], BF16)
    sga_d = dscr("sga_d", [NS, 128, 1024], BF16)
    x1_d = dscr("x1_d", [NS, 128, D], F32)
    h2T_d = dscr("h2T_d", [NS, 128, 1024], BF16)
    dbg = {}
    if debug:
        dbg["kT"] = nc.dram_tensor("dbg_kT", [128, 4, S], BF16, kind="ExternalOutput").ap()
        dbg["v"] = nc.dram_tensor("dbg_v", [128, NB, 8 * 66], BF16, kind="ExternalOutput").ap()
        dbg["kiT"] = nc.dram_tensor("dbg_kiT", [128, S], BF16, kind="ExternalOutput").ap()
        dbg["qT"] = nc.dram_tensor("dbg_qT", [NS, 128, 512], BF16, kind="ExternalOutput").ap()
        dbg["qiT"] = nc.dram_tensor("dbg_qiT", [NS, 128, 512], BF16, kind="ExternalOutput").ap()
        dbg["mc"] = nc.dram_tensor("dbg_mc", [NS, 128, 1024], BF16, kind="ExternalOutput").ap()
        dbg["sga"] = nc.dram_tensor("dbg_sga", [NS, 128, 1024], BF16, kind="ExternalOutput").ap()
        dbg["wi"] = nc.dram_tensor("dbg_wi", [128, NS * 8], F32, kind="ExternalOutput").ap()
        dbg["x1"] = nc.dram_tensor("dbg_x1", [NS, 128, D], F32, kind="ExternalOutput").ap()
        dbg["gw"] = nc.dram_tensor("dbg_gw", [128, NS * 16], F32, kind="ExternalOutput").ap()
        dbg["sc"] = nc.dram_tensor("dbg_sc", [NS, 128, S], F32, kind="ExternalOutput").ap()
        dbg["thr"] = nc.dram_tensor("dbg_thr", [NS, 128, 1], F32, kind="ExternalOutput").ap()
        dbg["attn"] = nc.dram_tensor("dbg_attn", [NS, 128, 512], F32, kind="ExternalOutput").ap()

    with ExitStack() as st:
        k = K(nc, st)
        pe, act, dve, pool, sp = k.pe, k.act, k.dve, k.pool, k.sp
        k.mk_banks()

        ident = k.sb("ident", [128, 128], BF16)
        identf = k.sb("identf", [128, 128], F32)
        wi_all = k.sb("wi_all", [128, NS * 8], F32)
        gw_all = k.sb("gw_all", [128, NS * 16], F32)
        gmix = k.sb("gmix", [128, 8], F32)
        gffn = k.sb("gffn", [128, 8], F32)
        ssL = [k.sb("ss%d" % i, [128, 1], F32) for i in range(2)]
        rsL = [k.sb("rs%d" % i, [128, 1], F32) for i in range(2)]
        hbL = [k.sb("hb%d" % i, [128, D], BF16) for i in range(2)]
        nrm_i = [0]

        k.dma(sp, identf[:], ident_d, writes=[identf])
        k.dma(pool, ident[:], ident_d, writes=[ident])
        k.dma(sp, gmix[:], gmix_d, writes=[gmix])
        k.dma(sp, gffn[:], gffn_d, writes=[gffn])

        def rope_tables(pos_d, n, cosT, sinT, stk):
            pi_ = k.sb("pos_i%d" % n, [128, n], I32, stk)
            pf = k.sb("pos_f%d" % n, [128, n], F32, stk)
            iv = k.sb("invf%d" % n, [128, 32], F32, stk)
            ang = k.sb("ang%d" % n, [128, n, 32], F32, stk)
            tmp = k.sb("angt%d" % n, [128, n, 32], F32, stk)
            k.dma(sp, pi_[:], pos_d, writes=[pi_])
            k.dma(sp, iv[:], invf, writes=[iv])
            k.op(dve, lambda e: e.tensor_copy(pf[:], pi_[:]), reads=[pi_], writes=[pf])
            k.op(dve, lambda e: e.tensor_tensor(ang[:], pf[:].unsqueeze(2).to_broadcast([128, n, 32]),
                                                iv[:].unsqueeze(1).to_broadcast([128, n, 32]), ALU.mult),
                 reads=[pf, iv], writes=[ang])
            ki_ = k.sb("angk%d" % n, [128, n, 32], I32, stk)
            kf_ = k.sb("angf%d" % n, [128, n, 32], F32, stk)
            two_pi = float(2 * np.pi)
            C1 = 6.28125
            C2 = float(2 * np.pi - 6.28125)
            PI_SAFE = 3.1415925

            def sin_of(dst, shift):
                if shift != 0.0:
                    k.op(dve, lambda e: e.tensor_scalar(tmp[:], ang[:], shift, None, ALU.add), reads=[ang], writes=[tmp])
                    src = tmp
                else:
                    src = ang
                k.op(dve, lambda e: e.tensor_scalar(kf_[:], src[:], 1.0 / two_pi, None, ALU.mult),
                     reads=[src], writes=[kf_])
                k.op(dve, lambda e: e.tensor_copy(ki_[:], kf_[:]), reads=[kf_], writes=[ki_])
                k.op(dve, lambda e: e.tensor_copy(kf_[:], ki_[:]), reads=[ki_], writes=[kf_])
                k.op(dve, lambda e: e.scalar_tensor_tensor(tmp[:], kf_[:], -C1, src[:], ALU.mult, ALU.add),
                     reads=[kf_, src], writes=[tmp])
                k.op(dve, lambda e: e.scalar_tensor_tensor(tmp[:], kf_[:], -C2, tmp[:], ALU.mult, ALU.add),
                     reads=[kf_, tmp], writes=[tmp])
                k.op(dve, lambda e: e.tensor_scalar(kf_[:], tmp[:], float(np.pi), -two_pi, ALU.is_gt, ALU.mult),
                     reads=[tmp], writes=[kf_])
                k.op(dve, lambda e: e.tensor_tensor(tmp[:], tmp[:], kf_[:], ALU.add), reads=[tmp, kf_], writes=[tmp])
                k.op(dve, lambda e: e.tensor_scalar(kf_[:], tmp[:], -float(np.pi), two_pi, ALU.is_lt, ALU.mult),
                     reads=[tmp], writes=[kf_])
                k.op(dve, lambda e: e.tensor_tensor(tmp[:], tmp[:], kf_[:], ALU.add), reads=[tmp, kf_], writes=[tmp])
                k.op(dve, lambda e: e.tensor_scalar(tmp[:], tmp[:], -PI_SAFE, PI_SAFE, ALU.max, ALU.min),
                     reads=[tmp], writes=[tmp])
                k.op(act, lambda e: e.activation(dst[:], tmp[:], AF.Sin), reads=[tmp], writes=[dst])

            sin_of(sinT, 0.0)
            sin_of(cosT, float(0.5 * np.pi))

        hbN = 4
        hbX = [k.sb("hbx%d" % i, [128, D], BF16) for i in range(hbN - 2)]

        def norm_pre(xt, n):
            hbs = hbL + hbX
            ss, rs, hb = ssL[nrm_i[0] % 2], rsL[nrm_i[0] % 2], hbs[nrm_i[0] % hbN]
            junk = hb
            nrm_i[0] += 1
            k.op(dve, lambda e: e.memset(ss[0:n, :], 0.0), writes=[ss])
            k.op(act, lambda e: e.activation(junk[0:n, :], xt[0:n, :], AF.Square, accum_out=ss[0:n, 0:1]),
                 reads=[xt, ss], writes=[junk, ss])
            k.op(act, lambda e: e.activation(rs[0:n, :], ss[0:n, :], AF.Sqrt, bias=EPS, scale=1.0 / D),
                 reads=[ss], writes=[rs])
            k.op(dve, lambda e: e.reciprocal(rs[0:n, :], rs[0:n, :]), reads=[rs], writes=[rs])
            k.op(act, lambda e: e.activation(hb[0:n, :], xt[0:n, :], AF.Copy, scale=rs[0:n, 0:1]),
                 reads=[xt, rs], writes=[hb])
            return hb

        def norm_post(hb, n, gT, hT, hT_ap=None):
            if hT_ap is None:
                hT_ap = hT[:, :, 0:n]
            tp = k.bank()
            tpv = bf(tp[:])[:, 0:8 * 128].rearrange("p (a b) -> p a b", a=8)
            for kc in range(8):
                k.op(pe, lambda e, kc=kc: e.transpose(tpv[:, kc, 0:n], hb[0:n, kc * 128:(kc + 1) * 128],
                                                      ident[0:n, 0:n]),
                     reads=[hb, ident], writes=[tp])
            k.op(dve, lambda e: e.tensor_tensor(hT_ap, tpv[:, :, 0:n],
                                                gT[:].unsqueeze(2).to_broadcast([128, 8, n]), ALU.mult),
                 reads=[tp, gT], writes=[hT])

        def norm_T(xt, n, gT, hT, hT_ap=None):
            hb = norm_pre(xt, n)
            norm_post(hb, n, gT, hT, hT_ap)

        def proj_tok(hT, w, c0, ncols):
            pb = k.bank()
            for kc in range(8):
                k.op(pe, lambda e, kc=kc: e.matmul(pb[:, 0:ncols], hT[:, kc, :], w[:, kc, c0:c0 + ncols],
                                                   start=(kc == 0), stop=(kc == 7)),
                     reads=[hT, w], writes=[pb])
            return pb

        def rope(pb, nh, cosT, sinT, ti, outb, tmpA, tmpB):
            pv = pb[:, 0:nh * 64].rearrange("p (h d) -> p h d", h=nh)
            ov = outb[:, 0:nh * 64].rearrange("p (h d) -> p h d", h=nh)
            av = tmpA[:, 0:nh * 64].rearrange("p (h d) -> p h d", h=nh)
            bv = tmpB[:, 0:nh * 64].rearrange("p (h d) -> p h d", h=nh)
            cb = cosT[:, ti, :].unsqueeze(1).to_broadcast([128, nh, 32])
            sb_ = sinT[:, ti, :].unsqueeze(1).to_broadcast([128, nh, 32])
            k.op(dve, lambda e: e.tensor_tensor(av[:, :, 0:32], pv[:, :, 0:32], cb, ALU.mult),
                 reads=[pb, cosT], writes=[tmpA])
            k.op(dve, lambda e: e.tensor_tensor(av[:, :, 32:64], pv[:, :, 32:64], cb, ALU.mult),
                 reads=[pb, cosT], writes=[tmpA])
            k.op(dve, lambda e: e.tensor_tensor(bv[:, :, 0:32], pv[:, :, 32:64], sb_, ALU.mult),
                 reads=[pb, sinT], writes=[tmpB])
            k.op(dve, lambda e: e.tensor_tensor(bv[:, :, 32:64], pv[:, :, 0:32], sb_, ALU.mult),
                 reads=[pb, sinT], writes=[tmpB])
            k.op(dve, lambda e: e.tensor_tensor(ov[:, :, 0:32], av[:, :, 0:32], bv[:, :, 0:32], ALU.subtract),
                 reads=[tmpA, tmpB], writes=[outb])
            k.op(dve, lambda e: e.tensor_tensor(ov[:, :, 32:64], av[:, :, 32:64], bv[:, :, 32:64], ALU.add),
                 reads=[tmpA, tmpB], writes=[outb])

        w_in_v = w_in.rearrange("(k p) c -> p k c", p=128)
        def phase_A1():
            with ExitStack() as s1:
                cosF = k.sb("cosF", [128, NB, 32], F32, s1)
                sinF = k.sb("sinF", [128, NB, 32], F32, s1)
                rope_tables(pos_full, NB, cosF, sinF, s1)
                wA = k.sb("wA1", [128, 8, 1088], BF16, s1)
                for kc in range(8):
                    k.dma(pool, wA[:, kc, 0:1024], w_in_v[:, kc, C_K:C_K + 1024], writes=[wA])
                    k.dma(pool, wA[:, kc, 1024:1088], w_in_v[:, kc, C_KI:C_KI + 64], writes=[wA])
                xt2 = [k.sb("xtA%d" % i, [128, D], F32, s1) for i in range(2)]
                hT2 = [k.sb("hTA%d" % i, [128, 8, 128], BF16, s1) for i in range(2)]
                krL = [k.sb("kr%d" % i, [128, 512], BF16, s1) for i in range(2)]
                kirL = [k.sb("kir%d" % i, [128, 128], BF16, s1) for i in range(2)]
                tAL = [k.sb("tA%d" % i, [128, 512], F32, s1) for i in range(2)]
                tBL = [k.sb("tB%d" % i, [128, 512], F32, s1) for i in range(2)]
                tCL = [k.sb("tC%d" % i, [128, 64], F32, s1) for i in range(2)]
                tDL = [k.sb("tD%d" % i, [128, 64], F32, s1) for i in range(2)]
                hbs1 = {}

                def pre1(ti):
                    k.dma(sp, xt2[ti % 2][:], x_full[ti * 128:(ti + 1) * 128, :], writes=[xt2[ti % 2]])
                    hbs1[ti] = norm_pre(xt2[ti % 2], 128)

                def post1(ti):
                    norm_post(hbs1.pop(ti), 128, gmix, hT2[ti % 2])

                pre1(0)
                post1(0)
                pre1(1)
                for ti in range(NB):
                    if ti + 1 < NB:
                        post1(ti + 1)
                    hT = hT2[ti % 2]
                    kr, kir, tA, tB = krL[ti % 2], kirL[ti % 2], tAL[ti % 2], tBL[ti % 2]
                    tC, tD = tCL[ti % 2], tDL[ti % 2]
                    pk = proj_tok(hT, wA, 0, 512)
                    pv = proj_tok(hT, wA, 512, 512)
                    pki = proj_tok(hT, wA, 1024, 64)
                    if ti + 2 < NB:
                        pre1(ti + 2)
                    rope(pk, 8, cosF, sinF, ti, kr, tA, tB)
                    Vv = V[:, ti, :].rearrange("p (h d) -> p h d", h=8)
                    k.op(act, lambda e: e.activation(Vv[:, :, 0:64],
                                                     pv[:, 0:512].rearrange("p (h d) -> p h d", h=8), AF.Copy),
                         reads=[pv], writes=[V])
                    rope(pki, 1, cosF, sinF, ti, kir, tC, tD)
                    k.op(dve, lambda e, kir=kir: e.tensor_copy(kir[:, 64:128], kir[:, 0:64]), reads=[kir], writes=[kir])
                    tb = k.bank()
                    tbv = bf(tb[:])
                    for pr in range(4):
                        k.op(pe, lambda e, pr=pr: e.transpose(tbv[:, pr * 128:(pr + 1) * 128],
                                                              kr[:, pr * 128:(pr + 1) * 128], ident[:]),
                             reads=[kr, ident], writes=[tb])
                    k.op(pe, lambda e: e.transpose(tbv[:, 512:640], kir[:], ident[:]), reads=[kir, ident], writes=[tb])
                    k.op(act, lambda e: e.activation(KT[:, :, ti * 128:(ti + 1) * 128],
                                                     tbv[:, 0:512].rearrange("p (a b) -> p a b", a=4), AF.Copy),
                         reads=[tb], writes=[KT])
                    k.op(act, lambda e: e.activation(kiT[:, ti * 128:(ti + 1) * 128], tbv[:, 512:640], AF.Copy),
                         reads=[tb], writes=[kiT])
                k.barrier()
        def dbg_A1():
            for pr in range(4):
                k.dma(sp, dbg["kT"][:, pr, :], KT[:, pr, :], reads=[KT])
            for t8 in range(0, NB, 4):
                k.dma(sp, dbg["v"][:, t8:t8 + 4, :], V[:, t8:t8 + 4, :], reads=[V])
            k.dma(sp, dbg["kiT"], kiT[:], reads=[kiT])

        def phase_A2():
            with ExitStack() as s2:
                cosO = k.sb("cosO", [128, NS, 32], F32, s2)
                sinO = k.sb("sinO", [128, NS, 32], F32, s2)
                rope_tables(pos_own, NS, cosO, sinO, s2)
                WU, WQ, WQI, WWI, WGC, WGA = 0, 1024, 1536, 2048, 2056, 3080
                wB = k.sb("wB", [128, 8, 4104], BF16, s2)
                for kc in range(8):
                    k.dma(pool, wB[:, kc, WU:WU + 1024], w_in_v[:, kc, C_U:C_U + 1024], writes=[wB])
                    k.dma(pool, wB[:, kc, WQ:WQ + 512], w_in_v[:, kc, C_Q:C_Q + 512], writes=[wB])
                    k.dma(pool, wB[:, kc, WQI:WQI + 512], w_in_v[:, kc, C_QI:C_QI + 512], writes=[wB])
                    k.dma(pool, wB[:, kc, WWI:WWI + 8], w_in_v[:, kc, C_WI:C_WI + 8], writes=[wB])
                    k.dma(pool, wB[:, kc, WGC:WGC + 2048], w_in_v[:, kc, C_GC:C_GC + 2048], writes=[wB])
                wco = k.sb("wco", [128, 4, D], BF16, s2)
                for kc in range(4):
                    k.dma(pool, wco[:, kc, :], wco_d[kc * 128:(kc + 1) * 128, :], writes=[wco])
                wdw = k.sb("wdw", [128, 4, 31], F32, s2)
                bdw = k.sb("bdw", [128, 4], F32, s2)
                lng = k.sb("lng", [128, 4], F32, s2)
                lnb = k.sb("lnb", [128, 4], F32, s2)
                k.dma(sp, wdw[:], wdw_d, writes=[wdw])
                k.dma(sp, bdw[:], bdw_d, writes=[bdw])
                k.dma(sp, lng[:], lng_d, writes=[lng])
                k.dma(sp, lnb[:], lnb_d, writes=[lnb])
                Dg = k.sb("Dg", [128, 124, 128], BF16, s2)
                for c in range(4):
                    for j in range(31):
                        k.op(dve, lambda e, c=c, j=j: e.tensor_scalar(Dg[:, c * 31 + j, :], identf[:],
                                                                       wdw[:, c, j:j + 1], None, ALU.mult),
                             reads=[identf, wdw], writes=[Dg])
                ones = k.sb("ones", [128, 128], F32, s2)
                k.op(pool, lambda e: e.memset(ones[:], 1.0), writes=[ones])
                xt2 = [k.sb("xtB%d" % i, [128, D], F32, s2) for i in range(2)]
                xh2 = [k.sb("xhB%d" % i, [32, D], F32, s2) for i in range(2)]
                hT_L = [k.sb("hTB_%d" % i_, [128, 8, 128], BF16, s2) for i_ in range(2)]
                hTh_L = [k.sb("hThB_%d" % i_, [128, 8, 32], BF16, s2) for i_ in range(2)]
                qr_L = [k.sb("qr_%d" % i_, [128, 512], BF16, s2) for i_ in range(2)]
                qir_L = [k.sb("qir_%d" % i_, [128, 512], BF16, s2) for i_ in range(2)]
                tA_L = [k.sb("tA2_%d" % i_, [128, 512], F32, s2) for i_ in range(2)]
                tB_L = [k.sb("tB2_%d" % i_, [128, 512], F32, s2) for i_ in range(2)]
                qT_s_L = [k.sb("qT_s_%d" % i_, [128, 512], BF16, s2) for i_ in range(2)]
                qiT_s_L = [k.sb("qiT_s_%d" % i_, [128, 512], BF16, s2) for i_ in range(2)]
                sg_L = [k.sb("sgl_%d" % i_, [128, 160], F32, s2) for i_ in range(2)]
                gT_L = [k.sb("gT_%d" % i_, [128, 4, 160], BF16, s2) for i_ in range(2)]
                csb_L = [k.sb("csb_%d" % i_, [128, 4, 128], F32, s2) for i_ in range(2)]
                csq_L = [k.sb("csq_%d" % i_, [128, 4, 128], F32, s2) for i_ in range(2)]
                mean_L = [k.sb("mean_%d" % i_, [128, 128], F32, s2) for i_ in range(2)]
                msq_L = [k.sb("msq_%d" % i_, [128, 128], F32, s2) for i_ in range(2)]
                rstd_L = [k.sb("rstd_%d" % i_, [128, 128], F32, s2) for i_ in range(2)]
                nrm_L = [k.sb("nrm_%d" % i_, [128, 4, 128], F32, s2) for i_ in range(2)]
                snT_L = [k.sb("snT_%d" % i_, [128, 4, 128], BF16, s2) for i_ in range(2)]
                sgc_L = [k.sb("sgc_%d" % i_, [128, 8, 128], F32, s2) for i_ in range(2)]
                mc_s_L = [k.sb("mc_s_%d" % i_, [128, 8, 128], BF16, s2) for i_ in range(2)]
                sga_s_L = [k.sb("sga_s_%d" % i_, [128, 8, 128], BF16, s2) for i_ in range(2)]
                qTd_b = Buf("qTd_b")
                mcd_b = Buf("mcd_b")

                hbs2 = {}

                def pre2(j):
                    k.dma(sp, xt2[j % 2][:], x_own[j * 128:(j + 1) * 128, :], writes=[xt2[j % 2]])
                    k.dma(sp, xh2[j % 2][:], x_halo[j * 32:(j + 1) * 32, :], writes=[xh2[j % 2]])
                    hbs2[j] = (norm_pre(xh2[j % 2], 32), norm_pre(xt2[j % 2], 128))

                def post2(j):
                    a_, b_ = hbs2.pop(j)
                    norm_post(a_, 32, gmix, hTh_L[j % 2])
                    norm_post(b_, 128, gmix, hT_L[j % 2])

                for j in range(nslots):
                    xt = xt2[j % 2]
                    xh = xh2[j % 2]
                    hT, hTh, qr, qir, tA, tB, qT_s, qiT_s, sg, gT, csb, csq, mean, msq, rstd, nrm, snT, sgc, mc_s, sga_s = hT_L[j % 2], hTh_L[j % 2], qr_L[j % 2], qir_L[j % 2], tA_L[j % 2], tB_L[j % 2], qT_s_L[j % 2], qiT_s_L[j % 2], sg_L[j % 2], gT_L[j % 2], csb_L[j % 2], csq_L[j % 2], mean_L[j % 2], msq_L[j % 2], rstd_L[j % 2], nrm_L[j % 2], snT_L[j % 2], sgc_L[j % 2], mc_s_L[j % 2], sga_s_L[j % 2]
                    if j == 0:
                        pre2(0)
                        post2(0)
                        if nslots > 1:
                            pre2(1)
                    if j + 1 < nslots:
                        post2(j + 1)
                    pq = proj_tok(hT, wB, WQ, 512)
                    rope(pq, 8, cosO, sinO, j, qr, tA, tB)
                    pqi = proj_tok(hT, wB, WQI, 512)
                    rope(pqi, 8, cosO, sinO, j, qir, tA, tB)
                    pw = proj_tok(hT, wB, WWI, 8)
                    k.op(act, lambda e: e.activation(wi_all[:, j * 8:(j + 1) * 8], pw[:, 0:8], AF.Copy,
                                                     scale=IDX_SCALE), reads=[pw], writes=[wi_all])
                    for src, dstb, dd in ((qr, qT_s, qT_d), (qir, qiT_s, qiT_d)):
                        tb = k.bank()
                        tbv = bf(tb[:])
                        for pr in range(4):
                            k.op(pe, lambda e, pr=pr, src=src, tbv=tbv: e.transpose(
                                tbv[:, pr * 128:(pr + 1) * 128], src[:, pr * 128:(pr + 1) * 128], ident[:]),
                                 reads=[src, ident], writes=[tb])
                        k.op(act, lambda e, dstb=dstb, tbv=tbv: e.activation(dstb[:], tbv[:, 0:512], AF.Copy),
                             reads=[tb], writes=[dstb])
                        k.dma(sp, dd[j], dstb[:], reads=[dstb], writes=[qTd_b], sembuf=dstb)
                    for c in range(4):
                        pu = k.bank()
                        puv = pu[:, 0:320].rearrange("p (a b) -> p a b", a=2)
                        for half, col0 in ((0, WU + c * 128), (1, WU + 512 + c * 128)):
                            for kc in range(8):
                                k.op(pe, lambda e, kc=kc, half=half, col0=col0, puv=puv: e.matmul(
                                    puv[:, half, 0:32], wB[:, kc, col0:col0 + 128], hTh[:, kc, :],
                                    start=(kc == 0), stop=(kc == 7)), reads=[wB, hTh], writes=[pu])
                            for kc in range(8):
                                k.op(pe, lambda e, kc=kc, half=half, col0=col0, puv=puv: e.matmul(
                                    puv[:, half, 32:160], wB[:, kc, col0:col0 + 128], hT[:, kc, :],
                                    start=(kc == 0), stop=(kc == 7)), reads=[wB, hT], writes=[pu])
                        k.op(act, lambda e, puv=puv: e.activation(sg[:], puv[:, 1, :], AF.Sigmoid),
                             reads=[pu], writes=[sg])
                        k.op(dve, lambda e, puv=puv, c=c: e.tensor_tensor(gT[:, c, :], puv[:, 0, :], sg[:], ALU.mult),
                             reads=[pu, sg], writes=[gT])
                    if j + 2 < nslots:
                        pre2(j + 2)
                    pc = k.bank()
                    pcv = pc[:, 0:512].rearrange("p (a b) -> p a b", a=4)
                    for c in range(4):
                        for jj in range(31):
                            k.op(pe, lambda e, c=c, jj=jj: e.matmul(pcv[:, c, :], Dg[:, c * 31 + jj, :],
                                                                    gT[:, c, 2 + jj:2 + jj + 128],
                                                                    start=(jj == 0), stop=(jj == 30)),
                                 reads=[Dg, gT], writes=[pc])
                    k.op(dve, lambda e: e.tensor_tensor(csb[:], pcv, bdw[:].unsqueeze(2).to_broadcast([128, 4, 128]),
                                                        ALU.add), reads=[pc, bdw], writes=[csb])
                    k.op(act, lambda e: e.activation(csq[:], csb[:], AF.Square), reads=[csb], writes=[csq])
                    pst = k.bank()
                    for c in range(4):
                        k.op(pe, lambda e, c=c: e.matmul(pst[:, 0:128], ones[:], csb[:, c, :], start=(c == 0),
                                                         stop=(c == 3)), reads=[ones, csb], writes=[pst])
                    for c in range(4):
                        k.op(pe, lambda e, c=c: e.matmul(pst[:, 128:256], ones[:], csq[:, c, :], start=(c == 0),
                                                         stop=(c == 3)), reads=[ones, csq], writes=[pst])
                    k.op(act, lambda e: e.activation(mean[:], pst[:, 0:128], AF.Copy, scale=1.0 / 512),
                         reads=[pst], writes=[mean])
                    k.op(dve, lambda e: e.tensor_tensor(msq[:], mean[:], mean[:], ALU.mult), reads=[mean], writes=[msq])
                    k.op(dve, lambda e: e.scalar_tensor_tensor(rstd[:], pst[:, 128:256], 1.0 / 512, msq[:],
                                                               ALU.mult, ALU.subtract),
                         reads=[pst, msq], writes=[rstd])
                    k.op(act, lambda e: e.activation(rstd[:], rstd[:], AF.Sqrt, bias=EPS, scale=1.0),
                         reads=[rstd], writes=[rstd])
                    k.op(dve, lambda e: e.reciprocal(rstd[:], rstd[:]), reads=[rstd], writes=[rstd])
                    k.op(dve, lambda e: e.tensor_tensor(nrm[:], csb[:], mean[:].unsqueeze(1).to_broadcast([128, 4, 128]),
                                                        ALU.subtract), reads=[csb, mean], writes=[nrm])
                    k.op(dve, lambda e: e.tensor_tensor(nrm[:], nrm[:], rstd[:].unsqueeze(1).to_broadcast([128, 4, 128]),
                                                        ALU.mult), reads=[nrm, rstd], writes=[nrm])
                    for c in range(4):
                        k.op(act, lambda e, c=c: e.activation(snT[:, c, :], nrm[:, c, :], AF.Silu,
                                                              bias=lnb[:, c:c + 1], scale=lng[:, c:c + 1]),
                             reads=[nrm, lnb, lng], writes=[snT])
                    for gi, (wcol, dst) in enumerate(((WGC, None), (WGA, sga_s))):
                        for hb_ in range(2):
                            pg = k.bank()
                            pgv = pg[:, 0:512].rearrange("p (a b) -> p a b", a=4)
                            for m in range(4):
                                col0 = wcol + (hb_ * 4 + m) * 128
                                for kc in range(8):
                                    k.op(pe, lambda e, kc=kc, m=m, col0=col0, pgv=pgv: e.matmul(
                                        pgv[:, m, :], wB[:, kc, col0:col0 + 128], hT[:, kc, :],
                                        start=(kc == 0), stop=(kc == 7)), reads=[wB, hT], writes=[pg])
                            tgt = sgc if gi == 0 else sga_s
                            k.op(act, lambda e, pgv=pgv, tgt=tgt, hb_=hb_: e.activation(
                                tgt[:, hb_ * 4:(hb_ + 1) * 4, :], pgv, AF.Sigmoid), reads=[pg], writes=[tgt])
                    for hb_ in range(2):
                        py = k.bank()
                        pyv = py[:, 0:512].rearrange("p (a b) -> p a b", a=4)
                        for m in range(4):
                            mm = hb_ * 4 + m
                            for kc in range(4):
                                k.op(pe, lambda e, kc=kc, m=m, mm=mm, pyv=pyv: e.matmul(
                                    pyv[:, m, :], wco[:, kc, mm * 128:(mm + 1) * 128], snT[:, kc, :],
                                    start=(kc == 0), stop=(kc == 3)), reads=[wco, snT], writes=[py])
                        k.op(dve, lambda e, pyv=pyv, hb_=hb_: e.tensor_tensor(
                            mc_s[:, hb_ * 4:(hb_ + 1) * 4, :], pyv, sgc[:, hb_ * 4:(hb_ + 1) * 4, :], ALU.mult),
                             reads=[py, sgc], writes=[mc_s])
                    k.dma(sp, mc_d[j], mc_s[:].rearrange("p a b -> p (a b)"), reads=[mc_s], writes=[mcd_b], sembuf=mc_s)
                    k.dma(sp, sga_d[j], sga_s[:].rearrange("p a b -> p (a b)"), reads=[sga_s], writes=[mcd_b],
                          sembuf=sga_s)
                k.barrier()
        def dbg_A2():
            k.dma(sp, dbg["wi"], wi_all[:], reads=[wi_all])
            for nm, src in (("qT", qT_d), ("qiT", qiT_d), ("mc", mc_d), ("sga", sga_d)):
                b_ = Buf("dbgc_" + nm)
                k.dma(sp, dbg[nm], src, writes=[b_])
            k.barrier()

        def phase_B():
            with ExitStack() as s3:
                cm2 = [k.sb("cm%d" % i, [128, 256], F32, s3) for i in range(2)]
                pw2 = k.sb("pw2", [128, NIT + 2], F32, s3)
                for it in range(NIT + 2):
                    k.op(dve, lambda e, it=it: e.memset(pw2[:, it:it + 1], float(2.0 ** -(it + 1))), writes=[pw2])
                qT2 = [k.sb("qTb%d" % i, [128, 4, 256], BF16, s3) for i in range(2)]
                for b_ in qT2:
                    k.op(dve, lambda e: e.memset(b_[:], 0.0), writes=[b_])
                qiT2 = [k.sb("qiTb%d" % i, [128, 512], BF16, s3) for i in range(2)]
                scL = [k.sb("sc%d" % i, [128, S], F32, s3) for i in range(2)]
                AL = [k.sb("Ab%d" % i, [128, 512], BF16, s3) for i in range(4)]
                wabs = k.sb("wabs", [128, 8], F32, s3)
                sgn = k.sb("sgn", [128, 8], F32, s3)
                sgd = k.sb("sgd", [128, 8, 128], BF16, s3)
                msk = k.sb("msk", [128, S], BF16, s3)
                Mb = k.sb("Mb", [128, NB, 128], BF16, s3)
                st8 = k.sb("st8", [128, 8], F32, s3)
                Q = k.sb("Qtab", [128, NIT + 2], F32, s3)
                Q2 = k.sb("Q2tab", [128, NIT + 2], F32, s3)
                E2 = [k.sb("Eb%d" % i, [128, 4, 128], BF16, s3) for i in range(3)]
                rden = k.sb("rden", [128, 8], F32, s3)
                Osb = k.sb("Osb", [128, 2, 4, 66], F32, s3)
                thr = st8[:, 6:7]
                ai = [0]
                MBIG = 30000.0
                if debug:
                    attnf = k.sb("attnf", [128, 512], F32, s3)

                def loads_a(j):
                    p = j % 2
                    k.dma(sp, qiT2[p][:], qiT_d[j], writes=[qiT2[p]])
                    k.dma(sp, cm2[p][:], cmask_d[:, j, :], writes=[cm2[p]])

                def loads_b(j):
                    p = j % 2
                    k.dma(sp, qT2[p][0:64, :, 0:128], qT_d[j][0:64, :].rearrange("p (a b) -> p a b", a=4), writes=[qT2[p]])
                    k.dma(sp, qT2[p][64:128, :, 128:256], qT_d[j][64:128, :].rearrange("p (a b) -> p a b", a=4),
                          writes=[qT2[p]])

                def prep(j):
                    wsl = wi_all[:, j * 8:(j + 1) * 8]
                    k.op(dve, lambda e: e.tensor_scalar(sgn[:], wsl, 0.0, 2.0, ALU.is_ge, ALU.mult), reads=[wi_all], writes=[sgn])
                    k.op(dve, lambda e: e.tensor_scalar(sgn[:], sgn[:], -1.0, None, ALU.add), reads=[sgn], writes=[sgn])
                    k.op(dve, lambda e: e.tensor_tensor(wabs[:], wsl, sgn[:], ALU.mult), reads=[wi_all, sgn], writes=[wabs])
                    k.op(dve, lambda e: e.tensor_tensor(sgd[:], ident[:].unsqueeze(1).to_broadcast([128, 8, 128]),
                                                        sgn[:].unsqueeze(2).to_broadcast([128, 8, 128]), ALU.mult),
                         reads=[ident, sgn], writes=[sgd])

                def indexer(j):
                    sc = scL[j % 2]
                    qiTs = qiT2[j % 2]
                    L = 256 * (j + 1)
                    nch = (L + 511) // 512
                    for cc in range(nch):
                        c0 = cc * 512
                        W = min(512, L - c0)
                        pacc = k.bank(pin=True)
                        pbs = {}

                        def qi_mm(h):
                            pb = k.bank()
                            pp = (h % 2) * 64
                            k.op(pe, lambda e: e.matmul(pb[:, 0:W], qiTs[pp:pp + 64, (h // 2) * 128:(h // 2 + 1) * 128],
                                                        kiT[pp:pp + 64, c0:c0 + W], start=True, stop=True),
                                 reads=[qiTs, kiT], writes=[pb])
                            pbs[h] = pb

                        qi_mm(0)
                        qi_mm(1)
                        qi_mm(2)
                        for h in range(8):
                            if h + 3 < 8:
                                qi_mm(h + 3)
                            pb = pbs[h]
                            A = AL[ai[0] % 4]
                            ai[0] += 1
                            k.op(act, lambda e: e.activation(A[:, 0:W], pb[:, 0:W], AF.Relu, scale=wabs[:, h:h + 1]),
                                 reads=[pb, wabs], writes=[A])
                            k.op(pe, lambda e: e.matmul(pacc[:, 0:W], sgd[:, h, :], A[:, 0:W], start=(h == 0), stop=(h == 7)),
                                 reads=[sgd, A], writes=[pacc])
                        k.op(act, lambda e: e.activation(sc[:, c0:c0 + W], pacc[:, 0:W], AF.Copy), reads=[pacc], writes=[sc])
                        k.unpin(pacc)

                def thresh(j):
                    sc = scL[j % 2]
                    L = 256 * (j + 1)
                    cmask = cm2[j % 2]
                    if j == 0:
                        k.op(dve, lambda e: e.tensor_tensor(sc[:, L - 256:L], sc[:, L - 256:L], cmask[:], ALU.add),
                             reads=[sc, cmask], writes=[sc])
                        k.op(dve, lambda e: e.memset(thr, -1.0e29), writes=[st8])
                        return
                    k.op(dve, lambda e: e.tensor_reduce(st8[:, 0:1], sc[:, 0:L], AX.X, ALU.max), reads=[sc], writes=[st8])
                    k.op(dve, lambda e: e.tensor_reduce(st8[:, 1:2], sc[:, 0:L], AX.X, ALU.min), reads=[sc], writes=[st8])
                    k.op(dve, lambda e: e.tensor_tensor(sc[:, L - 256:L], sc[:, L - 256:L], cmask[:], ALU.add),
                         reads=[sc, cmask], writes=[sc])
                    k.op(dve, lambda e: e.tensor_tensor(st8[:, 2:3], st8[:, 0:1], st8[:, 1:2], ALU.subtract),
                         reads=[st8], writes=[st8])
                    k.op(dve, lambda e: e.tensor_scalar(st8[:, 5:6], st8[:, 2:3], float(2.0 ** -10), None, ALU.mult),
                         reads=[st8], writes=[st8])
                    k.op(dve, lambda e: e.tensor_scalar(st8[:, 2:3], st8[:, 2:3], float(1.0 + 2.0 ** -9), 1e-30,
                                                        ALU.mult, ALU.add), reads=[st8], writes=[st8])
                    k.op(dve, lambda e: e.tensor_tensor(st8[:, 7:8], st8[:, 1:2], st8[:, 5:6], ALU.subtract),
                         reads=[st8], writes=[st8])
                    k.op(dve, lambda e: e.tensor_scalar(Q[:], pw2[:], st8[:, 2:3], None, ALU.mult),
                         reads=[pw2, st8], writes=[Q])
                    k.op(dve, lambda e: e.tensor_scalar(Q2[:], Q[:], 2.0, None, ALU.mult), reads=[Q], writes=[Q2])
                    k.op(dve, lambda e: e.tensor_tensor(st8[:, 3:4], st8[:, 7:8], Q[:, 0:1], ALU.add),
                         reads=[st8, Q], writes=[st8])
                    for it in range(NIT):
                        k.op(dve, lambda e: e.memset(st8[:, 4:5], 0.0), writes=[st8])
                        k.op(dve, lambda e: e.tensor_scalar(msk[:, 0:L], sc[:, 0:L], st8[:, 3:4], 0.0,
                                                            ALU.is_ge, ALU.add, accum_out=st8[:, 4:5]),
                             reads=[sc, st8], writes=[st8, msk])
                        k.op(dve, lambda e: e.tensor_scalar(st8[:, 5:6], st8[:, 4:5], TOPK - 0.5,
                                                            Q2[:, it + 1:it + 2], ALU.is_ge, ALU.mult),
                             reads=[st8, Q2], writes=[st8])
                        k.op(dve, lambda e: e.scalar_tensor_tensor(st8[:, 3:4], st8[:, 3:4], Q[:, it + 1:it + 2],
                                                                   st8[:, 5:6], ALU.subtract, ALU.add),
                             reads=[st8, Q], writes=[st8])
                    k.op(dve, lambda e: e.tensor_tensor(thr, st8[:, 3:4], Q[:, NIT:NIT + 1], ALU.subtract),
                         reads=[st8, Q], writes=[st8])

                def mask(j):
                    sc = scL[j % 2]
                    L = 256 * (j + 1)
                    nkb = 2 * (j + 1)
                    if debug:
                        b_ = Buf("dbg_sc%d" % j)
                        for c0_ in range(0, L, 1024):
                            c1_ = min(L, c0_ + 1024)
                            k.dma(sp, dbg["sc"][j][:, c0_:c1_], sc[:, c0_:c1_], reads=[sc], writes=[b_], sembuf=b_)
                        k.dma(sp, dbg["thr"][j], thr, reads=[st8], writes=[b_], sembuf=b_)
                    for g0 in range(0, nkb, 8):
                        gn = min(8, nkb - g0)
                        k.op(dve, lambda e: e.tensor_scalar(msk[:, g0 * 128:(g0 + gn) * 128],
                                                            sc[:, g0 * 128:(g0 + gn) * 128], thr, None, ALU.is_ge),
                             reads=[sc, st8], writes=[msk])
                        tb = k.bank()
                        tbv = bf(tb[:])
                        for q_ in range(gn):
                            k.op(pe, lambda e: e.transpose(tbv[:, q_ * 128:(q_ + 1) * 128],
                                                           msk[:, (g0 + q_) * 128:(g0 + q_ + 1) * 128], ident[:]),
                                 reads=[msk, ident], writes=[tb])
                        k.op(act, lambda e: e.activation(Mb[:, g0:g0 + gn, :],
                                                         tbv[:, 0:gn * 128].rearrange("p (a b) -> p a b", a=gn),
                                                         AF.Identity, bias=-MBIG, scale=MBIG),
                             reads=[tb], writes=[Mb])

                def attention(j):
                    qTs = qT2[j % 2]
                    nkb = 2 * (j + 1)
                    po = [k.bank(pin=True), k.bank(pin=True)]
                    pov = [b_[:, 0:512].rearrange("p (h d) -> p h d", h=4) for b_ in po]
                    units = [(kb, hg) for kb in range(nkb) for hg in range(2)]

                    def qk(u):
                        kb, hg = units[u]
                        pl = k.bank()
                        plv = pl[:, 0:512].rearrange("p (a b) -> p a b", a=4)
                        k.op(pe, lambda e: e.matmul(plv, ident[:], Mb[:, kb, :].unsqueeze(1).to_broadcast([128, 4, 128]),
                                                    start=True, stop=False), reads=[ident, Mb], writes=[pl])
                        for pi in range(2):
                            pr = hg * 2 + pi
                            k.op(pe, lambda e: e.matmul(pl[:, pi * 256:(pi + 1) * 256], KT[:, pr, kb * 128:(kb + 1) * 128],
                                                        qTs[:, pr, :], start=False, stop=(pi == 1)),
                                 reads=[KT, qTs], writes=[pl])
                        return pl, plv

                    LA = 2
                    pend = [qk(u) for u in range(min(LA, len(units)))]
                    for u in range(len(units)):
                        if u + LA < len(units):
                            pend.append(qk(u + LA))
                        kb, hg = units[u]
                        pl, plv = pend.pop(0)
                        E = E2[u % 3]
                        k.op(act, lambda e: e.activation(E[:], plv, AF.Exp, scale=ATT_SCALE), reads=[pl], writes=[E])
                        for hh in range(4):
                            h = hg * 4 + hh
                            k.op(pe, lambda e: e.matmul(pov[hg][:, hh, 0:66], E[:, hh, :], V[:, kb, h * 66:(h + 1) * 66],
                                                        start=(kb == 0 and hh == 0), stop=(kb == nkb - 1 and hh == 3)),
                                 reads=[E, V], writes=[po[hg]])
                    for hg in range(2):
                        k.op(act, lambda e: e.activation(Osb[:, hg, :, :], pov[hg][:, :, 0:66], AF.Copy),
                             reads=[po[hg]], writes=[Osb])
                        k.unpin(po[hg])

                def finalize(j):
                    for hg in range(2):
                        k.op(dve, lambda e: e.reciprocal(rden[:, hg * 4:(hg + 1) * 4].unsqueeze(2), Osb[:, hg, :, 64:65]),
                             reads=[Osb], writes=[rden])
                        k.op(dve, lambda e: e.tensor_tensor(
                            attn_all[:, j, hg * 256:(hg + 1) * 256].rearrange("p (h d) -> p h d", h=4), Osb[:, hg, :, 0:64],
                            rden[:, hg * 4:(hg + 1) * 4].unsqueeze(2).to_broadcast([128, 4, 64]), ALU.mult),
                             reads=[Osb, rden], writes=[attn_all])
                    if debug:
                        k.op(dve, lambda e: e.tensor_copy(attnf[:], attn_all[:, j, :]), reads=[attn_all], writes=[attnf])
                        k.dma(sp, dbg["attn"][j], attnf[:], reads=[attnf])

                loads_a(0)
                loads_b(0)
                if nslots > 1:
                    loads_a(1)
                    loads_b(1)
                prep(0)
                indexer(0)
                thresh(0)
                mask(0)
                if nslots > 1:
                    prep(1)
                    indexer(1)
                for i in range(nslots):
                    if i + 2 < nslots:
                        prep(i + 2)
                    if i + 1 < nslots:
                        thresh(i + 1)
                    attention(i)
                    if i + 2 < nslots:
                        loads_a(i + 2)
                        indexer(i + 2)
                    if i + 1 < nslots:
                        mask(i + 1)
                    finalize(i)
                    if i + 2 < nslots:
                        loads_b(i + 2)
                k.barrier()

            with ExitStack() as s3:
                wao = k.sb("wao", [128, 4, D], BF16, s3)
                wo = k.sb("wo", [128, 8, D], BF16, s3)
                wr = k.sb("wr", [128, 8, 20], BF16, s3)
                br = k.sb("br", [128, 20], F32, s3)
                for kc in range(4):
                    k.dma(pool, wao[:, kc, :], wao_d[kc * 128:(kc + 1) * 128, :], writes=[wao])
                for kc in range(8):
                    k.dma(pool, wo[:, kc, :], wo_d[kc * 128:(kc + 1) * 128, :], writes=[wo])
                k.dma(pool, wr[:], wr_d.rearrange("(k p) c -> p k c", p=128), writes=[wr])
                k.dma(sp, br[:], br_d, writes=[br])
                mc2 = [k.sb("mcb%d" % i, [128, 8, 128], BF16, s3) for i in range(2)]
                sga2 = [k.sb("sgab%d" % i, [128, 8, 128], BF16, s3) for i in range(2)]
                xoL = [k.sb("xob%d" % i, [128, D], F32, s3) for i in range(2)]
                h2L = [k.sb("h2s%d" % i, [128, 8, 128], BF16, s3) for i in range(2)]
                h2d_b = Buf("h2d_b")
                attnTL = [k.sb("attnT%d" % i, [128, 4, 128], BF16, s3) for i in range(2)]
                mTL = [k.sb("mT%d" % i, [128, 8, 128], BF16, s3) for i in range(2)]
                x1L = [k.sb("x1_%d" % i, [128, D], F32, s3) for i in range(2)]
                lg = k.sb("lg", [128, 20], F32, s3)
                r8 = k.sb("r8", [128, 16], F32, s3)
                ohg = k.sb("ohg", [128, 4], F32, s3)
                t16 = k.sb("t16", [128, 16], F32, s3)
                esel = k.sb("esel", [128, 4], F32, s3)
                oh1 = k.sb("oh1", [128, 4], F32, s3)
                oh2 = k.sb("oh2", [128, 4], F32, s3)
                em = k.sb("em", [128, 4], F32, s3)
                we = k.sb("we", [128, 4], F32, s3)
                x1d_b = Buf("x1d_b")

                def loads_c(j):
                    p = j % 2
                    k.dma(sp, mc2[p][:].rearrange("p a b -> p (a b)"), mc_d[j], writes=[mc2[p]])
                    k.dma(sp, sga2[p][:].rearrange("p a b -> p (a b)"), sga_d[j], writes=[sga2[p]])
                    k.dma(sp, xoL[p][:], x_own[j * 128:(j + 1) * 128, :], writes=[xoL[p]])

                def tail(j):
                    mcs, sgas = mc2[j % 2], sga2[j % 2]
                    xo, x1, mT, attnT = xoL[j % 2], x1L[j % 2], mTL[j % 2], attnTL[j % 2]
                    tb = k.bank()
                    tbv = bf(tb[:])
                    for q_ in range(4):
                        k.op(pe, lambda e: e.transpose(tbv[:, q_ * 128:(q_ + 1) * 128],
                                                       attn_all[:, j, q_ * 128:(q_ + 1) * 128], ident[:]),
                             reads=[attn_all, ident], writes=[tb])
                    k.op(act, lambda e: e.activation(attnT[:], tbv[:, 0:512].rearrange("p (a b) -> p a b", a=4), AF.Copy),
                         reads=[tb], writes=[attnT])
                    x1v = x1[:].rearrange("p (a b) -> p a b", a=8)
                    for hb_ in range(2):
                        py = k.bank()
                        pyv = py[:, 0:512].rearrange("p (a b) -> p a b", a=4)
                        for m in range(4):
                            mm = hb_ * 4 + m
                            for kc in range(4):
                                k.op(pe, lambda e: e.matmul(pyv[:, m, :], wao[:, kc, mm * 128:(mm + 1) * 128], attnT[:, kc, :],
                                                            start=(kc == 0), stop=(kc == 3)), reads=[wao, attnT], writes=[py])
                        k.op(dve, lambda e: e.tensor_tensor(x1v[:, hb_ * 4:(hb_ + 1) * 4, :], pyv,
                                                            sgas[:, hb_ * 4:(hb_ + 1) * 4, :], ALU.mult),
                             reads=[py, sgas], writes=[x1])
                    k.op(dve, lambda e: e.tensor_tensor(mT[:], x1v, mcs[:], ALU.add), reads=[x1, mcs], writes=[mT])
                    for half in range(2):
                        px = k.bank()
                        for kc in range(8):
                            k.op(pe, lambda e: e.matmul(px[:, 0:512], mT[:, kc, :], wo[:, kc, half * 512:(half + 1) * 512],
                                                        start=(kc == 0), stop=(kc == 7)), reads=[mT, wo], writes=[px])
                        k.op(dve, lambda e: e.tensor_tensor(x1[:, half * 512:(half + 1) * 512], px[:, 0:512],
                                                            xo[:, half * 512:(half + 1) * 512], ALU.add),
                             reads=[px, xo], writes=[x1])
                    k.dma(sp, x1_d[j], x1[:], reads=[x1], writes=[x1d_b], sembuf=x1)
                    if debug:
                        k.dma(sp, dbg["x1"][j], x1[:], reads=[x1], sembuf=x1)
                    hbsT[j] = norm_pre(x1, 128)

                def tailB(j):
                    h2s = h2L[j % 2]
                    norm_post(hbsT.pop(j), 128, gffn, h2s)
                    k.dma(sp, h2T_d[j], h2s[:].rearrange("p a b -> p (a b)"), reads=[h2s], writes=[h2d_b], sembuf=h2s)
                    pr_ = k.bank()
                    for kc in range(8):
                        k.op(pe, lambda e: e.matmul(pr_[:, 0:20], h2s[:, kc, :], wr[:, kc, :],
                                                    start=(kc == 0), stop=(kc == 7)), reads=[h2s, wr], writes=[pr_])
                    k.op(dve, lambda e: e.tensor_tensor(lg[:], pr_[:, 0:20], br[:], ALU.add), reads=[pr_, br], writes=[lg])
                    k.op(dve, lambda e: e.tensor_reduce(r8[:, 0:1], lg[:, 0:4], AX.X, ALU.max), reads=[lg], writes=[r8])
                    k.op(dve, lambda e: e.tensor_scalar(r8[:, 1:2], r8[:, 0:1], -1.0, None, ALU.mult), reads=[r8], writes=[r8])
                    k.op(dve, lambda e: e.memset(r8[:, 2:3], 0.0), writes=[r8])
                    k.op(act, lambda e: e.activation(t16[:, 0:4], lg[:, 0:4], AF.Exp, bias=r8[:, 1:2], scale=1.0,
                                                     accum_out=r8[:, 2:3]), reads=[lg, r8], writes=[t16, r8])
                    k.op(dve, lambda e: e.reciprocal(r8[:, 3:4], r8[:, 2:3]), reads=[r8], writes=[r8])
                    k.op(dve, lambda e: e.tensor_scalar(ohg[:], lg[:, 0:4], r8[:, 0:1], r8[:, 3:4], ALU.is_ge, ALU.mult),
                         reads=[lg, r8], writes=[ohg])
                    k.op(dve, lambda e: e.tensor_scalar(oh1[:], lg[:, 0:4], r8[:, 0:1], None, ALU.is_ge),
                         reads=[lg, r8], writes=[oh1])
                    k.op(dve, lambda e: e.tensor_tensor(t16[:].rearrange("p (g e) -> p g e", g=4),
                                                        lg[:, 4:20].rearrange("p (g e) -> p g e", g=4),
                                                        oh1[:].unsqueeze(2).to_broadcast([128, 4, 4]), ALU.mult),
                         reads=[lg, oh1], writes=[t16])
                    k.op(dve, lambda e: e.tensor_reduce(esel[:], t16[:].rearrange("p (g e) -> p e g", g=4), AX.X, ALU.add),
                         reads=[t16], writes=[esel])
                    k.op(dve, lambda e: e.tensor_reduce(r8[:, 4:5], esel[:], AX.X, ALU.max), reads=[esel], writes=[r8])
                    k.op(dve, lambda e: e.tensor_scalar(oh1[:], esel[:], r8[:, 4:5], None, ALU.is_ge),
                         reads=[esel, r8], writes=[oh1])
                    k.op(dve, lambda e: e.scalar_tensor_tensor(em[:], oh1[:], NEG, esel[:], ALU.mult, ALU.add),
                         reads=[oh1, esel], writes=[em])
                    k.op(dve, lambda e: e.tensor_reduce(r8[:, 5:6], em[:], AX.X, ALU.max), reads=[em], writes=[r8])
                    k.op(dve, lambda e: e.tensor_scalar(oh2[:], em[:], r8[:, 5:6], None, ALU.is_ge),
                         reads=[em, r8], writes=[oh2])
                    k.op(dve, lambda e: e.tensor_tensor(r8[:, 6:7], r8[:, 4:5], r8[:, 5:6], ALU.subtract),
                         reads=[r8], writes=[r8])
                    k.op(act, lambda e: e.activation(r8[:, 7:8], r8[:, 6:7], AF.Sigmoid), reads=[r8], writes=[r8])
                    k.op(dve, lambda e: e.tensor_scalar(r8[:, 8:9], r8[:, 7:8], -1.0, 1.0, ALU.mult, ALU.add),
                         reads=[r8], writes=[r8])
                    k.op(dve, lambda e: e.tensor_scalar(we[:], oh1[:], r8[:, 7:8], None, ALU.mult), reads=[oh1, r8], writes=[we])
                    k.op(dve, lambda e: e.scalar_tensor_tensor(we[:], oh2[:], r8[:, 8:9], we[:], ALU.mult, ALU.add),
                         reads=[oh2, r8, we], writes=[we])
                    k.op(dve, lambda e: e.tensor_tensor(gw_all[:, j * 16:(j + 1) * 16].rearrange("p (g e) -> p g e", g=4),
                                                        ohg[:].unsqueeze(2).to_broadcast([128, 4, 4]),
                                                        we[:].unsqueeze(1).to_broadcast([128, 4, 4]), ALU.mult),
                         reads=[ohg, we], writes=[gw_all])

                hbsT = {}
                loads_c(0)
                if nslots > 1:
                    loads_c(1)
                tail(0)
                for j in range(nslots):
                    if j + 1 < nslots:
                        tail(j + 1)
                    tailB(j)
                    if j + 2 < nslots:
                        loads_c(j + 2)
                k.barrier()
        def phase_C():
            with ExitStack() as s4:
                acc = k.sb("acc", [128, NS, D], F32, s4)
                h2T = k.sb("h2T", [128, 8, NS * 128], BF16, s4)
                for j in range(nslots):
                    k.dma(sp, h2T[:, :, j * 128:(j + 1) * 128], h2T_d[j].rearrange("p (a b) -> p a b", a=8), writes=[h2T])
                for j in range(nslots):
                    k.dma(sp, acc[:, j, :], x1_d[j], writes=[acc])
                gfin = k.sb("gfin", [128, D], F32, s4)
                k.dma(sp, gfin[:], gfin_d, writes=[gfin])
                wg2 = [k.sb("wg%d" % i, [128, 8, 256], BF16, s4) for i in range(2)]
                wu2 = [k.sb("wu%d" % i, [128, 8, 256], BF16, s4) for i in range(2)]
                wd2 = [k.sb("wd%d" % i, [128, 2, D], BF16, s4) for i in range(2)]
                sgl2 = [k.sb("sgm%d" % i, [128, 512], F32, s4) for i in range(2)]
                hid2 = [k.sb("hid%d" % i, [128, 2, 512], BF16, s4) for i in range(2)]
                ot2 = [k.sb("ot%d" % i, [128, D], F32, s4) for i in range(2)]

                def wload(ex):
                    p = ex % 2
                    k.dma(pool, wg2[p][:], wg_d[ex].rearrange("(k p) c -> p k c", p=128), writes=[wg2[p]])
                    k.dma(pool, wu2[p][:], wu_d[ex].rearrange("(k p) c -> p k c", p=128), writes=[wu2[p]])
                    k.dma(pool, wd2[p][:], wd_d[ex].rearrange("(k p) c -> p k c", p=128), writes=[wd2[p]])

                ngrp = (nslots + 3) // 4
                wload(0)
                hi = 0
                for ex in range(16):
                    if ex + 1 < 16:
                        wload(ex + 1)
                    p = ex % 2
                    wg, wu, wd = wg2[p], wu2[p], wd2[p]
                    for tg in range(ngrp):
                        ns_ = min(4, nslots - tg * 4)
                        N = ns_ * 128
                        hid = hid2[hi % 2]
                        hi += 1
                        for fc in range(2):
                            pg = k.bank()
                            pu = k.bank()
                            for kc in range(8):
                                k.op(pe, lambda e, kc=kc, fc=fc, pg=pg: e.matmul(
                                    pg[:, 0:N], wg[:, kc, fc * 128:(fc + 1) * 128], h2T[:, kc, tg * 512:tg * 512 + N],
                                    start=(kc == 0), stop=(kc == 7)), reads=[wg, h2T], writes=[pg])
                            for kc in range(8):
                                k.op(pe, lambda e, kc=kc, fc=fc, pu=pu: e.matmul(
                                    pu[:, 0:N], wu[:, kc, fc * 128:(fc + 1) * 128], h2T[:, kc, tg * 512:tg * 512 + N],
                                    start=(kc == 0), stop=(kc == 7)), reads=[wu, h2T], writes=[pu])
                            sgl = sgl2[fc]
                            k.op(act, lambda e, pg=pg, sgl=sgl: e.activation(sgl[:, 0:N], pg[:, 0:N], AF.Silu),
                                 reads=[pg], writes=[sgl])
                            k.op(dve, lambda e, pu=pu, sgl=sgl, fc=fc, hid=hid: e.tensor_tensor(
                                hid[:, fc, 0:N], pu[:, 0:N], sgl[:, 0:N], ALU.mult), reads=[pu, sgl], writes=[hid])
                        for s_ in range(ns_):
                            j = tg * 4 + s_
                            for half in range(2):
                                py = k.bank()
                                for fc in range(2):
                                    k.op(pe, lambda e, fc=fc, half=half, py=py, s_=s_, hid=hid: e.matmul(
                                        py[:, 0:512], hid[:, fc, s_ * 128:(s_ + 1) * 128],
                                        wd[:, fc, half * 512:(half + 1) * 512], start=(fc == 0), stop=(fc == 1)),
                                         reads=[hid, wd], writes=[py])
                                k.op(dve, lambda e, py=py, j=j, half=half, ex=ex: e.scalar_tensor_tensor(
                                    acc[:, j, half * 512:(half + 1) * 512], py[:, 0:512],
                                    gw_all[:, j * 16 + ex:j * 16 + ex + 1], acc[:, j, half * 512:(half + 1) * 512],
                                    ALU.mult, ALU.add), reads=[py, gw_all, acc], writes=[acc])
                for j in range(nslots):
                    ot = ot2[j % 2]
                    ss, rs, junk = ssL[j % 2], rsL[j % 2], hbL[j % 2]
                    k.op(dve, lambda e, ss=ss: e.memset(ss[:], 0.0), writes=[ss])
                    k.op(act, lambda e, j=j, junk=junk, ss=ss: e.activation(junk[:], acc[:, j, :], AF.Square, accum_out=ss[:, 0:1]),
                         reads=[acc, ss], writes=[junk, ss])
                    k.op(act, lambda e, rs=rs, ss=ss: e.activation(rs[:], ss[:], AF.Sqrt, bias=EPS, scale=1.0 / D), reads=[ss], writes=[rs])
                    k.op(dve, lambda e, rs=rs: e.reciprocal(rs[:], rs[:]), reads=[rs], writes=[rs])
                    k.op(dve, lambda e, j=j, ot=ot, rs=rs: e.scalar_tensor_tensor(ot[:], acc[:, j, :], rs[:, 0:1], gfin[:],
                                                                          ALU.mult, ALU.mult),
                         reads=[acc, rs, gfin], writes=[ot])
                    k.dma(sp, out_d[j * 128:(j + 1) * 128, :], ot[:], reads=[ot], sembuf=ot)
                k.barrier()
        if "A2" in phases:
            phase_A2()
            if debug:
                dbg_A2()
        with ExitStack() as skv:
            KT = k.sb("KT", [128, 4, S], BF16, skv)
            V = k.sb("V", [128, NB, 8 * 66], BF16, skv)
            kiT = k.sb("kiT", [128, S], BF16, skv)
            k.op(pool, lambda e: e.memset(V[:], 1.0), writes=[V])
            if "A1" in phases:
                phase_A1()
                if debug:
                    dbg_A1()
            attn_all = k.sb("attn_all", [128, NS, 512], BF16, skv)
            if "B" in phases:
                phase_B()
                if debug:
                    k.dma(sp, dbg["gw"], gw_all[:], reads=[gw_all])
            k.barrier()
        if "C" in phases:
            phase_C()
        k.barrier()
        build_nc.nins = k.nins
    return nc


def own_blocks(r):
    return [2 * j + ((j % 2) ^ r) for j in range(NS)]


def prep_core(inp, c):
    b, r = c // 2, c % 2
    x = np.asarray(inp["x"], dtype=np.float32)
    pos = np.asarray(inp["positions"]).astype(np.int32)
    blocks = own_blocks(r)
    xb = x[b]
    x_own = np.concatenate([xb[i * 128:(i + 1) * 128] for i in blocks], axis=0)
    x_halo = np.zeros((NS * 32, D), np.float32)
    for j, i in enumerate(blocks):
        if i > 0:
            x_halo[j * 32:(j + 1) * 32] = xb[i * 128 - 32:i * 128]
    pos_full = np.ascontiguousarray(pos[b].reshape(NB, 128).T)
    pos_own = np.ascontiguousarray(np.stack([pos[b][i * 128:(i + 1) * 128] for i in blocks], axis=1))
    invf = (1.0 / (np.float32(10000.0) ** (np.arange(0, 64, 2, dtype=np.float32) / np.float32(64)))).astype(np.float32)
    invf = np.ascontiguousarray(np.broadcast_to(invf[None, :], (128, 32)))
    cmask = np.zeros((128, NS, 256), np.float32)
    for j, i in enumerate(blocks):
        qidx = i * 128 + np.arange(128)[:, None]
        kidx = 256 * j + np.arange(256)[None, :]
        cmask[:, j, :] = np.where(kidx > qidx, np.float32(NEG), np.float32(0.0))

    def fm(v, nchunk):
        return np.ascontiguousarray(np.asarray(v, np.float32).reshape(nchunk, 128).T)

    w_dw = np.asarray(inp["w_dw"], np.float32)[0, :, 0, :]
    wdw = np.ascontiguousarray(w_dw.T.reshape(4, 128, 31).transpose(1, 0, 2))
    w_r = np.concatenate([np.asarray(inp["w_rg"], np.float32)[0],
                          np.asarray(inp["w_re"], np.float32)[0].reshape(D, 16)], axis=1)
    b_r = np.concatenate([np.asarray(inp["b_rg"], np.float32)[0], np.asarray(inp["b_re"], np.float32)[0].reshape(16)])
    m = {
        "x_full": np.ascontiguousarray(xb), "x_own": x_own, "x_halo": x_halo,
        "pos_full": pos_full, "pos_own": pos_own, "invf": invf, "cmask": cmask,
        "ident": np.eye(128, dtype=np.float32),
        "w_in": np.ascontiguousarray(np.asarray(inp["w_in"], np.float32)[0]),
        "wdw": wdw, "bdw": fm(inp["b_dw"][0], 4), "lng": fm(inp["ln_g"][0], 4), "lnb": fm(inp["ln_b"][0], 4),
        "gmix": fm(inp["g_mix"][0], 8), "gffn": fm(inp["g_ffn"][0], 8),
        "gfin": np.ascontiguousarray(np.broadcast_to(np.asarray(inp["g_final"], np.float32)[None, :], (128, D))),
        "b_r": np.ascontiguousarray(np.broadcast_to(b_r[None, :], (128, 20))),
        "w_r": np.ascontiguousarray(w_r),
        "w_conv_out": np.ascontiguousarray(np.asarray(inp["w_conv_out"], np.float32)[0]),
        "w_attn_out": np.ascontiguousarray(np.asarray(inp["w_attn_out"], np.float32)[0]),
        "w_o": np.ascontiguousarray(np.asarray(inp["w_o"], np.float32)[0]),
        "w_gate": np.ascontiguousarray(np.asarray(inp["w_gate"], np.float32)[0]),
        "w_up": np.ascontiguousarray(np.asarray(inp["w_up"], np.float32)[0]),
        "w_down": np.ascontiguousarray(np.asarray(inp["w_down"], np.float32)[0]),
    }
    return m


_NC_CACHE = {}


def kernel(**inputs):
    if "nc" not in _NC_CACHE:
        _NC_CACHE["nc"] = build_nc()
    nc = _NC_CACHE["nc"]
    in_maps = [prep_core(inputs, c) for c in range(8)]
    res = run_bass_kernel_spmd(nc, in_maps, core_ids=list(range(8)))
    out = np.empty((4, S, D), np.float32)
    for c in range(8):
        b, r = c // 2, c % 2
        o = np.asarray(res.results[c]["out"], dtype=np.float32)
        for j, i in enumerate(own_blocks(r)):
            out[b, i * 128:(i + 1) * 128] = o[j * 128:(j + 1) * 128]
    return out
```

```python
from contextlib import ExitStack
import numpy as np
import concourse.bass as bass
import concourse.mybir as mybir
from concourse.bass_utils import run_bass_kernel_spmd

F32 = mybir.dt.float32
BF16 = mybir.dt.bfloat16
I32 = mybir.dt.int32
ALU = mybir.AluOpType
AF = mybir.ActivationFunctionType
AX = mybir.AxisListType

D = 1024
S = 4096
NB = 32
NS = 16
EPS = 1e-6
NIT = 14
TOPK = 256
NEG = -1.0e30
C_U, C_Q, C_K, C_V, C_QI, C_KI, C_WI, C_GC, C_GA = 0, 1024, 1536, 2048, 2560, 3072, 3136, 3144, 4168
IDX_SCALE = float((8 ** -0.5) * (64 ** -0.5))
ATT_SCALE = float(64 ** -0.5)


class Buf:
    __slots__ = ("name", "t", "lw", "rd", "dsem", "dcnt")

    def __init__(self, name, t=None):
        self.name = name
        self.t = t
        self.lw = None
        self.rd = {}
        self.dsem = None
        self.dcnt = 0

    def __getitem__(self, key):
        return self.t[key]


class Eng:
    def __init__(self, k, name, eng):
        self.name = name
        self.eng = eng
        self.sem = k.new_sem("e_" + name)
        self.cnt = 0
        self.seen = {}

    def wait_tok(self, tok):
        if tok is None:
            return
        sem, val, _ = tok
        key = id(sem)
        if self.seen.get(key, 0) >= val:
            return
        self.eng.wait_ge(sem, val)
        self.seen[key] = val


class K:
    def __init__(self, nc, stack):
        self.nc = nc
        self.stack = stack
        self.pe = Eng(self, "pe", nc.tensor)
        self.act = Eng(self, "act", nc.scalar)
        self.dve = Eng(self, "dve", nc.vector)
        self.pool = Eng(self, "pool", nc.gpsimd)
        self.sp = Eng(self, "sp", nc.sync)
        self.engs = [self.pe, self.act, self.dve, self.pool, self.sp]
        self.dma_bufs = []
        self.nins = 0
        self.banks = []
        self.bank_i = 0
        self.pinned = set()

    def new_sem(self, name):
        return self.stack.enter_context(self.nc.semaphore(name))

    def sb(self, name, shape, dt, stack=None):
        t = (stack or self.stack).enter_context(self.nc.sbuf_tensor("s_" + name, list(shape), dt))
        return Buf(name, t)

    def mk_banks(self):
        for i in range(8):
            t = self.stack.enter_context(self.nc.psum_tensor("bank%d" % i, [128, 512], F32))
            self.banks.append(Buf("bank%d" % i, t))

    def bank(self, pin=False):
        for _ in range(16):
            b = self.banks[self.bank_i % 8]
            self.bank_i += 1
            if b.name not in self.pinned:
                if pin:
                    self.pinned.add(b.name)
                return b
        raise RuntimeError("no psum bank")

    def unpin(self, b):
        self.pinned.discard(b.name)

    def _deps(self, e, reads, writes):
        for b in reads:
            if b.lw is not None:
                e.wait_tok(b.lw)
        for b in writes:
            if b.lw is not None and b.lw[2] != e.name:
                e.wait_tok(b.lw)
            for en, tok in b.rd.items():
                if en != e.name:
                    e.wait_tok(tok)

    def _commit(self, tok, reads, writes):
        for b in reads:
            b.rd[tok[2]] = tok
        for b in writes:
            b.lw = tok
            b.rd = {}

    def op(self, e, fn, reads=(), writes=()):
        self._deps(e, reads, writes)
        ins = fn(e.eng)
        e.cnt += 1
        ins.then_inc(e.sem, 1)
        tok = (e.sem, e.cnt, e.name)
        self._commit(tok, reads, writes)
        self.nins += 1
        return tok

    def dma(self, e, out, in_, reads=(), writes=(), sembuf=None):
        self._deps(e, reads, writes)
        sb_ = sembuf if sembuf is not None else (writes[0] if writes else reads[0])
        if sb_.dsem is None:
            sb_.dsem = self.new_sem("d_" + sb_.name)
            self.dma_bufs.append(sb_)
        ins = e.eng.dma_start(out=out, in_=in_)
        sb_.dcnt += 16
        ins.then_inc(sb_.dsem, 16)
        tok = (sb_.dsem, sb_.dcnt, "dma_" + sb_.name)
        self._commit(tok, reads, writes)
        self.nins += 1
        return tok

    def barrier(self):
        toks = [(e.sem, e.cnt, e.name) for e in self.engs if e.cnt > 0]
        toks += [(b.dsem, b.dcnt, "dma") for b in self.dma_bufs]
        for e in self.engs:
            for t in toks:
                if t[2] != e.name:
                    e.wait_tok(t)


def bf(ap):
    return ap.bitcast(BF16)


def build_nc(phases=("A1", "A2", "B", "C"), debug=False, nslots=NS, bstop=9):
    nc = bass.Bass("TRN2", target_bir_lowering=False)

    def din(name, shape, dt=F32):
        return nc.dram_tensor(name, list(shape), dt, kind="ExternalInput").ap()

    def dscr(name, shape, dt):
        return nc.dram_tensor(name, list(shape), dt, kind="Internal").ap()

    x_full = din("x_full", [S, D])
    x_own = din("x_own", [NS * 128, D])
    x_halo = din("x_halo", [NS * 32, D])
    pos_full = din("pos_full", [128, NB], I32)
    pos_own = din("pos_own", [128, NS], I32)
    invf = din("invf", [128, 32])
    cmask_d = din("cmask", [128, NS, 256])
    ident_d = din("ident", [128, 128])
    w_in = din("w_in", [D, 5192])
    wdw_d = din("wdw", [128, 4, 31])
    bdw_d = din("bdw", [128, 4])
    lng_d = din("lng", [128, 4])
    lnb_d = din("lnb", [128, 4])
    gmix_d = din("gmix", [128, 8])
    gffn_d = din("gffn", [128, 8])
    gfin_d = din("gfin", [128, D])
    br_d = din("b_r", [128, 20])
    wr_d = din("w_r", [D, 20])
    wco_d = din("w_conv_out", [512, D])
    wao_d = din("w_attn_out", [512, D])
    wo_d = din("w_o", [D, D])
    wg_d = din("w_gate", [16, D, 256])
    wu_d = din("w_up", [16, D, 256])
    wd_d = din("w_down", [16, 256, D])
    out_d = nc.dram_tensor("out", [NS * 128, D], F32, kind="ExternalOutput").ap()

    qT_d = dscr("qT_d", [NS, 128, 512], BF16)
    qiT_d = dscr("qiT_d", [NS, 128, 512], BF16)
    mc_d = dscr("mc_d", [NS, 128, 1024], BF16)
    sga_d = dscr("sga_d", [NS, 128, 1024], BF16)
    x1_d = dscr("x1_d", [NS, 128, D], F32)
    h2T_d = dscr("h2T_d", [NS, 128, 1024], BF16)
    dbg = {}
    if debug:
        dbg["kT"] = nc.dram_tensor("dbg_kT", [128, 4, S], BF16, kind="ExternalOutput").ap()
        dbg["v"] = nc.dram_tensor("dbg_v", [128, NB, 8 * 66], BF16, kind="ExternalOutput").ap()
        dbg["kiT"] = nc.dram_tensor("dbg_kiT", [128, S], BF16, kind="ExternalOutput").ap()
        dbg["qT"] = nc.dram_tensor("dbg_qT", [NS, 128, 512], BF16, kind="ExternalOutput").ap()
        dbg["qiT"] = nc.dram_tensor("dbg_qiT", [NS, 128, 512], BF16, kind="ExternalOutput").ap()
        dbg["mc"] = nc.dram_tensor("dbg_mc", [NS, 128, 1024], BF16, kind="ExternalOutput").ap()
        dbg["sga"] = nc.dram_tensor("dbg_sga", [NS, 128, 1024], BF16, kind="ExternalOutput").ap()
        dbg["wi"] = nc.dram_tensor("dbg_wi", [128, NS * 8], F32, kind="ExternalOutput").ap()
        dbg["x1"] = nc.dram_tensor("dbg_x1", [NS, 128, D], F32, kind="ExternalOutput").ap()
        dbg["gw"] = nc.dram_tensor("dbg_gw", [128, NS * 16], F32, kind="ExternalOutput").ap()
        dbg["sc"] = nc.dram_tensor("dbg_sc", [NS, 128, S], F32, kind="ExternalOutput").ap()
        dbg["thr"] = nc.dram_tensor("dbg_thr", [NS, 128, 1], F32, kind="ExternalOutput").ap()
        dbg["attn"] = nc.dram_tensor("dbg_attn", [NS, 128, 512], F32, kind="ExternalOutput").ap()

    with ExitStack() as st:
        k = K(nc, st)
        pe, act, dve, pool, sp = k.pe, k.act, k.dve, k.pool, k.sp
        k.mk_banks()

        ident = k.sb("ident", [128, 128], BF16)
        identf = k.sb("identf", [128, 128], F32)
        wi_all = k.sb("wi_all", [128, NS * 8], F32)
        gw_all = k.sb("gw_all", [128, NS * 16], F32)
        gmix = k.sb("gmix", [128, 8], F32)
        gffn = k.sb("gffn", [128, 8], F32)
        ssL = [k.sb("ss%d" % i, [128, 1], F32) for i in range(2)]
        rsL = [k.sb("rs%d" % i, [128, 1], F32) for i in range(2)]
        hbL = [k.sb("hb%d" % i, [128, D], BF16) for i in range(2)]
        nrm_i = [0]

        k.dma(sp, identf[:], ident_d, writes=[identf])
        k.dma(pool, ident[:], ident_d, writes=[ident])
        k.dma(sp, gmix[:], gmix_d, writes=[gmix])
        k.dma(sp, gffn[:], gffn_d, writes=[gffn])

        def rope_tables(pos_d, n, cosT, sinT, stk):
            pi_ = k.sb("pos_i%d" % n, [128, n], I32, stk)
            pf = k.sb("pos_f%d" % n, [128, n], F32, stk)
            iv = k.sb("invf%d" % n, [128, 32], F32, stk)
            ang = k.sb("ang%d" % n, [128, n, 32], F32, stk)
            tmp = k.sb("angt%d" % n, [128, n, 32], F32, stk)
            k.dma(sp, pi_[:], pos_d, writes=[pi_])
            k.dma(sp, iv[:], invf, writes=[iv])
            k.op(dve, lambda e: e.tensor_copy(pf[:], pi_[:]), reads=[pi_], writes=[pf])
            k.op(dve, lambda e: e.tensor_tensor(ang[:], pf[:].unsqueeze(2).to_broadcast([128, n, 32]),
                                                iv[:].unsqueeze(1).to_broadcast([128, n, 32]), ALU.mult),
                 reads=[pf, iv], writes=[ang])
            ki_ = k.sb("angk%d" % n, [128, n, 32], I32, stk)
            kf_ = k.sb("angf%d" % n, [128, n, 32], F32, stk)
            two_pi = float(2 * np.pi)
            C1 = 6.28125
            C2 = float(2 * np.pi - 6.28125)
            PI_SAFE = 3.1415925

            def sin_of(dst, shift):
                if shift != 0.0:
                    k.op(dve, lambda e: e.tensor_scalar(tmp[:], ang[:], shift, None, ALU.add), reads=[ang], writes=[tmp])
                    src = tmp
                else:
                    src = ang
                k.op(dve, lambda e: e.tensor_scalar(kf_[:], src[:], 1.0 / two_pi, None, ALU.mult),
                     reads=[src], writes=[kf_])
                k.op(dve, lambda e: e.tensor_copy(ki_[:], kf_[:]), reads=[kf_], writes=[ki_])
                k.op(dve, lambda e: e.tensor_copy(kf_[:], ki_[:]), reads=[ki_], writes=[kf_])
                k.op(dve, lambda e: e.scalar_tensor_tensor(tmp[:], kf_[:], -C1, src[:], ALU.mult, ALU.add),
                     reads=[kf_, src], writes=[tmp])
                k.op(dve, lambda e: e.scalar_tensor_tensor(tmp[:], kf_[:], -C2, tmp[:], ALU.mult, ALU.add),
                     reads=[kf_, tmp], writes=[tmp])
                k.op(dve, lambda e: e.tensor_scalar(kf_[:], tmp[:], float(np.pi), -two_pi, ALU.is_gt, ALU.mult),
                     reads=[tmp], writes=[kf_])
                k.op(dve, lambda e: e.tensor_tensor(tmp[:], tmp[:], kf_[:], ALU.add), reads=[tmp, kf_], writes=[tmp])
                k.op(dve, lambda e: e.tensor_scalar(kf_[:], tmp[:], -float(np.pi), two_pi, ALU.is_lt, ALU.mult),
                     reads=[tmp], writes=[kf_])
                k.op(dve, lambda e: e.tensor_tensor(tmp[:], tmp[:], kf_[:], ALU.add), reads=[tmp, kf_], writes=[tmp])
                k.op(dve, lambda e: e.tensor_scalar(tmp[:], tmp[:], -PI_SAFE, PI_SAFE, ALU.max, ALU.min),
                     reads=[tmp], writes=[tmp])
                k.op(act, lambda e: e.activation(dst[:], tmp[:], AF.Sin), reads=[tmp], writes=[dst])

            sin_of(sinT, 0.0)
            sin_of(cosT, float(0.5 * np.pi))

        hbN = 4
        hbX = [k.sb("hbx%d" % i, [128, D], BF16) for i in range(hbN - 2)]

        def norm_pre(xt, n, src=None):
            if src is None:
                src = xt[0:n, :]
            hbs = hbL + hbX
            ss, rs, hb = ssL[nrm_i[0] % 2], rsL[nrm_i[0] % 2], hbs[nrm_i[0] % hbN]
            junk = hb
            nrm_i[0] += 1
            k.op(dve, lambda e: e.memset(ss[0:n, :], 0.0), writes=[ss])
            k.op(act, lambda e: e.activation(junk[0:n, :], src, AF.Square, accum_out=ss[0:n, 0:1]),
                 reads=[xt, ss], writes=[junk, ss])
            k.op(act, lambda e: e.activation(rs[0:n, :], ss[0:n, :], AF.Sqrt, bias=EPS, scale=1.0 / D),
                 reads=[ss], writes=[rs])
            k.op(dve, lambda e: e.reciprocal(rs[0:n, :], rs[0:n, :]), reads=[rs], writes=[rs])
            k.op(act, lambda e: e.activation(hb[0:n, :], src, AF.Copy, scale=rs[0:n, 0:1]),
                 reads=[xt, rs], writes=[hb])
            return hb

        def norm_post(hb, n, gT, hT, hT_ap=None):
            if hT_ap is None:
                hT_ap = hT[:, :, 0:n]
            tp = k.bank()
            tpv = bf(tp[:])[:, 0:8 * 128].rearrange("p (a b) -> p a b", a=8)
            for kc in range(8):
                k.op(pe, lambda e, kc=kc: e.transpose(tpv[:, kc, 0:n], hb[0:n, kc * 128:(kc + 1) * 128],
                                                      ident[0:n, 0:n]),
                     reads=[hb, ident], writes=[tp])
            k.op(dve, lambda e: e.tensor_tensor(hT_ap, tpv[:, :, 0:n],
                                                gT[:].unsqueeze(2).to_broadcast([128, 8, n]), ALU.mult),
                 reads=[tp, gT], writes=[hT])

        def norm_T(xt, n, gT, hT, hT_ap=None):
            hb = norm_pre(xt, n)
            norm_post(hb, n, gT, hT, hT_ap)

        def proj_tok(hT, w, c0, ncols):
            pb = k.bank()
            for kc in range(8):
                k.op(pe, lambda e, kc=kc: e.matmul(pb[:, 0:ncols], hT[:, kc, :], w[:, kc, c0:c0 + ncols],
                                                   start=(kc == 0), stop=(kc == 7)),
                     reads=[hT, w], writes=[pb])
            return pb

        def rope(pb, nh, cosT, sinT, ti, outb, tmpA, tmpB):
            pv = pb[:, 0:nh * 64].rearrange("p (h d) -> p h d", h=nh)
            ov = outb[:, 0:nh * 64].rearrange("p (h d) -> p h d", h=nh)
            av = tmpA[:, 0:nh * 64].rearrange("p (h d) -> p h d", h=nh)
            bv = tmpB[:, 0:nh * 64].rearrange("p (h d) -> p h d", h=nh)
            cb = cosT[:, ti, :].unsqueeze(1).to_broadcast([128, nh, 32])
            sb_ = sinT[:, ti, :].unsqueeze(1).to_broadcast([128, nh, 32])
            k.op(dve, lambda e: e.tensor_tensor(av[:, :, 0:32], pv[:, :, 0:32], cb, ALU.mult),
                 reads=[pb, cosT], writes=[tmpA])
            k.op(dve, lambda e: e.tensor_tensor(av[:, :, 32:64], pv[:, :, 32:64], cb, ALU.mult),
                 reads=[pb, cosT], writes=[tmpA])
            k.op(dve, lambda e: e.tensor_tensor(bv[:, :, 0:32], pv[:, :, 32:64], sb_, ALU.mult),
                 reads=[pb, sinT], writes=[tmpB])
            k.op(dve, lambda e: e.tensor_tensor(bv[:, :, 32:64], pv[:, :, 0:32], sb_, ALU.mult),
                 reads=[pb, sinT], writes=[tmpB])
            k.op(dve, lambda e: e.tensor_tensor(ov[:, :, 0:32], av[:, :, 0:32], bv[:, :, 0:32], ALU.subtract),
                 reads=[tmpA, tmpB], writes=[outb])
            k.op(dve, lambda e: e.tensor_tensor(ov[:, :, 32:64], av[:, :, 32:64], bv[:, :, 32:64], ALU.add),
                 reads=[tmpA, tmpB], writes=[outb])

        w_in_v = w_in.rearrange("(k p) c -> p k c", p=128)
        def phase_A1():
            with ExitStack() as s1:
                cosF = k.sb("cosF", [128, NB, 32], F32, s1)
                sinF = k.sb("sinF", [128, NB, 32], F32, s1)
                rope_tables(pos_full, NB, cosF, sinF, s1)
                wA = k.sb("wA1", [128, 8, 1088], BF16, s1)
                for kc in range(8):
                    k.dma(pool, wA[:, kc, 0:1024], w_in_v[:, kc, C_K:C_K + 1024], writes=[wA])
                    k.dma(pool, wA[:, kc, 1024:1088], w_in_v[:, kc, C_KI:C_KI + 64], writes=[wA])
                xt2 = [k.sb("xtA%d" % i, [128, D], F32, s1) for i in range(2)]
                hT2 = [k.sb("hTA%d" % i, [128, 8, 128], BF16, s1) for i in range(2)]
                krL = [k.sb("kr%d" % i, [128, 512], BF16, s1) for i in range(2)]
                kirL = [k.sb("kir%d" % i, [128, 128], BF16, s1) for i in range(2)]
                tAL = [k.sb("tA%d" % i, [128, 512], F32, s1) for i in range(2)]
                tBL = [k.sb("tB%d" % i, [128, 512], F32, s1) for i in range(2)]
                tCL = [k.sb("tC%d" % i, [128, 64], F32, s1) for i in range(2)]
                tDL = [k.sb("tD%d" % i, [128, 64], F32, s1) for i in range(2)]
                hbs1 = {}

                def pre1(ti):
                    k.dma(sp, xt2[ti % 2][:], x_full[ti * 128:(ti + 1) * 128, :], writes=[xt2[ti % 2]])
                    hbs1[ti] = norm_pre(xt2[ti % 2], 128)

                def post1(ti):
                    norm_post(hbs1.pop(ti), 128, gmix, hT2[ti % 2])

                pre1(0)
                post1(0)
                pre1(1)
                for ti in range(NB):
                    if ti + 1 < NB:
                        post1(ti + 1)
                    hT = hT2[ti % 2]
                    kr, kir, tA, tB = krL[ti % 2], kirL[ti % 2], tAL[ti % 2], tBL[ti % 2]
                    tC, tD = tCL[ti % 2], tDL[ti % 2]
                    pk = proj_tok(hT, wA, 0, 512)
                    pv = proj_tok(hT, wA, 512, 512)
                    pki = proj_tok(hT, wA, 1024, 64)
                    if ti + 2 < NB:
                        pre1(ti + 2)
                    rope(pk, 8, cosF, sinF, ti, kr, tA, tB)
                    Vv = V[:, ti, :].rearrange("p (h d) -> p h d", h=8)
                    k.op(act, lambda e: e.activation(Vv[:, :, 0:64],
                                                     pv[:, 0:512].rearrange("p (h d) -> p h d", h=8), AF.Copy),
                         reads=[pv], writes=[V])
                    rope(pki, 1, cosF, sinF, ti, kir, tC, tD)
                    k.op(dve, lambda e, kir=kir: e.tensor_copy(kir[:, 64:128], kir[:, 0:64]), reads=[kir], writes=[kir])
                    tb = k.bank()
                    tbv = bf(tb[:])
                    for pr in range(4):
                        k.op(pe, lambda e, pr=pr: e.transpose(tbv[:, pr * 128:(pr + 1) * 128],
                                                              kr[:, pr * 128:(pr + 1) * 128], ident[:]),
                             reads=[kr, ident], writes=[tb])
                    k.op(pe, lambda e: e.transpose(tbv[:, 512:640], kir[:], ident[:]), reads=[kir, ident], writes=[tb])
                    k.op(act, lambda e: e.activation(KT[:, :, ti * 128:(ti + 1) * 128],
                                                     tbv[:, 0:512].rearrange("p (a b) -> p a b", a=4), AF.Copy),
                         reads=[tb], writes=[KT])
                    k.op(act, lambda e: e.activation(kiT[:, ti * 128:(ti + 1) * 128], tbv[:, 512:640], AF.Copy),
                         reads=[tb], writes=[kiT])
                k.barrier()
        def dbg_A1():
            for pr in range(4):
                k.dma(sp, dbg["kT"][:, pr, :], KT[:, pr, :], reads=[KT])
            for t8 in range(0, NB, 4):
                k.dma(sp, dbg["v"][:, t8:t8 + 4, :], V[:, t8:t8 + 4, :], reads=[V])
            k.dma(sp, dbg["kiT"], kiT[:], reads=[kiT])

        def phase_A2():
            with ExitStack() as s2:
                cosO = k.sb("cosO", [128, NS, 32], F32, s2)
                sinO = k.sb("sinO", [128, NS, 32], F32, s2)
                rope_tables(pos_own, NS, cosO, sinO, s2)
                WU, WQ, WQI, WWI, WGC, WGA = 0, 1024, 1536, 2048, 2056, 3080
                wB = k.sb("wB", [128, 8, 4104], BF16, s2)
                for kc in range(8):
                    k.dma(pool, wB[:, kc, WU:WU + 1024], w_in_v[:, kc, C_U:C_U + 1024], writes=[wB])
                    k.dma(pool, wB[:, kc, WQ:WQ + 512], w_in_v[:, kc, C_Q:C_Q + 512], writes=[wB])
                    k.dma(pool, wB[:, kc, WQI:WQI + 512], w_in_v[:, kc, C_QI:C_QI + 512], writes=[wB])
                    k.dma(pool, wB[:, kc, WWI:WWI + 8], w_in_v[:, kc, C_WI:C_WI + 8], writes=[wB])
                    k.dma(pool, wB[:, kc, WGC:WGC + 2048], w_in_v[:, kc, C_GC:C_GC + 2048], writes=[wB])
                wco = k.sb("wco", [128, 4, D], BF16, s2)
                for kc in range(4):
                    k.dma(pool, wco[:, kc, :], wco_d[kc * 128:(kc + 1) * 128, :], writes=[wco])
                wdw = k.sb("wdw", [128, 4, 31], F32, s2)
                bdw = k.sb("bdw", [128, 4], F32, s2)
                lng = k.sb("lng", [128, 4], F32, s2)
                lnb = k.sb("lnb", [128, 4], F32, s2)
                k.dma(sp, wdw[:], wdw_d, writes=[wdw])
                k.dma(sp, bdw[:], bdw_d, writes=[bdw])
                k.dma(sp, lng[:], lng_d, writes=[lng])
                k.dma(sp, lnb[:], lnb_d, writes=[lnb])
                Dg = k.sb("Dg", [128, 124, 128], BF16, s2)
                for c in range(4):
                    for j in range(31):
                        k.op(dve, lambda e, c=c, j=j: e.tensor_scalar(Dg[:, c * 31 + j, :], identf[:],
                                                                       wdw[:, c, j:j + 1], None, ALU.mult),
                             reads=[identf, wdw], writes=[Dg])
                ones = k.sb("ones", [128, 128], F32, s2)
                k.op(pool, lambda e: e.memset(ones[:], 1.0), writes=[ones])
                xt2 = [k.sb("xtB%d" % i, [128, D], F32, s2) for i in range(2)]
                xh2 = [k.sb("xhB%d" % i, [32, D], F32, s2) for i in range(2)]
                hT_L = [k.sb("hTB_%d" % i_, [128, 8, 128], BF16, s2) for i_ in range(2)]
                hTh_L = [k.sb("hThB_%d" % i_, [128, 8, 32], BF16, s2) for i_ in range(2)]
                qr_L = [k.sb("qr_%d" % i_, [128, 512], BF16, s2) for i_ in range(2)]
                qir_L = [k.sb("qir_%d" % i_, [128, 512], BF16, s2) for i_ in range(2)]
                tA_L = [k.sb("tA2_%d" % i_, [128, 512], F32, s2) for i_ in range(2)]
                tB_L = [k.sb("tB2_%d" % i_, [128, 512], F32, s2) for i_ in range(2)]
                qT_s_L = [k.sb("qT_s_%d" % i_, [128, 512], BF16, s2) for i_ in range(2)]
                qiT_s_L = [k.sb("qiT_s_%d" % i_, [128, 512], BF16, s2) for i_ in range(2)]
                sg_L = [k.sb("sgl_%d" % i_, [128, 160], F32, s2) for i_ in range(2)]
                gT_L = [k.sb("gT_%d" % i_, [128, 4, 160], BF16, s2) for i_ in range(2)]
                csb_L = [k.sb("csb_%d" % i_, [128, 4, 128], F32, s2) for i_ in range(2)]
                csq_L = [k.sb("csq_%d" % i_, [128, 4, 128], F32, s2) for i_ in range(2)]
                mean_L = [k.sb("mean_%d" % i_, [128, 128], F32, s2) for i_ in range(2)]
                msq_L = [k.sb("msq_%d" % i_, [128, 128], F32, s2) for i_ in range(2)]
                rstd_L = [k.sb("rstd_%d" % i_, [128, 128], F32, s2) for i_ in range(2)]
                nrm_L = [k.sb("nrm_%d" % i_, [128, 4, 128], F32, s2) for i_ in range(2)]
                snT_L = [k.sb("snT_%d" % i_, [128, 4, 128], BF16, s2) for i_ in range(2)]
                sgc_L = [k.sb("sgc_%d" % i_, [128, 8, 128], F32, s2) for i_ in range(2)]
                mc_s_L = [k.sb("mc_s_%d" % i_, [128, 8, 128], BF16, s2) for i_ in range(2)]
                sga_s_L = [k.sb("sga_s_%d" % i_, [128, 8, 128], BF16, s2) for i_ in range(2)]
                qTd_b = Buf("qTd_b")
                mcd_b = Buf("mcd_b")

                hbs2 = {}

                def pre2(j):
                    k.dma(sp, xt2[j % 2][:], x_own[j * 128:(j + 1) * 128, :], writes=[xt2[j % 2]])
                    k.dma(sp, xh2[j % 2][:], x_halo[j * 32:(j + 1) * 32, :], writes=[xh2[j % 2]])
                    hbs2[j] = (norm_pre(xh2[j % 2], 32), norm_pre(xt2[j % 2], 128))

                def post2(j):
                    a_, b_ = hbs2.pop(j)
                    norm_post(a_, 32, gmix, hTh_L[j % 2])
                    norm_post(b_, 128, gmix, hT_L[j % 2])

                for j in range(nslots):
                    xt = xt2[j % 2]
                    xh = xh2[j % 2]
                    hT, hTh, qr, qir, tA, tB, qT_s, qiT_s, sg, gT, csb, csq, mean, msq, rstd, nrm, snT, sgc, mc_s, sga_s = hT_L[j % 2], hTh_L[j % 2], qr_L[j % 2], qir_L[j % 2], tA_L[j % 2], tB_L[j % 2], qT_s_L[j % 2], qiT_s_L[j % 2], sg_L[j % 2], gT_L[j % 2], csb_L[j % 2], csq_L[j % 2], mean_L[j % 2], msq_L[j % 2], rstd_L[j % 2], nrm_L[j % 2], snT_L[j % 2], sgc_L[j % 2], mc_s_L[j % 2], sga_s_L[j % 2]
                    if j == 0:
                        pre2(0)
                        post2(0)
                        if nslots > 1:
                            pre2(1)
                    if j + 1 < nslots:
                        post2(j + 1)
                    pq = proj_tok(hT, wB, WQ, 512)
                    rope(pq, 8, cosO, sinO, j, qr, tA, tB)
                    pqi = proj_tok(hT, wB, WQI, 512)
                    rope(pqi, 8, cosO, sinO, j, qir, tA, tB)
                    pw = proj_tok(hT, wB, WWI, 8)
                    k.op(act, lambda e: e.activation(wi_all[:, j * 8:(j + 1) * 8], pw[:, 0:8], AF.Copy,
                                                     scale=IDX_SCALE), reads=[pw], writes=[wi_all])
                    for src, dstb, dd in ((qr, qT_s, qT_d), (qir, qiT_s, qiT_d)):
                        tb = k.bank()
                        tbv = bf(tb[:])
                        for pr in range(4):
                            k.op(pe, lambda e, pr=pr, src=src, tbv=tbv: e.transpose(
                                tbv[:, pr * 128:(pr + 1) * 128], src[:, pr * 128:(pr + 1) * 128], ident[:]),
                                 reads=[src, ident], writes=[tb])
                        k.op(act, lambda e, dstb=dstb, tbv=tbv: e.activation(dstb[:], tbv[:, 0:512], AF.Copy),
                             reads=[tb], writes=[dstb])
                        k.dma(sp, dd[j], dstb[:], reads=[dstb], writes=[qTd_b], sembuf=dstb)
                    for c in range(4):
                        pu = k.bank()
                        puv = pu[:, 0:320].rearrange("p (a b) -> p a b", a=2)
                        for half, col0 in ((0, WU + c * 128), (1, WU + 512 + c * 128)):
                            for kc in range(8):
                                k.op(pe, lambda e, kc=kc, half=half, col0=col0, puv=puv: e.matmul(
                                    puv[:, half, 0:32], wB[:, kc, col0:col0 + 128], hTh[:, kc, :],
                                    start=(kc == 0), stop=(kc == 7)), reads=[wB, hTh], writes=[pu])
                            for kc in range(8):
                                k.op(pe, lambda e, kc=kc, half=half, col0=col0, puv=puv: e.matmul(
                                    puv[:, half, 32:160], wB[:, kc, col0:col0 + 128], hT[:, kc, :],
                                    start=(kc == 0), stop=(kc == 7)), reads=[wB, hT], writes=[pu])
                        k.op(act, lambda e, puv=puv: e.activation(sg[:], puv[:, 1, :], AF.Sigmoid),
                             reads=[pu], writes=[sg])
                        k.op(dve, lambda e, puv=puv, c=c: e.tensor_tensor(gT[:, c, :], puv[:, 0, :], sg[:], ALU.mult),
                             reads=[pu, sg], writes=[gT])
                    if j + 2 < nslots:
                        pre2(j + 2)
                    pc = k.bank()
                    pcv = pc[:, 0:512].rearrange("p (a b) -> p a b", a=4)
                    for c in range(4):
                        for jj in range(31):
                            k.op(pe, lambda e, c=c, jj=jj: e.matmul(pcv[:, c, :], Dg[:, c * 31 + jj, :],
                                                                    gT[:, c, 2 + jj:2 + jj + 128],
                                                                    start=(jj == 0), stop=(jj == 30)),
                                 reads=[Dg, gT], writes=[pc])
                    k.op(dve, lambda e: e.tensor_tensor(csb[:], pcv, bdw[:].unsqueeze(2).to_broadcast([128, 4, 128]),
                                                        ALU.add), reads=[pc, bdw], writes=[csb])
                    k.op(act, lambda e: e.activation(csq[:], csb[:], AF.Square), reads=[csb], writes=[csq])
                    pst = k.bank()
                    for c in range(4):
                        k.op(pe, lambda e, c=c: e.matmul(pst[:, 0:128], ones[:], csb[:, c, :], start=(c == 0),
                                                         stop=(c == 3)), reads=[ones, csb], writes=[pst])
                    for c in range(4):
                        k.op(pe, lambda e, c=c: e.matmul(pst[:, 128:256], ones[:], csq[:, c, :], start=(c == 0),
                                                         stop=(c == 3)), reads=[ones, csq], writes=[pst])
                    k.op(act, lambda e: e.activation(mean[:], pst[:, 0:128], AF.Copy, scale=1.0 / 512),
                         reads=[pst], writes=[mean])
                    k.op(dve, lambda e: e.tensor_tensor(msq[:], mean[:], mean[:], ALU.mult), reads=[mean], writes=[msq])
                    k.op(dve, lambda e: e.scalar_tensor_tensor(rstd[:], pst[:, 128:256], 1.0 / 512, msq[:],
                                                               ALU.mult, ALU.subtract),
                         reads=[pst, msq], writes=[rstd])
                    k.op(act, lambda e: e.activation(rstd[:], rstd[:], AF.Sqrt, bias=EPS, scale=1.0),
                         reads=[rstd], writes=[rstd])
                    k.op(dve, lambda e: e.reciprocal(rstd[:], rstd[:]), reads=[rstd], writes=[rstd])
                    k.op(dve, lambda e: e.tensor_tensor(nrm[:], csb[:], mean[:].unsqueeze(1).to_broadcast([128, 4, 128]),
                                                        ALU.subtract), reads=[csb, mean], writes=[nrm])
                    k.op(dve, lambda e: e.tensor_tensor(nrm[:], nrm[:], rstd[:].unsqueeze(1).to_broadcast([128, 4, 128]),
                                                        ALU.mult), reads=[nrm, rstd], writes=[nrm])
                    for c in range(4):
                        k.op(act, lambda e, c=c: e.activation(snT[:, c, :], nrm[:, c, :], AF.Silu,
                                                              bias=lnb[:, c:c + 1], scale=lng[:, c:c + 1]),
                             reads=[nrm, lnb, lng], writes=[snT])
                    for gi, (wcol, dst) in enumerate(((WGC, None), (WGA, sga_s))):
                        for hb_ in range(2):
                            pg = k.bank()
                            pgv = pg[:, 0:512].rearrange("p (a b) -> p a b", a=4)
                            for m in range(4):
                                col0 = wcol + (hb_ * 4 + m) * 128
                                for kc in range(8):
                                    k.op(pe, lambda e, kc=kc, m=m, col0=col0, pgv=pgv: e.matmul(
                                        pgv[:, m, :], wB[:, kc, col0:col0 + 128], hT[:, kc, :],
                                        start=(kc == 0), stop=(kc == 7)), reads=[wB, hT], writes=[pg])
                            tgt = sgc if gi == 0 else sga_s
                            k.op(act, lambda e, pgv=pgv, tgt=tgt, hb_=hb_: e.activation(
                                tgt[:, hb_ * 4:(hb_ + 1) * 4, :], pgv, AF.Sigmoid), reads=[pg], writes=[tgt])
                    for hb_ in range(2):
                        py = k.bank()
                        pyv = py[:, 0:512].rearrange("p (a b) -> p a b", a=4)
                        for m in range(4):
                            mm = hb_ * 4 + m
                            for kc in range(4):
                                k.op(pe, lambda e, kc=kc, m=m, mm=mm, pyv=pyv: e.matmul(
                                    pyv[:, m, :], wco[:, kc, mm * 128:(mm + 1) * 128], snT[:, kc, :],
                                    start=(kc == 0), stop=(kc == 3)), reads=[wco, snT], writes=[py])
                        k.op(dve, lambda e, pyv=pyv, hb_=hb_: e.tensor_tensor(
                            mc_s[:, hb_ * 4:(hb_ + 1) * 4, :], pyv, sgc[:, hb_ * 4:(hb_ + 1) * 4, :], ALU.mult),
                             reads=[py, sgc], writes=[mc_s])
                    k.dma(sp, mc_d[j], mc_s[:].rearrange("p a b -> p (a b)"), reads=[mc_s], writes=[mcd_b], sembuf=mc_s)
                    k.dma(sp, sga_d[j], sga_s[:].rearrange("p a b -> p (a b)"), reads=[sga_s], writes=[mcd_b],
                          sembuf=sga_s)
                k.barrier()
        def dbg_A2():
            k.dma(sp, dbg["wi"], wi_all[:], reads=[wi_all])
            for nm, src in (("qT", qT_d), ("qiT", qiT_d), ("mc", mc_d), ("sga", sga_d)):
                b_ = Buf("dbgc_" + nm)
                k.dma(sp, dbg[nm], src, writes=[b_])
            k.barrier()

        def phase_B():
            with ExitStack() as s3:
                cm2 = [k.sb("cm%d" % i, [128, 256], F32, s3) for i in range(2)]
                pw2 = k.sb("pw2", [128, NIT + 2], F32, s3)
                for it in range(NIT + 2):
                    k.op(dve, lambda e, it=it: e.memset(pw2[:, it:it + 1], float(2.0 ** -(it + 1))), writes=[pw2])
                qT2 = [k.sb("qTb%d" % i, [128, 4, 256], BF16, s3) for i in range(2)]
                for b_ in qT2:
                    k.op(dve, lambda e: e.memset(b_[:], 0.0), writes=[b_])
                qiT2 = [k.sb("qiTb%d" % i, [128, 512], BF16, s3) for i in range(2)]
                scL = [k.sb("sc%d" % i, [128, S], F32, s3) for i in range(2)]
                AL = [k.sb("Ab%d" % i, [128, 512], BF16, s3) for i in range(4)]
                wabs = k.sb("wabs", [128, 8], F32, s3)
                sgn = k.sb("sgn", [128, 8], F32, s3)
                sgd = k.sb("sgd", [128, 8, 128], BF16, s3)
                msk = k.sb("msk", [128, S], BF16, s3)
                Mb = k.sb("Mb", [128, NB, 128], BF16, s3)
                st8 = k.sb("st8", [128, 8], F32, s3)
                Q = k.sb("Qtab", [128, NIT + 2], F32, s3)
                Q2 = k.sb("Q2tab", [128, NIT + 2], F32, s3)
                E2 = [k.sb("Eb%d" % i, [128, 4, 128], BF16, s3) for i in range(3)]
                rden = k.sb("rden", [128, 8], F32, s3)
                Osb = k.sb("Osb", [128, 2, 4, 66], F32, s3)
                thr = st8[:, 6:7]
                ai = [0]
                MBIG = 30000.0
                if debug:
                    attnf = k.sb("attnf", [128, 512], F32, s3)

                def loads_a(j):
                    p = j % 2
                    k.dma(sp, qiT2[p][:], qiT_d[j], writes=[qiT2[p]])
                    k.dma(sp, cm2[p][:], cmask_d[:, j, :], writes=[cm2[p]])

                def loads_b(j):
                    p = j % 2
                    k.dma(sp, qT2[p][0:64, :, 0:128], qT_d[j][0:64, :].rearrange("p (a b) -> p a b", a=4), writes=[qT2[p]])
                    k.dma(sp, qT2[p][64:128, :, 128:256], qT_d[j][64:128, :].rearrange("p (a b) -> p a b", a=4),
                          writes=[qT2[p]])

                def prep(j):
                    wsl = wi_all[:, j * 8:(j + 1) * 8]
                    k.op(dve, lambda e: e.tensor_scalar(sgn[:], wsl, 0.0, 2.0, ALU.is_ge, ALU.mult), reads=[wi_all], writes=[sgn])
                    k.op(dve, lambda e: e.tensor_scalar(sgn[:], sgn[:], -1.0, None, ALU.add), reads=[sgn], writes=[sgn])
                    k.op(dve, lambda e: e.tensor_tensor(wabs[:], wsl, sgn[:], ALU.mult), reads=[wi_all, sgn], writes=[wabs])
                    k.op(dve, lambda e: e.tensor_tensor(sgd[:], ident[:].unsqueeze(1).to_broadcast([128, 8, 128]),
                                                        sgn[:].unsqueeze(2).to_broadcast([128, 8, 128]), ALU.mult),
                         reads=[ident, sgn], writes=[sgd])

                def indexer(j):
                    sc = scL[j % 2]
                    qiTs = qiT2[j % 2]
                    L = 256 * (j + 1)
                    nch = (L + 511) // 512
                    for cc in range(nch):
                        c0 = cc * 512
                        W = min(512, L - c0)
                        pacc = k.bank(pin=True)
                        pbs = {}

                        def qi_mm(h):
                            pb = k.bank()
                            pp = (h % 2) * 64
                            k.op(pe, lambda e: e.matmul(pb[:, 0:W], qiTs[pp:pp + 64, (h // 2) * 128:(h // 2 + 1) * 128],
                                                        kiT[pp:pp + 64, c0:c0 + W], start=True, stop=True),
                                 reads=[qiTs, kiT], writes=[pb])
                            pbs[h] = pb

                        qi_mm(0)
                        qi_mm(1)
                        qi_mm(2)
                        for h in range(8):
                            if h + 3 < 8:
                                qi_mm(h + 3)
                            pb = pbs[h]
                            A = AL[ai[0] % 4]
                            ai[0] += 1
                            k.op(act, lambda e: e.activation(A[:, 0:W], pb[:, 0:W], AF.Relu, scale=wabs[:, h:h + 1]),
                                 reads=[pb, wabs], writes=[A])
                            k.op(pe, lambda e: e.matmul(pacc[:, 0:W], sgd[:, h, :], A[:, 0:W], start=(h == 0), stop=(h == 7)),
                                 reads=[sgd, A], writes=[pacc])
                        k.op(act, lambda e: e.activation(sc[:, c0:c0 + W], pacc[:, 0:W], AF.Copy), reads=[pacc], writes=[sc])
                        k.unpin(pacc)

                def thresh(j):
                    sc = scL[j % 2]
                    L = 256 * (j + 1)
                    cmask = cm2[j % 2]
                    if j == 0:
                        k.op(dve, lambda e: e.tensor_tensor(sc[:, L - 256:L], sc[:, L - 256:L], cmask[:], ALU.add),
                             reads=[sc, cmask], writes=[sc])
                        k.op(dve, lambda e: e.memset(thr, -1.0e29), writes=[st8])
                        return
                    k.op(dve, lambda e: e.tensor_reduce(st8[:, 0:1], sc[:, 0:L], AX.X, ALU.max), reads=[sc], writes=[st8])
                    k.op(dve, lambda e: e.tensor_reduce(st8[:, 1:2], sc[:, 0:L], AX.X, ALU.min), reads=[sc], writes=[st8])
                    k.op(dve, lambda e: e.tensor_tensor(sc[:, L - 256:L], sc[:, L - 256:L], cmask[:], ALU.add),
                         reads=[sc, cmask], writes=[sc])
                    k.op(dve, lambda e: e.tensor_tensor(st8[:, 2:3], st8[:, 0:1], st8[:, 1:2], ALU.subtract),
                         reads=[st8], writes=[st8])
                    k.op(dve, lambda e: e.tensor_scalar(st8[:, 5:6], st8[:, 2:3], float(2.0 ** -10), None, ALU.mult),
                         reads=[st8], writes=[st8])
                    k.op(dve, lambda e: e.tensor_scalar(st8[:, 2:3], st8[:, 2:3], float(1.0 + 2.0 ** -9), 1e-30,
                                                        ALU.mult, ALU.add), reads=[st8], writes=[st8])
                    k.op(dve, lambda e: e.tensor_tensor(st8[:, 7:8], st8[:, 1:2], st8[:, 5:6], ALU.subtract),
                         reads=[st8], writes=[st8])
                    k.op(dve, lambda e: e.tensor_scalar(Q[:], pw2[:], st8[:, 2:3], None, ALU.mult),
                         reads=[pw2, st8], writes=[Q])
                    k.op(dve, lambda e: e.tensor_scalar(Q2[:], Q[:], 2.0, None, ALU.mult), reads=[Q], writes=[Q2])
                    k.op(dve, lambda e: e.tensor_tensor(st8[:, 3:4], st8[:, 7:8], Q[:, 0:1], ALU.add),
                         reads=[st8, Q], writes=[st8])
                    for it in range(NIT):
                        k.op(dve, lambda e: e.memset(st8[:, 4:5], 0.0), writes=[st8])
                        k.op(dve, lambda e: e.tensor_scalar(msk[:, 0:L], sc[:, 0:L], st8[:, 3:4], 0.0,
                                                            ALU.is_ge, ALU.add, accum_out=st8[:, 4:5]),
                             reads=[sc, st8], writes=[st8, msk])
                        k.op(dve, lambda e: e.tensor_scalar(st8[:, 5:6], st8[:, 4:5], TOPK - 0.5,
                                                            Q2[:, it + 1:it + 2], ALU.is_ge, ALU.mult),
                             reads=[st8, Q2], writes=[st8])
                        k.op(dve, lambda e: e.scalar_tensor_tensor(st8[:, 3:4], st8[:, 3:4], Q[:, it + 1:it + 2],
                                                                   st8[:, 5:6], ALU.subtract, ALU.add),
                             reads=[st8, Q], writes=[st8])
                    k.op(dve, lambda e: e.tensor_tensor(thr, st8[:, 3:4], Q[:, NIT:NIT + 1], ALU.subtract),
                         reads=[st8, Q], writes=[st8])

                def mask(j):
                    sc = scL[j % 2]
                    L = 256 * (j + 1)
                    nkb = 2 * (j + 1)
                    if debug:
                        b_ = Buf("dbg_sc%d" % j)
                        for c0_ in range(0, L, 1024):
                            c1_ = min(L, c0_ + 1024)
                            k.dma(sp, dbg["sc"][j][:, c0_:c1_], sc[:, c0_:c1_], reads=[sc], writes=[b_], sembuf=b_)
                        k.dma(sp, dbg["thr"][j], thr, reads=[st8], writes=[b_], sembuf=b_)
                    for g0 in range(0, nkb, 8):
                        gn = min(8, nkb - g0)
                        k.op(dve, lambda e: e.tensor_scalar(msk[:, g0 * 128:(g0 + gn) * 128],
                                                            sc[:, g0 * 128:(g0 + gn) * 128], thr, None, ALU.is_ge),
                             reads=[sc, st8], writes=[msk])
                        tb = k.bank()
                        tbv = bf(tb[:])
                        for q_ in range(gn):
                            k.op(pe, lambda e: e.transpose(tbv[:, q_ * 128:(q_ + 1) * 128],
                                                           msk[:, (g0 + q_) * 128:(g0 + q_ + 1) * 128], ident[:]),
                                 reads=[msk, ident], writes=[tb])
                        k.op(act, lambda e: e.activation(Mb[:, g0:g0 + gn, :],
                                                         tbv[:, 0:gn * 128].rearrange("p (a b) -> p a b", a=gn),
                                                         AF.Identity, bias=-MBIG, scale=MBIG),
                             reads=[tb], writes=[Mb])

                def attention(j):
                    qTs = qT2[j % 2]
                    nkb = 2 * (j + 1)
                    po = [k.bank(pin=True), k.bank(pin=True)]
                    pov = [b_[:, 0:512].rearrange("p (h d) -> p h d", h=4) for b_ in po]
                    units = [(kb, hg) for kb in range(nkb) for hg in range(2)]

                    def qk(u):
                        kb, hg = units[u]
                        pl = k.bank()
                        plv = pl[:, 0:512].rearrange("p (a b) -> p a b", a=4)
                        k.op(pe, lambda e: e.matmul(plv, ident[:], Mb[:, kb, :].unsqueeze(1).to_broadcast([128, 4, 128]),
                                                    start=True, stop=False), reads=[ident, Mb], writes=[pl])
                        for pi in range(2):
                            pr = hg * 2 + pi
                            k.op(pe, lambda e: e.matmul(pl[:, pi * 256:(pi + 1) * 256], KT[:, pr, kb * 128:(kb + 1) * 128],
                                                        qTs[:, pr, :], start=False, stop=(pi == 1)),
                                 reads=[KT, qTs], writes=[pl])
                        return pl, plv

                    LA = 2
                    pend = [qk(u) for u in range(min(LA, len(units)))]
                    for u in range(len(units)):
                        if u + LA < len(units):
                            pend.append(qk(u + LA))
                        kb, hg = units[u]
                        pl, plv = pend.pop(0)
                        E = E2[u % 3]
                        k.op(act, lambda e: e.activation(E[:], plv, AF.Exp, scale=ATT_SCALE), reads=[pl], writes=[E])
                        for hh in range(4):
                            h = hg * 4 + hh
                            k.op(pe, lambda e: e.matmul(pov[hg][:, hh, 0:66], E[:, hh, :], V[:, kb, h * 66:(h + 1) * 66],
                                                        start=(kb == 0 and hh == 0), stop=(kb == nkb - 1 and hh == 3)),
                                 reads=[E, V], writes=[po[hg]])
                    for hg in range(2):
                        k.op(act, lambda e: e.activation(Osb[:, hg, :, :], pov[hg][:, :, 0:66], AF.Copy),
                             reads=[po[hg]], writes=[Osb])
                        k.unpin(po[hg])

                def finalize(j):
                    for hg in range(2):
                        k.op(dve, lambda e: e.reciprocal(rden[:, hg * 4:(hg + 1) * 4].unsqueeze(2), Osb[:, hg, :, 64:65]),
                             reads=[Osb], writes=[rden])
                        k.op(dve, lambda e: e.tensor_tensor(
                            attn_all[:, j, hg * 256:(hg + 1) * 256].rearrange("p (h d) -> p h d", h=4), Osb[:, hg, :, 0:64],
                            rden[:, hg * 4:(hg + 1) * 4].unsqueeze(2).to_broadcast([128, 4, 64]), ALU.mult),
                             reads=[Osb, rden], writes=[attn_all])
                    if debug:
                        k.op(dve, lambda e: e.tensor_copy(attnf[:], attn_all[:, j, :]), reads=[attn_all], writes=[attnf])
                        k.dma(sp, dbg["attn"][j], attnf[:], reads=[attnf])

                loads_a(0)
                loads_b(0)
                if nslots > 1:
                    loads_a(1)
                    loads_b(1)
                prep(0)
                indexer(0)
                thresh(0)
                mask(0)
                if nslots > 1:
                    prep(1)
                    indexer(1)
                for i in range(nslots):
                    if i + 2 < nslots:
                        prep(i + 2)
                    if i + 1 < nslots:
                        thresh(i + 1)
                    attention(i)
                    if i + 2 < nslots:
                        loads_a(i + 2)
                        indexer(i + 2)
                    if i + 1 < nslots:
                        mask(i + 1)
                    finalize(i)
                    if i + 2 < nslots:
                        loads_b(i + 2)
                k.barrier()

        def phase_B2():
            with ExitStack() as s3:
                wao = k.sb("wao", [128, 4, D], BF16, s3)
                wo = k.sb("wo", [128, 8, D], BF16, s3)
                wr = k.sb("wr", [128, 8, 20], BF16, s3)
                br = k.sb("br", [128, 20], F32, s3)
                for kc in range(4):
                    k.dma(pool, wao[:, kc, :], wao_d[kc * 128:(kc + 1) * 128, :], writes=[wao])
                for kc in range(8):
                    k.dma(pool, wo[:, kc, :], wo_d[kc * 128:(kc + 1) * 128, :], writes=[wo])
                k.dma(pool, wr[:], wr_d.rearrange("(k p) c -> p k c", p=128), writes=[wr])
                k.dma(sp, br[:], br_d, writes=[br])
                mc2 = [k.sb("mcb%d" % i, [128, 8, 128], BF16, s3) for i in range(2)]
                sga2 = [k.sb("sgab%d" % i, [128, 8, 128], BF16, s3) for i in range(2)]
                xoL = [k.sb("xob%d" % i, [128, D], F32, s3) for i in range(2)]
                attnTL = [k.sb("attnT%d" % i, [128, 4, 128], BF16, s3) for i in range(2)]
                mTL = [k.sb("mT%d" % i, [128, 8, 128], BF16, s3) for i in range(2)]
                x1L = [k.sb("x1_%d" % i, [128, D], F32, s3) for i in range(2)]
                lg = k.sb("lg", [128, 20], F32, s3)
                r8 = k.sb("r8", [128, 16], F32, s3)
                ohg = k.sb("ohg", [128, 4], F32, s3)
                t16 = k.sb("t16", [128, 16], F32, s3)
                esel = k.sb("esel", [128, 4], F32, s3)
                oh1 = k.sb("oh1", [128, 4], F32, s3)
                oh2 = k.sb("oh2", [128, 4], F32, s3)
                em = k.sb("em", [128, 4], F32, s3)
                we = k.sb("we", [128, 4], F32, s3)
                x1d_b = Buf("x1d_b")

                def loads_c(j):
                    p = j % 2
                    k.dma(sp, mc2[p][:].rearrange("p a b -> p (a b)"), mc_d[j], writes=[mc2[p]])
                    k.dma(sp, sga2[p][:].rearrange("p a b -> p (a b)"), sga_d[j], writes=[sga2[p]])
                    k.dma(sp, xoL[p][:], x_own[j * 128:(j + 1) * 128, :], writes=[xoL[p]])

                def tail(j):
                    mcs, sgas = mc2[j % 2], sga2[j % 2]
                    xo, x1, mT, attnT = xoL[j % 2], x1L[j % 2], mTL[j % 2], attnTL[j % 2]
                    tb = k.bank()
                    tbv = bf(tb[:])
                    for q_ in range(4):
                        k.op(pe, lambda e: e.transpose(tbv[:, q_ * 128:(q_ + 1) * 128],
                                                       attn_all[:, j, q_ * 128:(q_ + 1) * 128], ident[:]),
                             reads=[attn_all, ident], writes=[tb])
                    k.op(act, lambda e: e.activation(attnT[:], tbv[:, 0:512].rearrange("p (a b) -> p a b", a=4), AF.Copy),
                         reads=[tb], writes=[attnT])
                    x1v = x1[:].rearrange("p (a b) -> p a b", a=8)
                    for hb_ in range(2):
                        py = k.bank()
                        pyv = py[:, 0:512].rearrange("p (a b) -> p a b", a=4)
                        for m in range(4):
                            mm = hb_ * 4 + m
                            for kc in range(4):
                                k.op(pe, lambda e: e.matmul(pyv[:, m, :], wao[:, kc, mm * 128:(mm + 1) * 128], attnT[:, kc, :],
                                                            start=(kc == 0), stop=(kc == 3)), reads=[wao, attnT], writes=[py])
                        k.op(dve, lambda e: e.tensor_tensor(x1v[:, hb_ * 4:(hb_ + 1) * 4, :], pyv,
                                                            sgas[:, hb_ * 4:(hb_ + 1) * 4, :], ALU.mult),
                             reads=[py, sgas], writes=[x1])
                    k.op(dve, lambda e: e.tensor_tensor(mT[:], x1v, mcs[:], ALU.add), reads=[x1, mcs], writes=[mT])
                    for half in range(2):
                        px = k.bank()
                        for kc in range(8):
                            k.op(pe, lambda e: e.matmul(px[:, 0:512], mT[:, kc, :], wo[:, kc, half * 512:(half + 1) * 512],
                                                        start=(kc == 0), stop=(kc == 7)), reads=[mT, wo], writes=[px])
                        k.op(dve, lambda e: e.tensor_tensor(acc[:, j, half * 512:(half + 1) * 512], px[:, 0:512],
                                                            xo[:, half * 512:(half + 1) * 512], ALU.add),
                             reads=[px, xo], writes=[acc])
                    if debug:
                        k.dma(sp, dbg["x1"][j], acc[:, j, :], reads=[acc], sembuf=x1)
                    hbsT[j] = norm_pre(acc, 128, acc[:, j, :])

                def tailB(j):
                    norm_post(hbsT.pop(j), 128, gffn, h2T, h2T[:, :, j * 128:(j + 1) * 128])
                    pr_ = k.bank()
                    for kc in range(8):
                        k.op(pe, lambda e: e.matmul(pr_[:, 0:20], h2T[:, kc, j * 128:(j + 1) * 128], wr[:, kc, :],
                                                    start=(kc == 0), stop=(kc == 7)), reads=[h2T, wr], writes=[pr_])
                    k.op(dve, lambda e: e.tensor_tensor(lg[:], pr_[:, 0:20], br[:], ALU.add), reads=[pr_, br], writes=[lg])
                    k.op(dve, lambda e: e.tensor_reduce(r8[:, 0:1], lg[:, 0:4], AX.X, ALU.max), reads=[lg], writes=[r8])
                    k.op(dve, lambda e: e.tensor_scalar(r8[:, 1:2], r8[:, 0:1], -1.0, None, ALU.mult), reads=[r8], writes=[r8])
                    k.op(dve, lambda e: e.memset(r8[:, 2:3], 0.0), writes=[r8])
                    k.op(act, lambda e: e.activation(t16[:, 0:4], lg[:, 0:4], AF.Exp, bias=r8[:, 1:2], scale=1.0,
                                                     accum_out=r8[:, 2:3]), reads=[lg, r8], writes=[t16, r8])
                    k.op(dve, lambda e: e.reciprocal(r8[:, 3:4], r8[:, 2:3]), reads=[r8], writes=[r8])
                    k.op(dve, lambda e: e.tensor_scalar(ohg[:], lg[:, 0:4], r8[:, 0:1], r8[:, 3:4], ALU.is_ge, ALU.mult),
                         reads=[lg, r8], writes=[ohg])
                    k.op(dve, lambda e: e.tensor_scalar(oh1[:], lg[:, 0:4], r8[:, 0:1], None, ALU.is_ge),
                         reads=[lg, r8], writes=[oh1])
                    k.op(dve, lambda e: e.tensor_tensor(t16[:].rearrange("p (g e) -> p g e", g=4),
                                                        lg[:, 4:20].rearrange("p (g e) -> p g e", g=4),
                                                        oh1[:].unsqueeze(2).to_broadcast([128, 4, 4]), ALU.mult),
                         reads=[lg, oh1], writes=[t16])
                    k.op(dve, lambda e: e.tensor_reduce(esel[:], t16[:].rearrange("p (g e) -> p e g", g=4), AX.X, ALU.add),
                         reads=[t16], writes=[esel])
                    k.op(dve, lambda e: e.tensor_reduce(r8[:, 4:5], esel[:], AX.X, ALU.max), reads=[esel], writes=[r8])
                    k.op(dve, lambda e: e.tensor_scalar(oh1[:], esel[:], r8[:, 4:5], None, ALU.is_ge),
                         reads=[esel, r8], writes=[oh1])
                    k.op(dve, lambda e: e.scalar_tensor_tensor(em[:], oh1[:], NEG, esel[:], ALU.mult, ALU.add),
                         reads=[oh1, esel], writes=[em])
                    k.op(dve, lambda e: e.tensor_reduce(r8[:, 5:6], em[:], AX.X, ALU.max), reads=[em], writes=[r8])
                    k.op(dve, lambda e: e.tensor_scalar(oh2[:], em[:], r8[:, 5:6], None, ALU.is_ge),
                         reads=[em, r8], writes=[oh2])
                    k.op(dve, lambda e: e.tensor_tensor(r8[:, 6:7], r8[:, 4:5], r8[:, 5:6], ALU.subtract),
                         reads=[r8], writes=[r8])
                    k.op(act, lambda e: e.activation(r8[:, 7:8], r8[:, 6:7], AF.Sigmoid), reads=[r8], writes=[r8])
                    k.op(dve, lambda e: e.tensor_scalar(r8[:, 8:9], r8[:, 7:8], -1.0, 1.0, ALU.mult, ALU.add),
                         reads=[r8], writes=[r8])
                    k.op(dve, lambda e: e.tensor_scalar(we[:], oh1[:], r8[:, 7:8], None, ALU.mult), reads=[oh1, r8], writes=[we])
                    k.op(dve, lambda e: e.scalar_tensor_tensor(we[:], oh2[:], r8[:, 8:9], we[:], ALU.mult, ALU.add),
                         reads=[oh2, r8, we], writes=[we])
                    k.op(dve, lambda e: e.tensor_tensor(gw_all[:, j * 16:(j + 1) * 16].rearrange("p (g e) -> p g e", g=4),
                                                        ohg[:].unsqueeze(2).to_broadcast([128, 4, 4]),
                                                        we[:].unsqueeze(1).to_broadcast([128, 4, 4]), ALU.mult),
                         reads=[ohg, we], writes=[gw_all])

                hbsT = {}
                loads_c(0)
                if nslots > 1:
                    loads_c(1)
                tail(0)
                for j in range(nslots):
                    if j + 1 < nslots:
                        tail(j + 1)
                    tailB(j)
                    if j + 2 < nslots:
                        loads_c(j + 2)
                k.barrier()
        def phase_C():
            with ExitStack() as s4:
                gfin = k.sb("gfin", [128, D], F32, s4)
                k.dma(sp, gfin[:], gfin_d, writes=[gfin])
                sgl2 = [k.sb("sgm%d" % i, [128, 512], F32, s4) for i in range(2)]
                hid2 = [k.sb("hid%d" % i, [128, 2, 512], BF16, s4) for i in range(2)]
                ot2 = [k.sb("ot%d" % i, [128, D], F32, s4) for i in range(2)]

                ngrp = (nslots + 3) // 4
                wload(0)
                hi = 0
                for ex in range(16):
                    if ex + 1 < 16:
                        wload(ex + 1)
                    p = ex % 2
                    wg, wu, wd = wg2[p], wu2[p], wd2[p]
                    for tg in range(ngrp):
                        ns_ = min(4, nslots - tg * 4)
                        N = ns_ * 128
                        hid = hid2[hi % 2]
                        hi += 1
                        for fc in range(2):
                            pg = k.bank()
                            pu = k.bank()
                            for kc in range(8):
                                k.op(pe, lambda e, kc=kc, fc=fc, pg=pg: e.matmul(
                                    pg[:, 0:N], wg[:, kc, fc * 128:(fc + 1) * 128], h2T[:, kc, tg * 512:tg * 512 + N],
                                    start=(kc == 0), stop=(kc == 7)), reads=[wg, h2T], writes=[pg])
                            for kc in range(8):
                                k.op(pe, lambda e, kc=kc, fc=fc, pu=pu: e.matmul(
                                    pu[:, 0:N], wu[:, kc, fc * 128:(fc + 1) * 128], h2T[:, kc, tg * 512:tg * 512 + N],
                                    start=(kc == 0), stop=(kc == 7)), reads=[wu, h2T], writes=[pu])
                            sgl = sgl2[fc]
                            k.op(act, lambda e, pg=pg, sgl=sgl: e.activation(sgl[:, 0:N], pg[:, 0:N], AF.Silu),
                                 reads=[pg], writes=[sgl])
                            k.op(dve, lambda e, pu=pu, sgl=sgl, fc=fc, hid=hid: e.tensor_tensor(
                                hid[:, fc, 0:N], pu[:, 0:N], sgl[:, 0:N], ALU.mult), reads=[pu, sgl], writes=[hid])
                        for s_ in range(ns_):
                            j = tg * 4 + s_
                            for half in range(2):
                                py = k.bank()
                                for fc in range(2):
                                    k.op(pe, lambda e, fc=fc, half=half, py=py, s_=s_, hid=hid: e.matmul(
                                        py[:, 0:512], hid[:, fc, s_ * 128:(s_ + 1) * 128],
                                        wd[:, fc, half * 512:(half + 1) * 512], start=(fc == 0), stop=(fc == 1)),
                                         reads=[hid, wd], writes=[py])
                                k.op(dve, lambda e, py=py, j=j, half=half, ex=ex: e.scalar_tensor_tensor(
                                    acc[:, j, half * 512:(half + 1) * 512], py[:, 0:512],
                                    gw_all[:, j * 16 + ex:j * 16 + ex + 1], acc[:, j, half * 512:(half + 1) * 512],
                                    ALU.mult, ALU.add), reads=[py, gw_all, acc], writes=[acc])
                for j in range(nslots):
                    ot = ot2[j % 2]
                    ss, rs, junk = ssL[j % 2], rsL[j % 2], hbL[j % 2]
                    k.op(dve, lambda e, ss=ss: e.memset(ss[:], 0.0), writes=[ss])
                    k.op(act, lambda e, j=j, junk=junk, ss=ss: e.activation(junk[:], acc[:, j, :], AF.Square, accum_out=ss[:, 0:1]),
                         reads=[acc, ss], writes=[junk, ss])
                    k.op(act, lambda e, rs=rs, ss=ss: e.activation(rs[:], ss[:], AF.Sqrt, bias=EPS, scale=1.0 / D), reads=[ss], writes=[rs])
                    k.op(dve, lambda e, rs=rs: e.reciprocal(rs[:], rs[:]), reads=[rs], writes=[rs])
                    k.op(dve, lambda e, j=j, ot=ot, rs=rs: e.scalar_tensor_tensor(ot[:], acc[:, j, :], rs[:, 0:1], gfin[:],
                                                                          ALU.mult, ALU.mult),
                         reads=[acc, rs, gfin], writes=[ot])
                    k.dma(sp, out_d[j * 128:(j + 1) * 128, :], ot[:], reads=[ot], sembuf=ot)
                k.barrier()
        if "A2" in phases:
            phase_A2()
            if debug:
                dbg_A2()
        attn_all = k.sb("attn_all", [128, NS, 512], BF16)
        wg2 = [k.sb("wg%d" % i, [128, 8, 256], BF16) for i in range(2)]
        wu2 = [k.sb("wu%d" % i, [128, 8, 256], BF16) for i in range(2)]
        wd2 = [k.sb("wd%d" % i, [128, 2, D], BF16) for i in range(2)]
        wloaded = set()

        def wload(ex):
            if ex in wloaded:
                return
            wloaded.add(ex)
            p = ex % 2
            k.dma(pool, wg2[p][:], wg_d[ex].rearrange("(k p) c -> p k c", p=128), writes=[wg2[p]])
            k.dma(pool, wu2[p][:], wu_d[ex].rearrange("(k p) c -> p k c", p=128), writes=[wu2[p]])
            k.dma(pool, wd2[p][:], wd_d[ex].rearrange("(k p) c -> p k c", p=128), writes=[wd2[p]])

        with ExitStack() as skv:
            KT = k.sb("KT", [128, 4, S], BF16, skv)
            V = k.sb("V", [128, NB, 8 * 66], BF16, skv)
            kiT = k.sb("kiT", [128, S], BF16, skv)
            k.op(pool, lambda e: e.memset(V[:], 1.0), writes=[V])
            if "A1" in phases:
                phase_A1()
                if debug:
                    dbg_A1()
            if "B" in phases:
                phase_B()
            k.barrier()
        acc = k.sb("acc", [128, NS, D], F32)
        h2T = k.sb("h2T", [128, 8, NS * 128], BF16)
        if "C" in phases:
            wload(0)
            wload(1)
        if "B" in phases:
            phase_B2()
            if debug:
                k.dma(sp, dbg["gw"], gw_all[:], reads=[gw_all])
        if "C" in phases:
            phase_C()
        k.barrier()
        build_nc.nins = k.nins
    return nc


def own_blocks(r):
    return [2 * j + ((j % 2) ^ r) for j in range(NS)]


def prep_core(inp, c):
    b, r = c // 2, c % 2
    x = np.asarray(inp["x"], dtype=np.float32)
    pos = np.asarray(inp["positions"]).astype(np.int32)
    blocks = own_blocks(r)
    xb = x[b]
    x_own = np.concatenate([xb[i * 128:(i + 1) * 128] for i in blocks], axis=0)
    x_halo = np.zeros((NS * 32, D), np.float32)
    for j, i in enumerate(blocks):
        if i > 0:
            x_halo[j * 32:(j + 1) * 32] = xb[i * 128 - 32:i * 128]
    pos_full = np.ascontiguousarray(pos[b].reshape(NB, 128).T)
    pos_own = np.ascontiguousarray(np.stack([pos[b][i * 128:(i + 1) * 128] for i in blocks], axis=1))
    invf = (1.0 / (np.float32(10000.0) ** (np.arange(0, 64, 2, dtype=np.float32) / np.float32(64)))).astype(np.float32)
    invf = np.ascontiguousarray(np.broadcast_to(invf[None, :], (128, 32)))
    cmask = np.zeros((128, NS, 256), np.float32)
    for j, i in enumerate(blocks):
        qidx = i * 128 + np.arange(128)[:, None]
        kidx = 256 * j + np.arange(256)[None, :]
        cmask[:, j, :] = np.where(kidx > qidx, np.float32(NEG), np.float32(0.0))

    def fm(v, nchunk):
        return np.ascontiguousarray(np.asarray(v, np.float32).reshape(nchunk, 128).T)

    w_dw = np.asarray(inp["w_dw"], np.float32)[0, :, 0, :]
    wdw = np.ascontiguousarray(w_dw.T.reshape(4, 128, 31).transpose(1, 0, 2))
    w_r = np.concatenate([np.asarray(inp["w_rg"], np.float32)[0],
                          np.asarray(inp["w_re"], np.float32)[0].reshape(D, 16)], axis=1)
    b_r = np.concatenate([np.asarray(inp["b_rg"], np.float32)[0], np.asarray(inp["b_re"], np.float32)[0].reshape(16)])
    m = {
        "x_full": np.ascontiguousarray(xb), "x_own": x_own, "x_halo": x_halo,
        "pos_full": pos_full, "pos_own": pos_own, "invf": invf, "cmask": cmask,
        "ident": np.eye(128, dtype=np.float32),
        "w_in": np.ascontiguousarray(np.asarray(inp["w_in"], np.float32)[0]),
        "wdw": wdw, "bdw": fm(inp["b_dw"][0], 4), "lng": fm(inp["ln_g"][0], 4), "lnb": fm(inp["ln_b"][0], 4),
        "gmix": fm(inp["g_mix"][0], 8), "gffn": fm(inp["g_ffn"][0], 8),
        "gfin": np.ascontiguousarray(np.broadcast_to(np.asarray(inp["g_final"], np.float32)[None, :], (128, D))),
        "b_r": np.ascontiguousarray(np.broadcast_to(b_r[None, :], (128, 20))),
        "w_r": np.ascontiguousarray(w_r),
        "w_conv_out": np.ascontiguousarray(np.asarray(inp["w_conv_out"], np.float32)[0]),
        "w_attn_out": np.ascontiguousarray(np.asarray(inp["w_attn_out"], np.float32)[0]),
        "w_o": np.ascontiguousarray(np.asarray(inp["w_o"], np.float32)[0]),
        "w_gate": np.ascontiguousarray(np.asarray(inp["w_gate"], np.float32)[0]),
        "w_up": np.ascontiguousarray(np.asarray(inp["w_up"], np.float32)[0]),
        "w_down": np.ascontiguousarray(np.asarray(inp["w_down"], np.float32)[0]),
    }
    return m


_NC_CACHE = {}


def kernel(**inputs):
    if "nc" not in _NC_CACHE:
        _NC_CACHE["nc"] = build_nc()
    nc = _NC_CACHE["nc"]
    in_maps = [prep_core(inputs, c) for c in range(8)]
    res = run_bass_kernel_spmd(nc, in_maps, core_ids=list(range(8)))
    out = np.empty((4, S, D), np.float32)
    for c in range(8):
        b, r = c // 2, c % 2
        o = np.asarray(res.results[c]["out"], dtype=np.float32)
        for j, i in enumerate(own_blocks(r)):
            out[b, i * 128:(i + 1) * 128] = o[j * 128:(j + 1) * 128]
    return out
```

```python
from contextlib import ExitStack
import numpy as np
import concourse.bass as bass
import concourse.mybir as mybir
from concourse.bass_utils import run_bass_kernel_spmd

F32 = mybir.dt.float32
BF16 = mybir.dt.bfloat16
I32 = mybir.dt.int32
ALU = mybir.AluOpType
AF = mybir.ActivationFunctionType
AX = mybir.AxisListType

D = 1024
S = 4096
NB = 32
NS = 16
EPS = 1e-6
NIT = 14
TOPK = 256
NEG = -1.0e30
C_U, C_Q, C_K, C_V, C_QI, C_KI, C_WI, C_GC, C_GA = 0, 1024, 1536, 2048, 2560, 3072, 3136, 3144, 4168
IDX_SCALE = float((8 ** -0.5) * (64 ** -0.5))
ATT_SCALE = float(64 ** -0.5)


class Buf:
    __slots__ = ("name", "t", "lw", "rd", "dsem", "dcnt")

    def __init__(self, name, t=None):
        self.name = name
        self.t = t
        self.lw = None
        self.rd = {}
        self.dsem = None
        self.dcnt = 0

    def __getitem__(self, key):
        return self.t[key]


class Eng:
    def __init__(self, k, name, eng):
        self.name = name
        self.eng = eng
        self.sem = k.new_sem("e_" + name)
        self.cnt = 0
        self.seen = {}

    def wait_tok(self, tok):
        if tok is None:
            return
        sem, val, _ = tok
        key = id(sem)
        if self.seen.get(key, 0) >= val:
            return
        self.eng.wait_ge(sem, val)
        self.seen[key] = val


class K:
    def __init__(self, nc, stack):
        self.nc = nc
        self.stack = stack
        self.pe = Eng(self, "pe", nc.tensor)
        self.act = Eng(self, "act", nc.scalar)
        self.dve = Eng(self, "dve", nc.vector)
        self.pool = Eng(self, "pool", nc.gpsimd)
        self.sp = Eng(self, "sp", nc.sync)
        self.engs = [self.pe, self.act, self.dve, self.pool, self.sp]
        self.dma_bufs = []
        self.nins = 0
        self.banks = []
        self.bank_i = 0
        self.pinned = set()

    def new_sem(self, name):
        return self.stack.enter_context(self.nc.semaphore(name))

    def sb(self, name, shape, dt, stack=None):
        t = (stack or self.stack).enter_context(self.nc.sbuf_tensor("s_" + name, list(shape), dt))
        return Buf(name, t)

    def mk_banks(self):
        for i in range(8):
            t = self.stack.enter_context(self.nc.psum_tensor("bank%d" % i, [128, 512], F32))
            self.banks.append(Buf("bank%d" % i, t))

    def bank(self, pin=False):
        for _ in range(16):
            b = self.banks[self.bank_i % 8]
            self.bank_i += 1
            if b.name not in self.pinned:
                if pin:
                    self.pinned.add(b.name)
                return b
        raise RuntimeError("no psum bank")

    def unpin(self, b):
        self.pinned.discard(b.name)

    def _deps(self, e, reads, writes):
        for b in reads:
            if b.lw is not None:
                e.wait_tok(b.lw)
        for b in writes:
            if b.lw is not None and b.lw[2] != e.name:
                e.wait_tok(b.lw)
            for en, tok in b.rd.items():
                if en != e.name:
                    e.wait_tok(tok)

    def _commit(self, tok, reads, writes):
        for b in reads:
            b.rd[tok[2]] = tok
        for b in writes:
            b.lw = tok
            b.rd = {}

    def op(self, e, fn, reads=(), writes=()):
        self._deps(e, reads, writes)
        ins = fn(e.eng)
        e.cnt += 1
        ins.then_inc(e.sem, 1)
        tok = (e.sem, e.cnt, e.name)
        self._commit(tok, reads, writes)
        self.nins += 1
        return tok

    def dma(self, e, out, in_, reads=(), writes=(), sembuf=None):
        self._deps(e, reads, writes)
        sb_ = sembuf if sembuf is not None else (writes[0] if writes else reads[0])
        if sb_.dsem is None:
            sb_.dsem = self.new_sem("d_" + sb_.name)
            self.dma_bufs.append(sb_)
        ins = e.eng.dma_start(out=out, in_=in_)
        sb_.dcnt += 16
        ins.then_inc(sb_.dsem, 16)
        tok = (sb_.dsem, sb_.dcnt, "dma_" + sb_.name)
        self._commit(tok, reads, writes)
        self.nins += 1
        return tok

    def barrier(self):
        toks = [(e.sem, e.cnt, e.name) for e in self.engs if e.cnt > 0]
        toks += [(b.dsem, b.dcnt, "dma") for b in self.dma_bufs]
        for e in self.engs:
            for t in toks:
                if t[2] != e.name:
                    e.wait_tok(t)


def bf(ap):
    return ap.bitcast(BF16)


def build_nc(phases=("A1", "A2", "B", "C"), debug=False, nslots=NS, bstop=9):
    nc = bass.Bass("TRN2", target_bir_lowering=False)

    def din(name, shape, dt=F32):
        return nc.dram_tensor(name, list(shape), dt, kind="ExternalInput").ap()

    def dscr(name, shape, dt):
        return nc.dram_tensor(name, list(shape), dt, kind="Internal").ap()

    x_full = din("x_full", [S, D])
    x_own = din("x_own", [NS * 128, D])
    x_halo = din("x_halo", [NS * 32, D])
    pos_full = din("pos_full", [128, NB], I32)
    pos_own = din("pos_own", [128, NS], I32)
    invf = din("invf", [128, 32])
    cmask_d = din("cmask", [128, NS, 256])
    ident_d = din("ident", [128, 128])
    w_in = din("w_in", [D, 5192])
    wdw_d = din("wdw", [128, 4, 31])
    bdw_d = din("bdw", [128, 4])
    lng_d = din("lng", [128, 4])
    lnb_d = din("lnb", [128, 4])
    gmix_d = din("gmix", [128, 8])
    gffn_d = din("gffn", [128, 8])
    gfin_d = din("gfin", [128, D])
    br_d = din("b_r", [128, 20])
    wr_d = din("w_r", [D, 20])
    wco_d = din("w_conv_out", [512, D])
    wao_d = din("w_attn_out", [512, D])
    wo_d = din("w_o", [D, D])
    wg_d = din("w_gate", [16, D, 256])
    wu_d = din("w_up", [16, D, 256])
    wd_d = din("w_down", [16, 256, D])
    out_d = nc.dram_tensor("out", [NS * 128, D], F32, kind="ExternalOutput").ap()

    qT_d = dscr("qT_d", [NS, 128, 512], BF16)
    qiT_d = dscr("qiT_d", [NS, 128, 512], BF16)
    mc_d = dscr("mc_d", [NS, 128, 1024], BF16)
    sga_d = dscr("sga_d", [NS, 128, 1024], BF16)
    x1_d = dscr("x1_d", [NS, 128, D], F32)
    h2T_d = dscr("h2T_d", [NS, 128, 1024], BF16)
    dbg = {}
    if debug:
        dbg["kT"] = nc.dram_tensor("dbg_kT", [128, 4, S], BF16, kind="ExternalOutput").ap()
        dbg["v"] = nc.dram_tensor("dbg_v", [128, NB, 8 * 66], BF16, kind="ExternalOutput").ap()
        dbg["kiT"] = nc.dram_tensor("dbg_kiT", [128, S], BF16, kind="ExternalOutput").ap()
        dbg["qT"] = nc.dram_tensor("dbg_qT", [NS, 128, 512], BF16, kind="ExternalOutput").ap()
        dbg["qiT"] = nc.dram_tensor("dbg_qiT", [NS, 128, 512], BF16, kind="ExternalOutput").ap()
        dbg["mc"] = nc.dram_tensor("dbg_mc", [NS, 128, 1024], BF16, kind="ExternalOutput").ap()
        dbg["sga"] = nc.dram_tensor("dbg_sga", [NS, 128, 1024], BF16, kind="ExternalOutput").ap()
        dbg["wi"] = nc.dram_tensor("dbg_wi", [128, NS * 8], F32, kind="ExternalOutput").ap()
        dbg["x1"] = nc.dram_tensor("dbg_x1", [NS, 128, D], F32, kind="ExternalOutput").ap()
        dbg["gw"] = nc.dram_tensor("dbg_gw", [128, NS * 16], F32, kind="ExternalOutput").ap()
        dbg["sc"] = nc.dram_tensor("dbg_sc", [NS, 128, S], F32, kind="ExternalOutput").ap()
        dbg["thr"] = nc.dram_tensor("dbg_thr", [NS, 128, 1], F32, kind="ExternalOutput").ap()
        dbg["attn"] = nc.dram_tensor("dbg_attn", [NS, 128, 512], F32, kind="ExternalOutput").ap()

    with ExitStack() as st:
        k = K(nc, st)
        pe, act, dve, pool, sp = k.pe, k.act, k.dve, k.pool, k.sp
        k.mk_banks()

        ident = k.sb("ident", [128, 128], BF16)
        identf = k.sb("identf", [128, 128], F32)
        wi_all = k.sb("wi_all", [128, NS * 8], F32)
        gw_all = k.sb("gw_all", [128, NS * 16], F32)
        gmix = k.sb("gmix", [128, 8], F32)
        gffn = k.sb("gffn", [128, 8], F32)
        ssL = [k.sb("ss%d" % i, [128, 1], F32) for i in range(2)]
        rsL = [k.sb("rs%d" % i, [128, 1], F32) for i in range(2)]
        hbL = [k.sb("hb%d" % i, [128, D], BF16) for i in range(2)]
        nrm_i = [0]

        k.dma(sp, identf[:], ident_d, writes=[identf])
        k.dma(pool, ident[:], ident_d, writes=[ident])
        k.dma(sp, gmix[:], gmix_d, writes=[gmix])
        k.dma(sp, gffn[:], gffn_d, writes=[gffn])

        def rope_tables(pos_d, n, cosT, sinT, stk):
            pi_ = k.sb("pos_i%d" % n, [128, n], I32, stk)
            pf = k.sb("pos_f%d" % n, [128, n], F32, stk)
            iv = k.sb("invf%d" % n, [128, 32], F32, stk)
            ang = k.sb("ang%d" % n, [128, n, 32], F32, stk)
            tmp = k.sb("angt%d" % n, [128, n, 32], F32, stk)
            k.dma(sp, pi_[:], pos_d, writes=[pi_])
            k.dma(sp, iv[:], invf, writes=[iv])
            k.op(dve, lambda e: e.tensor_copy(pf[:], pi_[:]), reads=[pi_], writes=[pf])
            k.op(dve, lambda e: e.tensor_tensor(ang[:], pf[:].unsqueeze(2).to_broadcast([128, n, 32]),
                                                iv[:].unsqueeze(1).to_broadcast([128, n, 32]), ALU.mult),
                 reads=[pf, iv], writes=[ang])
            ki_ = k.sb("angk%d" % n, [128, n, 32], I32, stk)
            kf_ = k.sb("angf%d" % n, [128, n, 32], F32, stk)
            two_pi = float(2 * np.pi)
            C1 = 6.28125
            C2 = float(2 * np.pi - 6.28125)
            PI_SAFE = 3.1415925

            def sin_of(dst, shift):
                if shift != 0.0:
                    k.op(dve, lambda e: e.tensor_scalar(tmp[:], ang[:], shift, None, ALU.add), reads=[ang], writes=[tmp])
                    src = tmp
                else:
                    src = ang
                k.op(dve, lambda e: e.tensor_scalar(kf_[:], src[:], 1.0 / two_pi, None, ALU.mult),
                     reads=[src], writes=[kf_])
                k.op(dve, lambda e: e.tensor_copy(ki_[:], kf_[:]), reads=[kf_], writes=[ki_])
                k.op(dve, lambda e: e.tensor_copy(kf_[:], ki_[:]), reads=[ki_], writes=[kf_])
                k.op(dve, lambda e: e.scalar_tensor_tensor(tmp[:], kf_[:], -C1, src[:], ALU.mult, ALU.add),
                     reads=[kf_, src], writes=[tmp])
                k.op(dve, lambda e: e.scalar_tensor_tensor(tmp[:], kf_[:], -C2, tmp[:], ALU.mult, ALU.add),
                     reads=[kf_, tmp], writes=[tmp])
                k.op(dve, lambda e: e.tensor_scalar(kf_[:], tmp[:], float(np.pi), -two_pi, ALU.is_gt, ALU.mult),
                     reads=[tmp], writes=[kf_])
                k.op(dve, lambda e: e.tensor_tensor(tmp[:], tmp[:], kf_[:], ALU.add), reads=[tmp, kf_], writes=[tmp])
                k.op(dve, lambda e: e.tensor_scalar(kf_[:], tmp[:], -float(np.pi), two_pi, ALU.is_lt, ALU.mult),
                     reads=[tmp], writes=[kf_])
                k.op(dve, lambda e: e.tensor_tensor(tmp[:], tmp[:], kf_[:], ALU.add), reads=[tmp, kf_], writes=[tmp])
                k.op(dve, lambda e: e.tensor_scalar(tmp[:], tmp[:], -PI_SAFE, PI_SAFE, ALU.max, ALU.min),
                     reads=[tmp], writes=[tmp])
                k.op(act, lambda e: e.activation(dst[:], tmp[:], AF.Sin), reads=[tmp], writes=[dst])

            sin_of(sinT, 0.0)
            sin_of(cosT, float(0.5 * np.pi))

        hbN = 4
        hbX = [k.sb("hbx%d" % i, [128, D], BF16) for i in range(hbN - 2)]

        def norm_pre(xt, n, src=None):
            if src is None:
                src = xt[0:n, :]
            hbs = hbL + hbX
            ss, rs, hb = ssL[nrm_i[0] % 2], rsL[nrm_i[0] % 2], hbs[nrm_i[0] % hbN]
            junk = hb
            nrm_i[0] += 1
            k.op(dve, lambda e: e.memset(ss[0:n, :], 0.0), writes=[ss])
            k.op(act, lambda e: e.activation(junk[0:n, :], src, AF.Square, accum_out=ss[0:n, 0:1]),
                 reads=[xt, ss], writes=[junk, ss])
            k.op(act, lambda e: e.activation(rs[0:n, :], ss[0:n, :], AF.Sqrt, bias=EPS, scale=1.0 / D),
                 reads=[ss], writes=[rs])
            k.op(dve, lambda e: e.reciprocal(rs[0:n, :], rs[0:n, :]), reads=[rs], writes=[rs])
            k.op(act, lambda e: e.activation(hb[0:n, :], src, AF.Copy, scale=rs[0:n, 0:1]),
                 reads=[xt, rs], writes=[hb])
            return hb

        def norm_post(hb, n, gT, hT, hT_ap=None):
            if hT_ap is None:
                hT_ap = hT[:, :, 0:n]
            tp = k.bank()
            tpv = bf(tp[:])[:, 0:8 * 128].rearrange("p (a b) -> p a b", a=8)
            for kc in range(8):
                k.op(pe, lambda e, kc=kc: e.transpose(tpv[:, kc, 0:n], hb[0:n, kc * 128:(kc + 1) * 128],
                                                      ident[0:n, 0:n]),
                     reads=[hb, ident], writes=[tp])
            k.op(dve, lambda e: e.tensor_tensor(hT_ap, tpv[:, :, 0:n],
                                                gT[:].unsqueeze(2).to_broadcast([128, 8, n]), ALU.mult),
                 reads=[tp, gT], writes=[hT])

        def norm_T(xt, n, gT, hT, hT_ap=None):
            hb = norm_pre(xt, n)
            norm_post(hb, n, gT, hT, hT_ap)

        def proj_tok(hT, w, c0, ncols):
            pb = k.bank()
            for kc in range(8):
                k.op(pe, lambda e, kc=kc: e.matmul(pb[:, 0:ncols], hT[:, kc, :], w[:, kc, c0:c0 + ncols],
                                                   start=(kc == 0), stop=(kc == 7)),
                     reads=[hT, w], writes=[pb])
            return pb

        def rope(pb, nh, cosT, sinT, ti, outb, tmpA, tmpB):
            pv = pb[:, 0:nh * 64].rearrange("p (h d) -> p h d", h=nh)
            ov = outb[:, 0:nh * 64].rearrange("p (h d) -> p h d", h=nh)
            av = tmpA[:, 0:nh * 64].rearrange("p (h d) -> p h d", h=nh)
            bv = tmpB[:, 0:nh * 64].rearrange("p (h d) -> p h d", h=nh)
            cb = cosT[:, ti, :].unsqueeze(1).to_broadcast([128, nh, 32])
            sb_ = sinT[:, ti, :].unsqueeze(1).to_broadcast([128, nh, 32])
            k.op(dve, lambda e: e.tensor_tensor(av[:, :, 0:32], pv[:, :, 0:32], cb, ALU.mult),
                 reads=[pb, cosT], writes=[tmpA])
            k.op(dve, lambda e: e.tensor_tensor(av[:, :, 32:64], pv[:, :, 32:64], cb, ALU.mult),
                 reads=[pb, cosT], writes=[tmpA])
            k.op(dve, lambda e: e.tensor_tensor(bv[:, :, 0:32], pv[:, :, 32:64], sb_, ALU.mult),
                 reads=[pb, sinT], writes=[tmpB])
            k.op(dve, lambda e: e.tensor_tensor(bv[:, :, 32:64], pv[:, :, 0:32], sb_, ALU.mult),
                 reads=[pb, sinT], writes=[tmpB])
            k.op(dve, lambda e: e.tensor_tensor(ov[:, :, 0:32], av[:, :, 0:32], bv[:, :, 0:32], ALU.subtract),
                 reads=[tmpA, tmpB], writes=[outb])
            k.op(dve, lambda e: e.tensor_tensor(ov[:, :, 32:64], av[:, :, 32:64], bv[:, :, 32:64], ALU.add),
                 reads=[tmpA, tmpB], writes=[outb])

        w_in_v = w_in.rearrange("(k p) c -> p k c", p=128)
        def phase_A1():
            with ExitStack() as s1:
                cosF = k.sb("cosF", [128, NB, 32], F32, s1)
                sinF = k.sb("sinF", [128, NB, 32], F32, s1)
                rope_tables(pos_full, NB, cosF, sinF, s1)
                wA = k.sb("wA1", [128, 8, 1088], BF16, s1)
                for kc in range(8):
                    k.dma(pool, wA[:, kc, 0:1024], w_in_v[:, kc, C_K:C_K + 1024], writes=[wA])
                    k.dma(pool, wA[:, kc, 1024:1088], w_in_v[:, kc, C_KI:C_KI + 64], writes=[wA])
                xt2 = [k.sb("xtA%d" % i, [128, D], F32, s1) for i in range(2)]
                hT2 = [k.sb("hTA%d" % i, [128, 8, 128], BF16, s1) for i in range(2)]
                krL = [k.sb("kr%d" % i, [128, 512], BF16, s1) for i in range(2)]
                kirL = [k.sb("kir%d" % i, [128, 128], BF16, s1) for i in range(2)]
                tAL = [k.sb("tA%d" % i, [128, 512], F32, s1) for i in range(2)]
                tBL = [k.sb("tB%d" % i, [128, 512], F32, s1) for i in range(2)]
                tCL = [k.sb("tC%d" % i, [128, 64], F32, s1) for i in range(2)]
                tDL = [k.sb("tD%d" % i, [128, 64], F32, s1) for i in range(2)]
                hbs1 = {}

                def pre1(ti):
                    k.dma(sp, xt2[ti % 2][:], x_full[ti * 128:(ti + 1) * 128, :], writes=[xt2[ti % 2]])
                    hbs1[ti] = norm_pre(xt2[ti % 2], 128)

                def post1(ti):
                    norm_post(hbs1.pop(ti), 128, gmix, hT2[ti % 2])

                pre1(0)
                post1(0)
                pre1(1)
                for ti in range(NB):
                    if ti + 1 < NB:
                        post1(ti + 1)
                    hT = hT2[ti % 2]
                    kr, kir, tA, tB = krL[ti % 2], kirL[ti % 2], tAL[ti % 2], tBL[ti % 2]
                    tC, tD = tCL[ti % 2], tDL[ti % 2]
                    pk = proj_tok(hT, wA, 0, 512)
                    pv = proj_tok(hT, wA, 512, 512)
                    pki = proj_tok(hT, wA, 1024, 64)
                    if ti + 2 < NB:
                        pre1(ti + 2)
                    rope(pk, 8, cosF, sinF, ti, kr, tA, tB)
                    Vv = V[:, ti, :].rearrange("p (h d) -> p h d", h=8)
                    k.op(act, lambda e: e.activation(Vv[:, :, 0:64],
                                                     pv[:, 0:512].rearrange("p (h d) -> p h d", h=8), AF.Copy),
                         reads=[pv], writes=[V])
                    rope(pki, 1, cosF, sinF, ti, kir, tC, tD)
                    k.op(dve, lambda e, kir=kir: e.tensor_copy(kir[:, 64:128], kir[:, 0:64]), reads=[kir], writes=[kir])
                    tb = k.bank()
                    tbv = bf(tb[:])
                    for pr in range(4):
                        k.op(pe, lambda e, pr=pr: e.transpose(tbv[:, pr * 128:(pr + 1) * 128],
                                                              kr[:, pr * 128:(pr + 1) * 128], ident[:]),
                             reads=[kr, ident], writes=[tb])
                    k.op(pe, lambda e: e.transpose(tbv[:, 512:640], kir[:], ident[:]), reads=[kir, ident], writes=[tb])
                    k.op(act, lambda e: e.activation(KT[:, :, ti * 128:(ti + 1) * 128],
                                                     tbv[:, 0:512].rearrange("p (a b) -> p a b", a=4), AF.Copy),
                         reads=[tb], writes=[KT])
                    k.op(act, lambda e: e.activation(kiT[:, ti * 128:(ti + 1) * 128], tbv[:, 512:640], AF.Copy),
                         reads=[tb], writes=[kiT])
                k.barrier()
        def dbg_A1():
            for pr in range(4):
                k.dma(sp, dbg["kT"][:, pr, :], KT[:, pr, :], reads=[KT])
            for t8 in range(0, NB, 4):
                k.dma(sp, dbg["v"][:, t8:t8 + 4, :], V[:, t8:t8 + 4, :], reads=[V])
            k.dma(sp, dbg["kiT"], kiT[:], reads=[kiT])

        def phase_A2():
            with ExitStack() as s2:
                cosO = k.sb("cosO", [128, NS, 32], F32, s2)
                sinO = k.sb("sinO", [128, NS, 32], F32, s2)
                rope_tables(pos_own, NS, cosO, sinO, s2)
                WU, WQ, WQI, WWI, WGC, WGA = 0, 1024, 1536, 2048, 2056, 3080
                wB = k.sb("wB", [128, 8, 4104], BF16, s2)
                for kc in range(8):
                    k.dma(pool, wB[:, kc, WU:WU + 1024], w_in_v[:, kc, C_U:C_U + 1024], writes=[wB])
                    k.dma(pool, wB[:, kc, WQ:WQ + 512], w_in_v[:, kc, C_Q:C_Q + 512], writes=[wB])
                    k.dma(pool, wB[:, kc, WQI:WQI + 512], w_in_v[:, kc, C_QI:C_QI + 512], writes=[wB])
                    k.dma(pool, wB[:, kc, WWI:WWI + 8], w_in_v[:, kc, C_WI:C_WI + 8], writes=[wB])
                    k.dma(pool, wB[:, kc, WGC:WGC + 2048], w_in_v[:, kc, C_GC:C_GC + 2048], writes=[wB])
                wco = k.sb("wco", [128, 4, D], BF16, s2)
                for kc in range(4):
                    k.dma(pool, wco[:, kc, :], wco_d[kc * 128:(kc + 1) * 128, :], writes=[wco])
                wdw = k.sb("wdw", [128, 4, 31], F32, s2)
                bdw = k.sb("bdw", [128, 4], F32, s2)
                lng = k.sb("lng", [128, 4], F32, s2)
                lnb = k.sb("lnb", [128, 4], F32, s2)
                k.dma(sp, wdw[:], wdw_d, writes=[wdw])
                k.dma(sp, bdw[:], bdw_d, writes=[bdw])
                k.dma(sp, lng[:], lng_d, writes=[lng])
                k.dma(sp, lnb[:], lnb_d, writes=[lnb])
                Dg = k.sb("Dg", [128, 124, 128], BF16, s2)
                for c in range(4):
                    for j in range(31):
                        k.op(dve, lambda e, c=c, j=j: e.tensor_scalar(Dg[:, c * 31 + j, :], identf[:],
                                                                       wdw[:, c, j:j + 1], None, ALU.mult),
                             reads=[identf, wdw], writes=[Dg])
                ones = k.sb("ones", [128, 128], F32, s2)
                k.op(pool, lambda e: e.memset(ones[:], 1.0), writes=[ones])
                xt2 = [k.sb("xtB%d" % i, [128, D], F32, s2) for i in range(2)]
                xh2 = [k.sb("xhB%d" % i, [32, D], F32, s2) for i in range(2)]
                hT_L = [k.sb("hTB_%d" % i_, [128, 8, 128], BF16, s2) for i_ in range(2)]
                hTh_L = [k.sb("hThB_%d" % i_, [128, 8, 32], BF16, s2) for i_ in range(2)]
                qr_L = [k.sb("qr_%d" % i_, [128, 512], BF16, s2) for i_ in range(2)]
                qir_L = [k.sb("qir_%d" % i_, [128, 512], BF16, s2) for i_ in range(2)]
                tA_L = [k.sb("tA2_%d" % i_, [128, 512], F32, s2) for i_ in range(2)]
                tB_L = [k.sb("tB2_%d" % i_, [128, 512], F32, s2) for i_ in range(2)]
                qT_s_L = [k.sb("qT_s_%d" % i_, [128, 512], BF16, s2) for i_ in range(2)]
                qiT_s_L = [k.sb("qiT_s_%d" % i_, [128, 512], BF16, s2) for i_ in range(2)]
                sg_L = [k.sb("sgl_%d" % i_, [128, 160], F32, s2) for i_ in range(2)]
                gT_L = [k.sb("gT_%d" % i_, [128, 4, 160], BF16, s2) for i_ in range(2)]
                csb_L = [k.sb("csb_%d" % i_, [128, 4, 128], F32, s2) for i_ in range(2)]
                csq_L = [k.sb("csq_%d" % i_, [128, 4, 128], F32, s2) for i_ in range(2)]
                mean_L = [k.sb("mean_%d" % i_, [128, 128], F32, s2) for i_ in range(2)]
                msq_L = [k.sb("msq_%d" % i_, [128, 128], F32, s2) for i_ in range(2)]
                rstd_L = [k.sb("rstd_%d" % i_, [128, 128], F32, s2) for i_ in range(2)]
                nrm_L = [k.sb("nrm_%d" % i_, [128, 4, 128], F32, s2) for i_ in range(2)]
                snT_L = [k.sb("snT_%d" % i_, [128, 4, 128], BF16, s2) for i_ in range(2)]
                sgc_L = [k.sb("sgc_%d" % i_, [128, 8, 128], F32, s2) for i_ in range(2)]
                mc_s_L = [k.sb("mc_s_%d" % i_, [128, 8, 128], BF16, s2) for i_ in range(2)]
                sga_s_L = [k.sb("sga_s_%d" % i_, [128, 8, 128], BF16, s2) for i_ in range(2)]
                qTd_b = Buf("qTd_b")
                mcd_b = Buf("mcd_b")

                hbs2 = {}

                def pre2(j):
                    k.dma(sp, xt2[j % 2][:], x_own[j * 128:(j + 1) * 128, :], writes=[xt2[j % 2]])
                    k.dma(sp, xh2[j % 2][:], x_halo[j * 32:(j + 1) * 32, :], writes=[xh2[j % 2]])
                    hbs2[j] = (norm_pre(xh2[j % 2], 32), norm_pre(xt2[j % 2], 128))

                def post2(j):
                    a_, b_ = hbs2.pop(j)
                    norm_post(a_, 32, gmix, hTh_L[j % 2])
                    norm_post(b_, 128, gmix, hT_L[j % 2])

                for j in range(nslots):
                    xt = xt2[j % 2]
                    xh = xh2[j % 2]
                    hT, hTh, qr, qir, tA, tB, qT_s, qiT_s, sg, gT, csb, csq, mean, msq, rstd, nrm, snT, sgc, mc_s, sga_s = hT_L[j % 2], hTh_L[j % 2], qr_L[j % 2], qir_L[j % 2], tA_L[j % 2], tB_L[j % 2], qT_s_L[j % 2], qiT_s_L[j % 2], sg_L[j % 2], gT_L[j % 2], csb_L[j % 2], csq_L[j % 2], mean_L[j % 2], msq_L[j % 2], rstd_L[j % 2], nrm_L[j % 2], snT_L[j % 2], sgc_L[j % 2], mc_s_L[j % 2], sga_s_L[j % 2]
                    if j == 0:
                        pre2(0)
                        post2(0)
                        if nslots > 1:
                            pre2(1)
                    if j + 1 < nslots:
                        post2(j + 1)
                    pq = proj_tok(hT, wB, WQ, 512)
                    rope(pq, 8, cosO, sinO, j, qr, tA, tB)
                    pqi = proj_tok(hT, wB, WQI, 512)
                    rope(pqi, 8, cosO, sinO, j, qir, tA, tB)
                    pw = proj_tok(hT, wB, WWI, 8)
                    k.op(act, lambda e: e.activation(wi_all[:, j * 8:(j + 1) * 8], pw[:, 0:8], AF.Copy,
                                                     scale=IDX_SCALE), reads=[pw], writes=[wi_all])
                    for src, dstb, dd in ((qr, qT_s, qT_d), (qir, qiT_s, qiT_d)):
                        tb = k.bank()
                        tbv = bf(tb[:])
                        for pr in range(4):
                            k.op(pe, lambda e, pr=pr, src=src, tbv=tbv: e.transpose(
                                tbv[:, pr * 128:(pr + 1) * 128], src[:, pr * 128:(pr + 1) * 128], ident[:]),
                                 reads=[src, ident], writes=[tb])
                        k.op(act, lambda e, dstb=dstb, tbv=tbv: e.activation(dstb[:], tbv[:, 0:512], AF.Copy),
                             reads=[tb], writes=[dstb])
                        k.dma(sp, dd[j], dstb[:], reads=[dstb], writes=[qTd_b], sembuf=dstb)
                    for c in range(4):
                        pu = k.bank()
                        puv = pu[:, 0:320].rearrange("p (a b) -> p a b", a=2)
                        for half, col0 in ((0, WU + c * 128), (1, WU + 512 + c * 128)):
                            for kc in range(8):
                                k.op(pe, lambda e, kc=kc, half=half, col0=col0, puv=puv: e.matmul(
                                    puv[:, half, 0:32], wB[:, kc, col0:col0 + 128], hTh[:, kc, :],
                                    start=(kc == 0), stop=(kc == 7)), reads=[wB, hTh], writes=[pu])
                            for kc in range(8):
                                k.op(pe, lambda e, kc=kc, half=half, col0=col0, puv=puv: e.matmul(
                                    puv[:, half, 32:160], wB[:, kc, col0:col0 + 128], hT[:, kc, :],
                                    start=(kc == 0), stop=(kc == 7)), reads=[wB, hT], writes=[pu])
                        k.op(act, lambda e, puv=puv: e.activation(sg[:], puv[:, 1, :], AF.Sigmoid),
                             reads=[pu], writes=[sg])
                        k.op(dve, lambda e, puv=puv, c=c: e.tensor_tensor(gT[:, c, :], puv[:, 0, :], sg[:], ALU.mult),
                             reads=[pu, sg], writes=[gT])
                    if j + 2 < nslots:
                        pre2(j + 2)
                    pc = k.bank()
                    pcv = pc[:, 0:512].rearrange("p (a b) -> p a b", a=4)
                    for c in range(4):
                        for jj in range(31):
                            k.op(pe, lambda e, c=c, jj=jj: e.matmul(pcv[:, c, :], Dg[:, c * 31 + jj, :],
                                                                    gT[:, c, 2 + jj:2 + jj + 128],
                                                                    start=(jj == 0), stop=(jj == 30)),
                                 reads=[Dg, gT], writes=[pc])
                    k.op(dve, lambda e: e.tensor_tensor(csb[:], pcv, bdw[:].unsqueeze(2).to_broadcast([128, 4, 128]),
                                                        ALU.add), reads=[pc, bdw], writes=[csb])
                    k.op(act, lambda e: e.activation(csq[:], csb[:], AF.Square), reads=[csb], writes=[csq])
                    pst = k.bank()
                    for c in range(4):
                        k.op(pe, lambda e, c=c: e.matmul(pst[:, 0:128], ones[:], csb[:, c, :], start=(c == 0),
                                                         stop=(c == 3)), reads=[ones, csb], writes=[pst])
                    for c in range(4):
                        k.op(pe, lambda e, c=c: e.matmul(pst[:, 128:256], ones[:], csq[:, c, :], start=(c == 0),
                                                         stop=(c == 3)), reads=[ones, csq], writes=[pst])
                    k.op(act, lambda e: e.activation(mean[:], pst[:, 0:128], AF.Copy, scale=1.0 / 512),
                         reads=[pst], writes=[mean])
                    k.op(dve, lambda e: e.tensor_tensor(msq[:], mean[:], mean[:], ALU.mult), reads=[mean], writes=[msq])
                    k.op(dve, lambda e: e.scalar_tensor_tensor(rstd[:], pst[:, 128:256], 1.0 / 512, msq[:],
                                                               ALU.mult, ALU.subtract),
                         reads=[pst, msq], writes=[rstd])
                    k.op(act, lambda e: e.activation(rstd[:], rstd[:], AF.Sqrt, bias=EPS, scale=1.0),
                         reads=[rstd], writes=[rstd])
                    k.op(dve, lambda e: e.reciprocal(rstd[:], rstd[:]), reads=[rstd], writes=[rstd])
                    k.op(dve, lambda e: e.tensor_tensor(nrm[:], csb[:], mean[:].unsqueeze(1).to_broadcast([128, 4, 128]),
                                                        ALU.subtract), reads=[csb, mean], writes=[nrm])
                    k.op(dve, lambda e: e.tensor_tensor(nrm[:], nrm[:], rstd[:].unsqueeze(1).to_broadcast([128, 4, 128]),
                                                        ALU.mult), reads=[nrm, rstd], writes=[nrm])
                    for c in range(4):
                        k.op(act, lambda e, c=c: e.activation(snT[:, c, :], nrm[:, c, :], AF.Silu,
                                                              bias=lnb[:, c:c + 1], scale=lng[:, c:c + 1]),
                             reads=[nrm, lnb, lng], writes=[snT])
                    for gi, (wcol, dst) in enumerate(((WGC, None), (WGA, sga_s))):
                        for hb_ in range(2):
                            pg = k.bank()
                            pgv = pg[:, 0:512].rearrange("p (a b) -> p a b", a=4)
                            for m in range(4):
                                col0 = wcol + (hb_ * 4 + m) * 128
                                for kc in range(8):
                                    k.op(pe, lambda e, kc=kc, m=m, col0=col0, pgv=pgv: e.matmul(
                                        pgv[:, m, :], wB[:, kc, col0:col0 + 128], hT[:, kc, :],
                                        start=(kc == 0), stop=(kc == 7)), reads=[wB, hT], writes=[pg])
                            tgt = sgc if gi == 0 else sga_s
                            k.op(act, lambda e, pgv=pgv, tgt=tgt, hb_=hb_: e.activation(
                                tgt[:, hb_ * 4:(hb_ + 1) * 4, :], pgv, AF.Sigmoid), reads=[pg], writes=[tgt])
                    for hb_ in range(2):
                        py = k.bank()
                        pyv = py[:, 0:512].rearrange("p (a b) -> p a b", a=4)
                        for m in range(4):
                            mm = hb_ * 4 + m
                            for kc in range(4):
                                k.op(pe, lambda e, kc=kc, m=m, mm=mm, pyv=pyv: e.matmul(
                                    pyv[:, m, :], wco[:, kc, mm * 128:(mm + 1) * 128], snT[:, kc, :],
                                    start=(kc == 0), stop=(kc == 3)), reads=[wco, snT], writes=[py])
                        k.op(dve, lambda e, pyv=pyv, hb_=hb_: e.tensor_tensor(
                            mc_s[:, hb_ * 4:(hb_ + 1) * 4, :], pyv, sgc[:, hb_ * 4:(hb_ + 1) * 4, :], ALU.mult),
                             reads=[py, sgc], writes=[mc_s])
                    k.dma(sp, mc_d[j], mc_s[:].rearrange("p a b -> p (a b)"), reads=[mc_s], writes=[mcd_b], sembuf=mc_s)
                    k.dma(sp, sga_d[j], sga_s[:].rearrange("p a b -> p (a b)"), reads=[sga_s], writes=[mcd_b],
                          sembuf=sga_s)
                k.barrier()
        def dbg_A2():
            k.dma(sp, dbg["wi"], wi_all[:], reads=[wi_all])
            for nm, src in (("qT", qT_d), ("qiT", qiT_d), ("mc", mc_d), ("sga", sga_d)):
                b_ = Buf("dbgc_" + nm)
                k.dma(sp, dbg[nm], src, writes=[b_])
            k.barrier()

        def phase_B():
            with ExitStack() as s3:
                cm2 = [k.sb("cm%d" % i, [128, 256], F32, s3) for i in range(2)]
                pw2 = k.sb("pw2", [128, NIT + 2], F32, s3)
                for it in range(NIT + 2):
                    k.op(dve, lambda e, it=it: e.memset(pw2[:, it:it + 1], float(2.0 ** -(it + 1))), writes=[pw2])
                qT2 = [k.sb("qTb%d" % i, [128, 4, 256], BF16, s3) for i in range(2)]
                for b_ in qT2:
                    k.op(dve, lambda e: e.memset(b_[:], 0.0), writes=[b_])
                qiT2 = [k.sb("qiTb%d" % i, [128, 512], BF16, s3) for i in range(2)]
                scL = [k.sb("sc%d" % i, [128, S], F32, s3) for i in range(2)]
                AL = [k.sb("Ab%d" % i, [128, 512], BF16, s3) for i in range(4)]
                wabs = k.sb("wabs", [128, 8], F32, s3)
                sgn = k.sb("sgn", [128, 8], F32, s3)
                sgd = k.sb("sgd", [128, 8, 128], BF16, s3)
                msk = k.sb("msk", [128, S], BF16, s3)
                Mb = k.sb("Mb", [128, NB, 128], BF16, s3)
                st8 = k.sb("st8", [128, 8], F32, s3)
                Q = k.sb("Qtab", [128, NIT + 2], F32, s3)
                Q2 = k.sb("Q2tab", [128, NIT + 2], F32, s3)
                E2 = [k.sb("Eb%d" % i, [128, 4, 128], BF16, s3) for i in range(3)]
                rden = k.sb("rden", [128, 8], F32, s3)
                Osb = k.sb("Osb", [128, 2, 4, 66], F32, s3)
                thr = st8[:, 6:7]
                ai = [0]
                MBIG = 30000.0
                if debug:
                    attnf = k.sb("attnf", [128, 512], F32, s3)

                def loads_a(j):
                    p = j % 2
                    k.dma(sp, qiT2[p][:], qiT_d[j], writes=[qiT2[p]])
                    k.dma(sp, cm2[p][:], cmask_d[:, j, :], writes=[cm2[p]])

                def loads_b(j):
                    p = j % 2
                    k.dma(sp, qT2[p][0:64, :, 0:128], qT_d[j][0:64, :].rearrange("p (a b) -> p a b", a=4), writes=[qT2[p]])
                    k.dma(sp, qT2[p][64:128, :, 128:256], qT_d[j][64:128, :].rearrange("p (a b) -> p a b", a=4),
                          writes=[qT2[p]])

                def prep(j):
                    wsl = wi_all[:, j * 8:(j + 1) * 8]
                    k.op(dve, lambda e: e.tensor_scalar(sgn[:], wsl, 0.0, 2.0, ALU.is_ge, ALU.mult), reads=[wi_all], writes=[sgn])
                    k.op(dve, lambda e: e.tensor_scalar(sgn[:], sgn[:], -1.0, None, ALU.add), reads=[sgn], writes=[sgn])
                    k.op(dve, lambda e: e.tensor_tensor(wabs[:], wsl, sgn[:], ALU.mult), reads=[wi_all, sgn], writes=[wabs])
                    k.op(dve, lambda e: e.tensor_tensor(sgd[:], ident[:].unsqueeze(1).to_broadcast([128, 8, 128]),
                                                        sgn[:].unsqueeze(2).to_broadcast([128, 8, 128]), ALU.mult),
                         reads=[ident, sgn], writes=[sgd])

                def indexer(j):
                    sc = scL[j % 2]
                    qiTs = qiT2[j % 2]
                    L = 256 * (j + 1)
                    nch = (L + 511) // 512
                    for cc in range(nch):
                        c0 = cc * 512
                        W = min(512, L - c0)
                        pacc = k.bank(pin=True)
                        pbs = {}

                        def qi_mm(h):
                            pb = k.bank()
                            pp = (h % 2) * 64
                            k.op(pe, lambda e: e.matmul(pb[:, 0:W], qiTs[pp:pp + 64, (h // 2) * 128:(h // 2 + 1) * 128],
                                                        kiT[pp:pp + 64, c0:c0 + W], start=True, stop=True),
                                 reads=[qiTs, kiT], writes=[pb])
                            pbs[h] = pb

                        qi_mm(0)
                        qi_mm(1)
                        qi_mm(2)
                        for h in range(8):
                            if h + 3 < 8:
                                qi_mm(h + 3)
                            pb = pbs[h]
                            A = AL[ai[0] % 4]
                            ai[0] += 1
                            k.op(act, lambda e: e.activation(A[:, 0:W], pb[:, 0:W], AF.Relu, scale=wabs[:, h:h + 1]),
                                 reads=[pb, wabs], writes=[A])
                            k.op(pe, lambda e: e.matmul(pacc[:, 0:W], sgd[:, h, :], A[:, 0:W], start=(h == 0), stop=(h == 7)),
                                 reads=[sgd, A], writes=[pacc])
                        k.op(act, lambda e: e.activation(sc[:, c0:c0 + W], pacc[:, 0:W], AF.Copy), reads=[pacc], writes=[sc])
                        k.unpin(pacc)

                def thresh(j):
                    sc = scL[j % 2]
                    L = 256 * (j + 1)
                    cmask = cm2[j % 2]
                    if j == 0:
                        k.op(dve, lambda e: e.tensor_tensor(sc[:, L - 256:L], sc[:, L - 256:L], cmask[:], ALU.add),
                             reads=[sc, cmask], writes=[sc])
                        k.op(dve, lambda e: e.memset(thr, -1.0e29), writes=[st8])
                        return
                    k.op(dve, lambda e: e.tensor_reduce(st8[:, 0:1], sc[:, 0:L], AX.X, ALU.max), reads=[sc], writes=[st8])
                    k.op(dve, lambda e: e.tensor_reduce(st8[:, 1:2], sc[:, 0:L], AX.X, ALU.min), reads=[sc], writes=[st8])
                    k.op(dve, lambda e: e.tensor_tensor(sc[:, L - 256:L], sc[:, L - 256:L], cmask[:], ALU.add),
                         reads=[sc, cmask], writes=[sc])
                    k.op(dve, lambda e: e.tensor_tensor(st8[:, 2:3], st8[:, 0:1], st8[:, 1:2], ALU.subtract),
                         reads=[st8], writes=[st8])
                    k.op(dve, lambda e: e.tensor_scalar(st8[:, 5:6], st8[:, 2:3], float(2.0 ** -10), None, ALU.mult),
                         reads=[st8], writes=[st8])
                    k.op(dve, lambda e: e.tensor_scalar(st8[:, 2:3], st8[:, 2:3], float(1.0 + 2.0 ** -9), 1e-30,
                                                        ALU.mult, ALU.add), reads=[st8], writes=[st8])
                    k.op(dve, lambda e: e.tensor_tensor(st8[:, 7:8], st8[:, 1:2], st8[:, 5:6], ALU.subtract),
                         reads=[st8], writes=[st8])
                    k.op(dve, lambda e: e.tensor_scalar(Q[:], pw2[:], st8[:, 2:3], None, ALU.mult),
                         reads=[pw2, st8], writes=[Q])
                    k.op(dve, lambda e: e.tensor_scalar(Q2[:], Q[:], 2.0, None, ALU.mult), reads=[Q], writes=[Q2])
                    k.op(dve, lambda e: e.tensor_tensor(st8[:, 3:4], st8[:, 7:8], Q[:, 0:1], ALU.add),
                         reads=[st8, Q], writes=[st8])
                    for it in range(NIT):
                        k.op(dve, lambda e: e.memset(st8[:, 4:5], 0.0), writes=[st8])
                        k.op(dve, lambda e: e.tensor_scalar(msk[:, 0:L], sc[:, 0:L], st8[:, 3:4], 0.0,
                                                            ALU.is_ge, ALU.add, accum_out=st8[:, 4:5]),
                             reads=[sc, st8], writes=[st8, msk])
                        k.op(dve, lambda e: e.tensor_scalar(st8[:, 5:6], st8[:, 4:5], TOPK - 0.5,
                                                            Q2[:, it + 1:it + 2], ALU.is_ge, ALU.mult),
                             reads=[st8, Q2], writes=[st8])
                        k.op(dve, lambda e: e.scalar_tensor_tensor(st8[:, 3:4], st8[:, 3:4], Q[:, it + 1:it + 2],
                                                                   st8[:, 5:6], ALU.subtract, ALU.add),
                             reads=[st8, Q], writes=[st8])
                    k.op(dve, lambda e: e.tensor_tensor(thr, st8[:, 3:4], Q[:, NIT:NIT + 1], ALU.subtract),
                         reads=[st8, Q], writes=[st8])

                def mask(j):
                    sc = scL[j % 2]
                    L = 256 * (j + 1)
                    nkb = 2 * (j + 1)
                    if debug:
                        b_ = Buf("dbg_sc%d" % j)
                        for c0_ in range(0, L, 1024):
                            c1_ = min(L, c0_ + 1024)
                            k.dma(sp, dbg["sc"][j][:, c0_:c1_], sc[:, c0_:c1_], reads=[sc], writes=[b_], sembuf=b_)
                        k.dma(sp, dbg["thr"][j], thr, reads=[st8], writes=[b_], sembuf=b_)
                    for g0 in range(0, nkb, 8):
                        gn = min(8, nkb - g0)
                        k.op(dve, lambda e: e.tensor_scalar(msk[:, g0 * 128:(g0 + gn) * 128],
                                                            sc[:, g0 * 128:(g0 + gn) * 128], thr, None, ALU.is_ge),
                             reads=[sc, st8], writes=[msk])
                        tb = k.bank()
                        tbv = bf(tb[:])
                        for q_ in range(gn):
                            k.op(pe, lambda e: e.transpose(tbv[:, q_ * 128:(q_ + 1) * 128],
                                                           msk[:, (g0 + q_) * 128:(g0 + q_ + 1) * 128], ident[:]),
                                 reads=[msk, ident], writes=[tb])
                        k.op(act, lambda e: e.activation(Mb[:, g0:g0 + gn, :],
                                                         tbv[:, 0:gn * 128].rearrange("p (a b) -> p a b", a=gn),
                                                         AF.Identity, bias=-MBIG, scale=MBIG),
                             reads=[tb], writes=[Mb])

                def attention(j):
                    qTs = qT2[j % 2]
                    nkb = 2 * (j + 1)
                    po = [k.bank(pin=True), k.bank(pin=True)]
                    pov = [b_[:, 0:512].rearrange("p (h d) -> p h d", h=4) for b_ in po]
                    units = [(kb, hg) for kb in range(nkb) for hg in range(2)]

                    def qk(u):
                        kb, hg = units[u]
                        pl = k.bank()
                        plv = pl[:, 0:512].rearrange("p (a b) -> p a b", a=4)
                        k.op(pe, lambda e: e.matmul(plv, ident[:], Mb[:, kb, :].unsqueeze(1).to_broadcast([128, 4, 128]),
                                                    start=True, stop=False), reads=[ident, Mb], writes=[pl])
                        for pi in range(2):
                            pr = hg * 2 + pi
                            k.op(pe, lambda e: e.matmul(pl[:, pi * 256:(pi + 1) * 256], KT[:, pr, kb * 128:(kb + 1) * 128],
                                                        qTs[:, pr, :], start=False, stop=(pi == 1)),
                                 reads=[KT, qTs], writes=[pl])
                        return pl, plv

                    LA = 2
                    pend = [qk(u) for u in range(min(LA, len(units)))]
                    for u in range(len(units)):
                        if u + LA < len(units):
                            pend.append(qk(u + LA))
                        kb, hg = units[u]
                        pl, plv = pend.pop(0)
                        E = E2[u % 3]
                        k.op(act, lambda e: e.activation(E[:], plv, AF.Exp, scale=ATT_SCALE), reads=[pl], writes=[E])
                        for hh in range(4):
                            h = hg * 4 + hh
                            k.op(pe, lambda e: e.matmul(pov[hg][:, hh, 0:66], E[:, hh, :], V[:, kb, h * 66:(h + 1) * 66],
                                                        start=(kb == 0 and hh == 0), stop=(kb == nkb - 1 and hh == 3)),
                                 reads=[E, V], writes=[po[hg]])
                    for hg in range(2):
                        k.op(act, lambda e: e.activation(Osb[:, hg, :, :], pov[hg][:, :, 0:66], AF.Copy),
                             reads=[po[hg]], writes=[Osb])
                        k.unpin(po[hg])

                def finalize(j):
                    for hg in range(2):
                        k.op(dve, lambda e: e.reciprocal(rden[:, hg * 4:(hg + 1) * 4].unsqueeze(2), Osb[:, hg, :, 64:65]),
                             reads=[Osb], writes=[rden])
                        k.op(dve, lambda e: e.tensor_tensor(
                            attn_all[:, j, hg * 256:(hg + 1) * 256].rearrange("p (h d) -> p h d", h=4), Osb[:, hg, :, 0:64],
                            rden[:, hg * 4:(hg + 1) * 4].unsqueeze(2).to_broadcast([128, 4, 64]), ALU.mult),
                             reads=[Osb, rden], writes=[attn_all])
                    if debug:
                        k.op(dve, lambda e: e.tensor_copy(attnf[:], attn_all[:, j, :]), reads=[attn_all], writes=[attnf])
                        k.dma(sp, dbg["attn"][j], attnf[:], reads=[attnf])

                loads_a(0)
                loads_b(0)
                if nslots > 1:
                    loads_a(1)
                    loads_b(1)
                prep(0)
                indexer(0)
                thresh(0)
                mask(0)
                if nslots > 1:
                    prep(1)
                    indexer(1)
                for i in range(nslots):
                    if i + 2 < nslots:
                        prep(i + 2)
                    if i + 1 < nslots:
                        thresh(i + 1)
                    attention(i)
                    if i + 2 < nslots:
                        loads_a(i + 2)
                        indexer(i + 2)
                    if i + 1 < nslots:
                        mask(i + 1)
                    finalize(i)
                    if i + 2 < nslots:
                        loads_b(i + 2)
                k.barrier()

        def phase_B2():
            with ExitStack() as s3:
                wao = k.sb("wao", [128, 4, D], BF16, s3)
                wo = k.sb("wo", [128, 8, D], BF16, s3)
                wr = k.sb("wr", [128, 8, 20], BF16, s3)
                br = k.sb("br", [128, 20], F32, s3)
                for kc in range(4):
                    k.dma(pool, wao[:, kc, :], wao_d[kc * 128:(kc + 1) * 128, :], writes=[wao])
                for kc in range(8):
                    k.dma(pool, wo[:, kc, :], wo_d[kc * 128:(kc + 1) * 128, :], writes=[wo])
                k.dma(pool, wr[:], wr_d.rearrange("(k p) c -> p k c", p=128), writes=[wr])
                k.dma(sp, br[:], br_d, writes=[br])
                if "C" in phases:
                    wload(0)
                    wload(1)
                mc2 = [k.sb("mcb%d" % i, [128, 8, 128], BF16, s3) for i in range(2)]
                sga2 = [k.sb("sgab%d" % i, [128, 8, 128], BF16, s3) for i in range(2)]
                xoL = [k.sb("xob%d" % i, [128, D], F32, s3) for i in range(2)]
                attnTL = [k.sb("attnT%d" % i, [128, 4, 128], BF16, s3) for i in range(2)]
                mTL = [k.sb("mT%d" % i, [128, 8, 128], BF16, s3) for i in range(2)]
                x1L = [k.sb("x1_%d" % i, [128, D], F32, s3) for i in range(2)]
                lgA = k.sb("lgA", [128, NS, 20], F32, s3)
                rA = k.sb("rA", [128, 8, NS], F32, s3)
                dG = k.sb("dG", [128, NS, 4], F32, s3)
                eG = k.sb("eG", [128, NS, 4], F32, s3)
                ohg1 = k.sb("ohg1", [128, NS, 4], F32, s3)
                ohgp = k.sb("ohgp", [128, NS, 4], F32, s3)
                t16A = k.sb("t16A", [128, NS, 16], F32, s3)
                eselA = k.sb("eselA", [128, NS, 4], F32, s3)
                oh1A = k.sb("oh1A", [128, NS, 4], F32, s3)
                oh2A = k.sb("oh2A", [128, NS, 4], F32, s3)
                emA = k.sb("emA", [128, NS, 4], F32, s3)
                x1d_b = Buf("x1d_b")

                def loads_c(j):
                    p = j % 2
                    k.dma(sp, mc2[p][:].rearrange("p a b -> p (a b)"), mc_d[j], writes=[mc2[p]])
                    k.dma(sp, sga2[p][:].rearrange("p a b -> p (a b)"), sga_d[j], writes=[sga2[p]])
                    k.dma(sp, xoL[p][:], x_own[j * 128:(j + 1) * 128, :], writes=[xoL[p]])

                def tail(j):
                    mcs, sgas = mc2[j % 2], sga2[j % 2]
                    xo, x1, mT, attnT = xoL[j % 2], x1L[j % 2], mTL[j % 2], attnTL[j % 2]
                    tb = k.bank()
                    tbv = bf(tb[:])
                    for q_ in range(4):
                        k.op(pe, lambda e: e.transpose(tbv[:, q_ * 128:(q_ + 1) * 128],
                                                       attn_all[:, j, q_ * 128:(q_ + 1) * 128], ident[:]),
                             reads=[attn_all, ident], writes=[tb])
                    k.op(act, lambda e: e.activation(attnT[:], tbv[:, 0:512].rearrange("p (a b) -> p a b", a=4), AF.Copy),
                         reads=[tb], writes=[attnT])
                    x1v = x1[:].rearrange("p (a b) -> p a b", a=8)
                    for hb_ in range(2):
                        py = k.bank()
                        pyv = py[:, 0:512].rearrange("p (a b) -> p a b", a=4)
                        for m in range(4):
                            mm = hb_ * 4 + m
                            for kc in range(4):
                                k.op(pe, lambda e: e.matmul(pyv[:, m, :], wao[:, kc, mm * 128:(mm + 1) * 128], attnT[:, kc, :],
                                                            start=(kc == 0), stop=(kc == 3)), reads=[wao, attnT], writes=[py])
                        k.op(dve, lambda e: e.tensor_tensor(x1v[:, hb_ * 4:(hb_ + 1) * 4, :], pyv,
                                                            sgas[:, hb_ * 4:(hb_ + 1) * 4, :], ALU.mult),
                             reads=[py, sgas], writes=[x1])
                    k.op(dve, lambda e: e.tensor_tensor(mT[:], x1v, mcs[:], ALU.add), reads=[x1, mcs], writes=[mT])
                    for half in range(2):
                        px = k.bank()
                        for kc in range(8):
                            k.op(pe, lambda e: e.matmul(px[:, 0:512], mT[:, kc, :], wo[:, kc, half * 512:(half + 1) * 512],
                                                        start=(kc == 0), stop=(kc == 7)), reads=[mT, wo], writes=[px])
                        k.op(dve, lambda e: e.tensor_tensor(acc[:, j, half * 512:(half + 1) * 512], px[:, 0:512],
                                                            xo[:, half * 512:(half + 1) * 512], ALU.add),
                             reads=[px, xo], writes=[acc])
                    if debug:
                        k.dma(sp, dbg["x1"][j], acc[:, j, :], reads=[acc], sembuf=x1)
                    hbsT[j] = norm_pre(acc, 128, acc[:, j, :])

                def tailB(j):
                    norm_post(hbsT.pop(j), 128, gffn, h2T, h2T[:, :, j * 128:(j + 1) * 128])
                    pr_ = k.bank()
                    for kc in range(8):
                        k.op(pe, lambda e: e.matmul(pr_[:, 0:20], h2T[:, kc, j * 128:(j + 1) * 128], wr[:, kc, :],
                                                    start=(kc == 0), stop=(kc == 7)), reads=[h2T, wr], writes=[pr_])
                    k.op(dve, lambda e: e.tensor_tensor(lgA[:, j, :], pr_[:, 0:20], br[:], ALU.add),
                         reads=[pr_, br], writes=[lgA])

                def router_all():
                    n = nslots
                    G = lgA[:, 0:n, 0:4]
                    Ex = lgA[:, 0:n, 4:20].rearrange("p n (g e) -> p n g e", g=4)
                    gmax, gsum, pg, m1, m2, w1, w2 = (rA[:, i, 0:n] for i in range(7))
                    bc4 = lambda ap: ap.unsqueeze(2).to_broadcast([128, n, 4])
                    k.op(dve, lambda e: e.tensor_reduce(gmax, G, AX.X, ALU.max), reads=[lgA], writes=[rA])
                    k.op(dve, lambda e: e.tensor_tensor(dG[:, 0:n, :], G, bc4(gmax), ALU.subtract), reads=[lgA, rA], writes=[dG])
                    k.op(act, lambda e: e.activation(eG[:, 0:n, :], dG[:, 0:n, :], AF.Exp), reads=[dG], writes=[eG])
                    k.op(dve, lambda e: e.tensor_reduce(gsum, eG[:, 0:n, :], AX.X, ALU.add), reads=[eG], writes=[rA])
                    k.op(dve, lambda e: e.reciprocal(pg, gsum), reads=[rA], writes=[rA])
                    k.op(dve, lambda e: e.tensor_scalar(ohg1[:, 0:n, :], dG[:, 0:n, :], 0.0, None, ALU.is_ge), reads=[dG], writes=[ohg1])
                    k.op(dve, lambda e: e.tensor_tensor(ohgp[:, 0:n, :], ohg1[:, 0:n, :], bc4(pg), ALU.mult),
                         reads=[ohg1, rA], writes=[ohgp])
                    t4 = t16A[:, 0:n, :].rearrange("p n (g e) -> p n g e", g=4)
                    k.op(dve, lambda e: e.tensor_tensor(t4, Ex, ohg1[:, 0:n, :].unsqueeze(3).to_broadcast([128, n, 4, 4]), ALU.mult),
                         reads=[lgA, ohg1], writes=[t16A])
                    k.op(dve, lambda e: e.tensor_reduce(eselA[:, 0:n, :], t16A[:, 0:n, :].rearrange("p n (g e) -> p n e g", g=4),
                                                        AX.X, ALU.add), reads=[t16A], writes=[eselA])
                    k.op(dve, lambda e: e.tensor_reduce(m1, eselA[:, 0:n, :], AX.X, ALU.max), reads=[eselA], writes=[rA])
                    k.op(dve, lambda e: e.tensor_tensor(dG[:, 0:n, :], eselA[:, 0:n, :], bc4(m1), ALU.subtract),
                         reads=[eselA, rA], writes=[dG])
                    k.op(dve, lambda e: e.tensor_scalar(oh1A[:, 0:n, :], dG[:, 0:n, :], 0.0, None, ALU.is_ge), reads=[dG], writes=[oh1A])
                    k.op(dve, lambda e: e.scalar_tensor_tensor(emA[:, 0:n, :], oh1A[:, 0:n, :], NEG, eselA[:, 0:n, :],
                                                               ALU.mult, ALU.add), reads=[oh1A, eselA], writes=[emA])
                    k.op(dve, lambda e: e.tensor_reduce(m2, emA[:, 0:n, :], AX.X, ALU.max), reads=[emA], writes=[rA])
                    k.op(dve, lambda e: e.tensor_tensor(dG[:, 0:n, :], emA[:, 0:n, :], bc4(m2), ALU.subtract),
                         reads=[emA, rA], writes=[dG])
                    k.op(dve, lambda e: e.tensor_scalar(oh2A[:, 0:n, :], dG[:, 0:n, :], 0.0, None, ALU.is_ge), reads=[dG], writes=[oh2A])
                    k.op(dve, lambda e: e.tensor_tensor(w2, m1, m2, ALU.subtract), reads=[rA], writes=[rA])
                    k.op(act, lambda e: e.activation(w1, w2, AF.Sigmoid), reads=[rA], writes=[rA])
                    k.op(dve, lambda e: e.tensor_scalar(w2, w1, -1.0, 1.0, ALU.mult, ALU.add), reads=[rA], writes=[rA])
                    k.op(dve, lambda e: e.tensor_tensor(oh1A[:, 0:n, :], oh1A[:, 0:n, :], bc4(w1), ALU.mult),
                         reads=[oh1A, rA], writes=[oh1A])
                    k.op(dve, lambda e: e.tensor_tensor(oh2A[:, 0:n, :], oh2A[:, 0:n, :], bc4(w2), ALU.mult),
                         reads=[oh2A, rA], writes=[oh2A])
                    k.op(dve, lambda e: e.tensor_tensor(oh1A[:, 0:n, :], oh1A[:, 0:n, :], oh2A[:, 0:n, :], ALU.add),
                         reads=[oh1A, oh2A], writes=[oh1A])
                    k.op(dve, lambda e: e.tensor_tensor(
                        gw_all[:, 0:n * 16].rearrange("p (n g e) -> p n g e", g=4, e=4),
                        ohgp[:, 0:n, :].unsqueeze(3).to_broadcast([128, n, 4, 4]),
                        oh1A[:, 0:n, :].unsqueeze(2).to_broadcast([128, n, 4, 4]), ALU.mult),
                         reads=[ohgp, oh1A], writes=[gw_all])

                hbsT = {}
                loads_c(0)
                if nslots > 1:
                    loads_c(1)
                tail(0)
                for j in range(nslots):
                    if j + 1 < nslots:
                        tail(j + 1)
                    tailB(j)
                    if j + 2 < nslots:
                        loads_c(j + 2)
                router_all()
                k.barrier()
        def phase_C():
            with ExitStack() as s4:
                gfin = k.sb("gfin", [128, D], F32, s4)
                k.dma(sp, gfin[:], gfin_d, writes=[gfin])
                sgl2 = [k.sb("sgm%d" % i, [128, 512], F32, s4) for i in range(2)]
                hid2 = [k.sb("hid%d" % i, [128, 2, 512], BF16, s4) for i in range(2)]
                ot2 = [k.sb("ot%d" % i, [128, D], F32, s4) for i in range(2)]

                ngrp = (nslots + 3) // 4
                wload(0)
                hi = 0
                for ex in range(16):
                    if ex + 1 < 16:
                        wload(ex + 1)
                    p = ex % 2
                    wg, wu, wd = wg2[p], wu2[p], wd2[p]
                    for tg in range(ngrp):
                        ns_ = min(4, nslots - tg * 4)
                        N = ns_ * 128
                        hid = hid2[hi % 2]
                        hi += 1
                        for fc in range(2):
                            pg = k.bank()
                            pu = k.bank()
                            for kc in range(8):
                                k.op(pe, lambda e, kc=kc, fc=fc, pg=pg: e.matmul(
                                    pg[:, 0:N], wg[:, kc, fc * 128:(fc + 1) * 128], h2T[:, kc, tg * 512:tg * 512 + N],
                                    start=(kc == 0), stop=(kc == 7)), reads=[wg, h2T], writes=[pg])
                            for kc in range(8):
                                k.op(pe, lambda e, kc=kc, fc=fc, pu=pu: e.matmul(
                                    pu[:, 0:N], wu[:, kc, fc * 128:(fc + 1) * 128], h2T[:, kc, tg * 512:tg * 512 + N],
                                    start=(kc == 0), stop=(kc == 7)), reads=[wu, h2T], writes=[pu])
                            sgl = sgl2[fc]
                            k.op(act, lambda e, pg=pg, sgl=sgl: e.activation(sgl[:, 0:N], pg[:, 0:N], AF.Silu),
                                 reads=[pg], writes=[sgl])
                            k.op(dve, lambda e, pu=pu, sgl=sgl, fc=fc, hid=hid: e.tensor_tensor(
                                hid[:, fc, 0:N], pu[:, 0:N], sgl[:, 0:N], ALU.mult), reads=[pu, sgl], writes=[hid])
                        for s_ in range(ns_):
                            j = tg * 4 + s_
                            for half in range(2):
                                py = k.bank()
                                for fc in range(2):
                                    k.op(pe, lambda e, fc=fc, half=half, py=py, s_=s_, hid=hid: e.matmul(
                                        py[:, 0:512], hid[:, fc, s_ * 128:(s_ + 1) * 128],
                                        wd[:, fc, half * 512:(half + 1) * 512], start=(fc == 0), stop=(fc == 1)),
                                         reads=[hid, wd], writes=[py])
                                k.op(dve, lambda e, py=py, j=j, half=half, ex=ex: e.scalar_tensor_tensor(
                                    acc[:, j, half * 512:(half + 1) * 512], py[:, 0:512],
                                    gw_all[:, j * 16 + ex:j * 16 + ex + 1], acc[:, j, half * 512:(half + 1) * 512],
                                    ALU.mult, ALU.add), reads=[py, gw_all, acc], writes=[acc])
                for j in range(nslots):
                    ot = ot2[j % 2]
                    ss, rs, junk = ssL[j % 2], rsL[j % 2], hbL[j % 2]
                    k.op(dve, lambda e, ss=ss: e.memset(ss[:], 0.0), writes=[ss])
                    k.op(act, lambda e, j=j, junk=junk, ss=ss: e.activation(junk[:], acc[:, j, :], AF.Square, accum_out=ss[:, 0:1]),
                         reads=[acc, ss], writes=[junk, ss])
                    k.op(act, lambda e, rs=rs, ss=ss: e.activation(rs[:], ss[:], AF.Sqrt, bias=EPS, scale=1.0 / D), reads=[ss], writes=[rs])
                    k.op(dve, lambda e, rs=rs: e.reciprocal(rs[:], rs[:]), reads=[rs], writes=[rs])
                    k.op(dve, lambda e, j=j, ot=ot, rs=rs: e.scalar_tensor_tensor(ot[:], acc[:, j, :], rs[:, 0:1], gfin[:],
                                                                          ALU.mult, ALU.mult),
                         reads=[acc, rs, gfin], writes=[ot])
                    k.dma(sp, out_d[j * 128:(j + 1) * 128, :], ot[:], reads=[ot], sembuf=ot)
                k.barrier()
        if "A2" in phases:
            phase_A2()
            if debug:
                dbg_A2()
        attn_all = k.sb("attn_all", [128, NS, 512], BF16)
        wg2 = [k.sb("wg%d" % i, [128, 8, 256], BF16) for i in range(2)]
        wu2 = [k.sb("wu%d" % i, [128, 8, 256], BF16) for i in range(2)]
        wd2 = [k.sb("wd%d" % i, [128, 2, D], BF16) for i in range(2)]
        wloaded = set()

        def wload(ex):
            if ex in wloaded:
                return
            wloaded.add(ex)
            p = ex % 2
            k.dma(pool, wg2[p][:], wg_d[ex].rearrange("(k p) c -> p k c", p=128), writes=[wg2[p]])
            k.dma(pool, wu2[p][:], wu_d[ex].rearrange("(k p) c -> p k c", p=128), writes=[wu2[p]])
            k.dma(pool, wd2[p][:], wd_d[ex].rearrange("(k p) c -> p k c", p=128), writes=[wd2[p]])

        with ExitStack() as skv:
            KT = k.sb("KT", [128, 4, S], BF16, skv)
            V = k.sb("V", [128, NB, 8 * 66], BF16, skv)
            kiT = k.sb("kiT", [128, S], BF16, skv)
            k.op(pool, lambda e: e.memset(V[:], 1.0), writes=[V])
            if "A1" in phases:
                phase_A1()
                if debug:
                    dbg_A1()
            if "B" in phases:
                phase_B()
            k.barrier()
        acc = k.sb("acc", [128, NS, D], F32)
        h2T = k.sb("h2T", [128, 8, NS * 128], BF16)
        if "B" in phases:
            phase_B2()
            if debug:
                k.dma(sp, dbg["gw"], gw_all[:], reads=[gw_all])
        if "C" in phases:
            phase_C()
        k.barrier()
        build_nc.nins = k.nins
    return nc


def own_blocks(r):
    return [2 * j + ((j % 2) ^ r) for j in range(NS)]


def prep_core(inp, c):
    b, r = c // 2, c % 2
    x = np.asarray(inp["x"], dtype=np.float32)
    pos = np.asarray(inp["positions"]).astype(np.int32)
    blocks = own_blocks(r)
    xb = x[b]
    x_own = np.concatenate([xb[i * 128:(i + 1) * 128] for i in blocks], axis=0)
    x_halo = np.zeros((NS * 32, D), np.float32)
    for j, i in enumerate(blocks):
        if i > 0:
            x_halo[j * 32:(j + 1) * 32] = xb[i * 128 - 32:i * 128]
    pos_full = np.ascontiguousarray(pos[b].reshape(NB, 128).T)
    pos_own = np.ascontiguousarray(np.stack([pos[b][i * 128:(i + 1) * 128] for i in blocks], axis=1))
    invf = (1.0 / (np.float32(10000.0) ** (np.arange(0, 64, 2, dtype=np.float32) / np.float32(64)))).astype(np.float32)
    invf = np.ascontiguousarray(np.broadcast_to(invf[None, :], (128, 32)))
    cmask = np.zeros((128, NS, 256), np.float32)
    for j, i in enumerate(blocks):
        qidx = i * 128 + np.arange(128)[:, None]
        kidx = 256 * j + np.arange(256)[None, :]
        cmask[:, j, :] = np.where(kidx > qidx, np.float32(NEG), np.float32(0.0))

    def fm(v, nchunk):
        return np.ascontiguousarray(np.asarray(v, np.float32).reshape(nchunk, 128).T)

    w_dw = np.asarray(inp["w_dw"], np.float32)[0, :, 0, :]
    wdw = np.ascontiguousarray(w_dw.T.reshape(4, 128, 31).transpose(1, 0, 2))
    w_r = np.concatenate([np.asarray(inp["w_rg"], np.float32)[0],
                          np.asarray(inp["w_re"], np.float32)[0].reshape(D, 16)], axis=1)
    b_r = np.concatenate([np.asarray(inp["b_rg"], np.float32)[0], np.asarray(inp["b_re"], np.float32)[0].reshape(16)])
    m = {
        "x_full": np.ascontiguousarray(xb), "x_own": x_own, "x_halo": x_halo,
        "pos_full": pos_full, "pos_own": pos_own, "invf": invf, "cmask": cmask,
        "ident": np.eye(128, dtype=np.float32),
        "w_in": np.ascontiguousarray(np.asarray(inp["w_in"], np.float32)[0]),
        "wdw": wdw, "bdw": fm(inp["b_dw"][0], 4), "lng": fm(inp["ln_g"][0], 4), "lnb": fm(inp["ln_b"][0], 4),
        "gmix": fm(inp["g_mix"][0], 8), "gffn": fm(inp["g_ffn"][0], 8),
        "gfin": np.ascontiguousarray(np.broadcast_to(np.asarray(inp["g_final"], np.float32)[None, :], (128, D))),
        "b_r": np.ascontiguousarray(np.broadcast_to(b_r[None, :], (128, 20))),
        "w_r": np.ascontiguousarray(w_r),
        "w_conv_out": np.ascontiguousarray(np.asarray(inp["w_conv_out"], np.float32)[0]),
        "w_attn_out": np.ascontiguousarray(np.asarray(inp["w_attn_out"], np.float32)[0]),
        "w_o": np.ascontiguousarray(np.asarray(inp["w_o"], np.float32)[0]),
        "w_gate": np.ascontiguousarray(np.asarray(inp["w_gate"], np.float32)[0]),
        "w_up": np.ascontiguousarray(np.asarray(inp["w_up"], np.float32)[0]),
        "w_down": np.ascontiguousarray(np.asarray(inp["w_down"], np.float32)[0]),
    }
    return m


_NC_CACHE = {}


def kernel(**inputs):
    if "nc" not in _NC_CACHE:
        _NC_CACHE["nc"] = build_nc()
    nc = _NC_CACHE["nc"]
    in_maps = [prep_core(inputs, c) for c in range(8)]
    res = run_bass_kernel_spmd(nc, in_maps, core_ids=list(range(8)))
    out = np.empty((4, S, D), np.float32)
    for c in range(8):
        b, r = c // 2, c % 2
        o = np.asarray(res.results[c]["out"], dtype=np.float32)
        for j, i in enumerate(own_blocks(r)):
            out[b, i * 128:(i + 1) * 128] = o[j * 128:(j + 1) * 128]
    return out
```

```python
from contextlib import ExitStack
import numpy as np
import concourse.bass as bass
import concourse.mybir as mybir
from concourse.bass_utils import run_bass_kernel_spmd

F32 = mybir.dt.float32
BF16 = mybir.dt.bfloat16
I32 = mybir.dt.int32
ALU = mybir.AluOpType
AF = mybir.ActivationFunctionType
AX = mybir.AxisListType

D = 1024
S = 4096
NB = 32
NS = 16
EPS = 1e-6
NIT = 14
TOPK = 256
NEG = -1.0e30
C_U, C_Q, C_K, C_V, C_QI, C_KI, C_WI, C_GC, C_GA = 0, 1024, 1536, 2048, 2560, 3072, 3136, 3144, 4168
IDX_SCALE = float((8 ** -0.5) * (64 ** -0.5))
ATT_SCALE = float(64 ** -0.5)


class Buf:
    __slots__ = ("name", "t", "lw", "rd", "dsem", "dcnt")

    def __init__(self, name, t=None):
        self.name = name
        self.t = t
        self.lw = None
        self.rd = {}
        self.dsem = None
        self.dcnt = 0

    def __getitem__(self, key):
        return self.t[key]


class Eng:
    def __init__(self, k, name, eng):
        self.name = name
        self.eng = eng
        self.sem = k.new_sem("e_" + name)
        self.cnt = 0
        self.seen = {}

    def wait_tok(self, tok):
        if tok is None:
            return
        sem, val, _ = tok
        key = id(sem)
        if self.seen.get(key, 0) >= val:
            return
        self.eng.wait_ge(sem, val)
        self.seen[key] = val


class K:
    def __init__(self, nc, stack):
        self.nc = nc
        self.stack = stack
        self.pe = Eng(self, "pe", nc.tensor)
        self.act = Eng(self, "act", nc.scalar)
        self.dve = Eng(self, "dve", nc.vector)
        self.pool = Eng(self, "pool", nc.gpsimd)
        self.sp = Eng(self, "sp", nc.sync)
        self.engs = [self.pe, self.act, self.dve, self.pool, self.sp]
        self.dma_bufs = []
        self.nins = 0
        self.banks = []
        self.bank_i = 0
        self.pinned = set()

    def new_sem(self, name):
        return self.stack.enter_context(self.nc.semaphore(name))

    def sb(self, name, shape, dt, stack=None):
        t = (stack or self.stack).enter_context(self.nc.sbuf_tensor("s_" + name, list(shape), dt))
        return Buf(name, t)

    def mk_banks(self):
        for i in range(8):
            t = self.stack.enter_context(self.nc.psum_tensor("bank%d" % i, [128, 512], F32))
            self.banks.append(Buf("bank%d" % i, t))

    def bank(self, pin=False):
        for _ in range(16):
            b = self.banks[self.bank_i % 8]
            self.bank_i += 1
            if b.name not in self.pinned:
                if pin:
                    self.pinned.add(b.name)
                return b
        raise RuntimeError("no psum bank")

    def unpin(self, b):
        self.pinned.discard(b.name)

    def _deps(self, e, reads, writes):
        for b in reads:
            if b.lw is not None:
                e.wait_tok(b.lw)
        for b in writes:
            if b.lw is not None and b.lw[2] != e.name:
                e.wait_tok(b.lw)
            for en, tok in b.rd.items():
                if en != e.name:
                    e.wait_tok(tok)

    def _commit(self, tok, reads, writes):
        for b in reads:
            b.rd[tok[2]] = tok
        for b in writes:
            b.lw = tok
            b.rd = {}

    def op(self, e, fn, reads=(), writes=()):
        self._deps(e, reads, writes)
        ins = fn(e.eng)
        e.cnt += 1
        ins.then_inc(e.sem, 1)
        tok = (e.sem, e.cnt, e.name)
        self._commit(tok, reads, writes)
        self.nins += 1
        return tok

    def dma(self, e, out, in_, reads=(), writes=(), sembuf=None):
        self._deps(e, reads, writes)
        sb_ = sembuf if sembuf is not None else (writes[0] if writes else reads[0])
        if sb_.dsem is None:
            sb_.dsem = self.new_sem("d_" + sb_.name)
            self.dma_bufs.append(sb_)
        ins = e.eng.dma_start(out=out, in_=in_)
        sb_.dcnt += 16
        ins.then_inc(sb_.dsem, 16)
        tok = (sb_.dsem, sb_.dcnt, "dma_" + sb_.name)
        self._commit(tok, reads, writes)
        self.nins += 1
        return tok

    def barrier(self):
        toks = [(e.sem, e.cnt, e.name) for e in self.engs if e.cnt > 0]
        toks += [(b.dsem, b.dcnt, "dma") for b in self.dma_bufs]
        for e in self.engs:
            for t in toks:
                if t[2] != e.name:
                    e.wait_tok(t)


def bf(ap):
    return ap.bitcast(BF16)


def build_nc(phases=("A1", "A2", "B", "C"), debug=False, nslots=NS, bstop=9):
    nc = bass.Bass("TRN2", target_bir_lowering=False)

    def din(name, shape, dt=F32):
        return nc.dram_tensor(name, list(shape), dt, kind="ExternalInput").ap()

    def dscr(name, shape, dt):
        return nc.dram_tensor(name, list(shape), dt, kind="Internal").ap()

    x_full = din("x_full", [S, D])
    x_own = din("x_own", [NS * 128, D])
    x_halo = din("x_halo", [NS * 32, D])
    pos_full = din("pos_full", [128, NB], I32)
    pos_own = din("pos_own", [128, NS], I32)
    invf = din("invf", [128, 32])
    cmask_d = din("cmask", [128, NS, 256])
    ident_d = din("ident", [128, 128])
    w_in = din("w_in", [D, 5192])
    wdw_d = din("wdw", [128, 4, 31])
    bdw_d = din("bdw", [128, 4])
    lng_d = din("lng", [128, 4])
    lnb_d = din("lnb", [128, 4])
    gmix_d = din("gmix", [128, 8])
    gffn_d = din("gffn", [128, 8])
    gfin_d = din("gfin", [128, D])
    br_d = din("b_r", [128, 20])
    wr_d = din("w_r", [D, 20])
    wco_d = din("w_conv_out", [512, D])
    wao_d = din("w_attn_out", [512, D])
    wo_d = din("w_o", [D, D])
    wg_d = din("w_gate", [16, D, 256])
    wu_d = din("w_up", [16, D, 256])
    wd_d = din("w_down", [16, 256, D])
    out_d = nc.dram_tensor("out", [NS * 128, D], F32, kind="ExternalOutput").ap()

    qT_d = dscr("qT_d", [NS, 128, 512], BF16)
    qiT_d = dscr("qiT_d", [NS, 128, 512], BF16)
    mc_d = dscr("mc_d", [NS, 128, 1024], BF16)
    sga_d = dscr("sga_d", [NS, 128, 1024], BF16)
    x1_d = dscr("x1_d", [NS, 128, D], F32)
    h2T_d = dscr("h2T_d", [NS, 128, 1024], BF16)
    dbg = {}
    if debug:
        dbg["kT"] = nc.dram_tensor("dbg_kT", [128, 4, S], BF16, kind="ExternalOutput").ap()
        dbg["v"] = nc.dram_tensor("dbg_v", [128, NB, 8 * 66], BF16, kind="ExternalOutput").ap()
        dbg["kiT"] = nc.dram_tensor("dbg_kiT", [128, S], BF16, kind="ExternalOutput").ap()
        dbg["qT"] = nc.dram_tensor("dbg_qT", [NS, 128, 512], BF16, kind="ExternalOutput").ap()
        dbg["qiT"] = nc.dram_tensor("dbg_qiT", [NS, 128, 512], BF16, kind="ExternalOutput").ap()
        dbg["mc"] = nc.dram_tensor("dbg_mc", [NS, 128, 1024], BF16, kind="ExternalOutput").ap()
        dbg["sga"] = nc.dram_tensor("dbg_sga", [NS, 128, 1024], BF16, kind="ExternalOutput").ap()
        dbg["wi"] = nc.dram_tensor("dbg_wi", [128, NS * 8], F32, kind="ExternalOutput").ap()
        dbg["x1"] = nc.dram_tensor("dbg_x1", [NS, 128, D], F32, kind="ExternalOutput").ap()
        dbg["gw"] = nc.dram_tensor("dbg_gw", [128, NS * 16], F32, kind="ExternalOutput").ap()
        dbg["sc"] = nc.dram_tensor("dbg_sc", [NS, 128, S], F32, kind="ExternalOutput").ap()
        dbg["thr"] = nc.dram_tensor("dbg_thr", [NS, 128, 1], F32, kind="ExternalOutput").ap()
        dbg["attn"] = nc.dram_tensor("dbg_attn", [NS, 128, 512], F32, kind="ExternalOutput").ap()

    with ExitStack() as st:
        k = K(nc, st)
        pe, act, dve, pool, sp = k.pe, k.act, k.dve, k.pool, k.sp
        k.mk_banks()

        ident = k.sb("ident", [128, 128], BF16)
        identf = k.sb("identf", [128, 128], F32)
        wi_all = k.sb("wi_all", [128, NS * 8], F32)
        gw_all = k.sb("gw_all", [128, NS * 16], F32)
        gmix = k.sb("gmix", [128, 8], F32)
        gffn = k.sb("gffn", [128, 8], F32)
        ssL = [k.sb("ss%d" % i, [128, 1], F32) for i in range(2)]
        rsL = [k.sb("rs%d" % i, [128, 1], F32) for i in range(2)]
        hbL = [k.sb("hb%d" % i, [128, D], BF16) for i in range(2)]
        nrm_i = [0]

        k.dma(sp, identf[:], ident_d, writes=[identf])
        k.dma(pool, ident[:], ident_d, writes=[ident])
        k.dma(sp, gmix[:], gmix_d, writes=[gmix])
        k.dma(sp, gffn[:], gffn_d, writes=[gffn])

        def rope_tables(pos_d, n, cosT, sinT, stk):
            pi_ = k.sb("pos_i%d" % n, [128, n], I32, stk)
            pf = k.sb("pos_f%d" % n, [128, n], F32, stk)
            iv = k.sb("invf%d" % n, [128, 32], F32, stk)
            ang = k.sb("ang%d" % n, [128, n, 32], F32, stk)
            tmp = k.sb("angt%d" % n, [128, n, 32], F32, stk)
            k.dma(sp, pi_[:], pos_d, writes=[pi_])
            k.dma(sp, iv[:], invf, writes=[iv])
            k.op(dve, lambda e: e.tensor_copy(pf[:], pi_[:]), reads=[pi_], writes=[pf])
            k.op(dve, lambda e: e.tensor_tensor(ang[:], pf[:].unsqueeze(2).to_broadcast([128, n, 32]),
                                                iv[:].unsqueeze(1).to_broadcast([128, n, 32]), ALU.mult),
                 reads=[pf, iv], writes=[ang])
            ki_ = k.sb("angk%d" % n, [128, n, 32], I32, stk)
            kf_ = k.sb("angf%d" % n, [128, n, 32], F32, stk)
            two_pi = float(2 * np.pi)
            C1 = 6.28125
            C2 = float(2 * np.pi - 6.28125)
            PI_SAFE = 3.1415925

            def sin_of(dst, shift):
                if shift != 0.0:
                    k.op(dve, lambda e: e.tensor_scalar(tmp[:], ang[:], shift, None, ALU.add), reads=[ang], writes=[tmp])
                    src = tmp
                else:
                    src = ang
                k.op(dve, lambda e: e.tensor_scalar(kf_[:], src[:], 1.0 / two_pi, None, ALU.mult),
                     reads=[src], writes=[kf_])
                k.op(dve, lambda e: e.tensor_copy(ki_[:], kf_[:]), reads=[kf_], writes=[ki_])
                k.op(dve, lambda e: e.tensor_copy(kf_[:], ki_[:]), reads=[ki_], writes=[kf_])
                k.op(dve, lambda e: e.scalar_tensor_tensor(tmp[:], kf_[:], -C1, src[:], ALU.mult, ALU.add),
                     reads=[kf_, src], writes=[tmp])
                k.op(dve, lambda e: e.scalar_tensor_tensor(tmp[:], kf_[:], -C2, tmp[:], ALU.mult, ALU.add),
                     reads=[kf_, tmp], writes=[tmp])
                k.op(dve, lambda e: e.tensor_scalar(kf_[:], tmp[:], float(np.pi), -two_pi, ALU.is_gt, ALU.mult),
                     reads=[tmp], writes=[kf_])
                k.op(dve, lambda e: e.tensor_tensor(tmp[:], tmp[:], kf_[:], ALU.add), reads=[tmp, kf_], writes=[tmp])
                k.op(dve, lambda e: e.tensor_scalar(kf_[:], tmp[:], -float(np.pi), two_pi, ALU.is_lt, ALU.mult),
                     reads=[tmp], writes=[kf_])
                k.op(dve, lambda e: e.tensor_tensor(tmp[:], tmp[:], kf_[:], ALU.add), reads=[tmp, kf_], writes=[tmp])
                k.op(dve, lambda e: e.tensor_scalar(tmp[:], tmp[:], -PI_SAFE, PI_SAFE, ALU.max, ALU.min),
                     reads=[tmp], writes=[tmp])
                k.op(act, lambda e: e.activation(dst[:], tmp[:], AF.Sin), reads=[tmp], writes=[dst])

            sin_of(sinT, 0.0)
            sin_of(cosT, float(0.5 * np.pi))

        hbN = 4
        hbX = [k.sb("hbx%d" % i, [128, D], BF16) for i in range(hbN - 2)]

        def norm_pre(xt, n, src=None):
            if src is None:
                src = xt[0:n, :]
            hbs = hbL + hbX
            ss, rs, hb = ssL[nrm_i[0] % 2], rsL[nrm_i[0] % 2], hbs[nrm_i[0] % hbN]
            junk = hb
            nrm_i[0] += 1
            k.op(dve, lambda e: e.memset(ss[0:n, :], 0.0), writes=[ss])
            k.op(act, lambda e: e.activation(junk[0:n, :], src, AF.Square, accum_out=ss[0:n, 0:1]),
                 reads=[xt, ss], writes=[junk, ss])
            k.op(act, lambda e: e.activation(rs[0:n, :], ss[0:n, :], AF.Sqrt, bias=EPS, scale=1.0 / D),
                 reads=[ss], writes=[rs])
            k.op(dve, lambda e: e.reciprocal(rs[0:n, :], rs[0:n, :]), reads=[rs], writes=[rs])
            k.op(act, lambda e: e.activation(hb[0:n, :], src, AF.Copy, scale=rs[0:n, 0:1]),
                 reads=[xt, rs], writes=[hb])
            return hb

        def norm_post(hb, n, gT, hT, hT_ap=None):
            if hT_ap is None:
                hT_ap = hT[:, :, 0:n]
            tp = k.bank()
            tpv = bf(tp[:])[:, 0:8 * 128].rearrange("p (a b) -> p a b", a=8)
            for kc in range(8):
                k.op(pe, lambda e, kc=kc: e.transpose(tpv[:, kc, 0:n], hb[0:n, kc * 128:(kc + 1) * 128],
                                                      ident[0:n, 0:n]),
                     reads=[hb, ident], writes=[tp])
            k.op(dve, lambda e: e.tensor_tensor(hT_ap, tpv[:, :, 0:n],
                                                gT[:].unsqueeze(2).to_broadcast([128, 8, n]), ALU.mult),
                 reads=[tp, gT], writes=[hT])

        def norm_T(xt, n, gT, hT, hT_ap=None):
            hb = norm_pre(xt, n)
            norm_post(hb, n, gT, hT, hT_ap)

        def proj_tok(hT, w, c0, ncols):
            pb = k.bank()
            for kc in range(8):
                k.op(pe, lambda e, kc=kc: e.matmul(pb[:, 0:ncols], hT[:, kc, :], w[:, kc, c0:c0 + ncols],
                                                   start=(kc == 0), stop=(kc == 7)),
                     reads=[hT, w], writes=[pb])
            return pb

        def rope(pb, nh, cosT, sinT, ti, outb, tmpA, tmpB):
            pv = pb[:, 0:nh * 64].rearrange("p (h d) -> p h d", h=nh)
            ov = outb[:, 0:nh * 64].rearrange("p (h d) -> p h d", h=nh)
            av = tmpA[:, 0:nh * 64].rearrange("p (h d) -> p h d", h=nh)
            bv = tmpB[:, 0:nh * 64].rearrange("p (h d) -> p h d", h=nh)
            cb = cosT[:, ti, :].unsqueeze(1).to_broadcast([128, nh, 32])
            sb_ = sinT[:, ti, :].unsqueeze(1).to_broadcast([128, nh, 32])
            k.op(dve, lambda e: e.tensor_tensor(av[:, :, 0:32], pv[:, :, 0:32], cb, ALU.mult),
                 reads=[pb, cosT], writes=[tmpA])
            k.op(dve, lambda e: e.tensor_tensor(av[:, :, 32:64], pv[:, :, 32:64], cb, ALU.mult),
                 reads=[pb, cosT], writes=[tmpA])
            k.op(dve, lambda e: e.tensor_tensor(bv[:, :, 0:32], pv[:, :, 32:64], sb_, ALU.mult),
                 reads=[pb, sinT], writes=[tmpB])
            k.op(dve, lambda e: e.tensor_tensor(bv[:, :, 32:64], pv[:, :, 0:32], sb_, ALU.mult),
                 reads=[pb, sinT], writes=[tmpB])
            k.op(dve, lambda e: e.tensor_tensor(ov[:, :, 0:32], av[:, :, 0:32], bv[:, :, 0:32], ALU.subtract),
                 reads=[tmpA, tmpB], writes=[outb])
            k.op(dve, lambda e: e.tensor_tensor(ov[:, :, 32:64], av[:, :, 32:64], bv[:, :, 32:64], ALU.add),
                 reads=[tmpA, tmpB], writes=[outb])

        w_in_v = w_in.rearrange("(k p) c -> p k c", p=128)
        def phase_A1():
            with ExitStack() as s1:
                cosF = k.sb("cosF", [128, NB, 32], F32, s1)
                sinF = k.sb("sinF", [128, NB, 32], F32, s1)
                rope_tables(pos_full, NB, cosF, sinF, s1)
                wA = k.sb("wA1", [128, 8, 1088], BF16, s1)
                for kc in range(8):
                    k.dma(pool, wA[:, kc, 0:1024], w_in_v[:, kc, C_K:C_K + 1024], writes=[wA])
                    k.dma(pool, wA[:, kc, 1024:1088], w_in_v[:, kc, C_KI:C_KI + 64], writes=[wA])
                xt2 = [k.sb("xtA%d" % i, [128, D], F32, s1) for i in range(2)]
                hT2 = [k.sb("hTA%d" % i, [128, 8, 128], BF16, s1) for i in range(2)]
                krL = [k.sb("kr%d" % i, [128, 512], BF16, s1) for i in range(2)]
                kirL = [k.sb("kir%d" % i, [128, 128], BF16, s1) for i in range(2)]
                tAL = [k.sb("tA%d" % i, [128, 512], F32, s1) for i in range(2)]
                tBL = [k.sb("tB%d" % i, [128, 512], F32, s1) for i in range(2)]
                tCL = [k.sb("tC%d" % i, [128, 64], F32, s1) for i in range(2)]
                tDL = [k.sb("tD%d" % i, [128, 64], F32, s1) for i in range(2)]
                hbs1 = {}

                def pre1(ti):
                    k.dma(sp, xt2[ti % 2][:], x_full[ti * 128:(ti + 1) * 128, :], writes=[xt2[ti % 2]])
                    hbs1[ti] = norm_pre(xt2[ti % 2], 128)

                def post1(ti):
                    norm_post(hbs1.pop(ti), 128, gmix, hT2[ti % 2])

                pre1(0)
                post1(0)
                pre1(1)
                for ti in range(NB):
                    if ti + 1 < NB:
                        post1(ti + 1)
                    hT = hT2[ti % 2]
                    kr, kir, tA, tB = krL[ti % 2], kirL[ti % 2], tAL[ti % 2], tBL[ti % 2]
                    tC, tD = tCL[ti % 2], tDL[ti % 2]
                    pk = proj_tok(hT, wA, 0, 512)
                    pv = proj_tok(hT, wA, 512, 512)
                    pki = proj_tok(hT, wA, 1024, 64)
                    if ti + 2 < NB:
                        pre1(ti + 2)
                    rope(pk, 8, cosF, sinF, ti, kr, tA, tB)
                    Vv = V[:, ti, :].rearrange("p (h d) -> p h d", h=8)
                    k.op(act, lambda e: e.activation(Vv[:, :, 0:64],
                                                     pv[:, 0:512].rearrange("p (h d) -> p h d", h=8), AF.Copy),
                         reads=[pv], writes=[V])
                    rope(pki, 1, cosF, sinF, ti, kir, tC, tD)
                    k.op(dve, lambda e, kir=kir: e.tensor_copy(kir[:, 64:128], kir[:, 0:64]), reads=[kir], writes=[kir])
                    tb = k.bank()
                    tbv = bf(tb[:])
                    for pr in range(4):
                        k.op(pe, lambda e, pr=pr: e.transpose(tbv[:, pr * 128:(pr + 1) * 128],
                                                              kr[:, pr * 128:(pr + 1) * 128], ident[:]),
                             reads=[kr, ident], writes=[tb])
                    k.op(pe, lambda e: e.transpose(tbv[:, 512:640], kir[:], ident[:]), reads=[kir, ident], writes=[tb])
                    k.op(act, lambda e: e.activation(KT[:, :, ti * 128:(ti + 1) * 128],
                                                     tbv[:, 0:512].rearrange("p (a b) -> p a b", a=4), AF.Copy),
                         reads=[tb], writes=[KT])
                    k.op(act, lambda e: e.activation(kiT[:, ti * 128:(ti + 1) * 128], tbv[:, 512:640], AF.Copy),
                         reads=[tb], writes=[kiT])
                k.barrier()
        def dbg_A1():
            for pr in range(4):
                k.dma(sp, dbg["kT"][:, pr, :], KT[:, pr, :], reads=[KT])
            for t8 in range(0, NB, 4):
                k.dma(sp, dbg["v"][:, t8:t8 + 4, :], V[:, t8:t8 + 4, :], reads=[V])
            k.dma(sp, dbg["kiT"], kiT[:], reads=[kiT])

        def phase_A2():
            with ExitStack() as s2:
                cosO = k.sb("cosO", [128, NS, 32], F32, s2)
                sinO = k.sb("sinO", [128, NS, 32], F32, s2)
                rope_tables(pos_own, NS, cosO, sinO, s2)
                WU, WQ, WQI, WWI, WGC, WGA = 0, 1024, 1536, 2048, 2056, 3080
                wB = k.sb("wB", [128, 8, 4104], BF16, s2)
                wBq, wBu, wBg = Buf("wBq", wB.t), Buf("wBu", wB.t), Buf("wBg", wB.t)
                for kc in range(8):
                    k.dma(pool, wB[:, kc, WQ:WQ + 512], w_in_v[:, kc, C_Q:C_Q + 512], writes=[wBq])
                    k.dma(pool, wB[:, kc, WQI:WQI + 512], w_in_v[:, kc, C_QI:C_QI + 512], writes=[wBq])
                    k.dma(pool, wB[:, kc, WWI:WWI + 8], w_in_v[:, kc, C_WI:C_WI + 8], writes=[wBq])
                for kc in range(8):
                    k.dma(pool, wB[:, kc, WU:WU + 1024], w_in_v[:, kc, C_U:C_U + 1024], writes=[wBu])
                for kc in range(8):
                    k.dma(pool, wB[:, kc, WGC:WGC + 2048], w_in_v[:, kc, C_GC:C_GC + 2048], writes=[wBg])
                wco = k.sb("wco", [128, 4, D], BF16, s2)
                for kc in range(4):
                    k.dma(pool, wco[:, kc, :], wco_d[kc * 128:(kc + 1) * 128, :], writes=[wco])
                wdw = k.sb("wdw", [128, 4, 31], F32, s2)
                bdw = k.sb("bdw", [128, 4], F32, s2)
                lng = k.sb("lng", [128, 4], F32, s2)
                lnb = k.sb("lnb", [128, 4], F32, s2)
                k.dma(sp, wdw[:], wdw_d, writes=[wdw])
                k.dma(sp, bdw[:], bdw_d, writes=[bdw])
                k.dma(sp, lng[:], lng_d, writes=[lng])
                k.dma(sp, lnb[:], lnb_d, writes=[lnb])
                Dg = k.sb("Dg", [128, 124, 128], BF16, s2)
                for c in range(4):
                    for j in range(31):
                        k.op(dve, lambda e, c=c, j=j: e.tensor_scalar(Dg[:, c * 31 + j, :], identf[:],
                                                                       wdw[:, c, j:j + 1], None, ALU.mult),
                             reads=[identf, wdw], writes=[Dg])
                ones = k.sb("ones", [128, 128], F32, s2)
                k.op(pool, lambda e: e.memset(ones[:], 1.0), writes=[ones])
                xt2 = [k.sb("xtB%d" % i, [128, D], F32, s2) for i in range(2)]
                xh2 = [k.sb("xhB%d" % i, [32, D], F32, s2) for i in range(2)]
                hT_L = [k.sb("hTB_%d" % i_, [128, 8, 128], BF16, s2) for i_ in range(2)]
                hTh_L = [k.sb("hThB_%d" % i_, [128, 8, 32], BF16, s2) for i_ in range(2)]
                qr_L = [k.sb("qr_%d" % i_, [128, 512], BF16, s2) for i_ in range(2)]
                qir_L = [k.sb("qir_%d" % i_, [128, 512], BF16, s2) for i_ in range(2)]
                tA_L = [k.sb("tA2_%d" % i_, [128, 512], F32, s2) for i_ in range(2)]
                tB_L = [k.sb("tB2_%d" % i_, [128, 512], F32, s2) for i_ in range(2)]
                qT_s_L = [k.sb("qT_s_%d" % i_, [128, 512], BF16, s2) for i_ in range(2)]
                qiT_s_L = [k.sb("qiT_s_%d" % i_, [128, 512], BF16, s2) for i_ in range(2)]
                sg_L = [k.sb("sgl_%d" % i_, [128, 160], F32, s2) for i_ in range(2)]
                gT_L = [k.sb("gT_%d" % i_, [128, 4, 160], BF16, s2) for i_ in range(2)]
                csb_L = [k.sb("csb_%d" % i_, [128, 4, 128], F32, s2) for i_ in range(2)]
                csq_L = [k.sb("csq_%d" % i_, [128, 4, 128], F32, s2) for i_ in range(2)]
                mean_L = [k.sb("mean_%d" % i_, [128, 128], F32, s2) for i_ in range(2)]
                msq_L = [k.sb("msq_%d" % i_, [128, 128], F32, s2) for i_ in range(2)]
                rstd_L = [k.sb("rstd_%d" % i_, [128, 128], F32, s2) for i_ in range(2)]
                nrm_L = [k.sb("nrm_%d" % i_, [128, 4, 128], F32, s2) for i_ in range(2)]
                snT_L = [k.sb("snT_%d" % i_, [128, 4, 128], BF16, s2) for i_ in range(2)]
                sgc_L = [k.sb("sgc_%d" % i_, [128, 8, 128], F32, s2) for i_ in range(2)]
                mc_s_L = [k.sb("mc_s_%d" % i_, [128, 8, 128], BF16, s2) for i_ in range(2)]
                sga_s_L = [k.sb("sga_s_%d" % i_, [128, 8, 128], BF16, s2) for i_ in range(2)]
                qTd_b = Buf("qTd_b")
                mcd_b = Buf("mcd_b")

                hbs2 = {}

                def pre2(j):
                    k.dma(sp, xt2[j % 2][:], x_own[j * 128:(j + 1) * 128, :], writes=[xt2[j % 2]])
                    k.dma(sp, xh2[j % 2][:], x_halo[j * 32:(j + 1) * 32, :], writes=[xh2[j % 2]])
                    hbs2[j] = (norm_pre(xh2[j % 2], 32), norm_pre(xt2[j % 2], 128))

                def post2(j):
                    a_, b_ = hbs2.pop(j)
                    norm_post(a_, 32, gmix, hTh_L[j % 2])
                    norm_post(b_, 128, gmix, hT_L[j % 2])

                for j in range(nslots):
                    xt = xt2[j % 2]
                    xh = xh2[j % 2]
                    hT, hTh, qr, qir, tA, tB, qT_s, qiT_s, sg, gT, csb, csq, mean, msq, rstd, nrm, snT, sgc, mc_s, sga_s = hT_L[j % 2], hTh_L[j % 2], qr_L[j % 2], qir_L[j % 2], tA_L[j % 2], tB_L[j % 2], qT_s_L[j % 2], qiT_s_L[j % 2], sg_L[j % 2], gT_L[j % 2], csb_L[j % 2], csq_L[j % 2], mean_L[j % 2], msq_L[j % 2], rstd_L[j % 2], nrm_L[j % 2], snT_L[j % 2], sgc_L[j % 2], mc_s_L[j % 2], sga_s_L[j % 2]
                    if j == 0:
                        pre2(0)
                        post2(0)
                        if nslots > 1:
                            pre2(1)
                    if j + 1 < nslots:
                        post2(j + 1)
                    pq = proj_tok(hT, wBq, WQ, 512)
                    rope(pq, 8, cosO, sinO, j, qr, tA, tB)
                    pqi = proj_tok(hT, wBq, WQI, 512)
                    rope(pqi, 8, cosO, sinO, j, qir, tA, tB)
                    pw = proj_tok(hT, wBq, WWI, 8)
                    k.op(act, lambda e: e.activation(wi_all[:, j * 8:(j + 1) * 8], pw[:, 0:8], AF.Copy,
                                                     scale=IDX_SCALE), reads=[pw], writes=[wi_all])
                    for src, dstb, dd in ((qr, qT_s, qT_d), (qir, qiT_s, qiT_d)):
                        tb = k.bank()
                        tbv = bf(tb[:])
                        for pr in range(4):
                            k.op(pe, lambda e, pr=pr, src=src, tbv=tbv: e.transpose(
                                tbv[:, pr * 128:(pr + 1) * 128], src[:, pr * 128:(pr + 1) * 128], ident[:]),
                                 reads=[src, ident], writes=[tb])
                        k.op(act, lambda e, dstb=dstb, tbv=tbv: e.activation(dstb[:], tbv[:, 0:512], AF.Copy),
                             reads=[tb], writes=[dstb])
                        k.dma(sp, dd[j], dstb[:], reads=[dstb], writes=[qTd_b], sembuf=dstb)
                    for c in range(4):
                        pu = k.bank()
                        puv = pu[:, 0:320].rearrange("p (a b) -> p a b", a=2)
                        for half, col0 in ((0, WU + c * 128), (1, WU + 512 + c * 128)):
                            for kc in range(8):
                                k.op(pe, lambda e, kc=kc, half=half, col0=col0, puv=puv: e.matmul(
                                    puv[:, half, 0:32], wB[:, kc, col0:col0 + 128], hTh[:, kc, :],
                                    start=(kc == 0), stop=(kc == 7)), reads=[wBu, hTh], writes=[pu])
                            for kc in range(8):
                                k.op(pe, lambda e, kc=kc, half=half, col0=col0, puv=puv: e.matmul(
                                    puv[:, half, 32:160], wB[:, kc, col0:col0 + 128], hT[:, kc, :],
                                    start=(kc == 0), stop=(kc == 7)), reads=[wBu, hT], writes=[pu])
                        k.op(act, lambda e, puv=puv: e.activation(sg[:], puv[:, 1, :], AF.Sigmoid),
                             reads=[pu], writes=[sg])
                        k.op(dve, lambda e, puv=puv, c=c: e.tensor_tensor(gT[:, c, :], puv[:, 0, :], sg[:], ALU.mult),
                             reads=[pu, sg], writes=[gT])
                    if j + 2 < nslots:
                        pre2(j + 2)
                    pc = k.bank()
                    pcv = pc[:, 0:512].rearrange("p (a b) -> p a b", a=4)
                    for c in range(4):
                        for jj in range(31):
                            k.op(pe, lambda e, c=c, jj=jj: e.matmul(pcv[:, c, :], Dg[:, c * 31 + jj, :],
                                                                    gT[:, c, 2 + jj:2 + jj + 128],
                                                                    start=(jj == 0), stop=(jj == 30)),
                                 reads=[Dg, gT], writes=[pc])
                    k.op(dve, lambda e: e.tensor_tensor(csb[:], pcv, bdw[:].unsqueeze(2).to_broadcast([128, 4, 128]),
                                                        ALU.add), reads=[pc, bdw], writes=[csb])
                    k.op(act, lambda e: e.activation(csq[:], csb[:], AF.Square), reads=[csb], writes=[csq])
                    pst = k.bank()
                    for c in range(4):
                        k.op(pe, lambda e, c=c: e.matmul(pst[:, 0:128], ones[:], csb[:, c, :], start=(c == 0),
                                                         stop=(c == 3)), reads=[ones, csb], writes=[pst])
                    for c in range(4):
                        k.op(pe, lambda e, c=c: e.matmul(pst[:, 128:256], ones[:], csq[:, c, :], start=(c == 0),
                                                         stop=(c == 3)), reads=[ones, csq], writes=[pst])
                    k.op(act, lambda e: e.activation(mean[:], pst[:, 0:128], AF.Copy, scale=1.0 / 512),
                         reads=[pst], writes=[mean])
                    k.op(dve, lambda e: e.tensor_tensor(msq[:], mean[:], mean[:], ALU.mult), reads=[mean], writes=[msq])
                    k.op(dve, lambda e: e.scalar_tensor_tensor(rstd[:], pst[:, 128:256], 1.0 / 512, msq[:],
                                                               ALU.mult, ALU.subtract),
                         reads=[pst, msq], writes=[rstd])
                    k.op(act, lambda e: e.activation(rstd[:], rstd[:], AF.Sqrt, bias=EPS, scale=1.0),
                         reads=[rstd], writes=[rstd])
                    k.op(dve, lambda e: e.reciprocal(rstd[:], rstd[:]), reads=[rstd], writes=[rstd])
                    k.op(dve, lambda e: e.tensor_tensor(nrm[:], csb[:], mean[:].unsqueeze(1).to_broadcast([128, 4, 128]),
                                                        ALU.subtract), reads=[csb, mean], writes=[nrm])
                    k.op(dve, lambda e: e.tensor_tensor(nrm[:], nrm[:], rstd[:].unsqueeze(1).to_broadcast([128, 4, 128]),
                                                        ALU.mult), reads=[nrm, rstd], writes=[nrm])
                    for c in range(4):
                        k.op(act, lambda e, c=c: e.activation(snT[:, c, :], nrm[:, c, :], AF.Silu,
                                                              bias=lnb[:, c:c + 1], scale=lng[:, c:c + 1]),
                             reads=[nrm, lnb, lng], writes=[snT])
                    for gi, (wcol, dst) in enumerate(((WGC, None), (WGA, sga_s))):
                        for hb_ in range(2):
                            pg = k.bank()
                            pgv = pg[:, 0:512].rearrange("p (a b) -> p a b", a=4)
                            for m in range(4):
                                col0 = wcol + (hb_ * 4 + m) * 128
                                for kc in range(8):
                                    k.op(pe, lambda e, kc=kc, m=m, col0=col0, pgv=pgv: e.matmul(
                                        pgv[:, m, :], wB[:, kc, col0:col0 + 128], hT[:, kc, :],
                                        start=(kc == 0), stop=(kc == 7)), reads=[wBg, hT], writes=[pg])
                            tgt = sgc if gi == 0 else sga_s
                            k.op(act, lambda e, pgv=pgv, tgt=tgt, hb_=hb_: e.activation(
                                tgt[:, hb_ * 4:(hb_ + 1) * 4, :], pgv, AF.Sigmoid), reads=[pg], writes=[tgt])
                    for hb_ in range(2):
                        py = k.bank()
                        pyv = py[:, 0:512].rearrange("p (a b) -> p a b", a=4)
                        for m in range(4):
                            mm = hb_ * 4 + m
                            for kc in range(4):
                                k.op(pe, lambda e, kc=kc, m=m, mm=mm, pyv=pyv: e.matmul(
                                    pyv[:, m, :], wco[:, kc, mm * 128:(mm + 1) * 128], snT[:, kc, :],
                                    start=(kc == 0), stop=(kc == 3)), reads=[wco, snT], writes=[py])
                        k.op(dve, lambda e, pyv=pyv, hb_=hb_: e.tensor_tensor(
                            mc_s[:, hb_ * 4:(hb_ + 1) * 4, :], pyv, sgc[:, hb_ * 4:(hb_ + 1) * 4, :], ALU.mult),
                             reads=[py, sgc], writes=[mc_s])
                    k.dma(sp, mc_d[j], mc_s[:].rearrange("p a b -> p (a b)"), reads=[mc_s], writes=[mcd_b], sembuf=mc_s)
                    k.dma(sp, sga_d[j], sga_s[:].rearrange("p a b -> p (a b)"), reads=[sga_s], writes=[mcd_b],
                          sembuf=sga_s)
                k.barrier()
        def dbg_A2():
            k.dma(sp, dbg["wi"], wi_all[:], reads=[wi_all])
            for nm, src in (("qT", qT_d), ("qiT", qiT_d), ("mc", mc_d), ("sga", sga_d)):
                b_ = Buf("dbgc_" + nm)
                k.dma(sp, dbg[nm], src, writes=[b_])
            k.barrier()

        def phase_B():
            with ExitStack() as s3:
                cm2 = [k.sb("cm%d" % i, [128, 256], F32, s3) for i in range(2)]
                pw2 = k.sb("pw2", [128, NIT + 2], F32, s3)
                for it in range(NIT + 2):
                    k.op(dve, lambda e, it=it: e.memset(pw2[:, it:it + 1], float(2.0 ** -(it + 1))), writes=[pw2])
                qT2 = [k.sb("qTb%d" % i, [128, 4, 256], BF16, s3) for i in range(2)]
                for b_ in qT2:
                    k.op(dve, lambda e: e.memset(b_[:], 0.0), writes=[b_])
                qiT2 = [k.sb("qiTb%d" % i, [128, 512], BF16, s3) for i in range(2)]
                scL = [k.sb("sc%d" % i, [128, S], F32, s3) for i in range(2)]
                AL = [k.sb("Ab%d" % i, [128, 512], BF16, s3) for i in range(4)]
                wabs = k.sb("wabs", [128, 8], F32, s3)
                sgn = k.sb("sgn", [128, 8], F32, s3)
                sgd = k.sb("sgd", [128, 8, 128], BF16, s3)
                msk = k.sb("msk", [128, S], BF16, s3)
                Mb = k.sb("Mb", [128, NB, 128], BF16, s3)
                st8 = k.sb("st8", [128, 8], F32, s3)
                Q = k.sb("Qtab", [128, NIT + 2], F32, s3)
                Q2 = k.sb("Q2tab", [128, NIT + 2], F32, s3)
                E2 = [k.sb("Eb%d" % i, [128, 4, 128], BF16, s3) for i in range(3)]
                rden = k.sb("rden", [128, 8], F32, s3)
                Osb = k.sb("Osb", [128, 2, 4, 66], F32, s3)
                thr = st8[:, 6:7]
                ai = [0]
                MBIG = 30000.0
                if debug:
                    attnf = k.sb("attnf", [128, 512], F32, s3)

                def loads_a(j):
                    p = j % 2
                    k.dma(sp, qiT2[p][:], qiT_d[j], writes=[qiT2[p]])
                    k.dma(sp, cm2[p][:], cmask_d[:, j, :], writes=[cm2[p]])

                def loads_b(j):
                    p = j % 2
                    k.dma(sp, qT2[p][0:64, :, 0:128], qT_d[j][0:64, :].rearrange("p (a b) -> p a b", a=4), writes=[qT2[p]])
                    k.dma(sp, qT2[p][64:128, :, 128:256], qT_d[j][64:128, :].rearrange("p (a b) -> p a b", a=4),
                          writes=[qT2[p]])

                def prep(j):
                    wsl = wi_all[:, j * 8:(j + 1) * 8]
                    k.op(dve, lambda e: e.tensor_scalar(sgn[:], wsl, 0.0, 2.0, ALU.is_ge, ALU.mult), reads=[wi_all], writes=[sgn])
                    k.op(dve, lambda e: e.tensor_scalar(sgn[:], sgn[:], -1.0, None, ALU.add), reads=[sgn], writes=[sgn])
                    k.op(dve, lambda e: e.tensor_tensor(wabs[:], wsl, sgn[:], ALU.mult), reads=[wi_all, sgn], writes=[wabs])
                    k.op(dve, lambda e: e.tensor_tensor(sgd[:], ident[:].unsqueeze(1).to_broadcast([128, 8, 128]),
                                                        sgn[:].unsqueeze(2).to_broadcast([128, 8, 128]), ALU.mult),
                         reads=[ident, sgn], writes=[sgd])

                def indexer(j):
                    sc = scL[j % 2]
                    qiTs = qiT2[j % 2]
                    L = 256 * (j + 1)
                    nch = (L + 511) // 512
                    for cc in range(nch):
                        c0 = cc * 512
                        W = min(512, L - c0)
                        pacc = k.bank(pin=True)
                        pbs = {}

                        def qi_mm(h):
                            pb = k.bank()
                            pp = (h % 2) * 64
                            k.op(pe, lambda e: e.matmul(pb[:, 0:W], qiTs[pp:pp + 64, (h // 2) * 128:(h // 2 + 1) * 128],
                                                        kiT[pp:pp + 64, c0:c0 + W], start=True, stop=True),
                                 reads=[qiTs, kiT], writes=[pb])
                            pbs[h] = pb

                        qi_mm(0)
                        qi_mm(1)
                        qi_mm(2)
                        for h in range(8):
                            if h + 3 < 8:
                                qi_mm(h + 3)
                            pb = pbs[h]
                            A = AL[ai[0] % 4]
                            ai[0] += 1
                            k.op(act, lambda e: e.activation(A[:, 0:W], pb[:, 0:W], AF.Relu, scale=wabs[:, h:h + 1]),
                                 reads=[pb, wabs], writes=[A])
                            k.op(pe, lambda e: e.matmul(pacc[:, 0:W], sgd[:, h, :], A[:, 0:W], start=(h == 0), stop=(h == 7)),
                                 reads=[sgd, A], writes=[pacc])
                        k.op(act, lambda e: e.activation(sc[:, c0:c0 + W], pacc[:, 0:W], AF.Copy), reads=[pacc], writes=[sc])
                        k.unpin(pacc)

                def thresh(j):
                    sc = scL[j % 2]
                    L = 256 * (j + 1)
                    cmask = cm2[j % 2]
                    if j == 0:
                        k.op(dve, lambda e: e.tensor_tensor(sc[:, L - 256:L], sc[:, L - 256:L], cmask[:], ALU.add),
                             reads=[sc, cmask], writes=[sc])
                        k.op(dve, lambda e: e.memset(thr, -1.0e29), writes=[st8])
                        return
                    k.op(dve, lambda e: e.tensor_reduce(st8[:, 0:1], sc[:, 0:L], AX.X, ALU.max), reads=[sc], writes=[st8])
                    k.op(dve, lambda e: e.tensor_reduce(st8[:, 1:2], sc[:, 0:L], AX.X, ALU.min), reads=[sc], writes=[st8])
                    k.op(dve, lambda e: e.tensor_tensor(sc[:, L - 256:L], sc[:, L - 256:L], cmask[:], ALU.add),
                         reads=[sc, cmask], writes=[sc])
                    k.op(dve, lambda e: e.tensor_tensor(st8[:, 2:3], st8[:, 0:1], st8[:, 1:2], ALU.subtract),
                         reads=[st8], writes=[st8])
                    k.op(dve, lambda e: e.tensor_scalar(st8[:, 5:6], st8[:, 2:3], float(2.0 ** -10), None, ALU.mult),
                         reads=[st8], writes=[st8])
                    k.op(dve, lambda e: e.tensor_scalar(st8[:, 2:3], st8[:, 2:3], float(1.0 + 2.0 ** -9), 1e-30,
                                                        ALU.mult, ALU.add), reads=[st8], writes=[st8])
                    k.op(dve, lambda e: e.tensor_tensor(st8[:, 7:8], st8[:, 1:2], st8[:, 5:6], ALU.subtract),
                         reads=[st8], writes=[st8])
                    k.op(dve, lambda e: e.tensor_scalar(Q[:], pw2[:], st8[:, 2:3], None, ALU.mult),
                         reads=[pw2, st8], writes=[Q])
                    k.op(dve, lambda e: e.tensor_scalar(Q2[:], Q[:], 2.0, None, ALU.mult), reads=[Q], writes=[Q2])
                    k.op(dve, lambda e: e.tensor_tensor(st8[:, 3:4], st8[:, 7:8], Q[:, 0:1], ALU.add),
                         reads=[st8, Q], writes=[st8])
                    for it in range(NIT):
                        k.op(dve, lambda e: e.memset(st8[:, 4:5], 0.0), writes=[st8])
                        k.op(dve, lambda e: e.tensor_scalar(msk[:, 0:L], sc[:, 0:L], st8[:, 3:4], 0.0,
                                                            ALU.is_ge, ALU.add, accum_out=st8[:, 4:5]),
                             reads=[sc, st8], writes=[st8, msk])
                        k.op(dve, lambda e: e.tensor_scalar(st8[:, 5:6], st8[:, 4:5], TOPK - 0.5,
                                                            Q2[:, it + 1:it + 2], ALU.is_ge, ALU.mult),
                             reads=[st8, Q2], writes=[st8])
                        k.op(dve, lambda e: e.scalar_tensor_tensor(st8[:, 3:4], st8[:, 3:4], Q[:, it + 1:it + 2],
                                                                   st8[:, 5:6], ALU.subtract, ALU.add),
                             reads=[st8, Q], writes=[st8])
                    k.op(dve, lambda e: e.tensor_tensor(thr, st8[:, 3:4], Q[:, NIT:NIT + 1], ALU.subtract),
                         reads=[st8, Q], writes=[st8])

                def mask(j):
                    sc = scL[j % 2]
                    L = 256 * (j + 1)
                    nkb = 2 * (j + 1)
                    if debug:
                        b_ = Buf("dbg_sc%d" % j)
                        for c0_ in range(0, L, 1024):
                            c1_ = min(L, c0_ + 1024)
                            k.dma(sp, dbg["sc"][j][:, c0_:c1_], sc[:, c0_:c1_], reads=[sc], writes=[b_], sembuf=b_)
                        k.dma(sp, dbg["thr"][j], thr, reads=[st8], writes=[b_], sembuf=b_)
                    for g0 in range(0, nkb, 8):
                        gn = min(8, nkb - g0)
                        k.op(dve, lambda e: e.tensor_scalar(msk[:, g0 * 128:(g0 + gn) * 128],
                                                            sc[:, g0 * 128:(g0 + gn) * 128], thr, None, ALU.is_ge),
                             reads=[sc, st8], writes=[msk])
                        tb = k.bank()
                        tbv = bf(tb[:])
                        for q_ in range(gn):
                            k.op(pe, lambda e: e.transpose(tbv[:, q_ * 128:(q_ + 1) * 128],
                                                           msk[:, (g0 + q_) * 128:(g0 + q_ + 1) * 128], ident[:]),
                                 reads=[msk, ident], writes=[tb])
                        k.op(act, lambda e: e.activation(Mb[:, g0:g0 + gn, :],
                                                         tbv[:, 0:gn * 128].rearrange("p (a b) -> p a b", a=gn),
                                                         AF.Identity, bias=-MBIG, scale=MBIG),
                             reads=[tb], writes=[Mb])

                def attention(j):
                    qTs = qT2[j % 2]
                    nkb = 2 * (j + 1)
                    po = [k.bank(pin=True), k.bank(pin=True)]
                    pov = [b_[:, 0:512].rearrange("p (h d) -> p h d", h=4) for b_ in po]
                    units = [(kb, hg) for kb in range(nkb) for hg in range(2)]

                    def qk(u):
                        kb, hg = units[u]
                        pl = k.bank()
                        plv = pl[:, 0:512].rearrange("p (a b) -> p a b", a=4)
                        k.op(pe, lambda e: e.matmul(plv, ident[:], Mb[:, kb, :].unsqueeze(1).to_broadcast([128, 4, 128]),
                                                    start=True, stop=False), reads=[ident, Mb], writes=[pl])
                        for pi in range(2):
                            pr = hg * 2 + pi
                            k.op(pe, lambda e: e.matmul(pl[:, pi * 256:(pi + 1) * 256], KT[:, pr, kb * 128:(kb + 1) * 128],
                                                        qTs[:, pr, :], start=False, stop=(pi == 1)),
                                 reads=[KT, qTs], writes=[pl])
                        return pl, plv

                    LA = 2
                    pend = [qk(u) for u in range(min(LA, len(units)))]
                    for u in range(len(units)):
                        if u + LA < len(units):
                            pend.append(qk(u + LA))
                        kb, hg = units[u]
                        pl, plv = pend.pop(0)
                        E = E2[u % 3]
                        k.op(act, lambda e: e.activation(E[:], plv, AF.Exp, scale=ATT_SCALE), reads=[pl], writes=[E])
                        for hh in range(4):
                            h = hg * 4 + hh
                            k.op(pe, lambda e: e.matmul(pov[hg][:, hh, 0:66], E[:, hh, :], V[:, kb, h * 66:(h + 1) * 66],
                                                        start=(kb == 0 and hh == 0), stop=(kb == nkb - 1 and hh == 3)),
                                 reads=[E, V], writes=[po[hg]])
                    for hg in range(2):
                        k.op(act, lambda e: e.activation(Osb[:, hg, :, :], pov[hg][:, :, 0:66], AF.Copy),
                             reads=[po[hg]], writes=[Osb])
                        k.unpin(po[hg])

                def finalize(j):
                    for hg in range(2):
                        k.op(dve, lambda e: e.reciprocal(rden[:, hg * 4:(hg + 1) * 4].unsqueeze(2), Osb[:, hg, :, 64:65]),
                             reads=[Osb], writes=[rden])
                        k.op(dve, lambda e: e.tensor_tensor(
                            attn_all[:, j, hg * 256:(hg + 1) * 256].rearrange("p (h d) -> p h d", h=4), Osb[:, hg, :, 0:64],
                            rden[:, hg * 4:(hg + 1) * 4].unsqueeze(2).to_broadcast([128, 4, 64]), ALU.mult),
                             reads=[Osb, rden], writes=[attn_all])
                    if debug:
                        k.op(dve, lambda e: e.tensor_copy(attnf[:], attn_all[:, j, :]), reads=[attn_all], writes=[attnf])
                        k.dma(sp, dbg["attn"][j], attnf[:], reads=[attnf])

                loads_a(0)
                loads_b(0)
                if nslots > 1:
                    loads_a(1)
                    loads_b(1)
                prep(0)
                indexer(0)
                thresh(0)
                mask(0)
                if nslots > 1:
                    prep(1)
                    indexer(1)
                for i in range(nslots):
                    if i + 2 < nslots:
                        prep(i + 2)
                    if i + 1 < nslots:
                        thresh(i + 1)
                    attention(i)
                    if i + 2 < nslots:
                        loads_a(i + 2)
                        indexer(i + 2)
                    if i + 1 < nslots:
                        mask(i + 1)
                    finalize(i)
                    if i + 2 < nslots:
                        loads_b(i + 2)
                k.barrier()

        def phase_B2():
            with ExitStack() as s3:
                wao = k.sb("wao", [128, 4, D], BF16, s3)
                wo = k.sb("wo", [128, 8, D], BF16, s3)
                wr = k.sb("wr", [128, 8, 20], BF16, s3)
                br = k.sb("br", [128, 20], F32, s3)
                for kc in range(4):
                    k.dma(pool, wao[:, kc, :], wao_d[kc * 128:(kc + 1) * 128, :], writes=[wao])
                for kc in range(8):
                    k.dma(pool, wo[:, kc, :], wo_d[kc * 128:(kc + 1) * 128, :], writes=[wo])
                k.dma(pool, wr[:], wr_d.rearrange("(k p) c -> p k c", p=128), writes=[wr])
                k.dma(sp, br[:], br_d, writes=[br])
                if "C" in phases:
                    wload(0)
                    wload(1)
                mc2 = [k.sb("mcb%d" % i, [128, 8, 128], BF16, s3) for i in range(2)]
                sga2 = [k.sb("sgab%d" % i, [128, 8, 128], BF16, s3) for i in range(2)]
                xoL = [k.sb("xob%d" % i, [128, D], F32, s3) for i in range(2)]
                attnTL = [k.sb("attnT%d" % i, [128, 4, 128], BF16, s3) for i in range(2)]
                mTL = [k.sb("mT%d" % i, [128, 8, 128], BF16, s3) for i in range(2)]
                x1L = [k.sb("x1_%d" % i, [128, D], F32, s3) for i in range(2)]
                lgA = k.sb("lgA", [128, NS, 20], F32, s3)
                rA = k.sb("rA", [128, 8, NS], F32, s3)
                dG = k.sb("dG", [128, NS, 4], F32, s3)
                eG = k.sb("eG", [128, NS, 4], F32, s3)
                ohg1 = k.sb("ohg1", [128, NS, 4], F32, s3)
                ohgp = k.sb("ohgp", [128, NS, 4], F32, s3)
                t16A = k.sb("t16A", [128, NS, 16], F32, s3)
                eselA = k.sb("eselA", [128, NS, 4], F32, s3)
                oh1A = k.sb("oh1A", [128, NS, 4], F32, s3)
                oh2A = k.sb("oh2A", [128, NS, 4], F32, s3)
                emA = k.sb("emA", [128, NS, 4], F32, s3)
                x1d_b = Buf("x1d_b")

                def loads_c(j):
                    p = j % 2
                    k.dma(sp, mc2[p][:].rearrange("p a b -> p (a b)"), mc_d[j], writes=[mc2[p]])
                    k.dma(sp, sga2[p][:].rearrange("p a b -> p (a b)"), sga_d[j], writes=[sga2[p]])
                    k.dma(sp, xoL[p][:], x_own[j * 128:(j + 1) * 128, :], writes=[xoL[p]])

                def tail(j):
                    mcs, sgas = mc2[j % 2], sga2[j % 2]
                    xo, x1, mT, attnT = xoL[j % 2], x1L[j % 2], mTL[j % 2], attnTL[j % 2]
                    tb = k.bank()
                    tbv = bf(tb[:])
                    for q_ in range(4):
                        k.op(pe, lambda e: e.transpose(tbv[:, q_ * 128:(q_ + 1) * 128],
                                                       attn_all[:, j, q_ * 128:(q_ + 1) * 128], ident[:]),
                             reads=[attn_all, ident], writes=[tb])
                    k.op(act, lambda e: e.activation(attnT[:], tbv[:, 0:512].rearrange("p (a b) -> p a b", a=4), AF.Copy),
                         reads=[tb], writes=[attnT])
                    x1v = x1[:].rearrange("p (a b) -> p a b", a=8)
                    for hb_ in range(2):
                        py = k.bank()
                        pyv = py[:, 0:512].rearrange("p (a b) -> p a b", a=4)
                        for m in range(4):
                            mm = hb_ * 4 + m
                            for kc in range(4):
                                k.op(pe, lambda e: e.matmul(pyv[:, m, :], wao[:, kc, mm * 128:(mm + 1) * 128], attnT[:, kc, :],
                                                            start=(kc == 0), stop=(kc == 3)), reads=[wao, attnT], writes=[py])
                        k.op(dve, lambda e: e.tensor_tensor(x1v[:, hb_ * 4:(hb_ + 1) * 4, :], pyv,
                                                            sgas[:, hb_ * 4:(hb_ + 1) * 4, :], ALU.mult),
                             reads=[py, sgas], writes=[x1])
                    k.op(dve, lambda e: e.tensor_tensor(mT[:], x1v, mcs[:], ALU.add), reads=[x1, mcs], writes=[mT])
                    for half in range(2):
                        px = k.bank()
                        for kc in range(8):
                            k.op(pe, lambda e: e.matmul(px[:, 0:512], mT[:, kc, :], wo[:, kc, half * 512:(half + 1) * 512],
                                                        start=(kc == 0), stop=(kc == 7)), reads=[mT, wo], writes=[px])
                        k.op(dve, lambda e: e.tensor_tensor(acc[:, j, half * 512:(half + 1) * 512], px[:, 0:512],
                                                            xo[:, half * 512:(half + 1) * 512], ALU.add),
                             reads=[px, xo], writes=[acc])
                    if debug:
                        k.dma(sp, dbg["x1"][j], acc[:, j, :], reads=[acc], sembuf=x1)
                    hbsT[j] = norm_pre(acc, 128, acc[:, j, :])

                def tailB(j):
                    norm_post(hbsT.pop(j), 128, gffn, h2T, h2T[:, :, j * 128:(j + 1) * 128])
                    pr_ = k.bank()
                    for kc in range(8):
                        k.op(pe, lambda e: e.matmul(pr_[:, 0:20], h2T[:, kc, j * 128:(j + 1) * 128], wr[:, kc, :],
                                                    start=(kc == 0), stop=(kc == 7)), reads=[h2T, wr], writes=[pr_])
                    k.op(dve, lambda e: e.tensor_tensor(lgA[:, j, :], pr_[:, 0:20], br[:], ALU.add),
                         reads=[pr_, br], writes=[lgA])

                def router_all():
                    n = nslots
                    G = lgA[:, 0:n, 0:4]
                    Ex = lgA[:, 0:n, 4:20].rearrange("p n (g e) -> p n g e", g=4)
                    gmax, gsum, pg, m1, m2, w1, w2 = (rA[:, i, 0:n] for i in range(7))
                    bc4 = lambda ap: ap.unsqueeze(2).to_broadcast([128, n, 4])
                    k.op(dve, lambda e: e.tensor_reduce(gmax, G, AX.X, ALU.max), reads=[lgA], writes=[rA])
                    k.op(dve, lambda e: e.tensor_tensor(dG[:, 0:n, :], G, bc4(gmax), ALU.subtract), reads=[lgA, rA], writes=[dG])
                    k.op(act, lambda e: e.activation(eG[:, 0:n, :], dG[:, 0:n, :], AF.Exp), reads=[dG], writes=[eG])
                    k.op(dve, lambda e: e.tensor_reduce(gsum, eG[:, 0:n, :], AX.X, ALU.add), reads=[eG], writes=[rA])
                    k.op(dve, lambda e: e.reciprocal(pg, gsum), reads=[rA], writes=[rA])
                    k.op(dve, lambda e: e.tensor_scalar(ohg1[:, 0:n, :], dG[:, 0:n, :], 0.0, None, ALU.is_ge), reads=[dG], writes=[ohg1])
                    k.op(dve, lambda e: e.tensor_tensor(ohgp[:, 0:n, :], ohg1[:, 0:n, :], bc4(pg), ALU.mult),
                         reads=[ohg1, rA], writes=[ohgp])
                    t4 = t16A[:, 0:n, :].rearrange("p n (g e) -> p n g e", g=4)
                    k.op(dve, lambda e: e.tensor_tensor(t4, Ex, ohg1[:, 0:n, :].unsqueeze(3).to_broadcast([128, n, 4, 4]), ALU.mult),
                         reads=[lgA, ohg1], writes=[t16A])
                    k.op(dve, lambda e: e.tensor_reduce(eselA[:, 0:n, :], t16A[:, 0:n, :].rearrange("p n (g e) -> p n e g", g=4),
                                                        AX.X, ALU.add), reads=[t16A], writes=[eselA])
                    k.op(dve, lambda e: e.tensor_reduce(m1, eselA[:, 0:n, :], AX.X, ALU.max), reads=[eselA], writes=[rA])
                    k.op(dve, lambda e: e.tensor_tensor(dG[:, 0:n, :], eselA[:, 0:n, :], bc4(m1), ALU.subtract),
                         reads=[eselA, rA], writes=[dG])
                    k.op(dve, lambda e: e.tensor_scalar(oh1A[:, 0:n, :], dG[:, 0:n, :], 0.0, None, ALU.is_ge), reads=[dG], writes=[oh1A])
                    k.op(dve, lambda e: e.scalar_tensor_tensor(emA[:, 0:n, :], oh1A[:, 0:n, :], NEG, eselA[:, 0:n, :],
                                                               ALU.mult, ALU.add), reads=[oh1A, eselA], writes=[emA])
                    k.op(dve, lambda e: e.tensor_reduce(m2, emA[:, 0:n, :], AX.X, ALU.max), reads=[emA], writes=[rA])
                    k.op(dve, lambda e: e.tensor_tensor(dG[:, 0:n, :], emA[:, 0:n, :], bc4(m2), ALU.subtract),
                         reads=[emA, rA], writes=[dG])
                    k.op(dve, lambda e: e.tensor_scalar(oh2A[:, 0:n, :], dG[:, 0:n, :], 0.0, None, ALU.is_ge), reads=[dG], writes=[oh2A])
                    k.op(dve, lambda e: e.tensor_tensor(w2, m1, m2, ALU.subtract), reads=[rA], writes=[rA])
                    k.op(act, lambda e: e.activation(w1, w2, AF.Sigmoid), reads=[rA], writes=[rA])
                    k.op(dve, lambda e: e.tensor_scalar(w2, w1, -1.0, 1.0, ALU.mult, ALU.add), reads=[rA], writes=[rA])
                    k.op(dve, lambda e: e.tensor_tensor(oh1A[:, 0:n, :], oh1A[:, 0:n, :], bc4(w1), ALU.mult),
                         reads=[oh1A, rA], writes=[oh1A])
                    k.op(dve, lambda e: e.tensor_tensor(oh2A[:, 0:n, :], oh2A[:, 0:n, :], bc4(w2), ALU.mult),
                         reads=[oh2A, rA], writes=[oh2A])
                    k.op(dve, lambda e: e.tensor_tensor(oh1A[:, 0:n, :], oh1A[:, 0:n, :], oh2A[:, 0:n, :], ALU.add),
                         reads=[oh1A, oh2A], writes=[oh1A])
                    k.op(dve, lambda e: e.tensor_tensor(
                        gw_all[:, 0:n * 16].rearrange("p (n g e) -> p n g e", g=4, e=4),
                        ohgp[:, 0:n, :].unsqueeze(3).to_broadcast([128, n, 4, 4]),
                        oh1A[:, 0:n, :].unsqueeze(2).to_broadcast([128, n, 4, 4]), ALU.mult),
                         reads=[ohgp, oh1A], writes=[gw_all])

                hbsT = {}
                loads_c(0)
                if nslots > 1:
                    loads_c(1)
                tail(0)
                for j in range(nslots):
                    if j + 1 < nslots:
                        tail(j + 1)
                    tailB(j)
                    if j + 2 < nslots:
                        loads_c(j + 2)
                router_all()
                k.barrier()
        def phase_C():
            with ExitStack() as s4:
                gfin = k.sb("gfin", [128, D], F32, s4)
                k.dma(sp, gfin[:], gfin_d, writes=[gfin])
                sgl2 = [k.sb("sgm%d" % i, [128, 512], F32, s4) for i in range(2)]
                hid2 = [k.sb("hid%d" % i, [128, 2, 512], BF16, s4) for i in range(2)]
                ot2 = [k.sb("ot%d" % i, [128, D], F32, s4) for i in range(2)]

                ngrp = (nslots + 3) // 4
                wload(0)
                hi = 0
                for ex in range(16):
                    if ex + 1 < 16:
                        wload(ex + 1)
                    p = ex % 2
                    wg, wu, wd = wg2[p], wu2[p], wd2[p]
                    for tg in range(ngrp):
                        ns_ = min(4, nslots - tg * 4)
                        N = ns_ * 128
                        hid = hid2[hi % 2]
                        hi += 1
                        for fc in range(2):
                            pg = k.bank()
                            pu = k.bank()
                            for kc in range(8):
                                k.op(pe, lambda e, kc=kc, fc=fc, pg=pg: e.matmul(
                                    pg[:, 0:N], wg[:, kc, fc * 128:(fc + 1) * 128], h2T[:, kc, tg * 512:tg * 512 + N],
                                    start=(kc == 0), stop=(kc == 7)), reads=[wg, h2T], writes=[pg])
                            for kc in range(8):
                                k.op(pe, lambda e, kc=kc, fc=fc, pu=pu: e.matmul(
                                    pu[:, 0:N], wu[:, kc, fc * 128:(fc + 1) * 128], h2T[:, kc, tg * 512:tg * 512 + N],
                                    start=(kc == 0), stop=(kc == 7)), reads=[wu, h2T], writes=[pu])
                            sgl = sgl2[fc]
                            k.op(act, lambda e, pg=pg, sgl=sgl: e.activation(sgl[:, 0:N], pg[:, 0:N], AF.Silu),
                                 reads=[pg], writes=[sgl])
                            k.op(dve, lambda e, pu=pu, sgl=sgl, fc=fc, hid=hid: e.tensor_tensor(
                                hid[:, fc, 0:N], pu[:, 0:N], sgl[:, 0:N], ALU.mult), reads=[pu, sgl], writes=[hid])
                        for s_ in range(ns_):
                            j = tg * 4 + s_
                            for half in range(2):
                                py = k.bank()
                                for fc in range(2):
                                    k.op(pe, lambda e, fc=fc, half=half, py=py, s_=s_, hid=hid: e.matmul(
                                        py[:, 0:512], hid[:, fc, s_ * 128:(s_ + 1) * 128],
                                        wd[:, fc, half * 512:(half + 1) * 512], start=(fc == 0), stop=(fc == 1)),
                                         reads=[hid, wd], writes=[py])
                                k.op(dve, lambda e, py=py, j=j, half=half, ex=ex: e.scalar_tensor_tensor(
                                    acc[:, j, half * 512:(half + 1) * 512], py[:, 0:512],
                                    gw_all[:, j * 16 + ex:j * 16 + ex + 1], acc[:, j, half * 512:(half + 1) * 512],
                                    ALU.mult, ALU.add), reads=[py, gw_all, acc], writes=[acc])
                for j in range(nslots):
                    ot = ot2[j % 2]
                    ss, rs, junk = ssL[j % 2], rsL[j % 2], hbL[j % 2]
                    k.op(dve, lambda e, ss=ss: e.memset(ss[:], 0.0), writes=[ss])
                    k.op(act, lambda e, j=j, junk=junk, ss=ss: e.activation(junk[:], acc[:, j, :], AF.Square, accum_out=ss[:, 0:1]),
                         reads=[acc, ss], writes=[junk, ss])
                    k.op(act, lambda e, rs=rs, ss=ss: e.activation(rs[:], ss[:], AF.Sqrt, bias=EPS, scale=1.0 / D), reads=[ss], writes=[rs])
                    k.op(dve, lambda e, rs=rs: e.reciprocal(rs[:], rs[:]), reads=[rs], writes=[rs])
                    k.op(dve, lambda e, j=j, ot=ot, rs=rs: e.scalar_tensor_tensor(ot[:], acc[:, j, :], rs[:, 0:1], gfin[:],
                                                                          ALU.mult, ALU.mult),
                         reads=[acc, rs, gfin], writes=[ot])
                    k.dma(sp, out_d[j * 128:(j + 1) * 128, :], ot[:], reads=[ot], sembuf=ot)
                k.barrier()
        if "A2" in phases:
            phase_A2()
            if debug:
                dbg_A2()
        attn_all = k.sb("attn_all", [128, NS, 512], BF16)
        wg2 = [k.sb("wg%d" % i, [128, 8, 256], BF16) for i in range(2)]
        wu2 = [k.sb("wu%d" % i, [128, 8, 256], BF16) for i in range(2)]
        wd2 = [k.sb("wd%d" % i, [128, 2, D], BF16) for i in range(2)]
        wloaded = set()

        def wload(ex):
            if ex in wloaded:
                return
            wloaded.add(ex)
            p = ex % 2
            k.dma(pool, wg2[p][:], wg_d[ex].rearrange("(k p) c -> p k c", p=128), writes=[wg2[p]])
            k.dma(pool, wu2[p][:], wu_d[ex].rearrange("(k p) c -> p k c", p=128), writes=[wu2[p]])
            k.dma(pool, wd2[p][:], wd_d[ex].rearrange("(k p) c -> p k c", p=128), writes=[wd2[p]])

        with ExitStack() as skv:
            KT = k.sb("KT", [128, 4, S], BF16, skv)
            V = k.sb("V", [128, NB, 8 * 66], BF16, skv)
            kiT = k.sb("kiT", [128, S], BF16, skv)
            k.op(pool, lambda e: e.memset(V[:], 1.0), writes=[V])
            if "A1" in phases:
                phase_A1()
                if debug:
                    dbg_A1()
            if "B" in phases:
                phase_B()
            k.barrier()
        acc = k.sb("acc", [128, NS, D], F32)
        h2T = k.sb("h2T", [128, 8, NS * 128], BF16)
        if "B" in phases:
            phase_B2()
            if debug:
                k.dma(sp, dbg["gw"], gw_all[:], reads=[gw_all])
        if "C" in phases:
            phase_C()
        k.barrier()
        build_nc.nins = k.nins
    return nc


def own_blocks(r):
    return [2 * j + ((j % 2) ^ r) for j in range(NS)]


def prep_core(inp, c):
    b, r = c // 2, c % 2
    x = np.asarray(inp["x"], dtype=np.float32)
    pos = np.asarray(inp["positions"]).astype(np.int32)
    blocks = own_blocks(r)
    xb = x[b]
    x_own = np.concatenate([xb[i * 128:(i + 1) * 128] for i in blocks], axis=0)
    x_halo = np.zeros((NS * 32, D), np.float32)
    for j, i in enumerate(blocks):
        if i > 0:
            x_halo[j * 32:(j + 1) * 32] = xb[i * 128 - 32:i * 128]
    pos_full = np.ascontiguousarray(pos[b].reshape(NB, 128).T)
    pos_own = np.ascontiguousarray(np.stack([pos[b][i * 128:(i + 1) * 128] for i in blocks], axis=1))
    invf = (1.0 / (np.float32(10000.0) ** (np.arange(0, 64, 2, dtype=np.float32) / np.float32(64)))).astype(np.float32)
    invf = np.ascontiguousarray(np.broadcast_to(invf[None, :], (128, 32)))
    cmask = np.zeros((128, NS, 256), np.float32)
    for j, i in enumerate(blocks):
        qidx = i * 128 + np.arange(128)[:, None]
        kidx = 256 * j + np.arange(256)[None, :]
        cmask[:, j, :] = np.where(kidx > qidx, np.float32(NEG), np.float32(0.0))

    def fm(v, nchunk):
        return np.ascontiguousarray(np.asarray(v, np.float32).reshape(nchunk, 128).T)

    w_dw = np.asarray(inp["w_dw"], np.float32)[0, :, 0, :]
    wdw = np.ascontiguousarray(w_dw.T.reshape(4, 128, 31).transpose(1, 0, 2))
    w_r = np.concatenate([np.asarray(inp["w_rg"], np.float32)[0],
                          np.asarray(inp["w_re"], np.float32)[0].reshape(D, 16)], axis=1)
    b_r = np.concatenate([np.asarray(inp["b_rg"], np.float32)[0], np.asarray(inp["b_re"], np.float32)[0].reshape(16)])
    m = {
        "x_full": np.ascontiguousarray(xb), "x_own": x_own, "x_halo": x_halo,
        "pos_full": pos_full, "pos_own": pos_own, "invf": invf, "cmask": cmask,
        "ident": np.eye(128, dtype=np.float32),
        "w_in": np.ascontiguousarray(np.asarray(inp["w_in"], np.float32)[0]),
        "wdw": wdw, "bdw": fm(inp["b_dw"][0], 4), "lng": fm(inp["ln_g"][0], 4), "lnb": fm(inp["ln_b"][0], 4),
        "gmix": fm(inp["g_mix"][0], 8), "gffn": fm(inp["g_ffn"][0], 8),
        "gfin": np.ascontiguousarray(np.broadcast_to(np.asarray(inp["g_final"], np.float32)[None, :], (128, D))),
        "b_r": np.ascontiguousarray(np.broadcast_to(b_r[None, :], (128, 20))),
        "w_r": np.ascontiguousarray(w_r),
        "w_conv_out": np.ascontiguousarray(np.asarray(inp["w_conv_out"], np.float32)[0]),
        "w_attn_out": np.ascontiguousarray(np.asarray(inp["w_attn_out"], np.float32)[0]),
        "w_o": np.ascontiguousarray(np.asarray(inp["w_o"], np.float32)[0]),
        "w_gate": np.ascontiguousarray(np.asarray(inp["w_gate"], np.float32)[0]),
        "w_up": np.ascontiguousarray(np.asarray(inp["w_up"], np.float32)[0]),
        "w_down": np.ascontiguousarray(np.asarray(inp["w_down"], np.float32)[0]),
    }
    return m


_NC_CACHE = {}


def kernel(**inputs):
    if "nc" not in _NC_CACHE:
        _NC_CACHE["nc"] = build_nc()
    nc = _NC_CACHE["nc"]
    in_maps = [prep_core(inputs, c) for c in range(8)]
    res = run_bass_kernel_spmd(nc, in_maps, core_ids=list(range(8)))
    out = np.empty((4, S, D), np.float32)
    for c in range(8):
        b, r = c // 2, c % 2
        o = np.asarray(res.results[c]["out"], dtype=np.float32)
        for j, i in enumerate(own_blocks(r)):
            out[b, i * 128:(i + 1) * 128] = o[j * 128:(j + 1) * 128]
    return out
```

```python
from contextlib import ExitStack
import numpy as np
import concourse.bass as bass
import concourse.mybir as mybir
from concourse.bass_utils import run_bass_kernel_spmd

F32 = mybir.dt.float32
BF16 = mybir.dt.bfloat16
I32 = mybir.dt.int32
ALU = mybir.AluOpType
AF = mybir.ActivationFunctionType
AX = mybir.AxisListType

D = 1024
S = 4096
NB = 32
NS = 16
EPS = 1e-6
NIT = 14
TOPK = 256
NEG = -1.0e30
C_U, C_Q, C_K, C_V, C_QI, C_KI, C_WI, C_GC, C_GA = 0, 1024, 1536, 2048, 2560, 3072, 3136, 3144, 4168
IDX_SCALE = float((8 ** -0.5) * (64 ** -0.5))
ATT_SCALE = float(64 ** -0.5)


class Buf:
    __slots__ = ("name", "t", "lw", "rd", "dsem", "dcnt")

    def __init__(self, name, t=None):
        self.name = name
        self.t = t
        self.lw = None
        self.rd = {}
        self.dsem = None
        self.dcnt = 0

    def __getitem__(self, key):
        return self.t[key]


class Eng:
    def __init__(self, k, name, eng):
        self.name = name
        self.eng = eng
        self.sem = k.new_sem("e_" + name)
        self.cnt = 0
        self.seen = {}

    def wait_tok(self, tok):
        if tok is None:
            return
        sem, val, _ = tok
        key = id(sem)
        if self.seen.get(key, 0) >= val:
            return
        self.eng.wait_ge(sem, val)
        self.seen[key] = val


class K:
    def __init__(self, nc, stack):
        self.nc = nc
        self.stack = stack
        self.pe = Eng(self, "pe", nc.tensor)
        self.act = Eng(self, "act", nc.scalar)
        self.dve = Eng(self, "dve", nc.vector)
        self.pool = Eng(self, "pool", nc.gpsimd)
        self.sp = Eng(self, "sp", nc.sync)
        self.engs = [self.pe, self.act, self.dve, self.pool, self.sp]
        self.dma_bufs = []
        self.nins = 0
        self.banks = []
        self.bank_i = 0
        self.pinned = set()

    def new_sem(self, name):
        return self.stack.enter_context(self.nc.semaphore(name))

    def sb(self, name, shape, dt, stack=None):
        t = (stack or self.stack).enter_context(self.nc.sbuf_tensor("s_" + name, list(shape), dt))
        return Buf(name, t)

    def mk_banks(self):
        for i in range(8):
            t = self.stack.enter_context(self.nc.psum_tensor("bank%d" % i, [128, 512], F32))
            self.banks.append(Buf("bank%d" % i, t))

    def bank(self, pin=False):
        for _ in range(16):
            b = self.banks[self.bank_i % 8]
            self.bank_i += 1
            if b.name not in self.pinned:
                if pin:
                    self.pinned.add(b.name)
                return b
        raise RuntimeError("no psum bank")

    def unpin(self, b):
        self.pinned.discard(b.name)

    def _deps(self, e, reads, writes):
        for b in reads:
            if b.lw is not None:
                e.wait_tok(b.lw)
        for b in writes:
            if b.lw is not None and b.lw[2] != e.name:
                e.wait_tok(b.lw)
            for en, tok in b.rd.items():
                if en != e.name:
                    e.wait_tok(tok)

    def _commit(self, tok, reads, writes):
        for b in reads:
            b.rd[tok[2]] = tok
        for b in writes:
            b.lw = tok
            b.rd = {}

    def op(self, e, fn, reads=(), writes=()):
        self._deps(e, reads, writes)
        ins = fn(e.eng)
        e.cnt += 1
        ins.then_inc(e.sem, 1)
        tok = (e.sem, e.cnt, e.name)
        self._commit(tok, reads, writes)
        self.nins += 1
        return tok

    def dma(self, e, out, in_, reads=(), writes=(), sembuf=None):
        self._deps(e, reads, writes)
        sb_ = sembuf if sembuf is not None else (writes[0] if writes else reads[0])
        if sb_.dsem is None:
            sb_.dsem = self.new_sem("d_" + sb_.name)
            self.dma_bufs.append(sb_)
        ins = e.eng.dma_start(out=out, in_=in_)
        sb_.dcnt += 16
        ins.then_inc(sb_.dsem, 16)
        tok = (sb_.dsem, sb_.dcnt, "dma_" + sb_.name)
        self._commit(tok, reads, writes)
        self.nins += 1
        return tok

    def barrier(self):
        toks = [(e.sem, e.cnt, e.name) for e in self.engs if e.cnt > 0]
        toks += [(b.dsem, b.dcnt, "dma") for b in self.dma_bufs]
        for e in self.engs:
            for t in toks:
                if t[2] != e.name:
                    e.wait_tok(t)


def bf(ap):
    return ap.bitcast(BF16)


def build_nc(phases=("A1", "A2", "B", "C"), debug=False, nslots=NS, bstop=9):
    nc = bass.Bass("TRN2", target_bir_lowering=False)

    def din(name, shape, dt=F32):
        return nc.dram_tensor(name, list(shape), dt, kind="ExternalInput").ap()

    def dscr(name, shape, dt):
        return nc.dram_tensor(name, list(shape), dt, kind="Internal").ap()

    x_full = din("x_full", [S, D])
    x_own = din("x_own", [NS * 128, D])
    x_halo = din("x_halo", [NS * 32, D])
    pos_full = din("pos_full", [128, NB], I32)
    pos_own = din("pos_own", [128, NS], I32)
    invf = din("invf", [128, 32])
    cmask_d = din("cmask", [128, NS, 256])
    ident_d = din("ident", [128, 128])
    w_in = din("w_in", [D, 5192])
    wdw_d = din("wdw", [128, 4, 31])
    bdw_d = din("bdw", [128, 4])
    lng_d = din("lng", [128, 4])
    lnb_d = din("lnb", [128, 4])
    gmix_d = din("gmix", [128, 8])
    gffn_d = din("gffn", [128, 8])
    gfin_d = din("gfin", [128, D])
    br_d = din("b_r", [128, 20])
    wr_d = din("w_r", [D, 20])
    wco_d = din("w_conv_out", [512, D])
    wao_d = din("w_attn_out", [512, D])
    wo_d = din("w_o", [D, D])
    wg_d = din("w_gate", [16, D, 256])
    wu_d = din("w_up", [16, D, 256])
    wd_d = din("w_down", [16, 256, D])
    out_d = nc.dram_tensor("out", [NS * 128, D], F32, kind="ExternalOutput").ap()

    qT_d = dscr("qT_d", [NS, 128, 512], BF16)
    qiT_d = dscr("qiT_d", [NS, 128, 512], BF16)
    mc_d = dscr("mc_d", [NS, 128, 1024], BF16)
    sga_d = dscr("sga_d", [NS, 128, 1024], BF16)
    x1_d = dscr("x1_d", [NS, 128, D], F32)
    h2T_d = dscr("h2T_d", [NS, 128, 1024], BF16)
    dbg = {}
    if debug:
        dbg["kT"] = nc.dram_tensor("dbg_kT", [128, 4, S], BF16, kind="ExternalOutput").ap()
        dbg["v"] = nc.dram_tensor("dbg_v", [128, NB, 8 * 66], BF16, kind="ExternalOutput").ap()
        dbg["kiT"] = nc.dram_tensor("dbg_kiT", [128, S], BF16, kind="ExternalOutput").ap()
        dbg["qT"] = nc.dram_tensor("dbg_qT", [NS, 128, 512], BF16, kind="ExternalOutput").ap()
        dbg["qiT"] = nc.dram_tensor("dbg_qiT", [NS, 128, 512], BF16, kind="ExternalOutput").ap()
        dbg["mc"] = nc.dram_tensor("dbg_mc", [NS, 128, 1024], BF16, kind="ExternalOutput").ap()
        dbg["sga"] = nc.dram_tensor("dbg_sga", [NS, 128, 1024], BF16, kind="ExternalOutput").ap()
        dbg["wi"] = nc.dram_tensor("dbg_wi", [128, NS * 8], F32, kind="ExternalOutput").ap()
        dbg["x1"] = nc.dram_tensor("dbg_x1", [NS, 128, D], F32, kind="ExternalOutput").ap()
        dbg["gw"] = nc.dram_tensor("dbg_gw", [128, NS * 16], F32, kind="ExternalOutput").ap()
        dbg["sc"] = nc.dram_tensor("dbg_sc", [NS, 128, S], F32, kind="ExternalOutput").ap()
        dbg["thr"] = nc.dram_tensor("dbg_thr", [NS, 128, 1], F32, kind="ExternalOutput").ap()
        dbg["attn"] = nc.dram_tensor("dbg_attn", [NS, 128, 512], F32, kind="ExternalOutput").ap()

    with ExitStack() as st:
        k = K(nc, st)
        pe, act, dve, pool, sp = k.pe, k.act, k.dve, k.pool, k.sp
        k.mk_banks()

        ident = k.sb("ident", [128, 128], BF16)
        identf = k.sb("identf", [128, 128], F32)
        wi_all = k.sb("wi_all", [128, NS * 8], F32)
        gw_all = k.sb("gw_all", [128, NS * 16], F32)
        gmix = k.sb("gmix", [128, 8], F32)
        gffn = k.sb("gffn", [128, 8], F32)
        ssL = [k.sb("ss%d" % i, [128, 1], F32) for i in range(2)]
        rsL = [k.sb("rs%d" % i, [128, 1], F32) for i in range(2)]
        hbL = [k.sb("hb%d" % i, [128, D], BF16) for i in range(2)]
        nrm_i = [0]

        k.dma(sp, identf[:], ident_d, writes=[identf])
        k.dma(pool, ident[:], ident_d, writes=[ident])
        k.dma(sp, gmix[:], gmix_d, writes=[gmix])
        k.dma(sp, gffn[:], gffn_d, writes=[gffn])

        def rope_tables(pos_d, n, cosT, sinT, stk):
            pi_ = k.sb("pos_i%d" % n, [128, n], I32, stk)
            pf = k.sb("pos_f%d" % n, [128, n], F32, stk)
            iv = k.sb("invf%d" % n, [128, 32], F32, stk)
            ang = k.sb("ang%d" % n, [128, n, 32], F32, stk)
            tmp = k.sb("angt%d" % n, [128, n, 32], F32, stk)
            k.dma(sp, pi_[:], pos_d, writes=[pi_])
            k.dma(sp, iv[:], invf, writes=[iv])
            k.op(dve, lambda e: e.tensor_copy(pf[:], pi_[:]), reads=[pi_], writes=[pf])
            k.op(dve, lambda e: e.tensor_tensor(ang[:], pf[:].unsqueeze(2).to_broadcast([128, n, 32]),
                                                iv[:].unsqueeze(1).to_broadcast([128, n, 32]), ALU.mult),
                 reads=[pf, iv], writes=[ang])
            ki_ = k.sb("angk%d" % n, [128, n, 32], I32, stk)
            kf_ = k.sb("angf%d" % n, [128, n, 32], F32, stk)
            two_pi = float(2 * np.pi)
            C1 = 6.28125
            C2 = float(2 * np.pi - 6.28125)
            PI_SAFE = 3.1415925

            def sin_of(dst, shift):
                if shift != 0.0:
                    k.op(dve, lambda e: e.tensor_scalar(tmp[:], ang[:], shift, None, ALU.add), reads=[ang], writes=[tmp])
                    src = tmp
                else:
                    src = ang
                k.op(dve, lambda e: e.tensor_scalar(kf_[:], src[:], 1.0 / two_pi, None, ALU.mult),
                     reads=[src], writes=[kf_])
                k.op(dve, lambda e: e.tensor_copy(ki_[:], kf_[:]), reads=[kf_], writes=[ki_])
                k.op(dve, lambda e: e.tensor_copy(kf_[:], ki_[:]), reads=[ki_], writes=[kf_])
                k.op(dve, lambda e: e.scalar_tensor_tensor(tmp[:], kf_[:], -C1, src[:], ALU.mult, ALU.add),
                     reads=[kf_, src], writes=[tmp])
                k.op(dve, lambda e: e.scalar_tensor_tensor(tmp[:], kf_[:], -C2, tmp[:], ALU.mult, ALU.add),
                     reads=[kf_, tmp], writes=[tmp])
                k.op(dve, lambda e: e.tensor_scalar(kf_[:], tmp[:], float(np.pi), -two_pi, ALU.is_gt, ALU.mult),
                     reads=[tmp], writes=[kf_])
                k.op(dve, lambda e: e.tensor_tensor(tmp[:], tmp[:], kf_[:], ALU.add), reads=[tmp, kf_], writes=[tmp])
                k.op(dve, lambda e: e.tensor_scalar(kf_[:], tmp[:], -float(np.pi), two_pi, ALU.is_lt, ALU.mult),
                     reads=[tmp], writes=[kf_])
                k.op(dve, lambda e: e.tensor_tensor(tmp[:], tmp[:], kf_[:], ALU.add), reads=[tmp, kf_], writes=[tmp])
                k.op(dve, lambda e: e.tensor_scalar(tmp[:], tmp[:], -PI_SAFE, PI_SAFE, ALU.max, ALU.min),
                     reads=[tmp], writes=[tmp])
                k.op(act, lambda e: e.activation(dst[:], tmp[:], AF.Sin), reads=[tmp], writes=[dst])

            sin_of(sinT, 0.0)
            sin_of(cosT, float(0.5 * np.pi))

        hbN = 4
        hbX = [k.sb("hbx%d" % i, [128, D], BF16) for i in range(hbN - 2)]

        def norm_pre(xt, n, src=None):
            if src is None:
                src = xt[0:n, :]
            hbs = hbL + hbX
            ss, rs, hb = ssL[nrm_i[0] % 2], rsL[nrm_i[0] % 2], hbs[nrm_i[0] % hbN]
            junk = hb
            nrm_i[0] += 1
            k.op(dve, lambda e: e.memset(ss[0:n, :], 0.0), writes=[ss])
            k.op(act, lambda e: e.activation(junk[0:n, :], src, AF.Square, accum_out=ss[0:n, 0:1]),
                 reads=[xt, ss], writes=[junk, ss])
            k.op(act, lambda e: e.activation(rs[0:n, :], ss[0:n, :], AF.Sqrt, bias=EPS, scale=1.0 / D),
                 reads=[ss], writes=[rs])
            k.op(dve, lambda e: e.reciprocal(rs[0:n, :], rs[0:n, :]), reads=[rs], writes=[rs])
            k.op(act, lambda e: e.activation(hb[0:n, :], src, AF.Copy, scale=rs[0:n, 0:1]),
                 reads=[xt, rs], writes=[hb])
            return hb

        def norm_post(hb, n, gT, hT, hT_ap=None):
            if hT_ap is None:
                hT_ap = hT[:, :, 0:n]
            tp = k.bank()
            tpv = bf(tp[:])[:, 0:8 * 128].rearrange("p (a b) -> p a b", a=8)
            for kc in range(8):
                k.op(pe, lambda e, kc=kc: e.transpose(tpv[:, kc, 0:n], hb[0:n, kc * 128:(kc + 1) * 128],
                                                      ident[0:n, 0:n]),
                     reads=[hb, ident], writes=[tp])
            k.op(dve, lambda e: e.tensor_tensor(hT_ap, tpv[:, :, 0:n],
                                                gT[:].unsqueeze(2).to_broadcast([128, 8, n]), ALU.mult),
                 reads=[tp, gT], writes=[hT])

        def norm_T(xt, n, gT, hT, hT_ap=None):
            hb = norm_pre(xt, n)
            norm_post(hb, n, gT, hT, hT_ap)

        def proj_tok(hT, w, c0, ncols):
            pb = k.bank()
            for kc in range(8):
                k.op(pe, lambda e, kc=kc: e.matmul(pb[:, 0:ncols], hT[:, kc, :], w[:, kc, c0:c0 + ncols],
                                                   start=(kc == 0), stop=(kc == 7)),
                     reads=[hT, w], writes=[pb])
            return pb

        def rope(pb, nh, cosT, sinT, ti, outb, tmpA, tmpB):
            pv = pb[:, 0:nh * 64].rearrange("p (h d) -> p h d", h=nh)
            ov = outb[:, 0:nh * 64].rearrange("p (h d) -> p h d", h=nh)
            av = tmpA[:, 0:nh * 64].rearrange("p (h d) -> p h d", h=nh)
            bv = tmpB[:, 0:nh * 64].rearrange("p (h d) -> p h d", h=nh)
            cb = cosT[:, ti, :].unsqueeze(1).to_broadcast([128, nh, 32])
            sb_ = sinT[:, ti, :].unsqueeze(1).to_broadcast([128, nh, 32])
            k.op(dve, lambda e: e.tensor_tensor(av[:, :, 0:32], pv[:, :, 0:32], cb, ALU.mult),
                 reads=[pb, cosT], writes=[tmpA])
            k.op(dve, lambda e: e.tensor_tensor(av[:, :, 32:64], pv[:, :, 32:64], cb, ALU.mult),
                 reads=[pb, cosT], writes=[tmpA])
            k.op(dve, lambda e: e.tensor_tensor(bv[:, :, 0:32], pv[:, :, 32:64], sb_, ALU.mult),
                 reads=[pb, sinT], writes=[tmpB])
            k.op(dve, lambda e: e.tensor_tensor(bv[:, :, 32:64], pv[:, :, 0:32], sb_, ALU.mult),
                 reads=[pb, sinT], writes=[tmpB])
            k.op(dve, lambda e: e.tensor_tensor(ov[:, :, 0:32], av[:, :, 0:32], bv[:, :, 0:32], ALU.subtract),
                 reads=[tmpA, tmpB], writes=[outb])
            k.op(dve, lambda e: e.tensor_tensor(ov[:, :, 32:64], av[:, :, 32:64], bv[:, :, 32:64], ALU.add),
                 reads=[tmpA, tmpB], writes=[outb])

        w_in_v = w_in.rearrange("(k p) c -> p k c", p=128)
        def phase_A1():
            with ExitStack() as s1:
                cosF = k.sb("cosF", [128, NB, 32], F32, s1)
                sinF = k.sb("sinF", [128, NB, 32], F32, s1)
                rope_tables(pos_full, NB, cosF, sinF, s1)
                wA = k.sb("wA1", [128, 8, 1088], BF16, s1)
                for kc in range(8):
                    k.dma(pool, wA[:, kc, 0:1024], w_in_v[:, kc, C_K:C_K + 1024], writes=[wA])
                    k.dma(pool, wA[:, kc, 1024:1088], w_in_v[:, kc, C_KI:C_KI + 64], writes=[wA])
                xt2 = [k.sb("xtA%d" % i, [128, D], F32, s1) for i in range(2)]
                hT2 = [k.sb("hTA%d" % i, [128, 8, 128], BF16, s1) for i in range(2)]
                krL = [k.sb("kr%d" % i, [128, 512], BF16, s1) for i in range(2)]
                kirL = [k.sb("kir%d" % i, [128, 128], BF16, s1) for i in range(2)]
                tAL = [k.sb("tA%d" % i, [128, 512], F32, s1) for i in range(2)]
                tBL = [k.sb("tB%d" % i, [128, 512], F32, s1) for i in range(2)]
                tCL = [k.sb("tC%d" % i, [128, 64], F32, s1) for i in range(2)]
                tDL = [k.sb("tD%d" % i, [128, 64], F32, s1) for i in range(2)]
                hbs1 = {}

                def pre1(ti):
                    k.dma(sp, xt2[ti % 2][:], x_full[ti * 128:(ti + 1) * 128, :], writes=[xt2[ti % 2]])
                    hbs1[ti] = norm_pre(xt2[ti % 2], 128)

                def post1(ti):
                    norm_post(hbs1.pop(ti), 128, gmix, hT2[ti % 2])

                def part2(ti):
                    kr, kir = krL[ti % 2], kirL[ti % 2]
                    tb = k.bank()
                    tbv = bf(tb[:])
                    for pr in range(4):
                        k.op(pe, lambda e, pr=pr: e.transpose(tbv[:, pr * 128:(pr + 1) * 128],
                                                              kr[:, pr * 128:(pr + 1) * 128], ident[:]),
                             reads=[kr, ident], writes=[tb])
                    k.op(pe, lambda e: e.transpose(tbv[:, 512:640], kir[:], ident[:]), reads=[kir, ident], writes=[tb])
                    k.op(act, lambda e: e.activation(KT[:, :, ti * 128:(ti + 1) * 128],
                                                     tbv[:, 0:512].rearrange("p (a b) -> p a b", a=4), AF.Copy),
                         reads=[tb], writes=[KT])
                    k.op(act, lambda e: e.activation(kiT[:, ti * 128:(ti + 1) * 128], tbv[:, 512:640], AF.Copy),
                         reads=[tb], writes=[kiT])

                pre1(0)
                post1(0)
                pre1(1)
                for ti in range(NB):
                    if ti + 1 < NB:
                        post1(ti + 1)
                    hT = hT2[ti % 2]
                    kr, kir, tA, tB = krL[ti % 2], kirL[ti % 2], tAL[ti % 2], tBL[ti % 2]
                    tC, tD = tCL[ti % 2], tDL[ti % 2]
                    pk = proj_tok(hT, wA, 0, 512)
                    pv = proj_tok(hT, wA, 512, 512)
                    pki = proj_tok(hT, wA, 1024, 64)
                    if ti + 2 < NB:
                        pre1(ti + 2)
                    rope(pk, 8, cosF, sinF, ti, kr, tA, tB)
                    Vv = V[:, ti, :].rearrange("p (h d) -> p h d", h=8)
                    k.op(act, lambda e: e.activation(Vv[:, :, 0:64],
                                                     pv[:, 0:512].rearrange("p (h d) -> p h d", h=8), AF.Copy),
                         reads=[pv], writes=[V])
                    rope(pki, 1, cosF, sinF, ti, kir, tC, tD)
                    k.op(dve, lambda e, kir=kir: e.tensor_copy(kir[:, 64:128], kir[:, 0:64]), reads=[kir], writes=[kir])
                    if ti >= 1:
                        part2(ti - 1)
                part2(NB - 1)
                k.barrier()
        def dbg_A1():
            for pr in range(4):
                k.dma(sp, dbg["kT"][:, pr, :], KT[:, pr, :], reads=[KT])
            for t8 in range(0, NB, 4):
                k.dma(sp, dbg["v"][:, t8:t8 + 4, :], V[:, t8:t8 + 4, :], reads=[V])
            k.dma(sp, dbg["kiT"], kiT[:], reads=[kiT])

        def phase_A2():
            with ExitStack() as s2:
                cosO = k.sb("cosO", [128, NS, 32], F32, s2)
                sinO = k.sb("sinO", [128, NS, 32], F32, s2)
                rope_tables(pos_own, NS, cosO, sinO, s2)
                WU, WQ, WQI, WWI, WGC, WGA = 0, 1024, 1536, 2048, 2056, 3080
                wB = k.sb("wB", [128, 8, 4104], BF16, s2)
                wBq, wBu, wBg = Buf("wBq", wB.t), Buf("wBu", wB.t), Buf("wBg", wB.t)
                for kc in range(8):
                    k.dma(pool, wB[:, kc, WQ:WQ + 512], w_in_v[:, kc, C_Q:C_Q + 512], writes=[wBq])
                    k.dma(pool, wB[:, kc, WQI:WQI + 512], w_in_v[:, kc, C_QI:C_QI + 512], writes=[wBq])
                    k.dma(pool, wB[:, kc, WWI:WWI + 8], w_in_v[:, kc, C_WI:C_WI + 8], writes=[wBq])
                for kc in range(8):
                    k.dma(pool, wB[:, kc, WU:WU + 1024], w_in_v[:, kc, C_U:C_U + 1024], writes=[wBu])
                for kc in range(8):
                    k.dma(pool, wB[:, kc, WGC:WGC + 2048], w_in_v[:, kc, C_GC:C_GC + 2048], writes=[wBg])
                wco = k.sb("wco", [128, 4, D], BF16, s2)
                for kc in range(4):
                    k.dma(pool, wco[:, kc, :], wco_d[kc * 128:(kc + 1) * 128, :], writes=[wco])
                wdw = k.sb("wdw", [128, 4, 31], F32, s2)
                bdw = k.sb("bdw", [128, 4], F32, s2)
                lng = k.sb("lng", [128, 4], F32, s2)
                lnb = k.sb("lnb", [128, 4], F32, s2)
                k.dma(sp, wdw[:], wdw_d, writes=[wdw])
                k.dma(sp, bdw[:], bdw_d, writes=[bdw])
                k.dma(sp, lng[:], lng_d, writes=[lng])
                k.dma(sp, lnb[:], lnb_d, writes=[lnb])
                Dg = k.sb("Dg", [128, 124, 128], BF16, s2)
                for c in range(4):
                    for j in range(31):
                        k.op(dve, lambda e, c=c, j=j: e.tensor_scalar(Dg[:, c * 31 + j, :], identf[:],
                                                                       wdw[:, c, j:j + 1], None, ALU.mult),
                             reads=[identf, wdw], writes=[Dg])
                ones = k.sb("ones", [128, 128], F32, s2)
                k.op(pool, lambda e: e.memset(ones[:], 1.0), writes=[ones])
                xt2 = [k.sb("xtB%d" % i, [128, D], F32, s2) for i in range(2)]
                xh2 = [k.sb("xhB%d" % i, [32, D], F32, s2) for i in range(2)]
                hT_L = [k.sb("hTB_%d" % i_, [128, 8, 128], BF16, s2) for i_ in range(2)]
                hTh_L = [k.sb("hThB_%d" % i_, [128, 8, 32], BF16, s2) for i_ in range(2)]
                qr_L = [k.sb("qr_%d" % i_, [128, 512], BF16, s2) for i_ in range(2)]
                qir_L = [k.sb("qir_%d" % i_, [128, 512], BF16, s2) for i_ in range(2)]
                tA_L = [k.sb("tA2_%d" % i_, [128, 512], F32, s2) for i_ in range(2)]
                tB_L = [k.sb("tB2_%d" % i_, [128, 512], F32, s2) for i_ in range(2)]
                qT_s_L = [k.sb("qT_s_%d" % i_, [128, 512], BF16, s2) for i_ in range(2)]
                qiT_s_L = [k.sb("qiT_s_%d" % i_, [128, 512], BF16, s2) for i_ in range(2)]
                sg_L = [k.sb("sgl_%d" % i_, [128, 160], F32, s2) for i_ in range(2)]
                gT_L = [k.sb("gT_%d" % i_, [128, 4, 160], BF16, s2) for i_ in range(2)]
                csb_L = [k.sb("csb_%d" % i_, [128, 4, 128], F32, s2) for i_ in range(2)]
                csq_L = [k.sb("csq_%d" % i_, [128, 4, 128], F32, s2) for i_ in range(2)]
                mean_L = [k.sb("mean_%d" % i_, [128, 128], F32, s2) for i_ in range(2)]
                msq_L = [k.sb("msq_%d" % i_, [128, 128], F32, s2) for i_ in range(2)]
                rstd_L = [k.sb("rstd_%d" % i_, [128, 128], F32, s2) for i_ in range(2)]
                nrm_L = [k.sb("nrm_%d" % i_, [128, 4, 128], F32, s2) for i_ in range(2)]
                snT_L = [k.sb("snT_%d" % i_, [128, 4, 128], BF16, s2) for i_ in range(2)]
                sgc_L = [k.sb("sgc_%d" % i_, [128, 8, 128], F32, s2) for i_ in range(2)]
                mc_s_L = [k.sb("mc_s_%d" % i_, [128, 8, 128], BF16, s2) for i_ in range(2)]
                sga_s_L = [k.sb("sga_s_%d" % i_, [128, 8, 128], BF16, s2) for i_ in range(2)]
                qTd_b = Buf("qTd_b")
                mcd_b = Buf("mcd_b")

                hbs2 = {}

                def pre2(j):
                    k.dma(sp, xt2[j % 2][:], x_own[j * 128:(j + 1) * 128, :], writes=[xt2[j % 2]])
                    k.dma(sp, xh2[j % 2][:], x_halo[j * 32:(j + 1) * 32, :], writes=[xh2[j % 2]])
                    hbs2[j] = (norm_pre(xh2[j % 2], 32), norm_pre(xt2[j % 2], 128))

                def post2(j):
                    a_, b_ = hbs2.pop(j)
                    norm_post(a_, 32, gmix, hTh_L[j % 2])
                    norm_post(b_, 128, gmix, hT_L[j % 2])

                for j in range(nslots):
                    xt = xt2[j % 2]
                    xh = xh2[j % 2]
                    hT, hTh, qr, qir, tA, tB, qT_s, qiT_s, sg, gT, csb, csq, mean, msq, rstd, nrm, snT, sgc, mc_s, sga_s = hT_L[j % 2], hTh_L[j % 2], qr_L[j % 2], qir_L[j % 2], tA_L[j % 2], tB_L[j % 2], qT_s_L[j % 2], qiT_s_L[j % 2], sg_L[j % 2], gT_L[j % 2], csb_L[j % 2], csq_L[j % 2], mean_L[j % 2], msq_L[j % 2], rstd_L[j % 2], nrm_L[j % 2], snT_L[j % 2], sgc_L[j % 2], mc_s_L[j % 2], sga_s_L[j % 2]
                    if j == 0:
                        pre2(0)
                        post2(0)
                        if nslots > 1:
                            pre2(1)
                    if j + 1 < nslots:
                        post2(j + 1)
                    pq = proj_tok(hT, wBq, WQ, 512)
                    rope(pq, 8, cosO, sinO, j, qr, tA, tB)
                    pqi = proj_tok(hT, wBq, WQI, 512)
                    rope(pqi, 8, cosO, sinO, j, qir, tA, tB)
                    pw = proj_tok(hT, wBq, WWI, 8)
                    k.op(act, lambda e: e.activation(wi_all[:, j * 8:(j + 1) * 8], pw[:, 0:8], AF.Copy,
                                                     scale=IDX_SCALE), reads=[pw], writes=[wi_all])
                    for src, dstb, dd in ((qr, qT_s, qT_d), (qir, qiT_s, qiT_d)):
                        tb = k.bank()
                        tbv = bf(tb[:])
                        for pr in range(4):
                            k.op(pe, lambda e, pr=pr, src=src, tbv=tbv: e.transpose(
                                tbv[:, pr * 128:(pr + 1) * 128], src[:, pr * 128:(pr + 1) * 128], ident[:]),
                                 reads=[src, ident], writes=[tb])
                        k.op(act, lambda e, dstb=dstb, tbv=tbv: e.activation(dstb[:], tbv[:, 0:512], AF.Copy),
                             reads=[tb], writes=[dstb])
                        k.dma(sp, dd[j], dstb[:], reads=[dstb], writes=[qTd_b], sembuf=dstb)
                    for c in range(4):
                        pu = k.bank()
                        puv = pu[:, 0:320].rearrange("p (a b) -> p a b", a=2)
                        for half, col0 in ((0, WU + c * 128), (1, WU + 512 + c * 128)):
                            for kc in range(8):
                                k.op(pe, lambda e, kc=kc, half=half, col0=col0, puv=puv: e.matmul(
                                    puv[:, half, 0:32], wB[:, kc, col0:col0 + 128], hTh[:, kc, :],
                                    start=(kc == 0), stop=(kc == 7)), reads=[wBu, hTh], writes=[pu])
                            for kc in range(8):
                                k.op(pe, lambda e, kc=kc, half=half, col0=col0, puv=puv: e.matmul(
                                    puv[:, half, 32:160], wB[:, kc, col0:col0 + 128], hT[:, kc, :],
                                    start=(kc == 0), stop=(kc == 7)), reads=[wBu, hT], writes=[pu])
                        k.op(act, lambda e, puv=puv: e.activation(sg[:], puv[:, 1, :], AF.Sigmoid),
                             reads=[pu], writes=[sg])
                        k.op(dve, lambda e, puv=puv, c=c: e.tensor_tensor(gT[:, c, :], puv[:, 0, :], sg[:], ALU.mult),
                             reads=[pu, sg], writes=[gT])
                    if j + 2 < nslots:
                        pre2(j + 2)
                    pc = k.bank()
                    pcv = pc[:, 0:512].rearrange("p (a b) -> p a b", a=4)
                    for c in range(4):
                        for jj in range(31):
                            k.op(pe, lambda e, c=c, jj=jj: e.matmul(pcv[:, c, :], Dg[:, c * 31 + jj, :],
                                                                    gT[:, c, 2 + jj:2 + jj + 128],
                                                                    start=(jj == 0), stop=(jj == 30)),
                                 reads=[Dg, gT], writes=[pc])
                    k.op(dve, lambda e: e.tensor_tensor(csb[:], pcv, bdw[:].unsqueeze(2).to_broadcast([128, 4, 128]),
                                                        ALU.add), reads=[pc, bdw], writes=[csb])
                    k.op(act, lambda e: e.activation(csq[:], csb[:], AF.Square), reads=[csb], writes=[csq])
                    pst = k.bank()
                    for c in range(4):
                        k.op(pe, lambda e, c=c: e.matmul(pst[:, 0:128], ones[:], csb[:, c, :], start=(c == 0),
                                                         stop=(c == 3)), reads=[ones, csb], writes=[pst])
                    for c in range(4):
                        k.op(pe, lambda e, c=c: e.matmul(pst[:, 128:256], ones[:], csq[:, c, :], start=(c == 0),
                                                         stop=(c == 3)), reads=[ones, csq], writes=[pst])
                    k.op(act, lambda e: e.activation(mean[:], pst[:, 0:128], AF.Copy, scale=1.0 / 512),
                         reads=[pst], writes=[mean])
                    k.op(dve, lambda e: e.tensor_tensor(msq[:], mean[:], mean[:], ALU.mult), reads=[mean], writes=[msq])
                    k.op(dve, lambda e: e.scalar_tensor_tensor(rstd[:], pst[:, 128:256], 1.0 / 512, msq[:],
                                                               ALU.mult, ALU.subtract),
                         reads=[pst, msq], writes=[rstd])
                    k.op(act, lambda e: e.activation(rstd[:], rstd[:], AF.Sqrt, bias=EPS, scale=1.0),
                         reads=[rstd], writes=[rstd])
                    k.op(dve, lambda e: e.reciprocal(rstd[:], rstd[:]), reads=[rstd], writes=[rstd])
                    k.op(dve, lambda e: e.tensor_tensor(nrm[:], csb[:], mean[:].unsqueeze(1).to_broadcast([128, 4, 128]),
                                                        ALU.subtract), reads=[csb, mean], writes=[nrm])
                    k.op(dve, lambda e: e.tensor_tensor(nrm[:], nrm[:], rstd[:].unsqueeze(1).to_broadcast([128, 4, 128]),
                                                        ALU.mult), reads=[nrm, rstd], writes=[nrm])
                    for c in range(4):
                        k.op(act, lambda e, c=c: e.activation(snT[:, c, :], nrm[:, c, :], AF.Silu,
                                                              bias=lnb[:, c:c + 1], scale=lng[:, c:c + 1]),
                             reads=[nrm, lnb, lng], writes=[snT])
                    for gi, (wcol, dst) in enumerate(((WGC, None), (WGA, sga_s))):
                        for hb_ in range(2):
                            pg = k.bank()
                            pgv = pg[:, 0:512].rearrange("p (a b) -> p a b", a=4)
                            for m in range(4):
                                col0 = wcol + (hb_ * 4 + m) * 128
                                for kc in range(8):
                                    k.op(pe, lambda e, kc=kc, m=m, col0=col0, pgv=pgv: e.matmul(
                                        pgv[:, m, :], wB[:, kc, col0:col0 + 128], hT[:, kc, :],
                                        start=(kc == 0), stop=(kc == 7)), reads=[wBg, hT], writes=[pg])
                            tgt = sgc if gi == 0 else sga_s
                            k.op(act, lambda e, pgv=pgv, tgt=tgt, hb_=hb_: e.activation(
                                tgt[:, hb_ * 4:(hb_ + 1) * 4, :], pgv, AF.Sigmoid), reads=[pg], writes=[tgt])
                    for hb_ in range(2):
                        py = k.bank()
                        pyv = py[:, 0:512].rearrange("p (a b) -> p a b", a=4)
                        for m in range(4):
                            mm = hb_ * 4 + m
                            for kc in range(4):
                                k.op(pe, lambda e, kc=kc, m=m, mm=mm, pyv=pyv: e.matmul(
                                    pyv[:, m, :], wco[:, kc, mm * 128:(mm + 1) * 128], snT[:, kc, :],
                                    start=(kc == 0), stop=(kc == 3)), reads=[wco, snT], writes=[py])
                        k.op(dve, lambda e, pyv=pyv, hb_=hb_: e.tensor_tensor(
                            mc_s[:, hb_ * 4:(hb_ + 1) * 4, :], pyv, sgc[:, hb_ * 4:(hb_ + 1) * 4, :], ALU.mult),
                             reads=[py, sgc], writes=[mc_s])
                    k.dma(sp, mc_d[j], mc_s[:].rearrange("p a b -> p (a b)"), reads=[mc_s], writes=[mcd_b], sembuf=mc_s)
                    k.dma(sp, sga_d[j], sga_s[:].rearrange("p a b -> p (a b)"), reads=[sga_s], writes=[mcd_b],
                          sembuf=sga_s)
                k.barrier()
        def dbg_A2():
            k.dma(sp, dbg["wi"], wi_all[:], reads=[wi_all])
            for nm, src in (("qT", qT_d), ("qiT", qiT_d), ("mc", mc_d), ("sga", sga_d)):
                b_ = Buf("dbgc_" + nm)
                k.dma(sp, dbg[nm], src, writes=[b_])
            k.barrier()

        def phase_B():
            with ExitStack() as s3:
                cm2 = [k.sb("cm%d" % i, [128, 256], F32, s3) for i in range(2)]
                pw2 = k.sb("pw2", [128, NIT + 2], F32, s3)
                for it in range(NIT + 2):
                    k.op(dve, lambda e, it=it: e.memset(pw2[:, it:it + 1], float(2.0 ** -(it + 1))), writes=[pw2])
                qT2 = [k.sb("qTb%d" % i, [128, 4, 256], BF16, s3) for i in range(2)]
                for b_ in qT2:
                    k.op(dve, lambda e: e.memset(b_[:], 0.0), writes=[b_])
                qiT2 = [k.sb("qiTb%d" % i, [128, 512], BF16, s3) for i in range(2)]
                scL = [k.sb("sc%d" % i, [128, S], F32, s3) for i in range(2)]
                AL = [k.sb("Ab%d" % i, [128, 512], BF16, s3) for i in range(4)]
                wabs = k.sb("wabs", [128, 8], F32, s3)
                sgn = k.sb("sgn", [128, 8], F32, s3)
                sgd = k.sb("sgd", [128, 8, 128], BF16, s3)
                msk = k.sb("msk", [128, S], BF16, s3)
                Mb = k.sb("Mb", [128, NB, 128], BF16, s3)
                st8 = k.sb("st8", [128, 8], F32, s3)
                Q = k.sb("Qtab", [128, NIT + 2], F32, s3)
                Q2 = k.sb("Q2tab", [128, NIT + 2], F32, s3)
                E2 = [k.sb("Eb%d" % i, [128, 4, 128], BF16, s3) for i in range(3)]
                rden = k.sb("rden", [128, 8], F32, s3)
                Osb = k.sb("Osb", [128, 2, 4, 66], F32, s3)
                thr = st8[:, 6:7]
                ai = [0]
                MBIG = 30000.0
                if debug:
                    attnf = k.sb("attnf", [128, 512], F32, s3)

                def loads_a(j):
                    p = j % 2
                    k.dma(sp, qiT2[p][:], qiT_d[j], writes=[qiT2[p]])
                    k.dma(sp, cm2[p][:], cmask_d[:, j, :], writes=[cm2[p]])

                def loads_b(j):
                    p = j % 2
                    k.dma(sp, qT2[p][0:64, :, 0:128], qT_d[j][0:64, :].rearrange("p (a b) -> p a b", a=4), writes=[qT2[p]])
                    k.dma(sp, qT2[p][64:128, :, 128:256], qT_d[j][64:128, :].rearrange("p (a b) -> p a b", a=4),
                          writes=[qT2[p]])

                def prep(j):
                    wsl = wi_all[:, j * 8:(j + 1) * 8]
                    k.op(dve, lambda e: e.tensor_scalar(sgn[:], wsl, 0.0, 2.0, ALU.is_ge, ALU.mult), reads=[wi_all], writes=[sgn])
                    k.op(dve, lambda e: e.tensor_scalar(sgn[:], sgn[:], -1.0, None, ALU.add), reads=[sgn], writes=[sgn])
                    k.op(dve, lambda e: e.tensor_tensor(wabs[:], wsl, sgn[:], ALU.mult), reads=[wi_all, sgn], writes=[wabs])
                    k.op(dve, lambda e: e.tensor_tensor(sgd[:], ident[:].unsqueeze(1).to_broadcast([128, 8, 128]),
                                                        sgn[:].unsqueeze(2).to_broadcast([128, 8, 128]), ALU.mult),
                         reads=[ident, sgn], writes=[sgd])

                def indexer(j):
                    sc = scL[j % 2]
                    qiTs = qiT2[j % 2]
                    L = 256 * (j + 1)
                    nch = (L + 511) // 512
                    for cc in range(nch):
                        c0 = cc * 512
                        W = min(512, L - c0)
                        pacc = k.bank(pin=True)
                        pbs = {}

                        def qi_mm(h):
                            pb = k.bank()
                            pp = (h % 2) * 64
                            k.op(pe, lambda e: e.matmul(pb[:, 0:W], qiTs[pp:pp + 64, (h // 2) * 128:(h // 2 + 1) * 128],
                                                        kiT[pp:pp + 64, c0:c0 + W], start=True, stop=True),
                                 reads=[qiTs, kiT], writes=[pb])
                            pbs[h] = pb

                        qi_mm(0)
                        qi_mm(1)
                        qi_mm(2)
                        for h in range(8):
                            if h + 3 < 8:
                                qi_mm(h + 3)
                            pb = pbs[h]
                            A = AL[ai[0] % 4]
                            ai[0] += 1
                            k.op(act, lambda e: e.activation(A[:, 0:W], pb[:, 0:W], AF.Relu, scale=wabs[:, h:h + 1]),
                                 reads=[pb, wabs], writes=[A])
                            k.op(pe, lambda e: e.matmul(pacc[:, 0:W], sgd[:, h, :], A[:, 0:W], start=(h == 0), stop=(h == 7)),
                                 reads=[sgd, A], writes=[pacc])
                        k.op(act, lambda e: e.activation(sc[:, c0:c0 + W], pacc[:, 0:W], AF.Copy), reads=[pacc], writes=[sc])
                        k.unpin(pacc)

                def thresh(j):
                    sc = scL[j % 2]
                    L = 256 * (j + 1)
                    cmask = cm2[j % 2]
                    if j == 0:
                        k.op(dve, lambda e: e.tensor_tensor(sc[:, L - 256:L], sc[:, L - 256:L], cmask[:], ALU.add),
                             reads=[sc, cmask], writes=[sc])
                        k.op(dve, lambda e: e.memset(thr, -1.0e29), writes=[st8])
                        return
                    k.op(dve, lambda e: e.tensor_reduce(st8[:, 0:1], sc[:, 0:L], AX.X, ALU.max), reads=[sc], writes=[st8])
                    k.op(dve, lambda e: e.tensor_reduce(st8[:, 1:2], sc[:, 0:L], AX.X, ALU.min), reads=[sc], writes=[st8])
                    k.op(dve, lambda e: e.tensor_tensor(sc[:, L - 256:L], sc[:, L - 256:L], cmask[:], ALU.add),
                         reads=[sc, cmask], writes=[sc])
                    k.op(dve, lambda e: e.tensor_tensor(st8[:, 2:3], st8[:, 0:1], st8[:, 1:2], ALU.subtract),
                         reads=[st8], writes=[st8])
                    k.op(dve, lambda e: e.tensor_scalar(st8[:, 5:6], st8[:, 2:3], float(2.0 ** -10), None, ALU.mult),
                         reads=[st8], writes=[st8])
                    k.op(dve, lambda e: e.tensor_scalar(st8[:, 2:3], st8[:, 2:3], float(1.0 + 2.0 ** -9), 1e-30,
                                                        ALU.mult, ALU.add), reads=[st8], writes=[st8])
                    k.op(dve, lambda e: e.tensor_tensor(st8[:, 7:8], st8[:, 1:2], st8[:, 5:6], ALU.subtract),
                         reads=[st8], writes=[st8])
                    k.op(dve, lambda e: e.tensor_scalar(Q[:], pw2[:], st8[:, 2:3], None, ALU.mult),
                         reads=[pw2, st8], writes=[Q])
                    k.op(dve, lambda e: e.tensor_scalar(Q2[:], Q[:], 2.0, None, ALU.mult), reads=[Q], writes=[Q2])
                    k.op(dve, lambda e: e.tensor_tensor(st8[:, 3:4], st8[:, 7:8], Q[:, 0:1], ALU.add),
                         reads=[st8, Q], writes=[st8])
                    for it in range(NIT):
                        k.op(dve, lambda e: e.memset(st8[:, 4:5], 0.0), writes=[st8])
                        k.op(dve, lambda e: e.tensor_scalar(msk[:, 0:L], sc[:, 0:L], st8[:, 3:4], 0.0,
                                                            ALU.is_ge, ALU.add, accum_out=st8[:, 4:5]),
                             reads=[sc, st8], writes=[st8, msk])
                        k.op(dve, lambda e: e.tensor_scalar(st8[:, 5:6], st8[:, 4:5], TOPK - 0.5,
                                                            Q2[:, it + 1:it + 2], ALU.is_ge, ALU.mult),
                             reads=[st8, Q2], writes=[st8])
                        k.op(dve, lambda e: e.scalar_tensor_tensor(st8[:, 3:4], st8[:, 3:4], Q[:, it + 1:it + 2],
                                                                   st8[:, 5:6], ALU.subtract, ALU.add),
                             reads=[st8, Q], writes=[st8])
                    k.op(dve, lambda e: e.tensor_tensor(thr, st8[:, 3:4], Q[:, NIT:NIT + 1], ALU.subtract),
                         reads=[st8, Q], writes=[st8])

                def mask(j):
                    sc = scL[j % 2]
                    L = 256 * (j + 1)
                    nkb = 2 * (j + 1)
                    if debug:
                        b_ = Buf("dbg_sc%d" % j)
                        for c0_ in range(0, L, 1024):
                            c1_ = min(L, c0_ + 1024)
                            k.dma(sp, dbg["sc"][j][:, c0_:c1_], sc[:, c0_:c1_], reads=[sc], writes=[b_], sembuf=b_)
                        k.dma(sp, dbg["thr"][j], thr, reads=[st8], writes=[b_], sembuf=b_)
                    for g0 in range(0, nkb, 8):
                        gn = min(8, nkb - g0)
                        k.op(dve, lambda e: e.tensor_scalar(msk[:, g0 * 128:(g0 + gn) * 128],
                                                            sc[:, g0 * 128:(g0 + gn) * 128], thr, None, ALU.is_ge),
                             reads=[sc, st8], writes=[msk])
                        tb = k.bank()
                        tbv = bf(tb[:])
                        for q_ in range(gn):
                            k.op(pe, lambda e: e.transpose(tbv[:, q_ * 128:(q_ + 1) * 128],
                                                           msk[:, (g0 + q_) * 128:(g0 + q_ + 1) * 128], ident[:]),
                                 reads=[msk, ident], writes=[tb])
                        k.op(act, lambda e: e.activation(Mb[:, g0:g0 + gn, :],
                                                         tbv[:, 0:gn * 128].rearrange("p (a b) -> p a b", a=gn),
                                                         AF.Identity, bias=-MBIG, scale=MBIG),
                             reads=[tb], writes=[Mb])

                def attention(j):
                    qTs = qT2[j % 2]
                    nkb = 2 * (j + 1)
                    po = [k.bank(pin=True), k.bank(pin=True)]
                    pov = [b_[:, 0:512].rearrange("p (h d) -> p h d", h=4) for b_ in po]
                    units = [(kb, hg) for kb in range(nkb) for hg in range(2)]

                    def qk(u):
                        kb, hg = units[u]
                        pl = k.bank()
                        plv = pl[:, 0:512].rearrange("p (a b) -> p a b", a=4)
                        k.op(pe, lambda e: e.matmul(plv, ident[:], Mb[:, kb, :].unsqueeze(1).to_broadcast([128, 4, 128]),
                                                    start=True, stop=False), reads=[ident, Mb], writes=[pl])
                        for pi in range(2):
                            pr = hg * 2 + pi
                            k.op(pe, lambda e: e.matmul(pl[:, pi * 256:(pi + 1) * 256], KT[:, pr, kb * 128:(kb + 1) * 128],
                                                        qTs[:, pr, :], start=False, stop=(pi == 1)),
                                 reads=[KT, qTs], writes=[pl])
                        return pl, plv

                    LA = 2
                    pend = [qk(u) for u in range(min(LA, len(units)))]
                    for u in range(len(units)):
                        if u + LA < len(units):
                            pend.append(qk(u + LA))
                        kb, hg = units[u]
                        pl, plv = pend.pop(0)
                        E = E2[u % 3]
                        k.op(act, lambda e: e.activation(E[:], plv, AF.Exp, scale=ATT_SCALE), reads=[pl], writes=[E])
                        for hh in range(4):
                            h = hg * 4 + hh
                            k.op(pe, lambda e: e.matmul(pov[hg][:, hh, 0:66], E[:, hh, :], V[:, kb, h * 66:(h + 1) * 66],
                                                        start=(kb == 0 and hh == 0), stop=(kb == nkb - 1 and hh == 3)),
                                 reads=[E, V], writes=[po[hg]])
                    for hg in range(2):
                        k.op(act, lambda e: e.activation(Osb[:, hg, :, :], pov[hg][:, :, 0:66], AF.Copy),
                             reads=[po[hg]], writes=[Osb])
                        k.unpin(po[hg])

                def finalize(j):
                    for hg in range(2):
                        k.op(dve, lambda e: e.reciprocal(rden[:, hg * 4:(hg + 1) * 4].unsqueeze(2), Osb[:, hg, :, 64:65]),
                             reads=[Osb], writes=[rden])
                        k.op(dve, lambda e: e.tensor_tensor(
                            attn_all[:, j, hg * 256:(hg + 1) * 256].rearrange("p (h d) -> p h d", h=4), Osb[:, hg, :, 0:64],
                            rden[:, hg * 4:(hg + 1) * 4].unsqueeze(2).to_broadcast([128, 4, 64]), ALU.mult),
                             reads=[Osb, rden], writes=[attn_all])
                    if debug:
                        k.op(dve, lambda e: e.tensor_copy(attnf[:], attn_all[:, j, :]), reads=[attn_all], writes=[attnf])
                        k.dma(sp, dbg["attn"][j], attnf[:], reads=[attnf])

                loads_a(0)
                loads_b(0)
                if nslots > 1:
                    loads_a(1)
                    loads_b(1)
                prep(0)
                indexer(0)
                thresh(0)
                mask(0)
                if nslots > 1:
                    prep(1)
                    indexer(1)
                for i in range(nslots):
                    if i + 2 < nslots:
                        prep(i + 2)
                    if i + 1 < nslots:
                        thresh(i + 1)
                    attention(i)
                    if i + 2 < nslots:
                        loads_a(i + 2)
                        indexer(i + 2)
                    if i + 1 < nslots:
                        mask(i + 1)
                    finalize(i)
                    if i + 2 < nslots:
                        loads_b(i + 2)
                k.barrier()

        def phase_B2():
            with ExitStack() as s3:
                wao = k.sb("wao", [128, 4, D], BF16, s3)
                wo = k.sb("wo", [128, 8, D], BF16, s3)
                wr = k.sb("wr", [128, 8, 20], BF16, s3)
                br = k.sb("br", [128, 20], F32, s3)
                for kc in range(4):
                    k.dma(pool, wao[:, kc, :], wao_d[kc * 128:(kc + 1) * 128, :], writes=[wao])
                for kc in range(8):
                    k.dma(pool, wo[:, kc, :], wo_d[kc * 128:(kc + 1) * 128, :], writes=[wo])
                k.dma(pool, wr[:], wr_d.rearrange("(k p) c -> p k c", p=128), writes=[wr])
                k.dma(sp, br[:], br_d, writes=[br])
                if "C" in phases:
                    wload(0)
                    wload(1)
                mc2 = [k.sb("mcb%d" % i, [128, 8, 128], BF16, s3) for i in range(2)]
                sga2 = [k.sb("sgab%d" % i, [128, 8, 128], BF16, s3) for i in range(2)]
                xoL = [k.sb("xob%d" % i, [128, D], F32, s3) for i in range(2)]
                attnTL = [k.sb("attnT%d" % i, [128, 4, 128], BF16, s3) for i in range(2)]
                mTL = [k.sb("mT%d" % i, [128, 8, 128], BF16, s3) for i in range(2)]
                x1L = [k.sb("x1_%d" % i, [128, D], F32, s3) for i in range(2)]
                lgA = k.sb("lgA", [128, NS, 20], F32, s3)
                rA = k.sb("rA", [128, 8, NS], F32, s3)
                dG = k.sb("dG", [128, NS, 4], F32, s3)
                eG = k.sb("eG", [128, NS, 4], F32, s3)
                ohg1 = k.sb("ohg1", [128, NS, 4], F32, s3)
                ohgp = k.sb("ohgp", [128, NS, 4], F32, s3)
                t16A = k.sb("t16A", [128, NS, 16], F32, s3)
                eselA = k.sb("eselA", [128, NS, 4], F32, s3)
                oh1A = k.sb("oh1A", [128, NS, 4], F32, s3)
                oh2A = k.sb("oh2A", [128, NS, 4], F32, s3)
                emA = k.sb("emA", [128, NS, 4], F32, s3)
                x1d_b = Buf("x1d_b")

                def loads_c(j):
                    p = j % 2
                    k.dma(sp, mc2[p][:].rearrange("p a b -> p (a b)"), mc_d[j], writes=[mc2[p]])
                    k.dma(sp, sga2[p][:].rearrange("p a b -> p (a b)"), sga_d[j], writes=[sga2[p]])
                    k.dma(sp, xoL[p][:], x_own[j * 128:(j + 1) * 128, :], writes=[xoL[p]])

                def tail(j):
                    mcs, sgas = mc2[j % 2], sga2[j % 2]
                    xo, x1, mT, attnT = xoL[j % 2], x1L[j % 2], mTL[j % 2], attnTL[j % 2]
                    tb = k.bank()
                    tbv = bf(tb[:])
                    for q_ in range(4):
                        k.op(pe, lambda e: e.transpose(tbv[:, q_ * 128:(q_ + 1) * 128],
                                                       attn_all[:, j, q_ * 128:(q_ + 1) * 128], ident[:]),
                             reads=[attn_all, ident], writes=[tb])
                    k.op(act, lambda e: e.activation(attnT[:], tbv[:, 0:512].rearrange("p (a b) -> p a b", a=4), AF.Copy),
                         reads=[tb], writes=[attnT])
                    x1v = x1[:].rearrange("p (a b) -> p a b", a=8)
                    for hb_ in range(2):
                        py = k.bank()
                        pyv = py[:, 0:512].rearrange("p (a b) -> p a b", a=4)
                        for m in range(4):
                            mm = hb_ * 4 + m
                            for kc in range(4):
                                k.op(pe, lambda e: e.matmul(pyv[:, m, :], wao[:, kc, mm * 128:(mm + 1) * 128], attnT[:, kc, :],
                                                            start=(kc == 0), stop=(kc == 3)), reads=[wao, attnT], writes=[py])
                        k.op(dve, lambda e: e.tensor_tensor(x1v[:, hb_ * 4:(hb_ + 1) * 4, :], pyv,
                                                            sgas[:, hb_ * 4:(hb_ + 1) * 4, :], ALU.mult),
                             reads=[py, sgas], writes=[x1])
                    k.op(dve, lambda e: e.tensor_tensor(mT[:], x1v, mcs[:], ALU.add), reads=[x1, mcs], writes=[mT])
                    for half in range(2):
                        px = k.bank()
                        for kc in range(8):
                            k.op(pe, lambda e: e.matmul(px[:, 0:512], mT[:, kc, :], wo[:, kc, half * 512:(half + 1) * 512],
                                                        start=(kc == 0), stop=(kc == 7)), reads=[mT, wo], writes=[px])
                        k.op(dve, lambda e: e.tensor_tensor(acc[:, j, half * 512:(half + 1) * 512], px[:, 0:512],
                                                            xo[:, half * 512:(half + 1) * 512], ALU.add),
                             reads=[px, xo], writes=[acc])
                    if debug:
                        k.dma(sp, dbg["x1"][j], acc[:, j, :], reads=[acc], sembuf=x1)
                    hbsT[j] = norm_pre(acc, 128, acc[:, j, :])

                def tailB(j):
                    norm_post(hbsT.pop(j), 128, gffn, h2T, h2T[:, :, j * 128:(j + 1) * 128])
                    pr_ = k.bank()
                    for kc in range(8):
                        k.op(pe, lambda e: e.matmul(pr_[:, 0:20], h2T[:, kc, j * 128:(j + 1) * 128], wr[:, kc, :],
                                                    start=(kc == 0), stop=(kc == 7)), reads=[h2T, wr], writes=[pr_])
                    k.op(dve, lambda e: e.tensor_tensor(lgA[:, j, :], pr_[:, 0:20], br[:], ALU.add),
                         reads=[pr_, br], writes=[lgA])

                def router_all():
                    n = nslots
                    G = lgA[:, 0:n, 0:4]
                    Ex = lgA[:, 0:n, 4:20].rearrange("p n (g e) -> p n g e", g=4)
                    gmax, gsum, pg, m1, m2, w1, w2 = (rA[:, i, 0:n] for i in range(7))
                    bc4 = lambda ap: ap.unsqueeze(2).to_broadcast([128, n, 4])
                    k.op(dve, lambda e: e.tensor_reduce(gmax, G, AX.X, ALU.max), reads=[lgA], writes=[rA])
                    k.op(dve, lambda e: e.tensor_tensor(dG[:, 0:n, :], G, bc4(gmax), ALU.subtract), reads=[lgA, rA], writes=[dG])
                    k.op(act, lambda e: e.activation(eG[:, 0:n, :], dG[:, 0:n, :], AF.Exp), reads=[dG], writes=[eG])
                    k.op(dve, lambda e: e.tensor_reduce(gsum, eG[:, 0:n, :], AX.X, ALU.add), reads=[eG], writes=[rA])
                    k.op(dve, lambda e: e.reciprocal(pg, gsum), reads=[rA], writes=[rA])
                    k.op(dve, lambda e: e.tensor_scalar(ohg1[:, 0:n, :], dG[:, 0:n, :], 0.0, None, ALU.is_ge), reads=[dG], writes=[ohg1])
                    k.op(dve, lambda e: e.tensor_tensor(ohgp[:, 0:n, :], ohg1[:, 0:n, :], bc4(pg), ALU.mult),
                         reads=[ohg1, rA], writes=[ohgp])
                    t4 = t16A[:, 0:n, :].rearrange("p n (g e) -> p n g e", g=4)
                    k.op(dve, lambda e: e.tensor_tensor(t4, Ex, ohg1[:, 0:n, :].unsqueeze(3).to_broadcast([128, n, 4, 4]), ALU.mult),
                         reads=[lgA, ohg1], writes=[t16A])
                    k.op(dve, lambda e: e.tensor_reduce(eselA[:, 0:n, :], t16A[:, 0:n, :].rearrange("p n (g e) -> p n e g", g=4),
                                                        AX.X, ALU.add), reads=[t16A], writes=[eselA])
                    k.op(dve, lambda e: e.tensor_reduce(m1, eselA[:, 0:n, :], AX.X, ALU.max), reads=[eselA], writes=[rA])
                    k.op(dve, lambda e: e.tensor_tensor(dG[:, 0:n, :], eselA[:, 0:n, :], bc4(m1), ALU.subtract),
                         reads=[eselA, rA], writes=[dG])
                    k.op(dve, lambda e: e.tensor_scalar(oh1A[:, 0:n, :], dG[:, 0:n, :], 0.0, None, ALU.is_ge), reads=[dG], writes=[oh1A])
                    k.op(dve, lambda e: e.scalar_tensor_tensor(emA[:, 0:n, :], oh1A[:, 0:n, :], NEG, eselA[:, 0:n, :],
                                                               ALU.mult, ALU.add), reads=[oh1A, eselA], writes=[emA])
                    k.op(dve, lambda e: e.tensor_reduce(m2, emA[:, 0:n, :], AX.X, ALU.max), reads=[emA], writes=[rA])
                    k.op(dve, lambda e: e.tensor_tensor(dG[:, 0:n, :], emA[:, 0:n, :], bc4(m2), ALU.subtract),
                         reads=[emA, rA], writes=[dG])
                    k.op(dve, lambda e: e.tensor_scalar(oh2A[:, 0:n, :], dG[:, 0:n, :], 0.0, None, ALU.is_ge), reads=[dG], writes=[oh2A])
                    k.op(dve, lambda e: e.tensor_tensor(w2, m1, m2, ALU.subtract), reads=[rA], writes=[rA])
                    k.op(act, lambda e: e.activation(w1, w2, AF.Sigmoid), reads=[rA], writes=[rA])
                    k.op(dve, lambda e: e.tensor_scalar(w2, w1, -1.0, 1.0, ALU.mult, ALU.add), reads=[rA], writes=[rA])
                    k.op(dve, lambda e: e.tensor_tensor(oh1A[:, 0:n, :], oh1A[:, 0:n, :], bc4(w1), ALU.mult),
                         reads=[oh1A, rA], writes=[oh1A])
                    k.op(dve, lambda e: e.tensor_tensor(oh2A[:, 0:n, :], oh2A[:, 0:n, :], bc4(w2), ALU.mult),
                         reads=[oh2A, rA], writes=[oh2A])
                    k.op(dve, lambda e: e.tensor_tensor(oh1A[:, 0:n, :], oh1A[:, 0:n, :], oh2A[:, 0:n, :], ALU.add),
                         reads=[oh1A, oh2A], writes=[oh1A])
                    k.op(dve, lambda e: e.tensor_tensor(
                        gw_all[:, 0:n * 16].rearrange("p (n g e) -> p n g e", g=4, e=4),
                        ohgp[:, 0:n, :].unsqueeze(3).to_broadcast([128, n, 4, 4]),
                        oh1A[:, 0:n, :].unsqueeze(2).to_broadcast([128, n, 4, 4]), ALU.mult),
                         reads=[ohgp, oh1A], writes=[gw_all])

                hbsT = {}
                loads_c(0)
                if nslots > 1:
                    loads_c(1)
                tail(0)
                for j in range(nslots):
                    if j + 1 < nslots:
                        tail(j + 1)
                    tailB(j)
                    if j + 2 < nslots:
                        loads_c(j + 2)
                router_all()
                k.barrier()
        def phase_C():
            with ExitStack() as s4:
                gfin = k.sb("gfin", [128, D], F32, s4)
                k.dma(sp, gfin[:], gfin_d, writes=[gfin])
                sgl2 = [k.sb("sgm%d" % i, [128, 512], F32, s4) for i in range(2)]
                hid2 = [k.sb("hid%d" % i, [128, 2, 512], BF16, s4) for i in range(2)]
                ot2 = [k.sb("ot%d" % i, [128, D], F32, s4) for i in range(2)]

                ngrp = (nslots + 3) // 4
                wload(0)
                hi = 0
                for ex in range(16):
                    if ex + 1 < 16:
                        wload(ex + 1)
                    p = ex % 2
                    wg, wu, wd = wg2[p], wu2[p], wd2[p]
                    for tg in range(ngrp):
                        ns_ = min(4, nslots - tg * 4)
                        N = ns_ * 128
                        hid = hid2[hi % 2]
                        hi += 1
                        for fc in range(2):
                            pg = k.bank()
                            pu = k.bank()
                            for kc in range(8):
                                k.op(pe, lambda e, kc=kc, fc=fc, pg=pg: e.matmul(
                                    pg[:, 0:N], wg[:, kc, fc * 128:(fc + 1) * 128], h2T[:, kc, tg * 512:tg * 512 + N],
                                    start=(kc == 0), stop=(kc == 7)), reads=[wg, h2T], writes=[pg])
                            for kc in range(8):
                                k.op(pe, lambda e, kc=kc, fc=fc, pu=pu: e.matmul(
                                    pu[:, 0:N], wu[:, kc, fc * 128:(fc + 1) * 128], h2T[:, kc, tg * 512:tg * 512 + N],
                                    start=(kc == 0), stop=(kc == 7)), reads=[wu, h2T], writes=[pu])
                            sgl = sgl2[fc]
                            k.op(act, lambda e, pg=pg, sgl=sgl: e.activation(sgl[:, 0:N], pg[:, 0:N], AF.Silu),
                                 reads=[pg], writes=[sgl])
                            k.op(dve, lambda e, pu=pu, sgl=sgl, fc=fc, hid=hid: e.tensor_tensor(
                                hid[:, fc, 0:N], pu[:, 0:N], sgl[:, 0:N], ALU.mult), reads=[pu, sgl], writes=[hid])
                        for s_ in range(ns_):
                            j = tg * 4 + s_
                            for half in range(2):
                                py = k.bank()
                                for fc in range(2):
                                    k.op(pe, lambda e, fc=fc, half=half, py=py, s_=s_, hid=hid: e.matmul(
                                        py[:, 0:512], hid[:, fc, s_ * 128:(s_ + 1) * 128],
                                        wd[:, fc, half * 512:(half + 1) * 512], start=(fc == 0), stop=(fc == 1)),
                                         reads=[hid, wd], writes=[py])
                                k.op(dve, lambda e, py=py, j=j, half=half, ex=ex: e.scalar_tensor_tensor(
                                    acc[:, j, half * 512:(half + 1) * 512], py[:, 0:512],
                                    gw_all[:, j * 16 + ex:j * 16 + ex + 1], acc[:, j, half * 512:(half + 1) * 512],
                                    ALU.mult, ALU.add), reads=[py, gw_all, acc], writes=[acc])
                for j in range(nslots):
                    ot = ot2[j % 2]
                    ss, rs, junk = ssL[j % 2], rsL[j % 2], hbL[j % 2]
                    k.op(dve, lambda e, ss=ss: e.memset(ss[:], 0.0), writes=[ss])
                    k.op(act, lambda e, j=j, junk=junk, ss=ss: e.activation(junk[:], acc[:, j, :], AF.Square, accum_out=ss[:, 0:1]),
                         reads=[acc, ss], writes=[junk, ss])
                    k.op(act, lambda e, rs=rs, ss=ss: e.activation(rs[:], ss[:], AF.Sqrt, bias=EPS, scale=1.0 / D), reads=[ss], writes=[rs])
                    k.op(dve, lambda e, rs=rs: e.reciprocal(rs[:], rs[:]), reads=[rs], writes=[rs])
                    k.op(dve, lambda e, j=j, ot=ot, rs=rs: e.scalar_tensor_tensor(ot[:], acc[:, j, :], rs[:, 0:1], gfin[:],
                                                                          ALU.mult, ALU.mult),
                         reads=[acc, rs, gfin], writes=[ot])
                    k.dma(sp, out_d[j * 128:(j + 1) * 128, :], ot[:], reads=[ot], sembuf=ot)
                k.barrier()
        if "A2" in phases:
            phase_A2()
            if debug:
                dbg_A2()
        attn_all = k.sb("attn_all", [128, NS, 512], BF16)
        wg2 = [k.sb("wg%d" % i, [128, 8, 256], BF16) for i in range(2)]
        wu2 = [k.sb("wu%d" % i, [128, 8, 256], BF16) for i in range(2)]
        wd2 = [k.sb("wd%d" % i, [128, 2, D], BF16) for i in range(2)]
        wloaded = set()

        def wload(ex):
            if ex in wloaded:
                return
            wloaded.add(ex)
            p = ex % 2
            k.dma(pool, wg2[p][:], wg_d[ex].rearrange("(k p) c -> p k c", p=128), writes=[wg2[p]])
            k.dma(pool, wu2[p][:], wu_d[ex].rearrange("(k p) c -> p k c", p=128), writes=[wu2[p]])
            k.dma(pool, wd2[p][:], wd_d[ex].rearrange("(k p) c -> p k c", p=128), writes=[wd2[p]])

        with ExitStack() as skv:
            KT = k.sb("KT", [128, 4, S], BF16, skv)
            V = k.sb("V", [128, NB, 8 * 66], BF16, skv)
            kiT = k.sb("kiT", [128, S], BF16, skv)
            k.op(pool, lambda e: e.memset(V[:], 1.0), writes=[V])
            if "A1" in phases:
                phase_A1()
                if debug:
                    dbg_A1()
            if "B" in phases:
                phase_B()
            k.barrier()
        acc = k.sb("acc", [128, NS, D], F32)
        h2T = k.sb("h2T", [128, 8, NS * 128], BF16)
        if "B" in phases:
            phase_B2()
            if debug:
                k.dma(sp, dbg["gw"], gw_all[:], reads=[gw_all])
        if "C" in phases:
            phase_C()
        k.barrier()
        build_nc.nins = k.nins
    return nc


def own_blocks(r):
    return [2 * j + ((j % 2) ^ r) for j in range(NS)]


def prep_core(inp, c):
    b, r = c // 2, c % 2
    x = np.asarray(inp["x"], dtype=np.float32)
    pos = np.asarray(inp["positions"]).astype(np.int32)
    blocks = own_blocks(r)
    xb = x[b]
    x_own = np.concatenate([xb[i * 128:(i + 1) * 128] for i in blocks], axis=0)
    x_halo = np.zeros((NS * 32, D), np.float32)
    for j, i in enumerate(blocks):
        if i > 0:
            x_halo[j * 32:(j + 1) * 32] = xb[i * 128 - 32:i * 128]
    pos_full = np.ascontiguousarray(pos[b].reshape(NB, 128).T)
    pos_own = np.ascontiguousarray(np.stack([pos[b][i * 128:(i + 1) * 128] for i in blocks], axis=1))
    invf = (1.0 / (np.float32(10000.0) ** (np.arange(0, 64, 2, dtype=np.float32) / np.float32(64)))).astype(np.float32)
    invf = np.ascontiguousarray(np.broadcast_to(invf[None, :], (128, 32)))
    cmask = np.zeros((128, NS, 256), np.float32)
    for j, i in enumerate(blocks):
        qidx = i * 128 + np.arange(128)[:, None]
        kidx = 256 * j + np.arange(256)[None, :]
        cmask[:, j, :] = np.where(kidx > qidx, np.float32(NEG), np.float32(0.0))

    def fm(v, nchunk):
        return np.ascontiguousarray(np.asarray(v, np.float32).reshape(nchunk, 128).T)

    w_dw = np.asarray(inp["w_dw"], np.float32)[0, :, 0, :]
    wdw = np.ascontiguousarray(w_dw.T.reshape(4, 128, 31).transpose(1, 0, 2))
    w_r = np.concatenate([np.asarray(inp["w_rg"], np.float32)[0],
                          np.asarray(inp["w_re"], np.float32)[0].reshape(D, 16)], axis=1)
    b_r = np.concatenate([np.asarray(inp["b_rg"], np.float32)[0], np.asarray(inp["b_re"], np.float32)[0].reshape(16)])
    m = {
        "x_full": np.ascontiguousarray(xb), "x_own": x_own, "x_halo": x_halo,
        "pos_full": pos_full, "pos_own": pos_own, "invf": invf, "cmask": cmask,
        "ident": np.eye(128, dtype=np.float32),
        "w_in": np.ascontiguousarray(np.asarray(inp["w_in"], np.float32)[0]),
        "wdw": wdw, "bdw": fm(inp["b_dw"][0], 4), "lng": fm(inp["ln_g"][0], 4), "lnb": fm(inp["ln_b"][0], 4),
        "gmix": fm(inp["g_mix"][0], 8), "gffn": fm(inp["g_ffn"][0], 8),
        "gfin": np.ascontiguousarray(np.broadcast_to(np.asarray(inp["g_final"], np.float32)[None, :], (128, D))),
        "b_r": np.ascontiguousarray(np.broadcast_to(b_r[None, :], (128, 20))),
        "w_r": np.ascontiguousarray(w_r),
        "w_conv_out": np.ascontiguousarray(np.asarray(inp["w_conv_out"], np.float32)[0]),
        "w_attn_out": np.ascontiguousarray(np.asarray(inp["w_attn_out"], np.float32)[0]),
        "w_o": np.ascontiguousarray(np.asarray(inp["w_o"], np.float32)[0]),
        "w_gate": np.ascontiguousarray(np.asarray(inp["w_gate"], np.float32)[0]),
        "w_up": np.ascontiguousarray(np.asarray(inp["w_up"], np.float32)[0]),
        "w_down": np.ascontiguousarray(np.asarray(inp["w_down"], np.float32)[0]),
    }
    return m


_NC_CACHE = {}


def kernel(**inputs):
    if "nc" not in _NC_CACHE:
        _NC_CACHE["nc"] = build_nc()
    nc = _NC_CACHE["nc"]
    in_maps = [prep_core(inputs, c) for c in range(8)]
    res = run_bass_kernel_spmd(nc, in_maps, core_ids=list(range(8)))
    out = np.empty((4, S, D), np.float32)
    for c in range(8):
        b, r = c // 2, c % 2
        o = np.asarray(res.results[c]["out"], dtype=np.float32)
        for j, i in enumerate(own_blocks(r)):
            out[b, i * 128:(i + 1) * 128] = o[j * 128:(j + 1) * 128]
    return out
```

```python
from contextlib import ExitStack
import numpy as np
import concourse.bass as bass
import concourse.mybir as mybir
from concourse.bass_utils import run_bass_kernel_spmd

F32 = mybir.dt.float32
BF16 = mybir.dt.bfloat16
I32 = mybir.dt.int32
ALU = mybir.AluOpType
AF = mybir.ActivationFunctionType
AX = mybir.AxisListType

D = 1024
S = 4096
NB = 32
NS = 16
EPS = 1e-6
NIT = 14
TOPK = 256
NEG = -1.0e30
C_U, C_Q, C_K, C_V, C_QI, C_KI, C_WI, C_GC, C_GA = 0, 1024, 1536, 2048, 2560, 3072, 3136, 3144, 4168
IDX_SCALE = float((8 ** -0.5) * (64 ** -0.5))
ATT_SCALE = float(64 ** -0.5)


class Buf:
    __slots__ = ("name", "t", "lw", "rd", "dsem", "dcnt")

    def __init__(self, name, t=None):
        self.name = name
        self.t = t
        self.lw = None
        self.rd = {}
        self.dsem = None
        self.dcnt = 0

    def __getitem__(self, key):
        return self.t[key]


class Eng:
    def __init__(self, k, name, eng):
        self.name = name
        self.eng = eng
        self.sem = k.new_sem("e_" + name)
        self.cnt = 0
        self.seen = {}

    def wait_tok(self, tok):
        if tok is None:
            return
        sem, val, _ = tok
        key = id(sem)
        if self.seen.get(key, 0) >= val:
            return
        self.eng.wait_ge(sem, val)
        self.seen[key] = val


class K:
    def __init__(self, nc, stack):
        self.nc = nc
        self.stack = stack
        self.pe = Eng(self, "pe", nc.tensor)
        self.act = Eng(self, "act", nc.scalar)
        self.dve = Eng(self, "dve", nc.vector)
        self.pool = Eng(self, "pool", nc.gpsimd)
        self.sp = Eng(self, "sp", nc.sync)
        self.engs = [self.pe, self.act, self.dve, self.pool, self.sp]
        self.dma_bufs = []
        self.nins = 0
        self.banks = []
        self.bank_i = 0
        self.pinned = set()

    def new_sem(self, name):
        return self.stack.enter_context(self.nc.semaphore(name))

    def sb(self, name, shape, dt, stack=None):
        t = (stack or self.stack).enter_context(self.nc.sbuf_tensor("s_" + name, list(shape), dt))
        return Buf(name, t)

    def mk_banks(self):
        for i in range(8):
            t = self.stack.enter_context(self.nc.psum_tensor("bank%d" % i, [128, 512], F32))
            self.banks.append(Buf("bank%d" % i, t))

    def bank(self, pin=False):
        for _ in range(16):
            b = self.banks[self.bank_i % 8]
            self.bank_i += 1
            if b.name not in self.pinned:
                if pin:
                    self.pinned.add(b.name)
                return b
        raise RuntimeError("no psum bank")

    def unpin(self, b):
        self.pinned.discard(b.name)

    def _deps(self, e, reads, writes):
        for b in reads:
            if b.lw is not None:
                e.wait_tok(b.lw)
        for b in writes:
            if b.lw is not None and b.lw[2] != e.name:
                e.wait_tok(b.lw)
            for en, tok in b.rd.items():
                if en != e.name:
                    e.wait_tok(tok)

    def _commit(self, tok, reads, writes):
        for b in reads:
            b.rd[tok[2]] = tok
        for b in writes:
            b.lw = tok
            b.rd = {}

    def op(self, e, fn, reads=(), writes=()):
        self._deps(e, reads, writes)
        ins = fn(e.eng)
        e.cnt += 1
        ins.then_inc(e.sem, 1)
        tok = (e.sem, e.cnt, e.name)
        self._commit(tok, reads, writes)
        self.nins += 1
        return tok

    def dma(self, e, out, in_, reads=(), writes=(), sembuf=None):
        self._deps(e, reads, writes)
        sb_ = sembuf if sembuf is not None else (writes[0] if writes else reads[0])
        if sb_.dsem is None:
            sb_.dsem = self.new_sem("d_" + sb_.name)
            self.dma_bufs.append(sb_)
        ins = e.eng.dma_start(out=out, in_=in_)
        sb_.dcnt += 16
        ins.then_inc(sb_.dsem, 16)
        tok = (sb_.dsem, sb_.dcnt, "dma_" + sb_.name)
        self._commit(tok, reads, writes)
        self.nins += 1
        return tok

    def barrier(self):
        toks = [(e.sem, e.cnt, e.name) for e in self.engs if e.cnt > 0]
        toks += [(b.dsem, b.dcnt, "dma") for b in self.dma_bufs]
        for e in self.engs:
            for t in toks:
                if t[2] != e.name:
                    e.wait_tok(t)


def bf(ap):
    return ap.bitcast(BF16)


def build_nc(phases=("A1", "A2", "B", "C"), debug=False, nslots=NS, bstop=9):
    nc = bass.Bass("TRN2", target_bir_lowering=False)

    def din(name, shape, dt=F32):
        return nc.dram_tensor(name, list(shape), dt, kind="ExternalInput").ap()

    def dscr(name, shape, dt):
        return nc.dram_tensor(name, list(shape), dt, kind="Internal").ap()

    x_full = din("x_full", [S, D])
    x_own = din("x_own", [NS * 128, D])
    x_halo = din("x_halo", [NS * 32, D])
    pos_full = din("pos_full", [128, NB], I32)
    pos_own = din("pos_own", [128, NS], I32)
    invf = din("invf", [128, 32])
    cmask_d = din("cmask", [128, NS, 256])
    ident_d = din("ident", [128, 128])
    w_in = din("w_in", [D, 5192])
    wdw_d = din("wdw", [128, 4, 31])
    bdw_d = din("bdw", [128, 4])
    lng_d = din("lng", [128, 4])
    lnb_d = din("lnb", [128, 4])
    gmix_d = din("gmix", [128, 8])
    gffn_d = din("gffn", [128, 8])
    gfin_d = din("gfin", [128, D])
    br_d = din("b_r", [128, 20])
    wr_d = din("w_r", [D, 20])
    wco_d = din("w_conv_out", [512, D])
    wao_d = din("w_attn_out", [512, D])
    wo_d = din("w_o", [D, D])
    wg_d = din("w_gate", [16, D, 256])
    wu_d = din("w_up", [16, D, 256])
    wd_d = din("w_down", [16, 256, D])
    out_d = nc.dram_tensor("out", [NS * 128, D], F32, kind="ExternalOutput").ap()

    qT_d = dscr("qT_d", [NS, 128, 512], BF16)
    qiT_d = dscr("qiT_d", [NS, 128, 512], BF16)
    mc_d = dscr("mc_d", [NS, 128, 1024], BF16)
    sga_d = dscr("sga_d", [NS, 128, 1024], BF16)
    x1_d = dscr("x1_d", [NS, 128, D], F32)
    h2T_d = dscr("h2T_d", [NS, 128, 1024], BF16)
    dbg = {}
    if debug:
        dbg["kT"] = nc.dram_tensor("dbg_kT", [128, 4, S], BF16, kind="ExternalOutput").ap()
        dbg["v"] = nc.dram_tensor("dbg_v", [128, NB, 8 * 66], BF16, kind="ExternalOutput").ap()
        dbg["kiT"] = nc.dram_tensor("dbg_kiT", [128, S], BF16, kind="ExternalOutput").ap()
        dbg["qT"] = nc.dram_tensor("dbg_qT", [NS, 128, 512], BF16, kind="ExternalOutput").ap()
        dbg["qiT"] = nc.dram_tensor("dbg_qiT", [NS, 128, 512], BF16, kind="ExternalOutput").ap()
        dbg["mc"] = nc.dram_tensor("dbg_mc", [NS, 128, 1024], BF16, kind="ExternalOutput").ap()
        dbg["sga"] = nc.dram_tensor("dbg_sga", [NS, 128, 1024], BF16, kind="ExternalOutput").ap()
        dbg["wi"] = nc.dram_tensor("dbg_wi", [128, NS * 8], F32, kind="ExternalOutput").ap()
        dbg["x1"] = nc.dram_tensor("dbg_x1", [NS, 128, D], F32, kind="ExternalOutput").ap()
        dbg["gw"] = nc.dram_tensor("dbg_gw", [128, NS * 16], F32, kind="ExternalOutput").ap()
        dbg["sc"] = nc.dram_tensor("dbg_sc", [NS, 128, S], F32, kind="ExternalOutput").ap()
        dbg["thr"] = nc.dram_tensor("dbg_thr", [NS, 128, 1], F32, kind="ExternalOutput").ap()
        dbg["attn"] = nc.dram_tensor("dbg_attn", [NS, 128, 512], F32, kind="ExternalOutput").ap()

    with ExitStack() as st:
        k = K(nc, st)
        pe, act, dve, pool, sp = k.pe, k.act, k.dve, k.pool, k.sp
        k.mk_banks()

        ident = k.sb("ident", [128, 128], BF16)
        identf = k.sb("identf", [128, 128], F32)
        wi_all = k.sb("wi_all", [128, NS * 8], F32)
        gw_all = k.sb("gw_all", [128, NS * 16], F32)
        gmix = k.sb("gmix", [128, 8], F32)
        gffn = k.sb("gffn", [128, 8], F32)
        ssL = [k.sb("ss%d" % i, [128, 1], F32) for i in range(2)]
        rsL = [k.sb("rs%d" % i, [128, 1], F32) for i in range(2)]
        hbL = [k.sb("hb%d" % i, [128, D], BF16) for i in range(2)]
        nrm_i = [0]

        k.dma(sp, identf[:], ident_d, writes=[identf])
        k.dma(pool, ident[:], ident_d, writes=[ident])
        k.dma(sp, gmix[:], gmix_d, writes=[gmix])
        k.dma(sp, gffn[:], gffn_d, writes=[gffn])

        def rope_tables(pos_d, n, cosT, sinT, stk):
            pi_ = k.sb("pos_i%d" % n, [128, n], I32, stk)
            pf = k.sb("pos_f%d" % n, [128, n], F32, stk)
            iv = k.sb("invf%d" % n, [128, 32], F32, stk)
            ang = k.sb("ang%d" % n, [128, n, 32], F32, stk)
            tmp = k.sb("angt%d" % n, [128, n, 32], F32, stk)
            k.dma(sp, pi_[:], pos_d, writes=[pi_])
            k.dma(sp, iv[:], invf, writes=[iv])
            k.op(dve, lambda e: e.tensor_copy(pf[:], pi_[:]), reads=[pi_], writes=[pf])
            k.op(dve, lambda e: e.tensor_tensor(ang[:], pf[:].unsqueeze(2).to_broadcast([128, n, 32]),
                                                iv[:].unsqueeze(1).to_broadcast([128, n, 32]), ALU.mult),
                 reads=[pf, iv], writes=[ang])
            ki_ = k.sb("angk%d" % n, [128, n, 32], I32, stk)
            kf_ = k.sb("angf%d" % n, [128, n, 32], F32, stk)
            two_pi = float(2 * np.pi)
            C1 = 6.28125
            C2 = float(2 * np.pi - 6.28125)
            PI_SAFE = 3.1415925

            def sin_of(dst, shift):
                if shift != 0.0:
                    k.op(dve, lambda e: e.tensor_scalar(tmp[:], ang[:], shift, None, ALU.add), reads=[ang], writes=[tmp])
                    src = tmp
                else:
                    src = ang
                k.op(dve, lambda e: e.tensor_scalar(kf_[:], src[:], 1.0 / two_pi, None, ALU.mult),
                     reads=[src], writes=[kf_])
                k.op(dve, lambda e: e.tensor_copy(ki_[:], kf_[:]), reads=[kf_], writes=[ki_])
                k.op(dve, lambda e: e.tensor_copy(kf_[:], ki_[:]), reads=[ki_], writes=[kf_])
                k.op(dve, lambda e: e.scalar_tensor_tensor(tmp[:], kf_[:], -C1, src[:], ALU.mult, ALU.add),
                     reads=[kf_, src], writes=[tmp])
                k.op(dve, lambda e: e.scalar_tensor_tensor(tmp[:], kf_[:], -C2, tmp[:], ALU.mult, ALU.add),
                     reads=[kf_, tmp], writes=[tmp])
                k.op(dve, lambda e: e.tensor_scalar(kf_[:], tmp[:], float(np.pi), -two_pi, ALU.is_gt, ALU.mult),
                     reads=[tmp], writes=[kf_])
                k.op(dve, lambda e: e.tensor_tensor(tmp[:], tmp[:], kf_[:], ALU.add), reads=[tmp, kf_], writes=[tmp])
                k.op(dve, lambda e: e.tensor_scalar(kf_[:], tmp[:], -float(np.pi), two_pi, ALU.is_lt, ALU.mult),
                     reads=[tmp], writes=[kf_])
                k.op(dve, lambda e: e.tensor_tensor(tmp[:], tmp[:], kf_[:], ALU.add), reads=[tmp, kf_], writes=[tmp])
                k.op(dve, lambda e: e.tensor_scalar(tmp[:], tmp[:], -PI_SAFE, PI_SAFE, ALU.max, ALU.min),
                     reads=[tmp], writes=[tmp])
                k.op(act, lambda e: e.activation(dst[:], tmp[:], AF.Sin), reads=[tmp], writes=[dst])

            sin_of(sinT, 0.0)
            sin_of(cosT, float(0.5 * np.pi))

        hbN = 4
        hbX = [k.sb("hbx%d" % i, [128, D], BF16) for i in range(hbN - 2)]

        def norm_pre(xt, n, src=None):
            if src is None:
                src = xt[0:n, :]
            hbs = hbL + hbX
            ss, rs, hb = ssL[nrm_i[0] % 2], rsL[nrm_i[0] % 2], hbs[nrm_i[0] % hbN]
            junk = hb
            nrm_i[0] += 1
            k.op(dve, lambda e: e.memset(ss[0:n, :], 0.0), writes=[ss])
            k.op(act, lambda e: e.activation(junk[0:n, :], src, AF.Square, accum_out=ss[0:n, 0:1]),
                 reads=[xt, ss], writes=[junk, ss])
            k.op(act, lambda e: e.activation(rs[0:n, :], ss[0:n, :], AF.Sqrt, bias=EPS, scale=1.0 / D),
                 reads=[ss], writes=[rs])
            k.op(dve, lambda e: e.reciprocal(rs[0:n, :], rs[0:n, :]), reads=[rs], writes=[rs])
            k.op(act, lambda e: e.activation(hb[0:n, :], src, AF.Copy, scale=rs[0:n, 0:1]),
                 reads=[xt, rs], writes=[hb])
            return hb

        def norm_post(hb, n, gT, hT, hT_ap=None):
            if hT_ap is None:
                hT_ap = hT[:, :, 0:n]
            tp = k.bank()
            tpv = bf(tp[:])[:, 0:8 * 128].rearrange("p (a b) -> p a b", a=8)
            for kc in range(8):
                k.op(pe, lambda e, kc=kc: e.transpose(tpv[:, kc, 0:n], hb[0:n, kc * 128:(kc + 1) * 128],
                                                      ident[0:n, 0:n]),
                     reads=[hb, ident], writes=[tp])
            k.op(dve, lambda e: e.tensor_tensor(hT_ap, tpv[:, :, 0:n],
                                                gT[:].unsqueeze(2).to_broadcast([128, 8, n]), ALU.mult),
                 reads=[tp, gT], writes=[hT])

        def norm_T(xt, n, gT, hT, hT_ap=None):
            hb = norm_pre(xt, n)
            norm_post(hb, n, gT, hT, hT_ap)

        def proj_tok(hT, w, c0, ncols):
            pb = k.bank()
            for kc in range(8):
                k.op(pe, lambda e, kc=kc: e.matmul(pb[:, 0:ncols], hT[:, kc, :], w[:, kc, c0:c0 + ncols],
                                                   start=(kc == 0), stop=(kc == 7)),
                     reads=[hT, w], writes=[pb])
            return pb

        def rope(pb, nh, cosT, sinT, ti, outb, tmpA, tmpB):
            pv = pb[:, 0:nh * 64].rearrange("p (h d) -> p h d", h=nh)
            ov = outb[:, 0:nh * 64].rearrange("p (h d) -> p h d", h=nh)
            av = tmpA[:, 0:nh * 64].rearrange("p (h d) -> p h d", h=nh)
            bv = tmpB[:, 0:nh * 64].rearrange("p (h d) -> p h d", h=nh)
            cb = cosT[:, ti, :].unsqueeze(1).to_broadcast([128, nh, 32])
            sb_ = sinT[:, ti, :].unsqueeze(1).to_broadcast([128, nh, 32])
            k.op(dve, lambda e: e.tensor_tensor(av[:, :, 0:32], pv[:, :, 0:32], cb, ALU.mult),
                 reads=[pb, cosT], writes=[tmpA])
            k.op(dve, lambda e: e.tensor_tensor(av[:, :, 32:64], pv[:, :, 32:64], cb, ALU.mult),
                 reads=[pb, cosT], writes=[tmpA])
            k.op(dve, lambda e: e.tensor_tensor(bv[:, :, 0:32], pv[:, :, 32:64], sb_, ALU.mult),
                 reads=[pb, sinT], writes=[tmpB])
            k.op(dve, lambda e: e.tensor_tensor(bv[:, :, 32:64], pv[:, :, 0:32], sb_, ALU.mult),
                 reads=[pb, sinT], writes=[tmpB])
            k.op(dve, lambda e: e.tensor_tensor(ov[:, :, 0:32], av[:, :, 0:32], bv[:, :, 0:32], ALU.subtract),
                 reads=[tmpA, tmpB], writes=[outb])
            k.op(dve, lambda e: e.tensor_tensor(ov[:, :, 32:64], av[:, :, 32:64], bv[:, :, 32:64], ALU.add),
                 reads=[tmpA, tmpB], writes=[outb])

        w_in_v = w_in.rearrange("(k p) c -> p k c", p=128)
        def phase_A1():
            with ExitStack() as s1:
                cosF = k.sb("cosF", [128, NB, 32], F32, s1)
                sinF = k.sb("sinF", [128, NB, 32], F32, s1)
                rope_tables(pos_full, NB, cosF, sinF, s1)
                wA = k.sb("wA1", [128, 8, 1088], BF16, s1)
                for kc in range(8):
                    k.dma(pool, wA[:, kc, 0:1024], w_in_v[:, kc, C_K:C_K + 1024], writes=[wA])
                    k.dma(pool, wA[:, kc, 1024:1088], w_in_v[:, kc, C_KI:C_KI + 64], writes=[wA])
                xt2 = [k.sb("xtA%d" % i, [128, D], F32, s1) for i in range(2)]
                hT2 = [k.sb("hTA%d" % i, [128, 8, 128], BF16, s1) for i in range(2)]
                krL = [k.sb("kr%d" % i, [128, 512], BF16, s1) for i in range(2)]
                kirL = [k.sb("kir%d" % i, [128, 128], BF16, s1) for i in range(2)]
                tAL = [k.sb("tA%d" % i, [128, 512], F32, s1) for i in range(2)]
                tBL = [k.sb("tB%d" % i, [128, 512], F32, s1) for i in range(2)]
                tCL = [k.sb("tC%d" % i, [128, 64], F32, s1) for i in range(2)]
                tDL = [k.sb("tD%d" % i, [128, 64], F32, s1) for i in range(2)]
                hbs1 = {}

                def pre1(ti):
                    k.dma(sp, xt2[ti % 2][:], x_full[ti * 128:(ti + 1) * 128, :], writes=[xt2[ti % 2]])
                    hbs1[ti] = norm_pre(xt2[ti % 2], 128)

                def post1(ti):
                    norm_post(hbs1.pop(ti), 128, gmix, hT2[ti % 2])

                def part2(ti):
                    kr, kir = krL[ti % 2], kirL[ti % 2]
                    tb = k.bank()
                    tbv = bf(tb[:])
                    for pr in range(4):
                        k.op(pe, lambda e, pr=pr: e.transpose(tbv[:, pr * 128:(pr + 1) * 128],
                                                              kr[:, pr * 128:(pr + 1) * 128], ident[:]),
                             reads=[kr, ident], writes=[tb])
                    k.op(pe, lambda e: e.transpose(tbv[:, 512:640], kir[:], ident[:]), reads=[kir, ident], writes=[tb])
                    k.op(act, lambda e: e.activation(KT[:, :, ti * 128:(ti + 1) * 128],
                                                     tbv[:, 0:512].rearrange("p (a b) -> p a b", a=4), AF.Copy),
                         reads=[tb], writes=[KT])
                    k.op(act, lambda e: e.activation(kiT[:, ti * 128:(ti + 1) * 128], tbv[:, 512:640], AF.Copy),
                         reads=[tb], writes=[kiT])

                pre1(0)
                post1(0)
                pre1(1)
                for ti in range(NB):
                    if ti + 1 < NB:
                        post1(ti + 1)
                    hT = hT2[ti % 2]
                    kr, kir, tA, tB = krL[ti % 2], kirL[ti % 2], tAL[ti % 2], tBL[ti % 2]
                    tC, tD = tCL[ti % 2], tDL[ti % 2]
                    pk = proj_tok(hT, wA, 0, 512)
                    pv = proj_tok(hT, wA, 512, 512)
                    pki = proj_tok(hT, wA, 1024, 64)
                    if ti + 2 < NB:
                        pre1(ti + 2)
                    rope(pk, 8, cosF, sinF, ti, kr, tA, tB)
                    Vv = V[:, ti, :].rearrange("p (h d) -> p h d", h=8)
                    k.op(act, lambda e: e.activation(Vv[:, :, 0:64],
                                                     pv[:, 0:512].rearrange("p (h d) -> p h d", h=8), AF.Copy),
                         reads=[pv], writes=[V])
                    rope(pki, 1, cosF, sinF, ti, kir, tC, tD)
                    k.op(dve, lambda e, kir=kir: e.tensor_copy(kir[:, 64:128], kir[:, 0:64]), reads=[kir], writes=[kir])
                    if ti >= 1:
                        part2(ti - 1)
                part2(NB - 1)
                k.barrier()
        def dbg_A1():
            for pr in range(4):
                k.dma(sp, dbg["kT"][:, pr, :], KT[:, pr, :], reads=[KT])
            for t8 in range(0, NB, 4):
                k.dma(sp, dbg["v"][:, t8:t8 + 4, :], V[:, t8:t8 + 4, :], reads=[V])
            k.dma(sp, dbg["kiT"], kiT[:], reads=[kiT])

        def phase_A2():
            with ExitStack() as s2:
                cosO = k.sb("cosO", [128, NS, 32], F32, s2)
                sinO = k.sb("sinO", [128, NS, 32], F32, s2)
                rope_tables(pos_own, NS, cosO, sinO, s2)
                WU, WQ, WQI, WWI, WGC, WGA = 0, 1024, 1536, 2048, 2056, 3080
                wB = k.sb("wB", [128, 8, 4104], BF16, s2)
                wBq, wBu, wBg = Buf("wBq", wB.t), Buf("wBu", wB.t), Buf("wBg", wB.t)
                for kc in range(8):
                    k.dma(pool, wB[:, kc, WQ:WQ + 512], w_in_v[:, kc, C_Q:C_Q + 512], writes=[wBq])
                    k.dma(pool, wB[:, kc, WQI:WQI + 512], w_in_v[:, kc, C_QI:C_QI + 512], writes=[wBq])
                    k.dma(pool, wB[:, kc, WWI:WWI + 8], w_in_v[:, kc, C_WI:C_WI + 8], writes=[wBq])
                for kc in range(8):
                    k.dma(pool, wB[:, kc, WU:WU + 1024], w_in_v[:, kc, C_U:C_U + 1024], writes=[wBu])
                for kc in range(8):
                    k.dma(pool, wB[:, kc, WGC:WGC + 2048], w_in_v[:, kc, C_GC:C_GC + 2048], writes=[wBg])
                wco = k.sb("wco", [128, 4, D], BF16, s2)
                for kc in range(4):
                    k.dma(pool, wco[:, kc, :], wco_d[kc * 128:(kc + 1) * 128, :], writes=[wco])
                wdw = k.sb("wdw", [128, 4, 31], F32, s2)
                bdw = k.sb("bdw", [128, 4], F32, s2)
                lng = k.sb("lng", [128, 4], F32, s2)
                lnb = k.sb("lnb", [128, 4], F32, s2)
                k.dma(sp, wdw[:], wdw_d, writes=[wdw])
                k.dma(sp, bdw[:], bdw_d, writes=[bdw])
                k.dma(sp, lng[:], lng_d, writes=[lng])
                k.dma(sp, lnb[:], lnb_d, writes=[lnb])
                Dg = k.sb("Dg", [128, 124, 128], BF16, s2)
                for c in range(4):
                    for j in range(31):
                        k.op(dve, lambda e, c=c, j=j: e.tensor_scalar(Dg[:, c * 31 + j, :], identf[:],
                                                                       wdw[:, c, j:j + 1], None, ALU.mult),
                             reads=[identf, wdw], writes=[Dg])
                ones = k.sb("ones", [128, 128], F32, s2)
                k.op(pool, lambda e: e.memset(ones[:], 1.0), writes=[ones])
                xt2 = [k.sb("xtB%d" % i, [128, D], F32, s2) for i in range(2)]
                xh2 = [k.sb("xhB%d" % i, [32, D], F32, s2) for i in range(2)]
                hT_L = [k.sb("hTB_%d" % i_, [128, 8, 128], BF16, s2) for i_ in range(2)]
                hTh_L = [k.sb("hThB_%d" % i_, [128, 8, 32], BF16, s2) for i_ in range(2)]
                qr_L = [k.sb("qr_%d" % i_, [128, 512], BF16, s2) for i_ in range(2)]
                qir_L = [k.sb("qir_%d" % i_, [128, 512], BF16, s2) for i_ in range(2)]
                tA_L = [k.sb("tA2_%d" % i_, [128, 512], F32, s2) for i_ in range(2)]
                tB_L = [k.sb("tB2_%d" % i_, [128, 512], F32, s2) for i_ in range(2)]
                qT_s_L = [k.sb("qT_s_%d" % i_, [128, 512], BF16, s2) for i_ in range(2)]
                qiT_s_L = [k.sb("qiT_s_%d" % i_, [128, 512], BF16, s2) for i_ in range(2)]
                sg_L = [k.sb("sgl_%d" % i_, [128, 160], F32, s2) for i_ in range(2)]
                gT_L = [k.sb("gT_%d" % i_, [128, 4, 160], BF16, s2) for i_ in range(2)]
                csb_L = [k.sb("csb_%d" % i_, [128, 4, 128], F32, s2) for i_ in range(2)]
                csq_L = [k.sb("csq_%d" % i_, [128, 4, 128], F32, s2) for i_ in range(2)]
                mean_L = [k.sb("mean_%d" % i_, [128, 128], F32, s2) for i_ in range(2)]
                msq_L = [k.sb("msq_%d" % i_, [128, 128], F32, s2) for i_ in range(2)]
                rstd_L = [k.sb("rstd_%d" % i_, [128, 128], F32, s2) for i_ in range(2)]
                nrm_L = [k.sb("nrm_%d" % i_, [128, 4, 128], F32, s2) for i_ in range(2)]
                snT_L = [k.sb("snT_%d" % i_, [128, 4, 128], BF16, s2) for i_ in range(2)]
                sgc_L = [k.sb("sgc_%d" % i_, [128, 8, 128], F32, s2) for i_ in range(2)]
                mc_s_L = [k.sb("mc_s_%d" % i_, [128, 8, 128], BF16, s2) for i_ in range(2)]
                sga_s_L = [k.sb("sga_s_%d" % i_, [128, 8, 128], BF16, s2) for i_ in range(2)]
                qTd_b = Buf("qTd_b")
                mcd_b = Buf("mcd_b")

                hbs2 = {}

                def pre2(j):
                    k.dma(sp, xt2[j % 2][:], x_own[j * 128:(j + 1) * 128, :], writes=[xt2[j % 2]])
                    k.dma(sp, xh2[j % 2][:], x_halo[j * 32:(j + 1) * 32, :], writes=[xh2[j % 2]])
                    hbs2[j] = (norm_pre(xh2[j % 2], 32), norm_pre(xt2[j % 2], 128))

                def post2(j):
                    a_, b_ = hbs2.pop(j)
                    norm_post(a_, 32, gmix, hTh_L[j % 2])
                    norm_post(b_, 128, gmix, hT_L[j % 2])

                for j in range(nslots):
                    xt = xt2[j % 2]
                    xh = xh2[j % 2]
                    hT, hTh, qr, qir, tA, tB, qT_s, qiT_s, sg, gT, csb, csq, mean, msq, rstd, nrm, snT, sgc, mc_s, sga_s = hT_L[j % 2], hTh_L[j % 2], qr_L[j % 2], qir_L[j % 2], tA_L[j % 2], tB_L[j % 2], qT_s_L[j % 2], qiT_s_L[j % 2], sg_L[j % 2], gT_L[j % 2], csb_L[j % 2], csq_L[j % 2], mean_L[j % 2], msq_L[j % 2], rstd_L[j % 2], nrm_L[j % 2], snT_L[j % 2], sgc_L[j % 2], mc_s_L[j % 2], sga_s_L[j % 2]
                    if j == 0:
                        pre2(0)
                        post2(0)
                        if nslots > 1:
                            pre2(1)
                    if j + 1 < nslots:
                        post2(j + 1)
                    pq = proj_tok(hT, wBq, WQ, 512)
                    rope(pq, 8, cosO, sinO, j, qr, tA, tB)
                    pqi = proj_tok(hT, wBq, WQI, 512)
                    rope(pqi, 8, cosO, sinO, j, qir, tA, tB)
                    pw = proj_tok(hT, wBq, WWI, 8)
                    k.op(act, lambda e: e.activation(wi_all[:, j * 8:(j + 1) * 8], pw[:, 0:8], AF.Copy,
                                                     scale=IDX_SCALE), reads=[pw], writes=[wi_all])
                    for src, dstb, dd in ((qr, qT_s, qT_d), (qir, qiT_s, qiT_d)):
                        tb = k.bank()
                        tbv = bf(tb[:])
                        for pr in range(4):
                            k.op(pe, lambda e, pr=pr, src=src, tbv=tbv: e.transpose(
                                tbv[:, pr * 128:(pr + 1) * 128], src[:, pr * 128:(pr + 1) * 128], ident[:]),
                                 reads=[src, ident], writes=[tb])
                        k.op(act, lambda e, dstb=dstb, tbv=tbv: e.activation(dstb[:], tbv[:, 0:512], AF.Copy),
                             reads=[tb], writes=[dstb])
                        k.dma(sp, dd[j], dstb[:], reads=[dstb], writes=[qTd_b], sembuf=dstb)
                    for c in range(4):
                        pu = k.bank()
                        puv = pu[:, 0:320].rearrange("p (a b) -> p a b", a=2)
                        for half, col0 in ((0, WU + c * 128), (1, WU + 512 + c * 128)):
                            for kc in range(8):
                                k.op(pe, lambda e, kc=kc, half=half, col0=col0, puv=puv: e.matmul(
                                    puv[:, half, 0:32], wB[:, kc, col0:col0 + 128], hTh[:, kc, :],
                                    start=(kc == 0), stop=(kc == 7)), reads=[wBu, hTh], writes=[pu])
                            for kc in range(8):
                                k.op(pe, lambda e, kc=kc, half=half, col0=col0, puv=puv: e.matmul(
                                    puv[:, half, 32:160], wB[:, kc, col0:col0 + 128], hT[:, kc, :],
                                    start=(kc == 0), stop=(kc == 7)), reads=[wBu, hT], writes=[pu])
                        k.op(act, lambda e, puv=puv: e.activation(sg[:], puv[:, 1, :], AF.Sigmoid),
                             reads=[pu], writes=[sg])
                        k.op(dve, lambda e, puv=puv, c=c: e.tensor_tensor(gT[:, c, :], puv[:, 0, :], sg[:], ALU.mult),
                             reads=[pu, sg], writes=[gT])
                    if j + 2 < nslots:
                        pre2(j + 2)
                    pc = k.bank()
                    pcv = pc[:, 0:512].rearrange("p (a b) -> p a b", a=4)
                    for c in range(4):
                        for jj in range(31):
                            k.op(pe, lambda e, c=c, jj=jj: e.matmul(pcv[:, c, :], Dg[:, c * 31 + jj, :],
                                                                    gT[:, c, 2 + jj:2 + jj + 128],
                                                                    start=(jj == 0), stop=(jj == 30)),
                                 reads=[Dg, gT], writes=[pc])
                    k.op(dve, lambda e: e.tensor_tensor(csb[:], pcv, bdw[:].unsqueeze(2).to_broadcast([128, 4, 128]),
                                                        ALU.add), reads=[pc, bdw], writes=[csb])
                    k.op(act, lambda e: e.activation(csq[:], csb[:], AF.Square), reads=[csb], writes=[csq])
                    pst = k.bank()
                    for c in range(4):
                        k.op(pe, lambda e, c=c: e.matmul(pst[:, 0:128], ones[:], csb[:, c, :], start=(c == 0),
                                                         stop=(c == 3)), reads=[ones, csb], writes=[pst])
                    for c in range(4):
                        k.op(pe, lambda e, c=c: e.matmul(pst[:, 128:256], ones[:], csq[:, c, :], start=(c == 0),
                                                         stop=(c == 3)), reads=[ones, csq], writes=[pst])
                    k.op(act, lambda e: e.activation(mean[:], pst[:, 0:128], AF.Copy, scale=1.0 / 512),
                         reads=[pst], writes=[mean])
                    k.op(dve, lambda e: e.tensor_tensor(msq[:], mean[:], mean[:], ALU.mult), reads=[mean], writes=[msq])
                    k.op(dve, lambda e: e.scalar_tensor_tensor(rstd[:], pst[:, 128:256], 1.0 / 512, msq[:],
                                                               ALU.mult, ALU.subtract),
                         reads=[pst, msq], writes=[rstd])
                    k.op(act, lambda e: e.activation(rstd[:], rstd[:], AF.Sqrt, bias=EPS, scale=1.0),
                         reads=[rstd], writes=[rstd])
                    k.op(dve, lambda e: e.reciprocal(rstd[:], rstd[:]), reads=[rstd], writes=[rstd])
                    k.op(dve, lambda e: e.tensor_tensor(nrm[:], csb[:], mean[:].unsqueeze(1).to_broadcast([128, 4, 128]),
                                                        ALU.subtract), reads=[csb, mean], writes=[nrm])
                    k.op(dve, lambda e: e.tensor_tensor(nrm[:], nrm[:], rstd[:].unsqueeze(1).to_broadcast([128, 4, 128]),
                                                        ALU.mult), reads=[nrm, rstd], writes=[nrm])
                    for c in range(4):
                        k.op(act, lambda e, c=c: e.activation(snT[:, c, :], nrm[:, c, :], AF.Silu,
                                                              bias=lnb[:, c:c + 1], scale=lng[:, c:c + 1]),
                             reads=[nrm, lnb, lng], writes=[snT])
                    for gi, (wcol, dst) in enumerate(((WGC, None), (WGA, sga_s))):
                        for hb_ in range(2):
                            pg = k.bank()
                            pgv = pg[:, 0:512].rearrange("p (a b) -> p a b", a=4)
                            for m in range(4):
                                col0 = wcol + (hb_ * 4 + m) * 128
                                for kc in range(8):
                                    k.op(pe, lambda e, kc=kc, m=m, col0=col0, pgv=pgv: e.matmul(
                                        pgv[:, m, :], wB[:, kc, col0:col0 + 128], hT[:, kc, :],
                                        start=(kc == 0), stop=(kc == 7)), reads=[wBg, hT], writes=[pg])
                            tgt = sgc if gi == 0 else sga_s
                            k.op(act, lambda e, pgv=pgv, tgt=tgt, hb_=hb_: e.activation(
                                tgt[:, hb_ * 4:(hb_ + 1) * 4, :], pgv, AF.Sigmoid), reads=[pg], writes=[tgt])
                    for hb_ in range(2):
                        py = k.bank()
                        pyv = py[:, 0:512].rearrange("p (a b) -> p a b", a=4)
                        for m in range(4):
                            mm = hb_ * 4 + m
                            for kc in range(4):
                                k.op(pe, lambda e, kc=kc, m=m, mm=mm, pyv=pyv: e.matmul(
                                    pyv[:, m, :], wco[:, kc, mm * 128:(mm + 1) * 128], snT[:, kc, :],
                                    start=(kc == 0), stop=(kc == 3)), reads=[wco, snT], writes=[py])
                        k.op(dve, lambda e, pyv=pyv, hb_=hb_: e.tensor_tensor(
                            mc_s[:, hb_ * 4:(hb_ + 1) * 4, :], pyv, sgc[:, hb_ * 4:(hb_ + 1) * 4, :], ALU.mult),
                             reads=[py, sgc], writes=[mc_s])
                    k.dma(sp, mc_d[j], mc_s[:].rearrange("p a b -> p (a b)"), reads=[mc_s], writes=[mcd_b], sembuf=mc_s)
                    k.dma(sp, sga_d[j], sga_s[:].rearrange("p a b -> p (a b)"), reads=[sga_s], writes=[mcd_b],
                          sembuf=sga_s)
                k.barrier()
        def dbg_A2():
            k.dma(sp, dbg["wi"], wi_all[:], reads=[wi_all])
            for nm, src in (("qT", qT_d), ("qiT", qiT_d), ("mc", mc_d), ("sga", sga_d)):
                b_ = Buf("dbgc_" + nm)
                k.dma(sp, dbg[nm], src, writes=[b_])
            k.barrier()

        def phase_B():
            with ExitStack() as s3:
                cm2 = [k.sb("cm%d" % i, [128, 256], F32, s3) for i in range(2)]
                pw2 = k.sb("pw2", [128, NIT + 2], F32, s3)
                for it in range(NIT + 2):
                    k.op(dve, lambda e, it=it: e.memset(pw2[:, it:it + 1], float(2.0 ** -(it + 1))), writes=[pw2])
                qT2 = [k.sb("qTb%d" % i, [128, 4, 256], BF16, s3) for i in range(2)]
                for b_ in qT2:
                    k.op(dve, lambda e: e.memset(b_[:], 0.0), writes=[b_])
                qiT2 = [k.sb("qiTb%d" % i, [128, 512], BF16, s3) for i in range(2)]
                scL = [k.sb("sc%d" % i, [128, S], F32, s3) for i in range(2)]
                AL = [k.sb("Ab%d" % i, [128, 512], BF16, s3) for i in range(4)]
                wabs = k.sb("wabs", [128, 8], F32, s3)
                sgn = k.sb("sgn", [128, 8], F32, s3)
                sgd = k.sb("sgd", [128, 8, 128], BF16, s3)
                msk = k.sb("msk", [128, S], BF16, s3)
                Mb = k.sb("Mb", [128, NB, 128], BF16, s3)
                st8 = k.sb("st8", [128, 8], F32, s3)
                Q = k.sb("Qtab", [128, NIT + 2], F32, s3)
                Q2 = k.sb("Q2tab", [128, NIT + 2], F32, s3)
                E2 = [k.sb("Eb%d" % i, [128, 4, 128], BF16, s3) for i in range(3)]
                rden = k.sb("rden", [128, 8], F32, s3)
                Osb = k.sb("Osb", [128, 2, 4, 66], F32, s3)
                thr = st8[:, 6:7]
                ai = [0]
                MBIG = 30000.0
                if debug:
                    attnf = k.sb("attnf", [128, 512], F32, s3)

                def loads_a(j):
                    p = j % 2
                    k.dma(sp, qiT2[p][:], qiT_d[j], writes=[qiT2[p]])
                    k.dma(sp, cm2[p][:], cmask_d[:, j, :], writes=[cm2[p]])

                def loads_b(j):
                    p = j % 2
                    k.dma(sp, qT2[p][0:64, :, 0:128], qT_d[j][0:64, :].rearrange("p (a b) -> p a b", a=4), writes=[qT2[p]])
                    k.dma(sp, qT2[p][64:128, :, 128:256], qT_d[j][64:128, :].rearrange("p (a b) -> p a b", a=4),
                          writes=[qT2[p]])

                def prep(j):
                    wsl = wi_all[:, j * 8:(j + 1) * 8]
                    k.op(dve, lambda e: e.tensor_scalar(sgn[:], wsl, 0.0, 2.0, ALU.is_ge, ALU.mult), reads=[wi_all], writes=[sgn])
                    k.op(dve, lambda e: e.tensor_scalar(sgn[:], sgn[:], -1.0, None, ALU.add), reads=[sgn], writes=[sgn])
                    k.op(dve, lambda e: e.tensor_tensor(wabs[:], wsl, sgn[:], ALU.mult), reads=[wi_all, sgn], writes=[wabs])
                    k.op(dve, lambda e: e.tensor_tensor(sgd[:], ident[:].unsqueeze(1).to_broadcast([128, 8, 128]),
                                                        sgn[:].unsqueeze(2).to_broadcast([128, 8, 128]), ALU.mult),
                         reads=[ident, sgn], writes=[sgd])

                def indexer(j):
                    sc = scL[j % 2]
                    qiTs = qiT2[j % 2]
                    L = 256 * (j + 1)
                    nch = (L + 511) // 512
                    for cc in range(nch):
                        c0 = cc * 512
                        W = min(512, L - c0)
                        pacc = k.bank(pin=True)
                        pbs = {}

                        def qi_mm(h):
                            pb = k.bank()
                            pp = (h % 2) * 64
                            k.op(pe, lambda e: e.matmul(pb[:, 0:W], qiTs[pp:pp + 64, (h // 2) * 128:(h // 2 + 1) * 128],
                                                        kiT[pp:pp + 64, c0:c0 + W], start=True, stop=True),
                                 reads=[qiTs, kiT], writes=[pb])
                            pbs[h] = pb

                        qi_mm(0)
                        qi_mm(1)
                        qi_mm(2)
                        for h in range(8):
                            if h + 3 < 8:
                                qi_mm(h + 3)
                            pb = pbs[h]
                            A = AL[ai[0] % 4]
                            ai[0] += 1
                            k.op(act, lambda e: e.activation(A[:, 0:W], pb[:, 0:W], AF.Relu, scale=wabs[:, h:h + 1]),
                                 reads=[pb, wabs], writes=[A])
                            k.op(pe, lambda e: e.matmul(pacc[:, 0:W], sgd[:, h, :], A[:, 0:W], start=(h == 0), stop=(h == 7)),
                                 reads=[sgd, A], writes=[pacc])
                        k.op(act, lambda e: e.activation(sc[:, c0:c0 + W], pacc[:, 0:W], AF.Copy), reads=[pacc], writes=[sc])
                        k.unpin(pacc)

                def thresh(j):
                    sc = scL[j % 2]
                    L = 256 * (j + 1)
                    cmask = cm2[j % 2]
                    if j == 0:
                        k.op(dve, lambda e: e.tensor_tensor(sc[:, L - 256:L], sc[:, L - 256:L], cmask[:], ALU.add),
                             reads=[sc, cmask], writes=[sc])
                        k.op(dve, lambda e: e.memset(thr, -1.0e29), writes=[st8])
                        return
                    k.op(dve, lambda e: e.tensor_reduce(st8[:, 0:1], sc[:, 0:L], AX.X, ALU.max), reads=[sc], writes=[st8])
                    k.op(dve, lambda e: e.tensor_reduce(st8[:, 1:2], sc[:, 0:L], AX.X, ALU.min), reads=[sc], writes=[st8])
                    k.op(dve, lambda e: e.tensor_tensor(sc[:, L - 256:L], sc[:, L - 256:L], cmask[:], ALU.add),
                         reads=[sc, cmask], writes=[sc])
                    k.op(dve, lambda e: e.tensor_tensor(st8[:, 2:3], st8[:, 0:1], st8[:, 1:2], ALU.subtract),
                         reads=[st8], writes=[st8])
                    k.op(dve, lambda e: e.tensor_scalar(st8[:, 5:6], st8[:, 2:3], float(2.0 ** -10), None, ALU.mult),
                         reads=[st8], writes=[st8])
                    k.op(dve, lambda e: e.tensor_scalar(st8[:, 2:3], st8[:, 2:3], float(1.0 + 2.0 ** -9), 1e-30,
                                                        ALU.mult, ALU.add), reads=[st8], writes=[st8])
                    k.op(dve, lambda e: e.tensor_tensor(st8[:, 7:8], st8[:, 1:2], st8[:, 5:6], ALU.subtract),
                         reads=[st8], writes=[st8])
                    k.op(dve, lambda e: e.tensor_scalar(Q[:], pw2[:], st8[:, 2:3], None, ALU.mult),
                         reads=[pw2, st8], writes=[Q])
                    k.op(dve, lambda e: e.tensor_scalar(Q2[:], Q[:], 2.0, None, ALU.mult), reads=[Q], writes=[Q2])
                    k.op(dve, lambda e: e.tensor_tensor(st8[:, 3:4], st8[:, 7:8], Q[:, 0:1], ALU.add),
                         reads=[st8, Q], writes=[st8])
                    for it in range(NIT):
                        k.op(dve, lambda e: e.memset(st8[:, 4:5], 0.0), writes=[st8])
                        k.op(dve, lambda e: e.tensor_scalar(msk[:, 0:L], sc[:, 0:L], st8[:, 3:4], 0.0,
                                                            ALU.is_ge, ALU.add, accum_out=st8[:, 4:5]),
                             reads=[sc, st8], writes=[st8, msk])
                        k.op(dve, lambda e: e.tensor_scalar(st8[:, 5:6], st8[:, 4:5], TOPK - 0.5,
                                                            Q2[:, it + 1:it + 2], ALU.is_ge, ALU.mult),
                             reads=[st8, Q2], writes=[st8])
                        k.op(dve, lambda e: e.scalar_tensor_tensor(st8[:, 3:4], st8[:, 3:4], Q[:, it + 1:it + 2],
                                                                   st8[:, 5:6], ALU.subtract, ALU.add),
                             reads=[st8, Q], writes=[st8])
                    k.op(dve, lambda e: e.tensor_tensor(thr, st8[:, 3:4], Q[:, NIT:NIT + 1], ALU.subtract),
                         reads=[st8, Q], writes=[st8])

                def mask(j):
                    sc = scL[j % 2]
                    L = 256 * (j + 1)
                    nkb = 2 * (j + 1)
                    if debug:
                        b_ = Buf("dbg_sc%d" % j)
                        for c0_ in range(0, L, 1024):
                            c1_ = min(L, c0_ + 1024)
                            k.dma(sp, dbg["sc"][j][:, c0_:c1_], sc[:, c0_:c1_], reads=[sc], writes=[b_], sembuf=b_)
                        k.dma(sp, dbg["thr"][j], thr, reads=[st8], writes=[b_], sembuf=b_)
                    for g0 in range(0, nkb, 8):
                        gn = min(8, nkb - g0)
                        k.op(dve, lambda e: e.tensor_scalar(msk[:, g0 * 128:(g0 + gn) * 128],
                                                            sc[:, g0 * 128:(g0 + gn) * 128], thr, None, ALU.is_ge),
                             reads=[sc, st8], writes=[msk])
                        tb = k.bank()
                        tbv = bf(tb[:])
                        for q_ in range(gn):
                            k.op(pe, lambda e: e.transpose(tbv[:, q_ * 128:(q_ + 1) * 128],
                                                           msk[:, (g0 + q_) * 128:(g0 + q_ + 1) * 128], ident[:]),
                                 reads=[msk, ident], writes=[tb])
                        k.op(act, lambda e: e.activation(Mb[:, g0:g0 + gn, :],
                                                         tbv[:, 0:gn * 128].rearrange("p (a b) -> p a b", a=gn),
                                                         AF.Identity, bias=-MBIG, scale=MBIG),
                             reads=[tb], writes=[Mb])

                def attention(j):
                    qTs = qT2[j % 2]
                    nkb = 2 * (j + 1)
                    po = [k.bank(pin=True), k.bank(pin=True)]
                    pov = [b_[:, 0:512].rearrange("p (h d) -> p h d", h=4) for b_ in po]
                    units = [(kb, hg) for kb in range(nkb) for hg in range(2)]

                    def qk(u):
                        kb, hg = units[u]
                        pl = k.bank()
                        plv = pl[:, 0:512].rearrange("p (a b) -> p a b", a=4)
                        k.op(pe, lambda e: e.matmul(plv, ident[:], Mb[:, kb, :].unsqueeze(1).to_broadcast([128, 4, 128]),
                                                    start=True, stop=False), reads=[ident, Mb], writes=[pl])
                        for pi in range(2):
                            pr = hg * 2 + pi
                            k.op(pe, lambda e: e.matmul(pl[:, pi * 256:(pi + 1) * 256], KT[:, pr, kb * 128:(kb + 1) * 128],
                                                        qTs[:, pr, :], start=False, stop=(pi == 1)),
                                 reads=[KT, qTs], writes=[pl])
                        return pl, plv

                    LA = 2
                    pend = [qk(u) for u in range(min(LA, len(units)))]
                    for u in range(len(units)):
                        if u + LA < len(units):
                            pend.append(qk(u + LA))
                        kb, hg = units[u]
                        pl, plv = pend.pop(0)
                        E = E2[u % 3]
                        k.op(act, lambda e: e.activation(E[:], plv, AF.Exp, scale=ATT_SCALE), reads=[pl], writes=[E])
                        for hh in range(4):
                            h = hg * 4 + hh
                            k.op(pe, lambda e: e.matmul(pov[hg][:, hh, 0:66], E[:, hh, :], V[:, kb, h * 66:(h + 1) * 66],
                                                        start=(kb == 0 and hh == 0), stop=(kb == nkb - 1 and hh == 3)),
                                 reads=[E, V], writes=[po[hg]])
                    for hg in range(2):
                        k.op(act, lambda e: e.activation(Osb[:, hg, :, :], pov[hg][:, :, 0:66], AF.Copy),
                             reads=[po[hg]], writes=[Osb])
                        k.unpin(po[hg])

                def finalize(j):
                    for hg in range(2):
                        k.op(dve, lambda e: e.reciprocal(rden[:, hg * 4:(hg + 1) * 4].unsqueeze(2), Osb[:, hg, :, 64:65]),
                             reads=[Osb], writes=[rden])
                        k.op(dve, lambda e: e.tensor_tensor(
                            attn_all[:, j, hg * 256:(hg + 1) * 256].rearrange("p (h d) -> p h d", h=4), Osb[:, hg, :, 0:64],
                            rden[:, hg * 4:(hg + 1) * 4].unsqueeze(2).to_broadcast([128, 4, 64]), ALU.mult),
                             reads=[Osb, rden], writes=[attn_all])
                    if debug:
                        k.op(dve, lambda e: e.tensor_copy(attnf[:], attn_all[:, j, :]), reads=[attn_all], writes=[attnf])
                        k.dma(sp, dbg["attn"][j], attnf[:], reads=[attnf])

                loads_a(0)
                loads_b(0)
                if nslots > 1:
                    loads_a(1)
                    loads_b(1)
                prep(0)
                indexer(0)
                thresh(0)
                mask(0)
                if nslots > 1:
                    prep(1)
                    indexer(1)
                for i in range(nslots):
                    if i + 2 < nslots:
                        prep(i + 2)
                    if i + 1 < nslots:
                        thresh(i + 1)
                    attention(i)
                    if i + 2 < nslots:
                        loads_a(i + 2)
                        indexer(i + 2)
                    if i + 1 < nslots:
                        mask(i + 1)
                    finalize(i)
                    if i + 2 < nslots:
                        loads_b(i + 2)
                k.barrier()

        def phase_B2():
            with ExitStack() as s3:
                if "C" in phases:
                    wload(0)
                    wload(1)
                mc2 = [k.sb("mcb%d" % i, [128, 8, 128], BF16, s3) for i in range(2)]
                sga2 = [k.sb("sgab%d" % i, [128, 8, 128], BF16, s3) for i in range(2)]
                xoL = [k.sb("xob%d" % i, [128, D], F32, s3) for i in range(2)]
                attnTL = [k.sb("attnT%d" % i, [128, 4, 128], BF16, s3) for i in range(2)]
                mTL = [k.sb("mT%d" % i, [128, 8, 128], BF16, s3) for i in range(2)]
                x1L = [k.sb("x1_%d" % i, [128, D], F32, s3) for i in range(2)]
                lgA = k.sb("lgA", [128, NS, 20], F32, s3)
                rA = k.sb("rA", [128, 8, NS], F32, s3)
                dG = k.sb("dG", [128, NS, 4], F32, s3)
                eG = k.sb("eG", [128, NS, 4], F32, s3)
                ohg1 = k.sb("ohg1", [128, NS, 4], F32, s3)
                ohgp = k.sb("ohgp", [128, NS, 4], F32, s3)
                t16A = k.sb("t16A", [128, NS, 16], F32, s3)
                eselA = k.sb("eselA", [128, NS, 4], F32, s3)
                oh1A = k.sb("oh1A", [128, NS, 4], F32, s3)
                oh2A = k.sb("oh2A", [128, NS, 4], F32, s3)
                emA = k.sb("emA", [128, NS, 4], F32, s3)
                x1d_b = Buf("x1d_b")

                def loads_m(j):
                    p = j % 2
                    k.dma(sp, mc2[p][:].rearrange("p a b -> p (a b)"), mc_d[j], writes=[mc2[p]])
                    k.dma(sp, sga2[p][:].rearrange("p a b -> p (a b)"), sga_d[j], writes=[sga2[p]])

                def loads_x(j):
                    p = j % 2
                    k.dma(sp, xoL[p][:], x_own[j * 128:(j + 1) * 128, :], writes=[xoL[p]])

                def s1(j):
                    attnT = attnTL[j % 2]
                    tb = k.bank()
                    tbv = bf(tb[:])
                    for q_ in range(4):
                        k.op(pe, lambda e: e.transpose(tbv[:, q_ * 128:(q_ + 1) * 128],
                                                       attn_all[:, j, q_ * 128:(q_ + 1) * 128], ident[:]),
                             reads=[attn_all, ident], writes=[tb])
                    k.op(act, lambda e: e.activation(attnT[:], tbv[:, 0:512].rearrange("p (a b) -> p a b", a=4), AF.Copy),
                         reads=[tb], writes=[attnT])

                def s2(j):
                    mcs, sgas = mc2[j % 2], sga2[j % 2]
                    x1, mT, attnT = x1L[j % 2], mTL[j % 2], attnTL[j % 2]
                    x1v = x1[:].rearrange("p (a b) -> p a b", a=8)
                    for hb_ in range(2):
                        py = k.bank()
                        pyv = py[:, 0:512].rearrange("p (a b) -> p a b", a=4)
                        for m in range(4):
                            mm = hb_ * 4 + m
                            for kc in range(4):
                                k.op(pe, lambda e: e.matmul(pyv[:, m, :], wao[:, kc, mm * 128:(mm + 1) * 128], attnT[:, kc, :],
                                                            start=(kc == 0), stop=(kc == 3)), reads=[wao, attnT], writes=[py])
                        k.op(dve, lambda e: e.tensor_tensor(x1v[:, hb_ * 4:(hb_ + 1) * 4, :], pyv,
                                                            sgas[:, hb_ * 4:(hb_ + 1) * 4, :], ALU.mult),
                             reads=[py, sgas], writes=[x1])
                    k.op(dve, lambda e: e.tensor_tensor(mT[:], x1v, mcs[:], ALU.add), reads=[x1, mcs], writes=[mT])

                def s3(j):
                    xo, mT = xoL[j % 2], mTL[j % 2]
                    for half in range(2):
                        px = k.bank()
                        for kc in range(8):
                            k.op(pe, lambda e: e.matmul(px[:, 0:512], mT[:, kc, :], wo[:, kc, half * 512:(half + 1) * 512],
                                                        start=(kc == 0), stop=(kc == 7)), reads=[mT, wo], writes=[px])
                        k.op(dve, lambda e: e.tensor_tensor(acc[:, j, half * 512:(half + 1) * 512], px[:, 0:512],
                                                            xo[:, half * 512:(half + 1) * 512], ALU.add),
                             reads=[px, xo], writes=[acc])
                    if debug:
                        k.dma(sp, dbg["x1"][j], acc[:, j, :], reads=[acc], sembuf=x1L[j % 2])
                    hbsT[j] = norm_pre(acc, 128, acc[:, j, :])

                def tailB(j):
                    norm_post(hbsT.pop(j), 128, gffn, h2T, h2T[:, :, j * 128:(j + 1) * 128])
                    pr_ = k.bank()
                    for kc in range(8):
                        k.op(pe, lambda e: e.matmul(pr_[:, 0:20], h2T[:, kc, j * 128:(j + 1) * 128], wr[:, kc, :],
                                                    start=(kc == 0), stop=(kc == 7)), reads=[h2T, wr], writes=[pr_])
                    k.op(dve, lambda e: e.tensor_tensor(lgA[:, j, :], pr_[:, 0:20], br[:], ALU.add),
                         reads=[pr_, br], writes=[lgA])

                def router_all():
                    n = nslots
                    G = lgA[:, 0:n, 0:4]
                    Ex = lgA[:, 0:n, 4:20].rearrange("p n (g e) -> p n g e", g=4)
                    gmax, gsum, pg, m1, m2, w1, w2 = (rA[:, i, 0:n] for i in range(7))
                    bc4 = lambda ap: ap.unsqueeze(2).to_broadcast([128, n, 4])
                    k.op(dve, lambda e: e.tensor_reduce(gmax, G, AX.X, ALU.max), reads=[lgA], writes=[rA])
                    k.op(dve, lambda e: e.tensor_tensor(dG[:, 0:n, :], G, bc4(gmax), ALU.subtract), reads=[lgA, rA], writes=[dG])
                    k.op(act, lambda e: e.activation(eG[:, 0:n, :], dG[:, 0:n, :], AF.Exp), reads=[dG], writes=[eG])
                    k.op(dve, lambda e: e.tensor_reduce(gsum, eG[:, 0:n, :], AX.X, ALU.add), reads=[eG], writes=[rA])
                    k.op(dve, lambda e: e.reciprocal(pg, gsum), reads=[rA], writes=[rA])
                    k.op(dve, lambda e: e.tensor_scalar(ohg1[:, 0:n, :], dG[:, 0:n, :], 0.0, None, ALU.is_ge), reads=[dG], writes=[ohg1])
                    k.op(dve, lambda e: e.tensor_tensor(ohgp[:, 0:n, :], ohg1[:, 0:n, :], bc4(pg), ALU.mult),
                         reads=[ohg1, rA], writes=[ohgp])
                    t4 = t16A[:, 0:n, :].rearrange("p n (g e) -> p n g e", g=4)
                    k.op(dve, lambda e: e.tensor_tensor(t4, Ex, ohg1[:, 0:n, :].unsqueeze(3).to_broadcast([128, n, 4, 4]), ALU.mult),
                         reads=[lgA, ohg1], writes=[t16A])
                    k.op(dve, lambda e: e.tensor_reduce(eselA[:, 0:n, :], t16A[:, 0:n, :].rearrange("p n (g e) -> p n e g", g=4),
                                                        AX.X, ALU.add), reads=[t16A], writes=[eselA])
                    k.op(dve, lambda e: e.tensor_reduce(m1, eselA[:, 0:n, :], AX.X, ALU.max), reads=[eselA], writes=[rA])
                    k.op(dve, lambda e: e.tensor_tensor(dG[:, 0:n, :], eselA[:, 0:n, :], bc4(m1), ALU.subtract),
                         reads=[eselA, rA], writes=[dG])
                    k.op(dve, lambda e: e.tensor_scalar(oh1A[:, 0:n, :], dG[:, 0:n, :], 0.0, None, ALU.is_ge), reads=[dG], writes=[oh1A])
                    k.op(dve, lambda e: e.scalar_tensor_tensor(emA[:, 0:n, :], oh1A[:, 0:n, :], NEG, eselA[:, 0:n, :],
                                                               ALU.mult, ALU.add), reads=[oh1A, eselA], writes=[emA])
                    k.op(dve, lambda e: e.tensor_reduce(m2, emA[:, 0:n, :], AX.X, ALU.max), reads=[emA], writes=[rA])
                    k.op(dve, lambda e: e.tensor_tensor(dG[:, 0:n, :], emA[:, 0:n, :], bc4(m2), ALU.subtract),
                         reads=[emA, rA], writes=[dG])
                    k.op(dve, lambda e: e.tensor_scalar(oh2A[:, 0:n, :], dG[:, 0:n, :], 0.0, None, ALU.is_ge), reads=[dG], writes=[oh2A])
                    k.op(dve, lambda e: e.tensor_tensor(w2, m1, m2, ALU.subtract), reads=[rA], writes=[rA])
                    k.op(act, lambda e: e.activation(w1, w2, AF.Sigmoid), reads=[rA], writes=[rA])
                    k.op(dve, lambda e: e.tensor_scalar(w2, w1, -1.0, 1.0, ALU.mult, ALU.add), reads=[rA], writes=[rA])
                    k.op(dve, lambda e: e.tensor_tensor(oh1A[:, 0:n, :], oh1A[:, 0:n, :], bc4(w1), ALU.mult),
                         reads=[oh1A, rA], writes=[oh1A])
                    k.op(dve, lambda e: e.tensor_tensor(oh2A[:, 0:n, :], oh2A[:, 0:n, :], bc4(w2), ALU.mult),
                         reads=[oh2A, rA], writes=[oh2A])
                    k.op(dve, lambda e: e.tensor_tensor(oh1A[:, 0:n, :], oh1A[:, 0:n, :], oh2A[:, 0:n, :], ALU.add),
                         reads=[oh1A, oh2A], writes=[oh1A])
                    k.op(dve, lambda e: e.tensor_tensor(
                        gw_all[:, 0:n * 16].rearrange("p (n g e) -> p n g e", g=4, e=4),
                        ohgp[:, 0:n, :].unsqueeze(3).to_broadcast([128, n, 4, 4]),
                        oh1A[:, 0:n, :].unsqueeze(2).to_broadcast([128, n, 4, 4]), ALU.mult),
                         reads=[ohgp, oh1A], writes=[gw_all])

                hbsT = {}
                for j in range(min(2, nslots)):
                    loads_m(j)
                    loads_x(j)
                for it in range(nslots + 3):
                    if 0 <= it - 3 < nslots:
                        tailB(it - 3)
                    if 0 <= it - 2 < nslots:
                        s3(it - 2)
                        if it < nslots:
                            loads_x(it)
                    if 0 <= it - 1 < nslots:
                        s2(it - 1)
                        if it + 1 < nslots:
                            loads_m(it + 1)
                    if it < nslots:
                        s1(it)
                router_all()
                k.barrier()
        def phase_C():
            with ExitStack() as s4:
                gfin = k.sb("gfin", [128, D], F32, s4)
                k.dma(sp, gfin[:], gfin_d, writes=[gfin])
                sgl2 = [k.sb("sgm%d" % i, [128, 512], F32, s4) for i in range(2)]
                hid2 = [k.sb("hid%d" % i, [128, 2, 512], BF16, s4) for i in range(2)]
                ot2 = [k.sb("ot%d" % i, [128, D], F32, s4) for i in range(2)]

                ngrp = (nslots + 3) // 4
                wload(0)
                hi = 0
                for ex in range(16):
                    if ex + 1 < 16:
                        wload(ex + 1)
                    p = ex % 2
                    wg, wu, wd = wg2[p], wu2[p], wd2[p]
                    for tg in range(ngrp):
                        ns_ = min(4, nslots - tg * 4)
                        N = ns_ * 128
                        hid = hid2[hi % 2]
                        hi += 1
                        for fc in range(2):
                            pg = k.bank()
                            pu = k.bank()
                            for kc in range(8):
                                k.op(pe, lambda e, kc=kc, fc=fc, pg=pg: e.matmul(
                                    pg[:, 0:N], wg[:, kc, fc * 128:(fc + 1) * 128], h2T[:, kc, tg * 512:tg * 512 + N],
                                    start=(kc == 0), stop=(kc == 7)), reads=[wg, h2T], writes=[pg])
                            for kc in range(8):
                                k.op(pe, lambda e, kc=kc, fc=fc, pu=pu: e.matmul(
                                    pu[:, 0:N], wu[:, kc, fc * 128:(fc + 1) * 128], h2T[:, kc, tg * 512:tg * 512 + N],
                                    start=(kc == 0), stop=(kc == 7)), reads=[wu, h2T], writes=[pu])
                            sgl = sgl2[fc]
                            k.op(act, lambda e, pg=pg, sgl=sgl: e.activation(sgl[:, 0:N], pg[:, 0:N], AF.Silu),
                                 reads=[pg], writes=[sgl])
                            k.op(dve, lambda e, pu=pu, sgl=sgl, fc=fc, hid=hid: e.tensor_tensor(
                                hid[:, fc, 0:N], pu[:, 0:N], sgl[:, 0:N], ALU.mult), reads=[pu, sgl], writes=[hid])
                        for s_ in range(ns_):
                            j = tg * 4 + s_
                            for half in range(2):
                                py = k.bank()
                                for fc in range(2):
                                    k.op(pe, lambda e, fc=fc, half=half, py=py, s_=s_, hid=hid: e.matmul(
                                        py[:, 0:512], hid[:, fc, s_ * 128:(s_ + 1) * 128],
                                        wd[:, fc, half * 512:(half + 1) * 512], start=(fc == 0), stop=(fc == 1)),
                                         reads=[hid, wd], writes=[py])
                                k.op(dve, lambda e, py=py, j=j, half=half, ex=ex: e.scalar_tensor_tensor(
                                    acc[:, j, half * 512:(half + 1) * 512], py[:, 0:512],
                                    gw_all[:, j * 16 + ex:j * 16 + ex + 1], acc[:, j, half * 512:(half + 1) * 512],
                                    ALU.mult, ALU.add), reads=[py, gw_all, acc], writes=[acc])
                for j in range(nslots):
                    ot = ot2[j % 2]
                    ss, rs, junk = ssL[j % 2], rsL[j % 2], hbL[j % 2]
                    k.op(dve, lambda e, ss=ss: e.memset(ss[:], 0.0), writes=[ss])
                    k.op(act, lambda e, j=j, junk=junk, ss=ss: e.activation(junk[:], acc[:, j, :], AF.Square, accum_out=ss[:, 0:1]),
                         reads=[acc, ss], writes=[junk, ss])
                    k.op(act, lambda e, rs=rs, ss=ss: e.activation(rs[:], ss[:], AF.Sqrt, bias=EPS, scale=1.0 / D), reads=[ss], writes=[rs])
                    k.op(dve, lambda e, rs=rs: e.reciprocal(rs[:], rs[:]), reads=[rs], writes=[rs])
                    k.op(dve, lambda e, j=j, ot=ot, rs=rs: e.scalar_tensor_tensor(ot[:], acc[:, j, :], rs[:, 0:1], gfin[:],
                                                                          ALU.mult, ALU.mult),
                         reads=[acc, rs, gfin], writes=[ot])
                    k.dma(sp, out_d[j * 128:(j + 1) * 128, :], ot[:], reads=[ot], sembuf=ot)
                k.barrier()
        if "A2" in phases:
            phase_A2()
            if debug:
                dbg_A2()
        attn_all = k.sb("attn_all", [128, NS, 512], BF16)
        wao = k.sb("wao", [128, 4, D], BF16)
        wo = k.sb("wo", [128, 8, D], BF16)
        wr = k.sb("wr", [128, 8, 20], BF16)
        br = k.sb("br", [128, 20], F32)

        def load_tail_weights():
            for kc in range(4):
                k.dma(pool, wao[:, kc, :], wao_d[kc * 128:(kc + 1) * 128, :], writes=[wao])
            for kc in range(8):
                k.dma(pool, wo[:, kc, :], wo_d[kc * 128:(kc + 1) * 128, :], writes=[wo])
            k.dma(pool, wr[:], wr_d.rearrange("(k p) c -> p k c", p=128), writes=[wr])
            k.dma(sp, br[:], br_d, writes=[br])

        wloaded = set()

        def wload(ex):
            if ex in wloaded:
                return
            wloaded.add(ex)
            p = ex % 2
            k.dma(pool, wg2[p][:], wg_d[ex].rearrange("(k p) c -> p k c", p=128), writes=[wg2[p]])
            k.dma(pool, wu2[p][:], wu_d[ex].rearrange("(k p) c -> p k c", p=128), writes=[wu2[p]])
            k.dma(pool, wd2[p][:], wd_d[ex].rearrange("(k p) c -> p k c", p=128), writes=[wd2[p]])

        with ExitStack() as skv:
            KT = k.sb("KT", [128, 4, S], BF16, skv)
            V = k.sb("V", [128, NB, 8 * 66], BF16, skv)
            kiT = k.sb("kiT", [128, S], BF16, skv)
            k.op(pool, lambda e: e.memset(V[:], 1.0), writes=[V])
            if "A1" in phases:
                phase_A1()
                if debug:
                    dbg_A1()
            if "B" in phases:
                load_tail_weights()
                phase_B()
            k.barrier()
        wg2 = [k.sb("wg%d" % i, [128, 8, 256], BF16) for i in range(2)]
        wu2 = [k.sb("wu%d" % i, [128, 8, 256], BF16) for i in range(2)]
        wd2 = [k.sb("wd%d" % i, [128, 2, D], BF16) for i in range(2)]
        acc = k.sb("acc", [128, NS, D], F32)
        h2T = k.sb("h2T", [128, 8, NS * 128], BF16)
        if "B" in phases:
            phase_B2()
            if debug:
                k.dma(sp, dbg["gw"], gw_all[:], reads=[gw_all])
        if "C" in phases:
            phase_C()
        k.barrier()
        build_nc.nins = k.nins
    return nc


def own_blocks(r):
    return [2 * j + ((j % 2) ^ r) for j in range(NS)]


def prep_core(inp, c):
    b, r = c // 2, c % 2
    x = np.asarray(inp["x"], dtype=np.float32)
    pos = np.asarray(inp["positions"]).astype(np.int32)
    blocks = own_blocks(r)
    xb = x[b]
    x_own = np.concatenate([xb[i * 128:(i + 1) * 128] for i in blocks], axis=0)
    x_halo = np.zeros((NS * 32, D), np.float32)
    for j, i in enumerate(blocks):
        if i > 0:
            x_halo[j * 32:(j + 1) * 32] = xb[i * 128 - 32:i * 128]
    pos_full = np.ascontiguousarray(pos[b].reshape(NB, 128).T)
    pos_own = np.ascontiguousarray(np.stack([pos[b][i * 128:(i + 1) * 128] for i in blocks], axis=1))
    invf = (1.0 / (np.float32(10000.0) ** (np.arange(0, 64, 2, dtype=np.float32) / np.float32(64)))).astype(np.float32)
    invf = np.ascontiguousarray(np.broadcast_to(invf[None, :], (128, 32)))
    cmask = np.zeros((128, NS, 256), np.float32)
    for j, i in enumerate(blocks):
        qidx = i * 128 + np.arange(128)[:, None]
        kidx = 256 * j + np.arange(256)[None, :]
        cmask[:, j, :] = np.where(kidx > qidx, np.float32(NEG), np.float32(0.0))

    def fm(v, nchunk):
        return np.ascontiguousarray(np.asarray(v, np.float32).reshape(nchunk, 128).T)

    w_dw = np.asarray(inp["w_dw"], np.float32)[0, :, 0, :]
    wdw = np.ascontiguousarray(w_dw.T.reshape(4, 128, 31).transpose(1, 0, 2))
    w_r = np.concatenate([np.asarray(inp["w_rg"], np.float32)[0],
                          np.asarray(inp["w_re"], np.float32)[0].reshape(D, 16)], axis=1)
    b_r = np.concatenate([np.asarray(inp["b_rg"], np.float32)[0], np.asarray(inp["b_re"], np.float32)[0].reshape(16)])
    m = {
        "x_full": np.ascontiguousarray(xb), "x_own": x_own, "x_halo": x_halo,
        "pos_full": pos_full, "pos_own": pos_own, "invf": invf, "cmask": cmask,
        "ident": np.eye(128, dtype=np.float32),
        "w_in": np.ascontiguousarray(np.asarray(inp["w_in"], np.float32)[0]),
        "wdw": wdw, "bdw": fm(inp["b_dw"][0], 4), "lng": fm(inp["ln_g"][0], 4), "lnb": fm(inp["ln_b"][0], 4),
        "gmix": fm(inp["g_mix"][0], 8), "gffn": fm(inp["g_ffn"][0], 8),
        "gfin": np.ascontiguousarray(np.broadcast_to(np.asarray(inp["g_final"], np.float32)[None, :], (128, D))),
        "b_r": np.ascontiguousarray(np.broadcast_to(b_r[None, :], (128, 20))),
        "w_r": np.ascontiguousarray(w_r),
        "w_conv_out": np.ascontiguousarray(np.asarray(inp["w_conv_out"], np.float32)[0]),
        "w_attn_out": np.ascontiguousarray(np.asarray(inp["w_attn_out"], np.float32)[0]),
        "w_o": np.ascontiguousarray(np.asarray(inp["w_o"], np.float32)[0]),
        "w_gate": np.ascontiguousarray(np.asarray(inp["w_gate"], np.float32)[0]),
        "w_up": np.ascontiguousarray(np.asarray(inp["w_up"], np.float32)[0]),
        "w_down": np.ascontiguousarray(np.asarray(inp["w_down"], np.float32)[0]),
    }
    return m


_NC_CACHE = {}


def kernel(**inputs):
    if "nc" not in _NC_CACHE:
        _NC_CACHE["nc"] = build_nc()
    nc = _NC_CACHE["nc"]
    in_maps = [prep_core(inputs, c) for c in range(8)]
    res = run_bass_kernel_spmd(nc, in_maps, core_ids=list(range(8)))
    out = np.empty((4, S, D), np.float32)
    for c in range(8):
        b, r = c // 2, c % 2
        o = np.asarray(res.results[c]["out"], dtype=np.float32)
        for j, i in enumerate(own_blocks(r)):
            out[b, i * 128:(i + 1) * 128] = o[j * 128:(j + 1) * 128]
    return out
```

```python
from contextlib import ExitStack
import numpy as np
import concourse.bass as bass
import concourse.mybir as mybir
from concourse.bass_utils import run_bass_kernel_spmd

F32 = mybir.dt.float32
BF16 = mybir.dt.bfloat16
I32 = mybir.dt.int32
ALU = mybir.AluOpType
AF = mybir.ActivationFunctionType
AX = mybir.AxisListType

D = 1024
S = 4096
NB = 32
NS = 16
EPS = 1e-6
NIT = 14
TOPK = 256
NEG = -1.0e30
C_U, C_Q, C_K, C_V, C_QI, C_KI, C_WI, C_GC, C_GA = 0, 1024, 1536, 2048, 2560, 3072, 3136, 3144, 4168
IDX_SCALE = float((8 ** -0.5) * (64 ** -0.5))
ATT_SCALE = float(64 ** -0.5)


class Buf:
    __slots__ = ("name", "t", "lw", "rd", "dsem", "dcnt")

    def __init__(self, name, t=None):
        self.name = name
        self.t = t
        self.lw = None
        self.rd = {}
        self.dsem = None
        self.dcnt = 0

    def __getitem__(self, key):
        return self.t[key]


class Eng:
    def __init__(self, k, name, eng):
        self.name = name
        self.eng = eng
        self.sem = k.new_sem("e_" + name)
        self.cnt = 0
        self.seen = {}

    def wait_tok(self, tok):
        if tok is None:
            return
        sem, val, _ = tok
        key = id(sem)
        if self.seen.get(key, 0) >= val:
            return
        self.eng.wait_ge(sem, val)
        self.seen[key] = val


class K:
    def __init__(self, nc, stack):
        self.nc = nc
        self.stack = stack
        self.pe = Eng(self, "pe", nc.tensor)
        self.act = Eng(self, "act", nc.scalar)
        self.dve = Eng(self, "dve", nc.vector)
        self.pool = Eng(self, "pool", nc.gpsimd)
        self.sp = Eng(self, "sp", nc.sync)
        self.engs = [self.pe, self.act, self.dve, self.pool, self.sp]
        self.dma_bufs = []
        self.nins = 0
        self.banks = []
        self.bank_i = 0
        self.pinned = set()

    def new_sem(self, name):
        return self.stack.enter_context(self.nc.semaphore(name))

    def sb(self, name, shape, dt, stack=None):
        t = (stack or self.stack).enter_context(self.nc.sbuf_tensor("s_" + name, list(shape), dt))
        return Buf(name, t)

    def mk_banks(self):
        for i in range(8):
            t = self.stack.enter_context(self.nc.psum_tensor("bank%d" % i, [128, 512], F32))
            self.banks.append(Buf("bank%d" % i, t))

    def bank(self, pin=False):
        for _ in range(16):
            b = self.banks[self.bank_i % 8]
            self.bank_i += 1
            if b.name not in self.pinned:
                if pin:
                    self.pinned.add(b.name)
                return b
        raise RuntimeError("no psum bank")

    def unpin(self, b):
        self.pinned.discard(b.name)

    def _deps(self, e, reads, writes):
        for b in reads:
            if b.lw is not None:
                e.wait_tok(b.lw)
        for b in writes:
            if b.lw is not None and b.lw[2] != e.name:
                e.wait_tok(b.lw)
            for en, tok in b.rd.items():
                if en != e.name:
                    e.wait_tok(tok)

    def _commit(self, tok, reads, writes):
        for b in reads:
            b.rd[tok[2]] = tok
        for b in writes:
            b.lw = tok
            b.rd = {}

    def op(self, e, fn, reads=(), writes=()):
        self._deps(e, reads, writes)
        ins = fn(e.eng)
        e.cnt += 1
        ins.then_inc(e.sem, 1)
        tok = (e.sem, e.cnt, e.name)
        self._commit(tok, reads, writes)
        self.nins += 1
        return tok

    def dma(self, e, out, in_, reads=(), writes=(), sembuf=None):
        self._deps(e, reads, writes)
        sb_ = sembuf if sembuf is not None else (writes[0] if writes else reads[0])
        if sb_.dsem is None:
            sb_.dsem = self.new_sem("d_" + sb_.name)
            self.dma_bufs.append(sb_)
        ins = e.eng.dma_start(out=out, in_=in_)
        sb_.dcnt += 16
        ins.then_inc(sb_.dsem, 16)
        tok = (sb_.dsem, sb_.dcnt, "dma_" + sb_.name)
        self._commit(tok, reads, writes)
        self.nins += 1
        return tok

    def barrier(self):
        toks = [(e.sem, e.cnt, e.name) for e in self.engs if e.cnt > 0]
        toks += [(b.dsem, b.dcnt, "dma") for b in self.dma_bufs]
        for e in self.engs:
            for t in toks:
                if t[2] != e.name:
                    e.wait_tok(t)


def bf(ap):
    return ap.bitcast(BF16)


def build_nc(phases=("A1", "A2", "B", "C"), debug=False, nslots=NS, bstop=9):
    nc = bass.Bass("TRN2", target_bir_lowering=False)

    def din(name, shape, dt=F32):
        return nc.dram_tensor(name, list(shape), dt, kind="ExternalInput").ap()

    def dscr(name, shape, dt):
        return nc.dram_tensor(name, list(shape), dt, kind="Internal").ap()

    x_full = din("x_full", [S, D])
    x_own = din("x_own", [NS * 128, D])
    x_halo = din("x_halo", [NS * 32, D])
    pos_full = din("pos_full", [128, NB], I32)
    pos_own = din("pos_own", [128, NS], I32)
    invf = din("invf", [128, 32])
    cmask_d = din("cmask", [128, NS, 256])
    ident_d = din("ident", [128, 128])
    w_in = din("w_in", [D, 5192])
    wdw_d = din("wdw", [128, 4, 31])
    bdw_d = din("bdw", [128, 4])
    lng_d = din("lng", [128, 4])
    lnb_d = din("lnb", [128, 4])
    gmix_d = din("gmix", [128, 8])
    gffn_d = din("gffn", [128, 8])
    gfin_d = din("gfin", [128, D])
    br_d = din("b_r", [128, 20])
    wr_d = din("w_r", [D, 20])
    wco_d = din("w_conv_out", [512, D])
    wao_d = din("w_attn_out", [512, D])
    wo_d = din("w_o", [D, D])
    wg_d = din("w_gate", [16, D, 256])
    wu_d = din("w_up", [16, D, 256])
    wd_d = din("w_down", [16, 256, D])
    out_d = nc.dram_tensor("out", [NS * 128, D], F32, kind="ExternalOutput").ap()

    qT_d = dscr("qT_d", [NS, 128, 512], BF16)
    qiT_d = dscr("qiT_d", [NS, 128, 512], BF16)
    mc_d = dscr("mc_d", [NS, 128, 1024], BF16)
    sga_d = dscr("sga_d", [NS, 128, 1024], BF16)
    x1_d = dscr("x1_d", [NS, 128, D], F32)
    h2T_d = dscr("h2T_d", [NS, 128, 1024], BF16)
    dbg = {}
    if debug:
        dbg["kT"] = nc.dram_tensor("dbg_kT", [128, 4, S], BF16, kind="ExternalOutput").ap()
        dbg["v"] = nc.dram_tensor("dbg_v", [128, NB, 8 * 66], BF16, kind="ExternalOutput").ap()
        dbg["kiT"] = nc.dram_tensor("dbg_kiT", [128, S], BF16, kind="ExternalOutput").ap()
        dbg["qT"] = nc.dram_tensor("dbg_qT", [NS, 128, 512], BF16, kind="ExternalOutput").ap()
        dbg["qiT"] = nc.dram_tensor("dbg_qiT", [NS, 128, 512], BF16, kind="ExternalOutput").ap()
        dbg["mc"] = nc.dram_tensor("dbg_mc", [NS, 128, 1024], BF16, kind="ExternalOutput").ap()
        dbg["sga"] = nc.dram_tensor("dbg_sga", [NS, 128, 1024], BF16, kind="ExternalOutput").ap()
        dbg["wi"] = nc.dram_tensor("dbg_wi", [128, NS * 8], F32, kind="ExternalOutput").ap()
        dbg["x1"] = nc.dram_tensor("dbg_x1", [NS, 128, D], F32, kind="ExternalOutput").ap()
        dbg["gw"] = nc.dram_tensor("dbg_gw", [128, NS * 16], F32, kind="ExternalOutput").ap()
        dbg["sc"] = nc.dram_tensor("dbg_sc", [NS, 128, S], F32, kind="ExternalOutput").ap()
        dbg["thr"] = nc.dram_tensor("dbg_thr", [NS, 128, 1], F32, kind="ExternalOutput").ap()
        dbg["attn"] = nc.dram_tensor("dbg_attn", [NS, 128, 512], F32, kind="ExternalOutput").ap()

    with ExitStack() as st:
        k = K(nc, st)
        pe, act, dve, pool, sp = k.pe, k.act, k.dve, k.pool, k.sp
        k.mk_banks()

        ident = k.sb("ident", [128, 128], BF16)
        identf = k.sb("identf", [128, 128], F32)
        wi_all = k.sb("wi_all", [128, NS * 8], F32)
        gw_all = k.sb("gw_all", [128, NS * 16], F32)
        gmix = k.sb("gmix", [128, 8], F32)
        gffn = k.sb("gffn", [128, 8], F32)
        ssL = [k.sb("ss%d" % i, [128, 1], F32) for i in range(2)]
        rsL = [k.sb("rs%d" % i, [128, 1], F32) for i in range(2)]
        hbL = [k.sb("hb%d" % i, [128, D], BF16) for i in range(2)]
        nrm_i = [0]

        k.dma(sp, identf[:], ident_d, writes=[identf])
        k.dma(pool, ident[:], ident_d, writes=[ident])
        k.dma(sp, gmix[:], gmix_d, writes=[gmix])
        k.dma(sp, gffn[:], gffn_d, writes=[gffn])

        def rope_tables(pos_d, n, cosT, sinT, stk):
            pi_ = k.sb("pos_i%d" % n, [128, n], I32, stk)
            pf = k.sb("pos_f%d" % n, [128, n], F32, stk)
            iv = k.sb("invf%d" % n, [128, 32], F32, stk)
            ang = k.sb("ang%d" % n, [128, n, 32], F32, stk)
            tmp = k.sb("angt%d" % n, [128, n, 32], F32, stk)
            k.dma(sp, pi_[:], pos_d, writes=[pi_])
            k.dma(sp, iv[:], invf, writes=[iv])
            k.op(dve, lambda e: e.tensor_copy(pf[:], pi_[:]), reads=[pi_], writes=[pf])
            k.op(dve, lambda e: e.tensor_tensor(ang[:], pf[:].unsqueeze(2).to_broadcast([128, n, 32]),
                                                iv[:].unsqueeze(1).to_broadcast([128, n, 32]), ALU.mult),
                 reads=[pf, iv], writes=[ang])
            ki_ = k.sb("angk%d" % n, [128, n, 32], I32, stk)
            kf_ = k.sb("angf%d" % n, [128, n, 32], F32, stk)
            two_pi = float(2 * np.pi)
            C1 = 6.28125
            C2 = float(2 * np.pi - 6.28125)
            PI_SAFE = 3.1415925

            def sin_of(dst, shift):
                if shift != 0.0:
                    k.op(dve, lambda e: e.tensor_scalar(tmp[:], ang[:], shift, None, ALU.add), reads=[ang], writes=[tmp])
                    src = tmp
                else:
                    src = ang
                k.op(dve, lambda e: e.tensor_scalar(kf_[:], src[:], 1.0 / two_pi, None, ALU.mult),
                     reads=[src], writes=[kf_])
                k.op(dve, lambda e: e.tensor_copy(ki_[:], kf_[:]), reads=[kf_], writes=[ki_])
                k.op(dve, lambda e: e.tensor_copy(kf_[:], ki_[:]), reads=[ki_], writes=[kf_])
                k.op(dve, lambda e: e.scalar_tensor_tensor(tmp[:], kf_[:], -C1, src[:], ALU.mult, ALU.add),
                     reads=[kf_, src], writes=[tmp])
                k.op(dve, lambda e: e.scalar_tensor_tensor(tmp[:], kf_[:], -C2, tmp[:], ALU.mult, ALU.add),
                     reads=[kf_, tmp], writes=[tmp])
                k.op(dve, lambda e: e.tensor_scalar(kf_[:], tmp[:], float(np.pi), -two_pi, ALU.is_gt, ALU.mult),
                     reads=[tmp], writes=[kf_])
                k.op(dve, lambda e: e.tensor_tensor(tmp[:], tmp[:], kf_[:], ALU.add), reads=[tmp, kf_], writes=[tmp])
                k.op(dve, lambda e: e.tensor_scalar(kf_[:], tmp[:], -float(np.pi), two_pi, ALU.is_lt, ALU.mult),
                     reads=[tmp], writes=[kf_])
                k.op(dve, lambda e: e.tensor_tensor(tmp[:], tmp[:], kf_[:], ALU.add), reads=[tmp, kf_], writes=[tmp])
                k.op(dve, lambda e: e.tensor_scalar(tmp[:], tmp[:], -PI_SAFE, PI_SAFE, ALU.max, ALU.min),
                     reads=[tmp], writes=[tmp])
                k.op(act, lambda e: e.activation(dst[:], tmp[:], AF.Sin), reads=[tmp], writes=[dst])

            sin_of(sinT, 0.0)
            sin_of(cosT, float(0.5 * np.pi))

        hbN = 4
        hbX = [k.sb("hbx%d" % i, [128, D], BF16) for i in range(hbN - 2)]

        def norm_pre(xt, n, src=None):
            if src is None:
                src = xt[0:n, :]
            hbs = hbL + hbX
            ss, rs, hb = ssL[nrm_i[0] % 2], rsL[nrm_i[0] % 2], hbs[nrm_i[0] % hbN]
            junk = hb
            nrm_i[0] += 1
            k.op(dve, lambda e: e.memset(ss[0:n, :], 0.0), writes=[ss])
            k.op(act, lambda e: e.activation(junk[0:n, :], src, AF.Square, accum_out=ss[0:n, 0:1]),
                 reads=[xt, ss], writes=[junk, ss])
            k.op(act, lambda e: e.activation(rs[0:n, :], ss[0:n, :], AF.Sqrt, bias=EPS, scale=1.0 / D),
                 reads=[ss], writes=[rs])
            k.op(dve, lambda e: e.reciprocal(rs[0:n, :], rs[0:n, :]), reads=[rs], writes=[rs])
            k.op(act, lambda e: e.activation(hb[0:n, :], src, AF.Copy, scale=rs[0:n, 0:1]),
                 reads=[xt, rs], writes=[hb])
            return hb

        def norm_post(hb, n, gT, hT, hT_ap=None):
            if hT_ap is None:
                hT_ap = hT[:, :, 0:n]
            tp = k.bank()
            tpv = bf(tp[:])[:, 0:8 * 128].rearrange("p (a b) -> p a b", a=8)
            for kc in range(8):
                k.op(pe, lambda e, kc=kc: e.transpose(tpv[:, kc, 0:n], hb[0:n, kc * 128:(kc + 1) * 128],
                                                      ident[0:n, 0:n]),
                     reads=[hb, ident], writes=[tp])
            k.op(dve, lambda e: e.tensor_tensor(hT_ap, tpv[:, :, 0:n],
                                                gT[:].unsqueeze(2).to_broadcast([128, 8, n]), ALU.mult),
                 reads=[tp, gT], writes=[hT])

        def norm_T(xt, n, gT, hT, hT_ap=None):
            hb = norm_pre(xt, n)
            norm_post(hb, n, gT, hT, hT_ap)

        def proj_tok(hT, w, c0, ncols):
            pb = k.bank()
            for kc in range(8):
                k.op(pe, lambda e, kc=kc: e.matmul(pb[:, 0:ncols], hT[:, kc, :], w[:, kc, c0:c0 + ncols],
                                                   start=(kc == 0), stop=(kc == 7)),
                     reads=[hT, w], writes=[pb])
            return pb

        def rope(pb, nh, cosT, sinT, ti, outb, tmpA, tmpB):
            pv = pb[:, 0:nh * 64].rearrange("p (h d) -> p h d", h=nh)
            ov = outb[:, 0:nh * 64].rearrange("p (h d) -> p h d", h=nh)
            av = tmpA[:, 0:nh * 64].rearrange("p (h d) -> p h d", h=nh)
            bv = tmpB[:, 0:nh * 64].rearrange("p (h d) -> p h d", h=nh)
            cb = cosT[:, ti, :].unsqueeze(1).to_broadcast([128, nh, 32])
            sb_ = sinT[:, ti, :].unsqueeze(1).to_broadcast([128, nh, 32])
            k.op(dve, lambda e: e.tensor_tensor(av[:, :, 0:32], pv[:, :, 0:32], cb, ALU.mult),
                 reads=[pb, cosT], writes=[tmpA])
            k.op(dve, lambda e: e.tensor_tensor(av[:, :, 32:64], pv[:, :, 32:64], cb, ALU.mult),
                 reads=[pb, cosT], writes=[tmpA])
            k.op(dve, lambda e: e.tensor_tensor(bv[:, :, 0:32], pv[:, :, 32:64], sb_, ALU.mult),
                 reads=[pb, sinT], writes=[tmpB])
            k.op(dve, lambda e: e.tensor_tensor(bv[:, :, 32:64], pv[:, :, 0:32], sb_, ALU.mult),
                 reads=[pb, sinT], writes=[tmpB])
            k.op(dve, lambda e: e.tensor_tensor(ov[:, :, 0:32], av[:, :, 0:32], bv[:, :, 0:32], ALU.subtract),
                 reads=[tmpA, tmpB], writes=[outb])
            k.op(dve, lambda e: e.tensor_tensor(ov[:, :, 32:64], av[:, :, 32:64], bv[:, :, 32:64], ALU.add),
                 reads=[tmpA, tmpB], writes=[outb])

        w_in_v = w_in.rearrange("(k p) c -> p k c", p=128)
        def phase_A1():
            with ExitStack() as s1:
                cosF = k.sb("cosF", [128, NB, 32], F32, s1)
                sinF = k.sb("sinF", [128, NB, 32], F32, s1)
                rope_tables(pos_full, NB, cosF, sinF, s1)
                wA = k.sb("wA1", [128, 8, 1088], BF16, s1)
                for kc in range(8):
                    k.dma(pool, wA[:, kc, 0:1024], w_in_v[:, kc, C_K:C_K + 1024], writes=[wA])
                    k.dma(pool, wA[:, kc, 1024:1088], w_in_v[:, kc, C_KI:C_KI + 64], writes=[wA])
                xt2 = [k.sb("xtA%d" % i, [128, D], F32, s1) for i in range(2)]
                hT2 = [k.sb("hTA%d" % i, [128, 8, 128], BF16, s1) for i in range(2)]
                krL = [k.sb("kr%d" % i, [128, 512], BF16, s1) for i in range(2)]
                kirL = [k.sb("kir%d" % i, [128, 128], BF16, s1) for i in range(2)]
                tAL = [k.sb("tA%d" % i, [128, 512], F32, s1) for i in range(2)]
                tBL = [k.sb("tB%d" % i, [128, 512], F32, s1) for i in range(2)]
                tCL = [k.sb("tC%d" % i, [128, 64], F32, s1) for i in range(2)]
                tDL = [k.sb("tD%d" % i, [128, 64], F32, s1) for i in range(2)]
                hbs1 = {}

                def pre1(ti):
                    k.dma(sp, xt2[ti % 2][:], x_full[ti * 128:(ti + 1) * 128, :], writes=[xt2[ti % 2]])
                    hbs1[ti] = norm_pre(xt2[ti % 2], 128)

                def post1(ti):
                    norm_post(hbs1.pop(ti), 128, gmix, hT2[ti % 2])

                def part2(ti):
                    kr, kir = krL[ti % 2], kirL[ti % 2]
                    tb = k.bank()
                    tbv = bf(tb[:])
                    for pr in range(4):
                        k.op(pe, lambda e, pr=pr: e.transpose(tbv[:, pr * 128:(pr + 1) * 128],
                                                              kr[:, pr * 128:(pr + 1) * 128], ident[:]),
                             reads=[kr, ident], writes=[tb])
                    k.op(pe, lambda e: e.transpose(tbv[:, 512:640], kir[:], ident[:]), reads=[kir, ident], writes=[tb])
                    k.op(act, lambda e: e.activation(KT[:, :, ti * 128:(ti + 1) * 128],
                                                     tbv[:, 0:512].rearrange("p (a b) -> p a b", a=4), AF.Copy),
                         reads=[tb], writes=[KT])
                    k.op(act, lambda e: e.activation(kiT[:, ti * 128:(ti + 1) * 128], tbv[:, 512:640], AF.Copy),
                         reads=[tb], writes=[kiT])

                pre1(0)
                post1(0)
                pre1(1)
                for ti in range(NB):
                    if ti + 1 < NB:
                        post1(ti + 1)
                    hT = hT2[ti % 2]
                    kr, kir, tA, tB = krL[ti % 2], kirL[ti % 2], tAL[ti % 2], tBL[ti % 2]
                    tC, tD = tCL[ti % 2], tDL[ti % 2]
                    pk = proj_tok(hT, wA, 0, 512)
                    pv = proj_tok(hT, wA, 512, 512)
                    pki = proj_tok(hT, wA, 1024, 64)
                    if ti + 2 < NB:
                        pre1(ti + 2)
                    rope(pk, 8, cosF, sinF, ti, kr, tA, tB)
                    Vv = V[:, ti, :].rearrange("p (h d) -> p h d", h=8)
                    k.op(act, lambda e: e.activation(Vv[:, :, 0:64],
                                                     pv[:, 0:512].rearrange("p (h d) -> p h d", h=8), AF.Copy),
                         reads=[pv], writes=[V])
                    rope(pki, 1, cosF, sinF, ti, kir, tC, tD)
                    k.op(dve, lambda e, kir=kir: e.tensor_copy(kir[:, 64:128], kir[:, 0:64]), reads=[kir], writes=[kir])
                    if ti >= 1:
                        part2(ti - 1)
                part2(NB - 1)
                k.barrier()
        def dbg_A1():
            for pr in range(4):
                k.dma(sp, dbg["kT"][:, pr, :], KT[:, pr, :], reads=[KT])
            for t8 in range(0, NB, 4):
                k.dma(sp, dbg["v"][:, t8:t8 + 4, :], V[:, t8:t8 + 4, :], reads=[V])
            k.dma(sp, dbg["kiT"], kiT[:], reads=[kiT])

        def phase_A2():
            with ExitStack() as s2:
                cosO = k.sb("cosO", [128, NS, 32], F32, s2)
                sinO = k.sb("sinO", [128, NS, 32], F32, s2)
                rope_tables(pos_own, NS, cosO, sinO, s2)
                WU, WQ, WQI, WWI, WGC, WGA = 0, 1024, 1536, 2048, 2056, 3080
                wB = k.sb("wB", [128, 8, 4104], BF16, s2)
                wBq, wBu, wBg = Buf("wBq", wB.t), Buf("wBu", wB.t), Buf("wBg", wB.t)
                for kc in range(8):
                    k.dma(pool, wB[:, kc, WQ:WQ + 512], w_in_v[:, kc, C_Q:C_Q + 512], writes=[wBq])
                    k.dma(pool, wB[:, kc, WQI:WQI + 512], w_in_v[:, kc, C_QI:C_QI + 512], writes=[wBq])
                    k.dma(pool, wB[:, kc, WWI:WWI + 8], w_in_v[:, kc, C_WI:C_WI + 8], writes=[wBq])
                for kc in range(8):
                    k.dma(pool, wB[:, kc, WU:WU + 1024], w_in_v[:, kc, C_U:C_U + 1024], writes=[wBu])
                for kc in range(8):
                    k.dma(pool, wB[:, kc, WGC:WGC + 2048], w_in_v[:, kc, C_GC:C_GC + 2048], writes=[wBg])
                wco = k.sb("wco", [128, 4, D], BF16, s2)
                for kc in range(4):
                    k.dma(pool, wco[:, kc, :], wco_d[kc * 128:(kc + 1) * 128, :], writes=[wco])
                wdw = k.sb("wdw", [128, 4, 31], F32, s2)
                bdw = k.sb("bdw", [128, 4], F32, s2)
                lng = k.sb("lng", [128, 4], F32, s2)
                lnb = k.sb("lnb", [128, 4], F32, s2)
                k.dma(sp, wdw[:], wdw_d, writes=[wdw])
                k.dma(sp, bdw[:], bdw_d, writes=[bdw])
                k.dma(sp, lng[:], lng_d, writes=[lng])
                k.dma(sp, lnb[:], lnb_d, writes=[lnb])
                Dg = k.sb("Dg", [128, 124, 128], BF16, s2)
                for c in range(4):
                    for j in range(31):
                        k.op(dve, lambda e, c=c, j=j: e.tensor_scalar(Dg[:, c * 31 + j, :], identf[:],
                                                                       wdw[:, c, j:j + 1], None, ALU.mult),
                             reads=[identf, wdw], writes=[Dg])
                ones = k.sb("ones", [128, 128], F32, s2)
                k.op(pool, lambda e: e.memset(ones[:], 1.0), writes=[ones])
                xt2 = [k.sb("xtB%d" % i, [128, D], F32, s2) for i in range(2)]
                xh2 = [k.sb("xhB%d" % i, [32, D], F32, s2) for i in range(2)]
                hT_L = [k.sb("hTB_%d" % i_, [128, 8, 128], BF16, s2) for i_ in range(2)]
                hTh_L = [k.sb("hThB_%d" % i_, [128, 8, 32], BF16, s2) for i_ in range(2)]
                qr_L = [k.sb("qr_%d" % i_, [128, 512], BF16, s2) for i_ in range(2)]
                qir_L = [k.sb("qir_%d" % i_, [128, 512], BF16, s2) for i_ in range(2)]
                tA_L = [k.sb("tA2_%d" % i_, [128, 512], F32, s2) for i_ in range(2)]
                tB_L = [k.sb("tB2_%d" % i_, [128, 512], F32, s2) for i_ in range(2)]
                qT_s_L = [k.sb("qT_s_%d" % i_, [128, 512], BF16, s2) for i_ in range(2)]
                qiT_s_L = [k.sb("qiT_s_%d" % i_, [128, 512], BF16, s2) for i_ in range(2)]
                sg_L = [k.sb("sgl_%d" % i_, [128, 160], F32, s2) for i_ in range(2)]
                gT_L = [k.sb("gT_%d" % i_, [128, 4, 160], BF16, s2) for i_ in range(2)]
                csb_L = [k.sb("csb_%d" % i_, [128, 4, 128], F32, s2) for i_ in range(2)]
                csq_L = [k.sb("csq_%d" % i_, [128, 4, 128], F32, s2) for i_ in range(2)]
                mean_L = [k.sb("mean_%d" % i_, [128, 128], F32, s2) for i_ in range(2)]
                msq_L = [k.sb("msq_%d" % i_, [128, 128], F32, s2) for i_ in range(2)]
                rstd_L = [k.sb("rstd_%d" % i_, [128, 128], F32, s2) for i_ in range(2)]
                nrm_L = [k.sb("nrm_%d" % i_, [128, 4, 128], F32, s2) for i_ in range(2)]
                snT_L = [k.sb("snT_%d" % i_, [128, 4, 128], BF16, s2) for i_ in range(2)]
                sgc_L = [k.sb("sgc_%d" % i_, [128, 8, 128], F32, s2) for i_ in range(2)]
                mc_s_L = [k.sb("mc_s_%d" % i_, [128, 8, 128], BF16, s2) for i_ in range(2)]
                sga_s_L = [k.sb("sga_s_%d" % i_, [128, 8, 128], BF16, s2) for i_ in range(2)]
                qTd_b = Buf("qTd_b")
                mcd_b = Buf("mcd_b")

                hbs2 = {}

                def pre2(j):
                    k.dma(sp, xt2[j % 2][:], x_own[j * 128:(j + 1) * 128, :], writes=[xt2[j % 2]])
                    k.dma(sp, xh2[j % 2][:], x_halo[j * 32:(j + 1) * 32, :], writes=[xh2[j % 2]])
                    hbs2[j] = (norm_pre(xh2[j % 2], 32), norm_pre(xt2[j % 2], 128))

                def post2(j):
                    a_, b_ = hbs2.pop(j)
                    norm_post(a_, 32, gmix, hTh_L[j % 2])
                    norm_post(b_, 128, gmix, hT_L[j % 2])

                for j in range(nslots):
                    xt = xt2[j % 2]
                    xh = xh2[j % 2]
                    hT, hTh, qr, qir, tA, tB, qT_s, qiT_s, sg, gT, csb, csq, mean, msq, rstd, nrm, snT, sgc, mc_s, sga_s = hT_L[j % 2], hTh_L[j % 2], qr_L[j % 2], qir_L[j % 2], tA_L[j % 2], tB_L[j % 2], qT_s_L[j % 2], qiT_s_L[j % 2], sg_L[j % 2], gT_L[j % 2], csb_L[j % 2], csq_L[j % 2], mean_L[j % 2], msq_L[j % 2], rstd_L[j % 2], nrm_L[j % 2], snT_L[j % 2], sgc_L[j % 2], mc_s_L[j % 2], sga_s_L[j % 2]
                    if j == 0:
                        pre2(0)
                        post2(0)
                        if nslots > 1:
                            pre2(1)
                    if j + 1 < nslots:
                        post2(j + 1)
                    pq = proj_tok(hT, wBq, WQ, 512)
                    rope(pq, 8, cosO, sinO, j, qr, tA, tB)
                    pqi = proj_tok(hT, wBq, WQI, 512)
                    rope(pqi, 8, cosO, sinO, j, qir, tA, tB)
                    pw = proj_tok(hT, wBq, WWI, 8)
                    k.op(act, lambda e: e.activation(wi_all[:, j * 8:(j + 1) * 8], pw[:, 0:8], AF.Copy,
                                                     scale=IDX_SCALE), reads=[pw], writes=[wi_all])
                    for src, dstb, dd in ((qr, qT_s, qT_d), (qir, qiT_s, qiT_d)):
                        tb = k.bank()
                        tbv = bf(tb[:])
                        for pr in range(4):
                            k.op(pe, lambda e, pr=pr, src=src, tbv=tbv: e.transpose(
                                tbv[:, pr * 128:(pr + 1) * 128], src[:, pr * 128:(pr + 1) * 128], ident[:]),
                                 reads=[src, ident], writes=[tb])
                        k.op(act, lambda e, dstb=dstb, tbv=tbv: e.activation(dstb[:], tbv[:, 0:512], AF.Copy),
                             reads=[tb], writes=[dstb])
                        k.dma(sp, dd[j], dstb[:], reads=[dstb], writes=[qTd_b], sembuf=dstb)
                    for c in range(4):
                        pu = k.bank()
                        puv = pu[:, 0:320].rearrange("p (a b) -> p a b", a=2)
                        for half, col0 in ((0, WU + c * 128), (1, WU + 512 + c * 128)):
                            for kc in range(8):
                                k.op(pe, lambda e, kc=kc, half=half, col0=col0, puv=puv: e.matmul(
                                    puv[:, half, 0:32], wB[:, kc, col0:col0 + 128], hTh[:, kc, :],
                                    start=(kc == 0), stop=(kc == 7)), reads=[wBu, hTh], writes=[pu])
                            for kc in range(8):
                                k.op(pe, lambda e, kc=kc, half=half, col0=col0, puv=puv: e.matmul(
                                    puv[:, half, 32:160], wB[:, kc, col0:col0 + 128], hT[:, kc, :],
                                    start=(kc == 0), stop=(kc == 7)), reads=[wBu, hT], writes=[pu])
                        k.op(act, lambda e, puv=puv: e.activation(sg[:], puv[:, 1, :], AF.Sigmoid),
                             reads=[pu], writes=[sg])
                        k.op(dve, lambda e, puv=puv, c=c: e.tensor_tensor(gT[:, c, :], puv[:, 0, :], sg[:], ALU.mult),
                             reads=[pu, sg], writes=[gT])
                    if j + 2 < nslots:
                        pre2(j + 2)
                    pc = k.bank()
                    pcv = pc[:, 0:512].rearrange("p (a b) -> p a b", a=4)
                    for c in range(4):
                        for jj in range(31):
                            k.op(pe, lambda e, c=c, jj=jj: e.matmul(pcv[:, c, :], Dg[:, c * 31 + jj, :],
                                                                    gT[:, c, 2 + jj:2 + jj + 128],
                                                                    start=(jj == 0), stop=(jj == 30)),
                                 reads=[Dg, gT], writes=[pc])
                    k.op(dve, lambda e: e.tensor_tensor(csb[:], pcv, bdw[:].unsqueeze(2).to_broadcast([128, 4, 128]),
                                                        ALU.add), reads=[pc, bdw], writes=[csb])
                    k.op(act, lambda e: e.activation(csq[:], csb[:], AF.Square), reads=[csb], writes=[csq])
                    pst = k.bank()
                    for c in range(4):
                        k.op(pe, lambda e, c=c: e.matmul(pst[:, 0:128], ones[:], csb[:, c, :], start=(c == 0),
                                                         stop=(c == 3)), reads=[ones, csb], writes=[pst])
                    for c in range(4):
                        k.op(pe, lambda e, c=c: e.matmul(pst[:, 128:256], ones[:], csq[:, c, :], start=(c == 0),
                                                         stop=(c == 3)), reads=[ones, csq], writes=[pst])
                    k.op(act, lambda e: e.activation(mean[:], pst[:, 0:128], AF.Copy, scale=1.0 / 512),
                         reads=[pst], writes=[mean])
                    k.op(dve, lambda e: e.tensor_tensor(msq[:], mean[:], mean[:], ALU.mult), reads=[mean], writes=[msq])
                    k.op(dve, lambda e: e.scalar_tensor_tensor(rstd[:], pst[:, 128:256], 1.0 / 512, msq[:],
                                                               ALU.mult, ALU.subtract),
                         reads=[pst, msq], writes=[rstd])
                    k.op(act, lambda e: e.activation(rstd[:], rstd[:], AF.Sqrt, bias=EPS, scale=1.0),
                         reads=[rstd], writes=[rstd])
                    k.op(dve, lambda e: e.reciprocal(rstd[:], rstd[:]), reads=[rstd], writes=[rstd])
                    k.op(dve, lambda e: e.tensor_tensor(nrm[:], csb[:], mean[:].unsqueeze(1).to_broadcast([128, 4, 128]),
                                                        ALU.subtract), reads=[csb, mean], writes=[nrm])
                    k.op(dve, lambda e: e.tensor_tensor(nrm[:], nrm[:], rstd[:].unsqueeze(1).to_broadcast([128, 4, 128]),
                                                        ALU.mult), reads=[nrm, rstd], writes=[nrm])
                    for c in range(4):
                        k.op(act, lambda e, c=c: e.activation(snT[:, c, :], nrm[:, c, :], AF.Silu,
                                                              bias=lnb[:, c:c + 1], scale=lng[:, c:c + 1]),
                             reads=[nrm, lnb, lng], writes=[snT])
                    for gi, (wcol, dst) in enumerate(((WGC, None), (WGA, sga_s))):
                        for hb_ in range(2):
                            pg = k.bank()
                            pgv = pg[:, 0:512].rearrange("p (a b) -> p a b", a=4)
                            for m in range(4):
                                col0 = wcol + (hb_ * 4 + m) * 128
                                for kc in range(8):
                                    k.op(pe, lambda e, kc=kc, m=m, col0=col0, pgv=pgv: e.matmul(
                                        pgv[:, m, :], wB[:, kc, col0:col0 + 128], hT[:, kc, :],
                                        start=(kc == 0), stop=(kc == 7)), reads=[wBg, hT], writes=[pg])
                            tgt = sgc if gi == 0 else sga_s
                            k.op(act, lambda e, pgv=pgv, tgt=tgt, hb_=hb_: e.activation(
                                tgt[:, hb_ * 4:(hb_ + 1) * 4, :], pgv, AF.Sigmoid), reads=[pg], writes=[tgt])
                    for hb_ in range(2):
                        py = k.bank()
                        pyv = py[:, 0:512].rearrange("p (a b) -> p a b", a=4)
                        for m in range(4):
                            mm = hb_ * 4 + m
                            for kc in range(4):
                                k.op(pe, lambda e, kc=kc, m=m, mm=mm, pyv=pyv: e.matmul(
                                    pyv[:, m, :], wco[:, kc, mm * 128:(mm + 1) * 128], snT[:, kc, :],
                                    start=(kc == 0), stop=(kc == 3)), reads=[wco, snT], writes=[py])
                        k.op(dve, lambda e, pyv=pyv, hb_=hb_: e.tensor_tensor(
                            mc_s[:, hb_ * 4:(hb_ + 1) * 4, :], pyv, sgc[:, hb_ * 4:(hb_ + 1) * 4, :], ALU.mult),
                             reads=[py, sgc], writes=[mc_s])
                    k.dma(sp, mc_d[j], mc_s[:].rearrange("p a b -> p (a b)"), reads=[mc_s], writes=[mcd_b], sembuf=mc_s)
                    k.dma(sp, sga_d[j], sga_s[:].rearrange("p a b -> p (a b)"), reads=[sga_s], writes=[mcd_b],
                          sembuf=sga_s)
                k.barrier()
        def dbg_A2():
            k.dma(sp, dbg["wi"], wi_all[:], reads=[wi_all])
            for nm, src in (("qT", qT_d), ("qiT", qiT_d), ("mc", mc_d), ("sga", sga_d)):
                b_ = Buf("dbgc_" + nm)
                k.dma(sp, dbg[nm], src, writes=[b_])
            k.barrier()

        def phase_B():
            with ExitStack() as s3:
                cm2 = [k.sb("cm%d" % i, [128, 256], F32, s3) for i in range(2)]
                pw2 = k.sb("pw2", [128, NIT + 2], F32, s3)
                for it in range(NIT + 2):
                    k.op(dve, lambda e, it=it: e.memset(pw2[:, it:it + 1], float(2.0 ** -(it + 1))), writes=[pw2])
                qT2 = [k.sb("qTb%d" % i, [128, 4, 256], BF16, s3) for i in range(2)]
                for b_ in qT2:
                    k.op(dve, lambda e: e.memset(b_[:], 0.0), writes=[b_])
                qiT2 = [k.sb("qiTb%d" % i, [128, 512], BF16, s3) for i in range(2)]
                scL = [k.sb("sc%d" % i, [128, S], F32, s3) for i in range(2)]
                AL = [k.sb("Ab%d" % i, [128, 512], BF16, s3) for i in range(4)]
                wabs = k.sb("wabs", [128, 8], F32, s3)
                sgn = k.sb("sgn", [128, 8], F32, s3)
                sgd = k.sb("sgd", [128, 8, 128], BF16, s3)
                msk = k.sb("msk", [128, S], BF16, s3)
                Mb = k.sb("Mb", [128, NB, 128], BF16, s3)
                st8 = k.sb("st8", [128, 8], F32, s3)
                Q = k.sb("Qtab", [128, NIT + 2], F32, s3)
                Q2 = k.sb("Q2tab", [128, NIT + 2], F32, s3)
                E2 = [k.sb("Eb%d" % i, [128, 4, 128], BF16, s3) for i in range(3)]
                rden = k.sb("rden", [128, 8], F32, s3)
                Osb = k.sb("Osb", [128, 2, 4, 66], F32, s3)
                thr = st8[:, 6:7]
                ai = [0]
                MBIG = 30000.0
                if debug:
                    attnf = k.sb("attnf", [128, 512], F32, s3)

                def loads_a(j):
                    p = j % 2
                    k.dma(sp, qiT2[p][:], qiT_d[j], writes=[qiT2[p]])
                    k.dma(sp, cm2[p][:], cmask_d[:, j, :], writes=[cm2[p]])

                def loads_b(j):
                    p = j % 2
                    k.dma(sp, qT2[p][0:64, :, 0:128], qT_d[j][0:64, :].rearrange("p (a b) -> p a b", a=4), writes=[qT2[p]])
                    k.dma(sp, qT2[p][64:128, :, 128:256], qT_d[j][64:128, :].rearrange("p (a b) -> p a b", a=4),
                          writes=[qT2[p]])

                def prep(j):
                    wsl = wi_all[:, j * 8:(j + 1) * 8]
                    k.op(dve, lambda e: e.tensor_scalar(sgn[:], wsl, 0.0, 2.0, ALU.is_ge, ALU.mult), reads=[wi_all], writes=[sgn])
                    k.op(dve, lambda e: e.tensor_scalar(sgn[:], sgn[:], -1.0, None, ALU.add), reads=[sgn], writes=[sgn])
                    k.op(dve, lambda e: e.tensor_tensor(wabs[:], wsl, sgn[:], ALU.mult), reads=[wi_all, sgn], writes=[wabs])
                    k.op(dve, lambda e: e.tensor_tensor(sgd[:], ident[:].unsqueeze(1).to_broadcast([128, 8, 128]),
                                                        sgn[:].unsqueeze(2).to_broadcast([128, 8, 128]), ALU.mult),
                         reads=[ident, sgn], writes=[sgd])

                def indexer(j):
                    sc = scL[j % 2]
                    qiTs = qiT2[j % 2]
                    L = 256 * (j + 1)
                    nch = (L + 511) // 512
                    for cc in range(nch):
                        c0 = cc * 512
                        W = min(512, L - c0)
                        pacc = k.bank(pin=True)
                        pbs = {}

                        def qi_mm(h):
                            pb = k.bank()
                            pp = (h % 2) * 64
                            k.op(pe, lambda e: e.matmul(pb[:, 0:W], qiTs[pp:pp + 64, (h // 2) * 128:(h // 2 + 1) * 128],
                                                        kiT[pp:pp + 64, c0:c0 + W], start=True, stop=True),
                                 reads=[qiTs, kiT], writes=[pb])
                            pbs[h] = pb

                        qi_mm(0)
                        qi_mm(1)
                        qi_mm(2)
                        for h in range(8):
                            if h + 3 < 8:
                                qi_mm(h + 3)
                            pb = pbs[h]
                            A = AL[ai[0] % 4]
                            ai[0] += 1
                            k.op(act, lambda e: e.activation(A[:, 0:W], pb[:, 0:W], AF.Relu, scale=wabs[:, h:h + 1]),
                                 reads=[pb, wabs], writes=[A])
                            k.op(pe, lambda e: e.matmul(pacc[:, 0:W], sgd[:, h, :], A[:, 0:W], start=(h == 0), stop=(h == 7)),
                                 reads=[sgd, A], writes=[pacc])
                        k.op(act, lambda e: e.activation(sc[:, c0:c0 + W], pacc[:, 0:W], AF.Copy), reads=[pacc], writes=[sc])
                        k.unpin(pacc)

                def thresh(j):
                    sc = scL[j % 2]
                    L = 256 * (j + 1)
                    cmask = cm2[j % 2]
                    if j == 0:
                        k.op(dve, lambda e: e.tensor_tensor(sc[:, L - 256:L], sc[:, L - 256:L], cmask[:], ALU.add),
                             reads=[sc, cmask], writes=[sc])
                        k.op(dve, lambda e: e.memset(thr, -1.0e29), writes=[st8])
                        return
                    k.op(dve, lambda e: e.tensor_reduce(st8[:, 0:1], sc[:, 0:L], AX.X, ALU.max), reads=[sc], writes=[st8])
                    k.op(dve, lambda e: e.tensor_reduce(st8[:, 1:2], sc[:, 0:L], AX.X, ALU.min), reads=[sc], writes=[st8])
                    k.op(dve, lambda e: e.tensor_tensor(sc[:, L - 256:L], sc[:, L - 256:L], cmask[:], ALU.add),
                         reads=[sc, cmask], writes=[sc])
                    k.op(dve, lambda e: e.tensor_tensor(st8[:, 2:3], st8[:, 0:1], st8[:, 1:2], ALU.subtract),
                         reads=[st8], writes=[st8])
                    k.op(dve, lambda e: e.tensor_scalar(st8[:, 5:6], st8[:, 2:3], float(2.0 ** -10), None, ALU.mult),
                         reads=[st8], writes=[st8])
                    k.op(dve, lambda e: e.tensor_scalar(st8[:, 2:3], st8[:, 2:3], float(1.0 + 2.0 ** -9), 1e-30,
                                                        ALU.mult, ALU.add), reads=[st8], writes=[st8])
                    k.op(dve, lambda e: e.tensor_tensor(st8[:, 7:8], st8[:, 1:2], st8[:, 5:6], ALU.subtract),
                         reads=[st8], writes=[st8])
                    k.op(dve, lambda e: e.tensor_scalar(Q[:], pw2[:], st8[:, 2:3], None, ALU.mult),
                         reads=[pw2, st8], writes=[Q])
                    k.op(dve, lambda e: e.tensor_scalar(Q2[:], Q[:], 2.0, None, ALU.mult), reads=[Q], writes=[Q2])
                    k.op(dve, lambda e: e.tensor_tensor(st8[:, 3:4], st8[:, 7:8], Q[:, 0:1], ALU.add),
                         reads=[st8, Q], writes=[st8])
                    for it in range(NIT):
                        k.op(dve, lambda e: e.memset(st8[:, 4:5], 0.0), writes=[st8])
                        k.op(dve, lambda e: e.tensor_scalar(msk[:, 0:L], sc[:, 0:L], st8[:, 3:4], 0.0,
                                                            ALU.is_ge, ALU.add, accum_out=st8[:, 4:5]),
                             reads=[sc, st8], writes=[st8, msk])
                        k.op(dve, lambda e: e.tensor_scalar(st8[:, 5:6], st8[:, 4:5], TOPK - 0.5,
                                                            Q2[:, it + 1:it + 2], ALU.is_ge, ALU.mult),
                             reads=[st8, Q2], writes=[st8])
                        k.op(dve, lambda e: e.scalar_tensor_tensor(st8[:, 3:4], st8[:, 3:4], Q[:, it + 1:it + 2],
                                                                   st8[:, 5:6], ALU.subtract, ALU.add),
                             reads=[st8, Q], writes=[st8])
                    k.op(dve, lambda e: e.tensor_tensor(thr, st8[:, 3:4], Q[:, NIT:NIT + 1], ALU.subtract),
                         reads=[st8, Q], writes=[st8])

                def mask(j):
                    sc = scL[j % 2]
                    L = 256 * (j + 1)
                    nkb = 2 * (j + 1)
                    if debug:
                        b_ = Buf("dbg_sc%d" % j)
                        for c0_ in range(0, L, 1024):
                            c1_ = min(L, c0_ + 1024)
                            k.dma(sp, dbg["sc"][j][:, c0_:c1_], sc[:, c0_:c1_], reads=[sc], writes=[b_], sembuf=b_)
                        k.dma(sp, dbg["thr"][j], thr, reads=[st8], writes=[b_], sembuf=b_)
                    for g0 in range(0, nkb, 8):
                        gn = min(8, nkb - g0)
                        k.op(dve, lambda e: e.tensor_scalar(msk[:, g0 * 128:(g0 + gn) * 128],
                                                            sc[:, g0 * 128:(g0 + gn) * 128], thr, None, ALU.is_ge),
                             reads=[sc, st8], writes=[msk])
                        tb = k.bank()
                        tbv = bf(tb[:])
                        for q_ in range(gn):
                            k.op(pe, lambda e: e.transpose(tbv[:, q_ * 128:(q_ + 1) * 128],
                                                           msk[:, (g0 + q_) * 128:(g0 + q_ + 1) * 128], ident[:]),
                                 reads=[msk, ident], writes=[tb])
                        k.op(act, lambda e: e.activation(Mb[:, g0:g0 + gn, :],
                                                         tbv[:, 0:gn * 128].rearrange("p (a b) -> p a b", a=gn),
                                                         AF.Identity, bias=-MBIG, scale=MBIG),
                             reads=[tb], writes=[Mb])

                def attention(j):
                    qTs = qT2[j % 2]
                    nkb = 2 * (j + 1)
                    po = [k.bank(pin=True), k.bank(pin=True)]
                    pov = [b_[:, 0:512].rearrange("p (h d) -> p h d", h=4) for b_ in po]
                    units = [(kb, hg) for kb in range(nkb) for hg in range(2)]

                    def qk(u):
                        kb, hg = units[u]
                        pl = k.bank()
                        plv = pl[:, 0:512].rearrange("p (a b) -> p a b", a=4)
                        k.op(pe, lambda e: e.matmul(plv, ident[:], Mb[:, kb, :].unsqueeze(1).to_broadcast([128, 4, 128]),
                                                    start=True, stop=False), reads=[ident, Mb], writes=[pl])
                        for pi in range(2):
                            pr = hg * 2 + pi
                            k.op(pe, lambda e: e.matmul(pl[:, pi * 256:(pi + 1) * 256], KT[:, pr, kb * 128:(kb + 1) * 128],
                                                        qTs[:, pr, :], start=False, stop=(pi == 1)),
                                 reads=[KT, qTs], writes=[pl])
                        return pl, plv

                    LA = 2
                    pend = [qk(u) for u in range(min(LA, len(units)))]
                    for u in range(len(units)):
                        if u + LA < len(units):
                            pend.append(qk(u + LA))
                        kb, hg = units[u]
                        pl, plv = pend.pop(0)
                        E = E2[u % 3]
                        k.op(act, lambda e: e.activation(E[:], plv, AF.Exp, scale=ATT_SCALE), reads=[pl], writes=[E])
                        for hh in range(4):
                            h = hg * 4 + hh
                            k.op(pe, lambda e: e.matmul(pov[hg][:, hh, 0:66], E[:, hh, :], V[:, kb, h * 66:(h + 1) * 66],
                                                        start=(kb == 0 and hh == 0), stop=(kb == nkb - 1 and hh == 3)),
                                 reads=[E, V], writes=[po[hg]])
                    for hg in range(2):
                        k.op(act, lambda e: e.activation(Osb[:, hg, :, :], pov[hg][:, :, 0:66], AF.Copy),
                             reads=[po[hg]], writes=[Osb])
                        k.unpin(po[hg])

                def finalize(j):
                    for hg in range(2):
                        k.op(dve, lambda e: e.reciprocal(rden[:, hg * 4:(hg + 1) * 4].unsqueeze(2), Osb[:, hg, :, 64:65]),
                             reads=[Osb], writes=[rden])
                        k.op(dve, lambda e: e.tensor_tensor(
                            attn_all[:, j, hg * 256:(hg + 1) * 256].rearrange("p (h d) -> p h d", h=4), Osb[:, hg, :, 0:64],
                            rden[:, hg * 4:(hg + 1) * 4].unsqueeze(2).to_broadcast([128, 4, 64]), ALU.mult),
                             reads=[Osb, rden], writes=[attn_all])
                    if debug:
                        k.op(dve, lambda e: e.tensor_copy(attnf[:], attn_all[:, j, :]), reads=[attn_all], writes=[attnf])
                        k.dma(sp, dbg["attn"][j], attnf[:], reads=[attnf])

                loads_a(0)
                loads_b(0)
                if nslots > 1:
                    loads_a(1)
                    loads_b(1)
                prep(0)
                indexer(0)
                thresh(0)
                mask(0)
                if nslots > 1:
                    prep(1)
                    indexer(1)
                for i in range(nslots):
                    if i + 2 < nslots:
                        prep(i + 2)
                    if i + 1 < nslots:
                        thresh(i + 1)
                    attention(i)
                    if i + 2 < nslots:
                        loads_a(i + 2)
                        indexer(i + 2)
                    if i + 1 < nslots:
                        mask(i + 1)
                    finalize(i)
                    if i + 2 < nslots:
                        loads_b(i + 2)
                k.barrier()

        def phase_B2():
            with ExitStack() as s3:
                if "C" in phases:
                    wload(0)
                    wload(1)
                mc2 = [k.sb("mcb%d" % i, [128, 8, 128], BF16, s3) for i in range(2)]
                sga2 = [k.sb("sgab%d" % i, [128, 8, 128], BF16, s3) for i in range(2)]
                xoL = [k.sb("xob%d" % i, [128, D], F32, s3) for i in range(2)]
                attnTL = [k.sb("attnT%d" % i, [128, 4, 128], BF16, s3) for i in range(2)]
                mTL = [k.sb("mT%d" % i, [128, 8, 128], BF16, s3) for i in range(2)]
                x1L = [k.sb("x1_%d" % i, [128, D], F32, s3) for i in range(2)]
                lgA = k.sb("lgA", [128, NS, 20], F32, s3)
                rA = k.sb("rA", [128, 8, NS], F32, s3)
                dG = k.sb("dG", [128, NS, 4], F32, s3)
                eG = k.sb("eG", [128, NS, 4], F32, s3)
                ohg1 = k.sb("ohg1", [128, NS, 4], F32, s3)
                ohgp = k.sb("ohgp", [128, NS, 4], F32, s3)
                t16A = k.sb("t16A", [128, NS, 16], F32, s3)
                eselA = k.sb("eselA", [128, NS, 4], F32, s3)
                oh1A = k.sb("oh1A", [128, NS, 4], F32, s3)
                oh2A = k.sb("oh2A", [128, NS, 4], F32, s3)
                emA = k.sb("emA", [128, NS, 4], F32, s3)
                x1d_b = Buf("x1d_b")

                def loads_m(j):
                    p = j % 2
                    k.dma(sp, mc2[p][:].rearrange("p a b -> p (a b)"), mc_d[j], writes=[mc2[p]])
                    k.dma(sp, sga2[p][:].rearrange("p a b -> p (a b)"), sga_d[j], writes=[sga2[p]])

                def loads_x(j):
                    p = j % 2
                    k.dma(sp, xoL[p][:], x_own[j * 128:(j + 1) * 128, :], writes=[xoL[p]])

                def s1(j):
                    attnT = attnTL[j % 2]
                    tb = k.bank()
                    tbv = bf(tb[:])
                    for q_ in range(4):
                        k.op(pe, lambda e: e.transpose(tbv[:, q_ * 128:(q_ + 1) * 128],
                                                       attn_all[:, j, q_ * 128:(q_ + 1) * 128], ident[:]),
                             reads=[attn_all, ident], writes=[tb])
                    k.op(act, lambda e: e.activation(attnT[:], tbv[:, 0:512].rearrange("p (a b) -> p a b", a=4), AF.Copy),
                         reads=[tb], writes=[attnT])

                def s2(j):
                    mcs, sgas = mc2[j % 2], sga2[j % 2]
                    x1, mT, attnT = x1L[j % 2], mTL[j % 2], attnTL[j % 2]
                    x1v = x1[:].rearrange("p (a b) -> p a b", a=8)
                    for hb_ in range(2):
                        py = k.bank()
                        pyv = py[:, 0:512].rearrange("p (a b) -> p a b", a=4)
                        for m in range(4):
                            mm = hb_ * 4 + m
                            for kc in range(4):
                                k.op(pe, lambda e: e.matmul(pyv[:, m, :], wao[:, kc, mm * 128:(mm + 1) * 128], attnT[:, kc, :],
                                                            start=(kc == 0), stop=(kc == 3)), reads=[wao, attnT], writes=[py])
                        k.op(dve, lambda e: e.tensor_tensor(x1v[:, hb_ * 4:(hb_ + 1) * 4, :], pyv,
                                                            sgas[:, hb_ * 4:(hb_ + 1) * 4, :], ALU.mult),
                             reads=[py, sgas], writes=[x1])
                    k.op(dve, lambda e: e.tensor_tensor(mT[:], x1v, mcs[:], ALU.add), reads=[x1, mcs], writes=[mT])

                def s3(j):
                    xo, mT = xoL[j % 2], mTL[j % 2]
                    for half in range(2):
                        px = k.bank()
                        for kc in range(8):
                            k.op(pe, lambda e: e.matmul(px[:, 0:512], mT[:, kc, :], wo[:, kc, half * 512:(half + 1) * 512],
                                                        start=(kc == 0), stop=(kc == 7)), reads=[mT, wo], writes=[px])
                        k.op(dve, lambda e: e.tensor_tensor(acc[:, j, half * 512:(half + 1) * 512], px[:, 0:512],
                                                            xo[:, half * 512:(half + 1) * 512], ALU.add),
                             reads=[px, xo], writes=[acc])
                    if debug:
                        k.dma(sp, dbg["x1"][j], acc[:, j, :], reads=[acc], sembuf=x1L[j % 2])
                    hbsT[j] = norm_pre(acc, 128, acc[:, j, :])

                def tailB(j):
                    norm_post(hbsT.pop(j), 128, gffn, h2T, h2T[:, :, j * 128:(j + 1) * 128])
                    pr_ = k.bank()
                    for kc in range(8):
                        k.op(pe, lambda e: e.matmul(pr_[:, 0:20], h2T[:, kc, j * 128:(j + 1) * 128], wr[:, kc, :],
                                                    start=(kc == 0), stop=(kc == 7)), reads=[h2T, wr], writes=[pr_])
                    k.op(dve, lambda e: e.tensor_tensor(lgA[:, j, :], pr_[:, 0:20], br[:], ALU.add),
                         reads=[pr_, br], writes=[lgA])

                def router_all():
                    n = nslots
                    G = lgA[:, 0:n, 0:4]
                    Ex = lgA[:, 0:n, 4:20].rearrange("p n (g e) -> p n g e", g=4)
                    gmax, gsum, pg, m1, m2, w1, w2 = (rA[:, i, 0:n] for i in range(7))
                    bc4 = lambda ap: ap.unsqueeze(2).to_broadcast([128, n, 4])
                    k.op(dve, lambda e: e.tensor_reduce(gmax, G, AX.X, ALU.max), reads=[lgA], writes=[rA])
                    k.op(dve, lambda e: e.tensor_tensor(dG[:, 0:n, :], G, bc4(gmax), ALU.subtract), reads=[lgA, rA], writes=[dG])
                    k.op(act, lambda e: e.activation(eG[:, 0:n, :], dG[:, 0:n, :], AF.Exp), reads=[dG], writes=[eG])
                    k.op(dve, lambda e: e.tensor_reduce(gsum, eG[:, 0:n, :], AX.X, ALU.add), reads=[eG], writes=[rA])
                    k.op(dve, lambda e: e.reciprocal(pg, gsum), reads=[rA], writes=[rA])
                    k.op(dve, lambda e: e.tensor_scalar(ohg1[:, 0:n, :], dG[:, 0:n, :], 0.0, None, ALU.is_ge), reads=[dG], writes=[ohg1])
                    k.op(dve, lambda e: e.tensor_tensor(ohgp[:, 0:n, :], ohg1[:, 0:n, :], bc4(pg), ALU.mult),
                         reads=[ohg1, rA], writes=[ohgp])
                    t4 = t16A[:, 0:n, :].rearrange("p n (g e) -> p n g e", g=4)
                    k.op(dve, lambda e: e.tensor_tensor(t4, Ex, ohg1[:, 0:n, :].unsqueeze(3).to_broadcast([128, n, 4, 4]), ALU.mult),
                         reads=[lgA, ohg1], writes=[t16A])
                    k.op(dve, lambda e: e.tensor_reduce(eselA[:, 0:n, :], t16A[:, 0:n, :].rearrange("p n (g e) -> p n e g", g=4),
                                                        AX.X, ALU.add), reads=[t16A], writes=[eselA])
                    k.op(dve, lambda e: e.tensor_reduce(m1, eselA[:, 0:n, :], AX.X, ALU.max), reads=[eselA], writes=[rA])
                    k.op(dve, lambda e: e.tensor_tensor(dG[:, 0:n, :], eselA[:, 0:n, :], bc4(m1), ALU.subtract),
                         reads=[eselA, rA], writes=[dG])
                    k.op(dve, lambda e: e.tensor_scalar(oh1A[:, 0:n, :], dG[:, 0:n, :], 0.0, None, ALU.is_ge), reads=[dG], writes=[oh1A])
                    k.op(dve, lambda e: e.scalar_tensor_tensor(emA[:, 0:n, :], oh1A[:, 0:n, :], NEG, eselA[:, 0:n, :],
                                                               ALU.mult, ALU.add), reads=[oh1A, eselA], writes=[emA])
                    k.op(dve, lambda e: e.tensor_reduce(m2, emA[:, 0:n, :], AX.X, ALU.max), reads=[emA], writes=[rA])
                    k.op(dve, lambda e: e.tensor_tensor(dG[:, 0:n, :], emA[:, 0:n, :], bc4(m2), ALU.subtract),
                         reads=[emA, rA], writes=[dG])
                    k.op(dve, lambda e: e.tensor_scalar(oh2A[:, 0:n, :], dG[:, 0:n, :], 0.0, None, ALU.is_ge), reads=[dG], writes=[oh2A])
                    k.op(dve, lambda e: e.tensor_tensor(w2, m1, m2, ALU.subtract), reads=[rA], writes=[rA])
                    k.op(act, lambda e: e.activation(w1, w2, AF.Sigmoid), reads=[rA], writes=[rA])
                    k.op(dve, lambda e: e.tensor_scalar(w2, w1, -1.0, 1.0, ALU.mult, ALU.add), reads=[rA], writes=[rA])
                    k.op(dve, lambda e: e.tensor_tensor(oh1A[:, 0:n, :], oh1A[:, 0:n, :], bc4(w1), ALU.mult),
                         reads=[oh1A, rA], writes=[oh1A])
                    k.op(dve, lambda e: e.tensor_tensor(oh2A[:, 0:n, :], oh2A[:, 0:n, :], bc4(w2), ALU.mult),
                         reads=[oh2A, rA], writes=[oh2A])
                    k.op(dve, lambda e: e.tensor_tensor(oh1A[:, 0:n, :], oh1A[:, 0:n, :], oh2A[:, 0:n, :], ALU.add),
                         reads=[oh1A, oh2A], writes=[oh1A])
                    k.op(dve, lambda e: e.tensor_tensor(
                        gw_all[:, 0:n * 16].rearrange("p (n g e) -> p n g e", g=4, e=4),
                        ohgp[:, 0:n, :].unsqueeze(3).to_broadcast([128, n, 4, 4]),
                        oh1A[:, 0:n, :].unsqueeze(2).to_broadcast([128, n, 4, 4]), ALU.mult),
                         reads=[ohgp, oh1A], writes=[gw_all])

                hbsT = {}
                for j in range(min(2, nslots)):
                    loads_m(j)
                    loads_x(j)
                for it in range(nslots + 3):
                    if 0 <= it - 3 < nslots:
                        tailB(it - 3)
                    if 0 <= it - 2 < nslots:
                        s3(it - 2)
                        if it < nslots:
                            loads_x(it)
                    if 0 <= it - 1 < nslots:
                        s2(it - 1)
                        if it + 1 < nslots:
                            loads_m(it + 1)
                    if it < nslots:
                        s1(it)
                router_all()
                k.barrier()
        def phase_C():
            with ExitStack() as s4:
                gfin = k.sb("gfin", [128, D], F32, s4)
                k.dma(sp, gfin[:], gfin_d, writes=[gfin])
                sgl2 = [k.sb("sgm%d" % i, [128, 512], F32, s4) for i in range(2)]
                hid2 = [k.sb("hid%d" % i, [128, 2, 512], BF16, s4) for i in range(2)]
                ot2 = [k.sb("ot%d" % i, [128, D], F32, s4) for i in range(2)]

                ngrp = (nslots + 3) // 4
                wload(0)
                hi = [0]

                def gu(ex, tg):
                    wg, wu = wg2[ex % 2], wu2[ex % 2]
                    ns_ = min(4, nslots - tg * 4)
                    N = ns_ * 128
                    hid = hid2[hi[0] % 2]
                    hi[0] += 1
                    for fc in range(2):
                        pg = k.bank()
                        pu = k.bank()
                        for kc in range(8):
                            k.op(pe, lambda e, kc=kc, fc=fc, pg=pg: e.matmul(
                                pg[:, 0:N], wg[:, kc, fc * 128:(fc + 1) * 128], h2T[:, kc, tg * 512:tg * 512 + N],
                                start=(kc == 0), stop=(kc == 7)), reads=[wg, h2T], writes=[pg])
                        for kc in range(8):
                            k.op(pe, lambda e, kc=kc, fc=fc, pu=pu: e.matmul(
                                pu[:, 0:N], wu[:, kc, fc * 128:(fc + 1) * 128], h2T[:, kc, tg * 512:tg * 512 + N],
                                start=(kc == 0), stop=(kc == 7)), reads=[wu, h2T], writes=[pu])
                        sgl = sgl2[fc]
                        k.op(act, lambda e, pg=pg, sgl=sgl: e.activation(sgl[:, 0:N], pg[:, 0:N], AF.Silu),
                             reads=[pg], writes=[sgl])
                        k.op(dve, lambda e, pu=pu, sgl=sgl, fc=fc, hid=hid: e.tensor_tensor(
                            hid[:, fc, 0:N], pu[:, 0:N], sgl[:, 0:N], ALU.mult), reads=[pu, sgl], writes=[hid])
                    return hid

                def final_norm(j):
                    ot = ot2[j % 2]
                    ss, rs, junk = ssL[j % 2], rsL[j % 2], hbL[j % 2]
                    k.op(dve, lambda e, ss=ss: e.memset(ss[:], 0.0), writes=[ss])
                    k.op(act, lambda e, j=j, junk=junk, ss=ss: e.activation(junk[:], acc[:, j, :], AF.Square, accum_out=ss[:, 0:1]),
                         reads=[acc, ss], writes=[junk, ss])
                    k.op(act, lambda e, rs=rs, ss=ss: e.activation(rs[:], ss[:], AF.Sqrt, bias=EPS, scale=1.0 / D), reads=[ss], writes=[rs])
                    k.op(dve, lambda e, rs=rs: e.reciprocal(rs[:], rs[:]), reads=[rs], writes=[rs])
                    k.op(dve, lambda e, j=j, ot=ot, rs=rs: e.scalar_tensor_tensor(ot[:], acc[:, j, :], rs[:, 0:1], gfin[:],
                                                                          ALU.mult, ALU.mult),
                         reads=[acc, rs, gfin], writes=[ot])
                    k.dma(sp, out_d[j * 128:(j + 1) * 128, :], ot[:], reads=[ot], sembuf=ot)

                def down(ex, tg, hid):
                    wd = wd2[ex % 2]
                    ns_ = min(4, nslots - tg * 4)
                    for s_ in range(ns_):
                        j = tg * 4 + s_
                        for half in range(2):
                            py = k.bank()
                            for fc in range(2):
                                k.op(pe, lambda e, fc=fc, half=half, py=py, s_=s_, hid=hid: e.matmul(
                                    py[:, 0:512], hid[:, fc, s_ * 128:(s_ + 1) * 128],
                                    wd[:, fc, half * 512:(half + 1) * 512], start=(fc == 0), stop=(fc == 1)),
                                     reads=[hid, wd], writes=[py])
                            k.op(dve, lambda e, py=py, j=j, half=half, ex=ex: e.scalar_tensor_tensor(
                                acc[:, j, half * 512:(half + 1) * 512], py[:, 0:512],
                                gw_all[:, j * 16 + ex:j * 16 + ex + 1], acc[:, j, half * 512:(half + 1) * 512],
                                ALU.mult, ALU.add), reads=[py, gw_all, acc], writes=[acc])
                    if ex == 15:
                        for s_ in range(ns_):
                            final_norm(tg * 4 + s_)

                prev = None
                for ex in range(16):
                    for tg in range(ngrp):
                        h_ = gu(ex, tg)
                        if tg == ngrp - 1 and ex + 2 < 16:
                            wload_gu(ex + 2)
                        if prev is not None:
                            down(*prev)
                        if tg == 0 and ex + 1 < 16:
                            wload_d(ex + 1)
                        prev = (ex, tg, h_)
                down(*prev)
                k.barrier()
        if "A2" in phases:
            phase_A2()
            if debug:
                dbg_A2()
        attn_all = k.sb("attn_all", [128, NS, 512], BF16)
        wao = k.sb("wao", [128, 4, D], BF16)
        wo = k.sb("wo", [128, 8, D], BF16)
        wr = k.sb("wr", [128, 8, 20], BF16)
        br = k.sb("br", [128, 20], F32)

        def load_tail_weights():
            for kc in range(4):
                k.dma(pool, wao[:, kc, :], wao_d[kc * 128:(kc + 1) * 128, :], writes=[wao])
            for kc in range(8):
                k.dma(pool, wo[:, kc, :], wo_d[kc * 128:(kc + 1) * 128, :], writes=[wo])
            k.dma(pool, wr[:], wr_d.rearrange("(k p) c -> p k c", p=128), writes=[wr])
            k.dma(sp, br[:], br_d, writes=[br])

        wloaded = set()

        def wload_gu(ex):
            if ("gu", ex) in wloaded:
                return
            wloaded.add(("gu", ex))
            p = ex % 2
            k.dma(pool, wg2[p][:], wg_d[ex].rearrange("(k p) c -> p k c", p=128), writes=[wg2[p]])
            k.dma(pool, wu2[p][:], wu_d[ex].rearrange("(k p) c -> p k c", p=128), writes=[wu2[p]])

        def wload_d(ex):
            if ("d", ex) in wloaded:
                return
            wloaded.add(("d", ex))
            p = ex % 2
            k.dma(pool, wd2[p][:], wd_d[ex].rearrange("(k p) c -> p k c", p=128), writes=[wd2[p]])

        def wload(ex):
            wload_gu(ex)
            wload_d(ex)

        with ExitStack() as skv:
            KT = k.sb("KT", [128, 4, S], BF16, skv)
            V = k.sb("V", [128, NB, 8 * 66], BF16, skv)
            kiT = k.sb("kiT", [128, S], BF16, skv)
            k.op(pool, lambda e: e.memset(V[:], 1.0), writes=[V])
            if "A1" in phases:
                phase_A1()
                if debug:
                    dbg_A1()
            if "B" in phases:
                load_tail_weights()
                phase_B()
            k.barrier()
        wg2 = [k.sb("wg%d" % i, [128, 8, 256], BF16) for i in range(2)]
        wu2 = [k.sb("wu%d" % i, [128, 8, 256], BF16) for i in range(2)]
        wd2 = [k.sb("wd%d" % i, [128, 2, D], BF16) for i in range(2)]
        acc = k.sb("acc", [128, NS, D], F32)
        h2T = k.sb("h2T", [128, 8, NS * 128], BF16)
        if "B" in phases:
            phase_B2()
            if debug:
                k.dma(sp, dbg["gw"], gw_all[:], reads=[gw_all])
        if "C" in phases:
            phase_C()
        k.barrier()
        build_nc.nins = k.nins
    return nc


def own_blocks(r):
    return [2 * j + ((j % 2) ^ r) for j in range(NS)]


def prep_core(inp, c):
    b, r = c // 2, c % 2
    x = np.asarray(inp["x"], dtype=np.float32)
    pos = np.asarray(inp["positions"]).astype(np.int32)
    blocks = own_blocks(r)
    xb = x[b]
    x_own = np.concatenate([xb[i * 128:(i + 1) * 128] for i in blocks], axis=0)
    x_halo = np.zeros((NS * 32, D), np.float32)
    for j, i in enumerate(blocks):
        if i > 0:
            x_halo[j * 32:(j + 1) * 32] = xb[i * 128 - 32:i * 128]
    pos_full = np.ascontiguousarray(pos[b].reshape(NB, 128).T)
    pos_own = np.ascontiguousarray(np.stack([pos[b][i * 128:(i + 1) * 128] for i in blocks], axis=1))
    invf = (1.0 / (np.float32(10000.0) ** (np.arange(0, 64, 2, dtype=np.float32) / np.float32(64)))).astype(np.float32)
    invf = np.ascontiguousarray(np.broadcast_to(invf[None, :], (128, 32)))
    cmask = np.zeros((128, NS, 256), np.float32)
    for j, i in enumerate(blocks):
        qidx = i * 128 + np.arange(128)[:, None]
        kidx = 256 * j + np.arange(256)[None, :]
        cmask[:, j, :] = np.where(kidx > qidx, np.float32(NEG), np.float32(0.0))

    def fm(v, nchunk):
        return np.ascontiguousarray(np.asarray(v, np.float32).reshape(nchunk, 128).T)

    w_dw = np.asarray(inp["w_dw"], np.float32)[0, :, 0, :]
    wdw = np.ascontiguousarray(w_dw.T.reshape(4, 128, 31).transpose(1, 0, 2))
    w_r = np.concatenate([np.asarray(inp["w_rg"], np.float32)[0],
                          np.asarray(inp["w_re"], np.float32)[0].reshape(D, 16)], axis=1)
    b_r = np.concatenate([np.asarray(inp["b_rg"], np.float32)[0], np.asarray(inp["b_re"], np.float32)[0].reshape(16)])
    m = {
        "x_full": np.ascontiguousarray(xb), "x_own": x_own, "x_halo": x_halo,
        "pos_full": pos_full, "pos_own": pos_own, "invf": invf, "cmask": cmask,
        "ident": np.eye(128, dtype=np.float32),
        "w_in": np.ascontiguousarray(np.asarray(inp["w_in"], np.float32)[0]),
        "wdw": wdw, "bdw": fm(inp["b_dw"][0], 4), "lng": fm(inp["ln_g"][0], 4), "lnb": fm(inp["ln_b"][0], 4),
        "gmix": fm(inp["g_mix"][0], 8), "gffn": fm(inp["g_ffn"][0], 8),
        "gfin": np.ascontiguousarray(np.broadcast_to(np.asarray(inp["g_final"], np.float32)[None, :], (128, D))),
        "b_r": np.ascontiguousarray(np.broadcast_to(b_r[None, :], (128, 20))),
        "w_r": np.ascontiguousarray(w_r),
        "w_conv_out": np.ascontiguousarray(np.asarray(inp["w_conv_out"], np.float32)[0]),
        "w_attn_out": np.ascontiguousarray(np.asarray(inp["w_attn_out"], np.float32)[0]),
        "w_o": np.ascontiguousarray(np.asarray(inp["w_o"], np.float32)[0]),
        "w_gate": np.ascontiguousarray(np.asarray(inp["w_gate"], np.float32)[0]),
        "w_up": np.ascontiguousarray(np.asarray(inp["w_up"], np.float32)[0]),
        "w_down": np.ascontiguousarray(np.asarray(inp["w_down"], np.float32)[0]),
    }
    return m


_NC_CACHE = {}


def kernel(**inputs):
    if "nc" not in _NC_CACHE:
        _NC_CACHE["nc"] = build_nc()
    nc = _NC_CACHE["nc"]
    in_maps = [prep_core(inputs, c) for c in range(8)]
    res = run_bass_kernel_spmd(nc, in_maps, core_ids=list(range(8)))
    out = np.empty((4, S, D), np.float32)
    for c in range(8):
        b, r = c // 2, c % 2
        o = np.asarray(res.results[c]["out"], dtype=np.float32)
        for j, i in enumerate(own_blocks(r)):
            out[b, i * 128:(i + 1) * 128] = o[j * 128:(j + 1) * 128]
    return out
```

```python
from contextlib import ExitStack
import numpy as np
import concourse.bass as bass
import concourse.mybir as mybir
from concourse.bass_utils import run_bass_kernel_spmd

F32 = mybir.dt.float32
BF16 = mybir.dt.bfloat16
I32 = mybir.dt.int32
ALU = mybir.AluOpType
AF = mybir.ActivationFunctionType
AX = mybir.AxisListType

D = 1024
S = 4096
NB = 32
NS = 16
EPS = 1e-6
NIT = 14
TOPK = 256
NEG = -1.0e30
C_U, C_Q, C_K, C_V, C_QI, C_KI, C_WI, C_GC, C_GA = 0, 1024, 1536, 2048, 2560, 3072, 3136, 3144, 4168
IDX_SCALE = float((8 ** -0.5) * (64 ** -0.5))
ATT_SCALE = float(64 ** -0.5)


class Buf:
    __slots__ = ("name", "t", "lw", "rd", "dsem", "dcnt")

    def __init__(self, name, t=None):
        self.name = name
        self.t = t
        self.lw = None
        self.rd = {}
        self.dsem = None
        self.dcnt = 0

    def __getitem__(self, key):
        return self.t[key]


class Eng:
    def __init__(self, k, name, eng):
        self.name = name
        self.eng = eng
        self.sem = k.new_sem("e_" + name)
        self.cnt = 0
        self.seen = {}

    def wait_tok(self, tok):
        if tok is None:
            return
        sem, val, _ = tok
        key = id(sem)
        if self.seen.get(key, 0) >= val:
            return
        self.eng.wait_ge(sem, val)
        self.seen[key] = val


class K:
    def __init__(self, nc, stack):
        self.nc = nc
        self.stack = stack
        self.pe = Eng(self, "pe", nc.tensor)
        self.act = Eng(self, "act", nc.scalar)
        self.dve = Eng(self, "dve", nc.vector)
        self.pool = Eng(self, "pool", nc.gpsimd)
        self.sp = Eng(self, "sp", nc.sync)
        self.engs = [self.pe, self.act, self.dve, self.pool, self.sp]
        self.dma_bufs = []
        self.nins = 0
        self.banks = []
        self.bank_i = 0
        self.pinned = set()

    def new_sem(self, name):
        return self.stack.enter_context(self.nc.semaphore(name))

    def sb(self, name, shape, dt, stack=None):
        t = (stack or self.stack).enter_context(self.nc.sbuf_tensor("s_" + name, list(shape), dt))
        return Buf(name, t)

    def mk_banks(self):
        for i in range(8):
            t = self.stack.enter_context(self.nc.psum_tensor("bank%d" % i, [128, 512], F32))
            self.banks.append(Buf("bank%d" % i, t))

    def bank(self, pin=False):
        for _ in range(16):
            b = self.banks[self.bank_i % 8]
            self.bank_i += 1
            if b.name not in self.pinned:
                if pin:
                    self.pinned.add(b.name)
                return b
        raise RuntimeError("no psum bank")

    def unpin(self, b):
        self.pinned.discard(b.name)

    def _deps(self, e, reads, writes):
        for b in reads:
            if b.lw is not None:
                e.wait_tok(b.lw)
        for b in writes:
            if b.lw is not None and b.lw[2] != e.name:
                e.wait_tok(b.lw)
            for en, tok in b.rd.items():
                if en != e.name:
                    e.wait_tok(tok)

    def _commit(self, tok, reads, writes):
        for b in reads:
            b.rd[tok[2]] = tok
        for b in writes:
            b.lw = tok
            b.rd = {}

    def op(self, e, fn, reads=(), writes=()):
        self._deps(e, reads, writes)
        ins = fn(e.eng)
        e.cnt += 1
        ins.then_inc(e.sem, 1)
        tok = (e.sem, e.cnt, e.name)
        self._commit(tok, reads, writes)
        self.nins += 1
        return tok

    def dma(self, e, out, in_, reads=(), writes=(), sembuf=None):
        self._deps(e, reads, writes)
        sb_ = sembuf if sembuf is not None else (writes[0] if writes else reads[0])
        if sb_.dsem is None:
            sb_.dsem = self.new_sem("d_" + sb_.name)
            self.dma_bufs.append(sb_)
        ins = e.eng.dma_start(out=out, in_=in_)
        sb_.dcnt += 16
        ins.then_inc(sb_.dsem, 16)
        tok = (sb_.dsem, sb_.dcnt, "dma_" + sb_.name)
        self._commit(tok, reads, writes)
        self.nins += 1
        return tok

    def barrier(self):
        toks = [(e.sem, e.cnt, e.name) for e in self.engs if e.cnt > 0]
        toks += [(b.dsem, b.dcnt, "dma") for b in self.dma_bufs]
        for e in self.engs:
            for t in toks:
                if t[2] != e.name:
                    e.wait_tok(t)


def bf(ap):
    return ap.bitcast(BF16)


def build_nc(phases=("A1", "A2", "B", "C"), debug=False, nslots=NS, bstop=9):
    nc = bass.Bass("TRN2", target_bir_lowering=False)

    def din(name, shape, dt=F32):
        return nc.dram_tensor(name, list(shape), dt, kind="ExternalInput").ap()

    def dscr(name, shape, dt):
        return nc.dram_tensor(name, list(shape), dt, kind="Internal").ap()

    x_full = din("x_full", [S, D])
    x_own = din("x_own", [NS * 128, D])
    x_halo = din("x_halo", [NS * 32, D])
    pos_full = din("pos_full", [128, NB], I32)
    pos_own = din("pos_own", [128, NS], I32)
    invf = din("invf", [128, 32])
    cmask_d = din("cmask", [128, NS, 256])
    ident_d = din("ident", [128, 128])
    w_in = din("w_in", [D, 5192])
    wdw_d = din("wdw", [128, 4, 31])
    bdw_d = din("bdw", [128, 4])
    lng_d = din("lng", [128, 4])
    lnb_d = din("lnb", [128, 4])
    gmix_d = din("gmix", [128, 8])
    gffn_d = din("gffn", [128, 8])
    gfin_d = din("gfin", [128, D])
    br_d = din("b_r", [128, 20])
    wr_d = din("w_r", [D, 20])
    wco_d = din("w_conv_out", [512, D])
    wao_d = din("w_attn_out", [512, D])
    wo_d = din("w_o", [D, D])
    wg_d = din("w_gate", [16, D, 256])
    wu_d = din("w_up", [16, D, 256])
    wd_d = din("w_down", [16, 256, D])
    out_d = nc.dram_tensor("out", [NS * 128, D], F32, kind="ExternalOutput").ap()

    qT_d = dscr("qT_d", [NS, 128, 512], BF16)
    qiT_d = dscr("qiT_d", [NS, 128, 512], BF16)
    mc_d = dscr("mc_d", [NS, 128, 1024], BF16)
    sga_d = dscr("sga_d", [NS, 128, 1024], BF16)
    x1_d = dscr("x1_d", [NS, 128, D], F32)
    h2T_d = dscr("h2T_d", [NS, 128, 1024], BF16)
    dbg = {}
    if debug:
        dbg["kT"] = nc.dram_tensor("dbg_kT", [128, 4, S], BF16, kind="ExternalOutput").ap()
        dbg["v"] = nc.dram_tensor("dbg_v", [128, NB, 8 * 66], BF16, kind="ExternalOutput").ap()
        dbg["kiT"] = nc.dram_tensor("dbg_kiT", [128, S], BF16, kind="ExternalOutput").ap()
        dbg["qT"] = nc.dram_tensor("dbg_qT", [NS, 128, 512], BF16, kind="ExternalOutput").ap()
        dbg["qiT"] = nc.dram_tensor("dbg_qiT", [NS, 128, 512], BF16, kind="ExternalOutput").ap()
        dbg["mc"] = nc.dram_tensor("dbg_mc", [NS, 128, 1024], BF16, kind="ExternalOutput").ap()
        dbg["sga"] = nc.dram_tensor("dbg_sga", [NS, 128, 1024], BF16, kind="ExternalOutput").ap()
        dbg["wi"] = nc.dram_tensor("dbg_wi", [128, NS * 8], F32, kind="ExternalOutput").ap()
        dbg["x1"] = nc.dram_tensor("dbg_x1", [NS, 128, D], F32, kind="ExternalOutput").ap()
        dbg["gw"] = nc.dram_tensor("dbg_gw", [128, NS * 16], F32, kind="ExternalOutput").ap()
        dbg["sc"] = nc.dram_tensor("dbg_sc", [NS, 128, S], F32, kind="ExternalOutput").ap()
        dbg["thr"] = nc.dram_tensor("dbg_thr", [NS, 128, 1], F32, kind="ExternalOutput").ap()
        dbg["attn"] = nc.dram_tensor("dbg_attn", [NS, 128, 512], F32, kind="ExternalOutput").ap()

    with ExitStack() as st:
        k = K(nc, st)
        pe, act, dve, pool, sp = k.pe, k.act, k.dve, k.pool, k.sp
        k.mk_banks()

        ident = k.sb("ident", [128, 128], BF16)
        identf = k.sb("identf", [128, 128], F32)
        wi_all = k.sb("wi_all", [128, NS * 8], F32)
        gw_all = k.sb("gw_all", [128, NS * 16], F32)
        gmix = k.sb("gmix", [128, 8], F32)
        gffn = k.sb("gffn", [128, 8], F32)
        ssL = [k.sb("ss%d" % i, [128, 1], F32) for i in range(2)]
        rsL = [k.sb("rs%d" % i, [128, 1], F32) for i in range(2)]
        hbL = [k.sb("hb%d" % i, [128, D], BF16) for i in range(2)]
        nrm_i = [0]

        k.dma(sp, identf[:], ident_d, writes=[identf])
        k.dma(pool, ident[:], ident_d, writes=[ident])
        k.dma(sp, gmix[:], gmix_d, writes=[gmix])
        k.dma(sp, gffn[:], gffn_d, writes=[gffn])

        def rope_tables(pos_d, n, cosT, sinT, stk):
            pi_ = k.sb("pos_i%d" % n, [128, n], I32, stk)
            pf = k.sb("pos_f%d" % n, [128, n], F32, stk)
            iv = k.sb("invf%d" % n, [128, 32], F32, stk)
            ang = k.sb("ang%d" % n, [128, n, 32], F32, stk)
            tmp = k.sb("angt%d" % n, [128, n, 32], F32, stk)
            k.dma(sp, pi_[:], pos_d, writes=[pi_])
            k.dma(sp, iv[:], invf, writes=[iv])
            k.op(dve, lambda e: e.tensor_copy(pf[:], pi_[:]), reads=[pi_], writes=[pf])
            k.op(dve, lambda e: e.tensor_tensor(ang[:], pf[:].unsqueeze(2).to_broadcast([128, n, 32]),
                                                iv[:].unsqueeze(1).to_broadcast([128, n, 32]), ALU.mult),
                 reads=[pf, iv], writes=[ang])
            ki_ = k.sb("angk%d" % n, [128, n, 32], I32, stk)
            kf_ = k.sb("angf%d" % n, [128, n, 32], F32, stk)
            two_pi = float(2 * np.pi)
            C1 = 6.28125
            C2 = float(2 * np.pi - 6.28125)
            PI_SAFE = 3.1415925

            def sin_of(dst, shift):
                if shift != 0.0:
                    k.op(dve, lambda e: e.tensor_scalar(tmp[:], ang[:], shift, None, ALU.add), reads=[ang], writes=[tmp])
                    src = tmp
                else:
                    src = ang
                k.op(dve, lambda e: e.tensor_scalar(kf_[:], src[:], 1.0 / two_pi, None, ALU.mult),
                     reads=[src], writes=[kf_])
                k.op(dve, lambda e: e.tensor_copy(ki_[:], kf_[:]), reads=[kf_], writes=[ki_])
                k.op(dve, lambda e: e.tensor_copy(kf_[:], ki_[:]), reads=[ki_], writes=[kf_])
                k.op(dve, lambda e: e.scalar_tensor_tensor(tmp[:], kf_[:], -C1, src[:], ALU.mult, ALU.add),
                     reads=[kf_, src], writes=[tmp])
                k.op(dve, lambda e: e.scalar_tensor_tensor(tmp[:], kf_[:], -C2, tmp[:], ALU.mult, ALU.add),
                     reads=[kf_, tmp], writes=[tmp])
                k.op(dve, lambda e: e.tensor_scalar(kf_[:], tmp[:], float(np.pi), -two_pi, ALU.is_gt, ALU.mult),
                     reads=[tmp], writes=[kf_])
                k.op(dve, lambda e: e.tensor_tensor(tmp[:], tmp[:], kf_[:], ALU.add), reads=[tmp, kf_], writes=[tmp])
                k.op(dve, lambda e: e.tensor_scalar(kf_[:], tmp[:], -float(np.pi), two_pi, ALU.is_lt, ALU.mult),
                     reads=[tmp], writes=[kf_])
                k.op(dve, lambda e: e.tensor_tensor(tmp[:], tmp[:], kf_[:], ALU.add), reads=[tmp, kf_], writes=[tmp])
                k.op(dve, lambda e: e.tensor_scalar(tmp[:], tmp[:], -PI_SAFE, PI_SAFE, ALU.max, ALU.min),
                     reads=[tmp], writes=[tmp])
                k.op(act, lambda e: e.activation(dst[:], tmp[:], AF.Sin), reads=[tmp], writes=[dst])

            sin_of(sinT, 0.0)
            sin_of(cosT, float(0.5 * np.pi))

        hbN = 4
        hbX = [k.sb("hbx%d" % i, [128, D], BF16) for i in range(hbN - 2)]

        def norm_pre(xt, n, src=None):
            if src is None:
                src = xt[0:n, :]
            hbs = hbL + hbX
            ss, rs, hb = ssL[nrm_i[0] % 2], rsL[nrm_i[0] % 2], hbs[nrm_i[0] % hbN]
            junk = hb
            nrm_i[0] += 1
            k.op(dve, lambda e: e.memset(ss[0:n, :], 0.0), writes=[ss])
            k.op(act, lambda e: e.activation(junk[0:n, :], src, AF.Square, accum_out=ss[0:n, 0:1]),
                 reads=[xt, ss], writes=[junk, ss])
            k.op(act, lambda e: e.activation(rs[0:n, :], ss[0:n, :], AF.Sqrt, bias=EPS, scale=1.0 / D),
                 reads=[ss], writes=[rs])
            k.op(dve, lambda e: e.reciprocal(rs[0:n, :], rs[0:n, :]), reads=[rs], writes=[rs])
            k.op(act, lambda e: e.activation(hb[0:n, :], src, AF.Copy, scale=rs[0:n, 0:1]),
                 reads=[xt, rs], writes=[hb])
            return hb

        def norm_post(hb, n, gT, hT, hT_ap=None):
            if hT_ap is None:
                hT_ap = hT[:, :, 0:n]
            tp = k.bank()
            tpv = bf(tp[:])[:, 0:8 * 128].rearrange("p (a b) -> p a b", a=8)
            for kc in range(8):
                k.op(pe, lambda e, kc=kc: e.transpose(tpv[:, kc, 0:n], hb[0:n, kc * 128:(kc + 1) * 128],
                                                      ident[0:n, 0:n]),
                     reads=[hb, ident], writes=[tp])
            k.op(dve, lambda e: e.tensor_tensor(hT_ap, tpv[:, :, 0:n],
                                                gT[:].unsqueeze(2).to_broadcast([128, 8, n]), ALU.mult),
                 reads=[tp, gT], writes=[hT])

        def norm_T(xt, n, gT, hT, hT_ap=None):
            hb = norm_pre(xt, n)
            norm_post(hb, n, gT, hT, hT_ap)

        def proj_tok(hT, w, c0, ncols):
            pb = k.bank()
            for kc in range(8):
                k.op(pe, lambda e, kc=kc: e.matmul(pb[:, 0:ncols], hT[:, kc, :], w[:, kc, c0:c0 + ncols],
                                                   start=(kc == 0), stop=(kc == 7)),
                     reads=[hT, w], writes=[pb])
            return pb

        def rope(pb, nh, cosT, sinT, ti, outb, tmpA, tmpB):
            pv = pb[:, 0:nh * 64].rearrange("p (h d) -> p h d", h=nh)
            ov = outb[:, 0:nh * 64].rearrange("p (h d) -> p h d", h=nh)
            av = tmpA[:, 0:nh * 64].rearrange("p (h d) -> p h d", h=nh)
            bv = tmpB[:, 0:nh * 64].rearrange("p (h d) -> p h d", h=nh)
            cb = cosT[:, ti, :].unsqueeze(1).to_broadcast([128, nh, 32])
            sb_ = sinT[:, ti, :].unsqueeze(1).to_broadcast([128, nh, 32])
            k.op(dve, lambda e: e.tensor_tensor(av[:, :, 0:32], pv[:, :, 0:32], cb, ALU.mult),
                 reads=[pb, cosT], writes=[tmpA])
            k.op(dve, lambda e: e.tensor_tensor(av[:, :, 32:64], pv[:, :, 32:64], cb, ALU.mult),
                 reads=[pb, cosT], writes=[tmpA])
            k.op(dve, lambda e: e.tensor_tensor(bv[:, :, 0:32], pv[:, :, 32:64], sb_, ALU.mult),
                 reads=[pb, sinT], writes=[tmpB])
            k.op(dve, lambda e: e.tensor_tensor(bv[:, :, 32:64], pv[:, :, 0:32], sb_, ALU.mult),
                 reads=[pb, sinT], writes=[tmpB])
            k.op(dve, lambda e: e.tensor_tensor(ov[:, :, 0:32], av[:, :, 0:32], bv[:, :, 0:32], ALU.subtract),
                 reads=[tmpA, tmpB], writes=[outb])
            k.op(dve, lambda e: e.tensor_tensor(ov[:, :, 32:64], av[:, :, 32:64], bv[:, :, 32:64], ALU.add),
                 reads=[tmpA, tmpB], writes=[outb])

        w_in_v = w_in.rearrange("(k p) c -> p k c", p=128)
        def phase_A1():
            with ExitStack() as s1:
                cosF = k.sb("cosF", [128, NB, 32], F32, s1)
                sinF = k.sb("sinF", [128, NB, 32], F32, s1)
                rope_tables(pos_full, NB, cosF, sinF, s1)
                wA = k.sb("wA1", [128, 8, 1088], BF16, s1)
                for kc in range(8):
                    k.dma(pool, wA[:, kc, 0:1024], w_in_v[:, kc, C_K:C_K + 1024], writes=[wA])
                    k.dma(pool, wA[:, kc, 1024:1088], w_in_v[:, kc, C_KI:C_KI + 64], writes=[wA])
                k.op(pool, lambda e: e.memset(V[:], 1.0), writes=[V])
                xt2 = [k.sb("xtA%d" % i, [128, D], F32, s1) for i in range(2)]
                hT2 = [k.sb("hTA%d" % i, [128, 8, 128], BF16, s1) for i in range(2)]
                krL = [k.sb("kr%d" % i, [128, 512], BF16, s1) for i in range(2)]
                kirL = [k.sb("kir%d" % i, [128, 128], BF16, s1) for i in range(2)]
                tAL = [k.sb("tA%d" % i, [128, 512], F32, s1) for i in range(2)]
                tBL = [k.sb("tB%d" % i, [128, 512], F32, s1) for i in range(2)]
                tCL = [k.sb("tC%d" % i, [128, 64], F32, s1) for i in range(2)]
                tDL = [k.sb("tD%d" % i, [128, 64], F32, s1) for i in range(2)]
                hbs1 = {}

                def pre1(ti):
                    k.dma(sp, xt2[ti % 2][:], x_full[ti * 128:(ti + 1) * 128, :], writes=[xt2[ti % 2]])
                    hbs1[ti] = norm_pre(xt2[ti % 2], 128)

                def post1(ti):
                    norm_post(hbs1.pop(ti), 128, gmix, hT2[ti % 2])

                def part2(ti):
                    kr, kir = krL[ti % 2], kirL[ti % 2]
                    tb = k.bank()
                    tbv = bf(tb[:])
                    for pr in range(4):
                        k.op(pe, lambda e, pr=pr: e.transpose(tbv[:, pr * 128:(pr + 1) * 128],
                                                              kr[:, pr * 128:(pr + 1) * 128], ident[:]),
                             reads=[kr, ident], writes=[tb])
                    k.op(pe, lambda e: e.transpose(tbv[:, 512:640], kir[:], ident[:]), reads=[kir, ident], writes=[tb])
                    k.op(act, lambda e: e.activation(KT[:, :, ti * 128:(ti + 1) * 128],
                                                     tbv[:, 0:512].rearrange("p (a b) -> p a b", a=4), AF.Copy),
                         reads=[tb], writes=[KT])
                    k.op(act, lambda e: e.activation(kiT[:, ti * 128:(ti + 1) * 128], tbv[:, 512:640], AF.Copy),
                         reads=[tb], writes=[kiT])

                pre1(0)
                post1(0)
                pre1(1)
                for ti in range(NB):
                    if ti + 1 < NB:
                        post1(ti + 1)
                    hT = hT2[ti % 2]
                    kr, kir, tA, tB = krL[ti % 2], kirL[ti % 2], tAL[ti % 2], tBL[ti % 2]
                    tC, tD = tCL[ti % 2], tDL[ti % 2]
                    pk = proj_tok(hT, wA, 0, 512)
                    pv = proj_tok(hT, wA, 512, 512)
                    pki = proj_tok(hT, wA, 1024, 64)
                    if ti + 2 < NB:
                        pre1(ti + 2)
                    rope(pk, 8, cosF, sinF, ti, kr, tA, tB)
                    Vv = V[:, ti, :].rearrange("p (h d) -> p h d", h=8)
                    k.op(act, lambda e: e.activation(Vv[:, :, 0:64],
                                                     pv[:, 0:512].rearrange("p (h d) -> p h d", h=8), AF.Copy),
                         reads=[pv], writes=[V])
                    rope(pki, 1, cosF, sinF, ti, kir, tC, tD)
                    k.op(dve, lambda e, kir=kir: e.tensor_copy(kir[:, 64:128], kir[:, 0:64]), reads=[kir], writes=[kir])
                    if ti >= 1:
                        part2(ti - 1)
                part2(NB - 1)
                k.barrier()
        def dbg_A1():
            for pr in range(4):
                k.dma(sp, dbg["kT"][:, pr, :], KT[:, pr, :], reads=[KT])
            for t8 in range(0, NB, 4):
                k.dma(sp, dbg["v"][:, t8:t8 + 4, :], V[:, t8:t8 + 4, :], reads=[V])
            k.dma(sp, dbg["kiT"], kiT[:], reads=[kiT])

        def phase_A2():
            with ExitStack() as s2:
                cosO = k.sb("cosO", [128, NS, 32], F32, s2)
                sinO = k.sb("sinO", [128, NS, 32], F32, s2)
                rope_tables(pos_own, NS, cosO, sinO, s2)
                WU, WQ, WQI, WWI, WGC, WGA = 0, 1024, 1536, 2048, 2056, 3080
                wB = k.sb("wB", [128, 8, 4104], BF16, s2)
                wBq, wBu, wBg = Buf("wBq", wB.t), Buf("wBu", wB.t), Buf("wBg", wB.t)
                for kc in range(8):
                    k.dma(pool, wB[:, kc, WQ:WQ + 512], w_in_v[:, kc, C_Q:C_Q + 512], writes=[wBq])
                    k.dma(pool, wB[:, kc, WQI:WQI + 512], w_in_v[:, kc, C_QI:C_QI + 512], writes=[wBq])
                    k.dma(pool, wB[:, kc, WWI:WWI + 8], w_in_v[:, kc, C_WI:C_WI + 8], writes=[wBq])
                for kc in range(8):
                    k.dma(pool, wB[:, kc, WU:WU + 1024], w_in_v[:, kc, C_U:C_U + 1024], writes=[wBu])
                for kc in range(8):
                    k.dma(pool, wB[:, kc, WGC:WGC + 2048], w_in_v[:, kc, C_GC:C_GC + 2048], writes=[wBg])
                wco = k.sb("wco", [128, 4, D], BF16, s2)
                for kc in range(4):
                    k.dma(pool, wco[:, kc, :], wco_d[kc * 128:(kc + 1) * 128, :], writes=[wco])
                wdw = k.sb("wdw", [128, 4, 31], F32, s2)
                bdw = k.sb("bdw", [128, 4], F32, s2)
                lng = k.sb("lng", [128, 4], F32, s2)
                lnb = k.sb("lnb", [128, 4], F32, s2)
                k.dma(sp, wdw[:], wdw_d, writes=[wdw])
                k.dma(sp, bdw[:], bdw_d, writes=[bdw])
                k.dma(sp, lng[:], lng_d, writes=[lng])
                k.dma(sp, lnb[:], lnb_d, writes=[lnb])
                Dg = k.sb("Dg", [128, 124, 128], BF16, s2)
                for c in range(4):
                    for j in range(31):
                        k.op(dve, lambda e, c=c, j=j: e.tensor_scalar(Dg[:, c * 31 + j, :], identf[:],
                                                                       wdw[:, c, j:j + 1], None, ALU.mult),
                             reads=[identf, wdw], writes=[Dg])
                ones = k.sb("ones", [128, 128], F32, s2)
                k.op(pool, lambda e: e.memset(ones[:], 1.0), writes=[ones])
                xt2 = [k.sb("xtB%d" % i, [128, D], F32, s2) for i in range(2)]
                xh2 = [k.sb("xhB%d" % i, [32, D], F32, s2) for i in range(2)]
                hT_L = [k.sb("hTB_%d" % i_, [128, 8, 128], BF16, s2) for i_ in range(2)]
                hTh_L = [k.sb("hThB_%d" % i_, [128, 8, 32], BF16, s2) for i_ in range(2)]
                qr_L = [k.sb("qr_%d" % i_, [128, 512], BF16, s2) for i_ in range(2)]
                qir_L = [k.sb("qir_%d" % i_, [128, 512], BF16, s2) for i_ in range(2)]
                tA_L = [k.sb("tA2_%d" % i_, [128, 512], F32, s2) for i_ in range(2)]
                tB_L = [k.sb("tB2_%d" % i_, [128, 512], F32, s2) for i_ in range(2)]
                qT_s_L = [k.sb("qT_s_%d" % i_, [128, 512], BF16, s2) for i_ in range(2)]
                qiT_s_L = [k.sb("qiT_s_%d" % i_, [128, 512], BF16, s2) for i_ in range(2)]
                sg_L = [k.sb("sgl_%d" % i_, [128, 160], F32, s2) for i_ in range(2)]
                gT_L = [k.sb("gT_%d" % i_, [128, 4, 160], BF16, s2) for i_ in range(2)]
                csb_L = [k.sb("csb_%d" % i_, [128, 4, 128], F32, s2) for i_ in range(2)]
                csq_L = [k.sb("csq_%d" % i_, [128, 4, 128], F32, s2) for i_ in range(2)]
                mean_L = [k.sb("mean_%d" % i_, [128, 128], F32, s2) for i_ in range(2)]
                msq_L = [k.sb("msq_%d" % i_, [128, 128], F32, s2) for i_ in range(2)]
                rstd_L = [k.sb("rstd_%d" % i_, [128, 128], F32, s2) for i_ in range(2)]
                nrm_L = [k.sb("nrm_%d" % i_, [128, 4, 128], F32, s2) for i_ in range(2)]
                snT_L = [k.sb("snT_%d" % i_, [128, 4, 128], BF16, s2) for i_ in range(2)]
                sgc_L = [k.sb("sgc_%d" % i_, [128, 8, 128], F32, s2) for i_ in range(2)]
                mc_s_L = [k.sb("mc_s_%d" % i_, [128, 8, 128], BF16, s2) for i_ in range(2)]
                sga_s_L = [k.sb("sga_s_%d" % i_, [128, 8, 128], BF16, s2) for i_ in range(2)]
                qTd_b = Buf("qTd_b")
                mcd_b = Buf("mcd_b")

                hbs2 = {}

                def pre2(j):
                    k.dma(sp, xt2[j % 2][:], x_own[j * 128:(j + 1) * 128, :], writes=[xt2[j % 2]])
                    k.dma(sp, xh2[j % 2][:], x_halo[j * 32:(j + 1) * 32, :], writes=[xh2[j % 2]])
                    hbs2[j] = (norm_pre(xh2[j % 2], 32), norm_pre(xt2[j % 2], 128))

                def post2(j):
                    a_, b_ = hbs2.pop(j)
                    norm_post(a_, 32, gmix, hTh_L[j % 2])
                    norm_post(b_, 128, gmix, hT_L[j % 2])

                for j in range(nslots):
                    xt = xt2[j % 2]
                    xh = xh2[j % 2]
                    hT, hTh, qr, qir, tA, tB, qT_s, qiT_s, sg, gT, csb, csq, mean, msq, rstd, nrm, snT, sgc, mc_s, sga_s = hT_L[j % 2], hTh_L[j % 2], qr_L[j % 2], qir_L[j % 2], tA_L[j % 2], tB_L[j % 2], qT_s_L[j % 2], qiT_s_L[j % 2], sg_L[j % 2], gT_L[j % 2], csb_L[j % 2], csq_L[j % 2], mean_L[j % 2], msq_L[j % 2], rstd_L[j % 2], nrm_L[j % 2], snT_L[j % 2], sgc_L[j % 2], mc_s_L[j % 2], sga_s_L[j % 2]
                    if j == 0:
                        pre2(0)
                        post2(0)
                        if nslots > 1:
                            pre2(1)
                    if j + 1 < nslots:
                        post2(j + 1)
                    pq = proj_tok(hT, wBq, WQ, 512)
                    rope(pq, 8, cosO, sinO, j, qr, tA, tB)
                    pqi = proj_tok(hT, wBq, WQI, 512)
                    rope(pqi, 8, cosO, sinO, j, qir, tA, tB)
                    pw = proj_tok(hT, wBq, WWI, 8)
                    k.op(act, lambda e: e.activation(wi_all[:, j * 8:(j + 1) * 8], pw[:, 0:8], AF.Copy,
                                                     scale=IDX_SCALE), reads=[pw], writes=[wi_all])
                    for src, dstb, dd in ((qr, qT_s, qT_d), (qir, qiT_s, qiT_d)):
                        tb = k.bank()
                        tbv = bf(tb[:])
                        for pr in range(4):
                            k.op(pe, lambda e, pr=pr, src=src, tbv=tbv: e.transpose(
                                tbv[:, pr * 128:(pr + 1) * 128], src[:, pr * 128:(pr + 1) * 128], ident[:]),
                                 reads=[src, ident], writes=[tb])
                        k.op(act, lambda e, dstb=dstb, tbv=tbv: e.activation(dstb[:], tbv[:, 0:512], AF.Copy),
                             reads=[tb], writes=[dstb])
                        k.dma(sp, dd[j], dstb[:], reads=[dstb], writes=[qTd_b], sembuf=dstb)
                    for c in range(4):
                        pu = k.bank()
                        puv = pu[:, 0:320].rearrange("p (a b) -> p a b", a=2)
                        for half, col0 in ((0, WU + c * 128), (1, WU + 512 + c * 128)):
                            for kc in range(8):
                                k.op(pe, lambda e, kc=kc, half=half, col0=col0, puv=puv: e.matmul(
                                    puv[:, half, 0:32], wB[:, kc, col0:col0 + 128], hTh[:, kc, :],
                                    start=(kc == 0), stop=(kc == 7)), reads=[wBu, hTh], writes=[pu])
                            for kc in range(8):
                                k.op(pe, lambda e, kc=kc, half=half, col0=col0, puv=puv: e.matmul(
                                    puv[:, half, 32:160], wB[:, kc, col0:col0 + 128], hT[:, kc, :],
                                    start=(kc == 0), stop=(kc == 7)), reads=[wBu, hT], writes=[pu])
                        k.op(act, lambda e, puv=puv: e.activation(sg[:], puv[:, 1, :], AF.Sigmoid),
                             reads=[pu], writes=[sg])
                        k.op(dve, lambda e, puv=puv, c=c: e.tensor_tensor(gT[:, c, :], puv[:, 0, :], sg[:], ALU.mult),
                             reads=[pu, sg], writes=[gT])
                    if j + 2 < nslots:
                        pre2(j + 2)
                    pc = k.bank()
                    pcv = pc[:, 0:512].rearrange("p (a b) -> p a b", a=4)
                    for c in range(4):
                        for jj in range(31):
                            k.op(pe, lambda e, c=c, jj=jj: e.matmul(pcv[:, c, :], Dg[:, c * 31 + jj, :],
                                                                    gT[:, c, 2 + jj:2 + jj + 128],
                                                                    start=(jj == 0), stop=(jj == 30)),
                                 reads=[Dg, gT], writes=[pc])
                    k.op(dve, lambda e: e.tensor_tensor(csb[:], pcv, bdw[:].unsqueeze(2).to_broadcast([128, 4, 128]),
                                                        ALU.add), reads=[pc, bdw], writes=[csb])
                    k.op(act, lambda e: e.activation(csq[:], csb[:], AF.Square), reads=[csb], writes=[csq])
                    pst = k.bank()
                    for c in range(4):
                        k.op(pe, lambda e, c=c: e.matmul(pst[:, 0:128], ones[:], csb[:, c, :], start=(c == 0),
                                                         stop=(c == 3)), reads=[ones, csb], writes=[pst])
                    for c in range(4):
                        k.op(pe, lambda e, c=c: e.matmul(pst[:, 128:256], ones[:], csq[:, c, :], start=(c == 0),
                                                         stop=(c == 3)), reads=[ones, csq], writes=[pst])
                    k.op(act, lambda e: e.activation(mean[:], pst[:, 0:128], AF.Copy, scale=1.0 / 512),
                         reads=[pst], writes=[mean])
                    k.op(dve, lambda e: e.tensor_tensor(msq[:], mean[:], mean[:], ALU.mult), reads=[mean], writes=[msq])
                    k.op(dve, lambda e: e.scalar_tensor_tensor(rstd[:], pst[:, 128:256], 1.0 / 512, msq[:],
                                                               ALU.mult, ALU.subtract),
                         reads=[pst, msq], writes=[rstd])
                    k.op(act, lambda e: e.activation(rstd[:], rstd[:], AF.Sqrt, bias=EPS, scale=1.0),
                         reads=[rstd], writes=[rstd])
                    k.op(dve, lambda e: e.reciprocal(rstd[:], rstd[:]), reads=[rstd], writes=[rstd])
                    k.op(dve, lambda e: e.tensor_tensor(nrm[:], csb[:], mean[:].unsqueeze(1).to_broadcast([128, 4, 128]),
                                                        ALU.subtract), reads=[csb, mean], writes=[nrm])
                    k.op(dve, lambda e: e.tensor_tensor(nrm[:], nrm[:], rstd[:].unsqueeze(1).to_broadcast([128, 4, 128]),
                                                        ALU.mult), reads=[nrm, rstd], writes=[nrm])
                    for c in range(4):
                        k.op(act, lambda e, c=c: e.activation(snT[:, c, :], nrm[:, c, :], AF.Silu,
                                                              bias=lnb[:, c:c + 1], scale=lng[:, c:c + 1]),
                             reads=[nrm, lnb, lng], writes=[snT])
                    for gi, (wcol, dst) in enumerate(((WGC, None), (WGA, sga_s))):
                        for hb_ in range(2):
                            pg = k.bank()
                            pgv = pg[:, 0:512].rearrange("p (a b) -> p a b", a=4)
                            for m in range(4):
                                col0 = wcol + (hb_ * 4 + m) * 128
                                for kc in range(8):
                                    k.op(pe, lambda e, kc=kc, m=m, col0=col0, pgv=pgv: e.matmul(
                                        pgv[:, m, :], wB[:, kc, col0:col0 + 128], hT[:, kc, :],
                                        start=(kc == 0), stop=(kc == 7)), reads=[wBg, hT], writes=[pg])
                            tgt = sgc if gi == 0 else sga_s
                            k.op(act, lambda e, pgv=pgv, tgt=tgt, hb_=hb_: e.activation(
                                tgt[:, hb_ * 4:(hb_ + 1) * 4, :], pgv, AF.Sigmoid), reads=[pg], writes=[tgt])
                    for hb_ in range(2):
                        py = k.bank()
                        pyv = py[:, 0:512].rearrange("p (a b) -> p a b", a=4)
                        for m in range(4):
                            mm = hb_ * 4 + m
                            for kc in range(4):
                                k.op(pe, lambda e, kc=kc, m=m, mm=mm, pyv=pyv: e.matmul(
                                    pyv[:, m, :], wco[:, kc, mm * 128:(mm + 1) * 128], snT[:, kc, :],
                                    start=(kc == 0), stop=(kc == 3)), reads=[wco, snT], writes=[py])
                        k.op(dve, lambda e, pyv=pyv, hb_=hb_: e.tensor_tensor(
                            mc_s[:, hb_ * 4:(hb_ + 1) * 4, :], pyv, sgc[:, hb_ * 4:(hb_ + 1) * 4, :], ALU.mult),
                             reads=[py, sgc], writes=[mc_s])
                    k.dma(sp, mc_d[j], mc_s[:].rearrange("p a b -> p (a b)"), reads=[mc_s], writes=[mcd_b], sembuf=mc_s)
                    k.dma(sp, sga_d[j], sga_s[:].rearrange("p a b -> p (a b)"), reads=[sga_s], writes=[mcd_b],
                          sembuf=sga_s)
                k.barrier()
        def dbg_A2():
            k.dma(sp, dbg["wi"], wi_all[:], reads=[wi_all])
            for nm, src in (("qT", qT_d), ("qiT", qiT_d), ("mc", mc_d), ("sga", sga_d)):
                b_ = Buf("dbgc_" + nm)
                k.dma(sp, dbg[nm], src, writes=[b_])
            k.barrier()

        def phase_B():
            with ExitStack() as s3:
                cm2 = [k.sb("cm%d" % i, [128, 256], F32, s3) for i in range(2)]
                pw2 = k.sb("pw2", [128, NIT + 2], F32, s3)
                for it in range(NIT + 2):
                    k.op(dve, lambda e, it=it: e.memset(pw2[:, it:it + 1], float(2.0 ** -(it + 1))), writes=[pw2])
                qT2 = [k.sb("qTb%d" % i, [128, 4, 256], BF16, s3) for i in range(2)]
                for b_ in qT2:
                    k.op(dve, lambda e: e.memset(b_[:], 0.0), writes=[b_])
                qiT2 = [k.sb("qiTb%d" % i, [128, 512], BF16, s3) for i in range(2)]
                scL = [k.sb("sc%d" % i, [128, S], F32, s3) for i in range(2)]
                AL = [k.sb("Ab%d" % i, [128, 512], BF16, s3) for i in range(4)]
                wabs = k.sb("wabs", [128, 8], F32, s3)
                sgn = k.sb("sgn", [128, 8], F32, s3)
                sgd = k.sb("sgd", [128, 8, 128], BF16, s3)
                msk = k.sb("msk", [128, S], BF16, s3)
                Mb = k.sb("Mb", [128, NB, 128], BF16, s3)
                st8 = k.sb("st8", [128, 8], F32, s3)
                Q = k.sb("Qtab", [128, NIT + 2], F32, s3)
                Q2 = k.sb("Q2tab", [128, NIT + 2], F32, s3)
                E2 = [k.sb("Eb%d" % i, [128, 4, 128], BF16, s3) for i in range(3)]
                rden = k.sb("rden", [128, 8], F32, s3)
                Osb = k.sb("Osb", [128, 2, 4, 66], F32, s3)
                thr = st8[:, 6:7]
                ai = [0]
                MBIG = 30000.0
                if debug:
                    attnf = k.sb("attnf", [128, 512], F32, s3)

                def loads_a(j):
                    p = j % 2
                    k.dma(sp, qiT2[p][:], qiT_d[j], writes=[qiT2[p]])
                    k.dma(sp, cm2[p][:], cmask_d[:, j, :], writes=[cm2[p]])

                def loads_b(j):
                    p = j % 2
                    k.dma(sp, qT2[p][0:64, :, 0:128], qT_d[j][0:64, :].rearrange("p (a b) -> p a b", a=4), writes=[qT2[p]])
                    k.dma(sp, qT2[p][64:128, :, 128:256], qT_d[j][64:128, :].rearrange("p (a b) -> p a b", a=4),
                          writes=[qT2[p]])

                def prep(j):
                    wsl = wi_all[:, j * 8:(j + 1) * 8]
                    k.op(dve, lambda e: e.tensor_scalar(sgn[:], wsl, 0.0, 2.0, ALU.is_ge, ALU.mult), reads=[wi_all], writes=[sgn])
                    k.op(dve, lambda e: e.tensor_scalar(sgn[:], sgn[:], -1.0, None, ALU.add), reads=[sgn], writes=[sgn])
                    k.op(dve, lambda e: e.tensor_tensor(wabs[:], wsl, sgn[:], ALU.mult), reads=[wi_all, sgn], writes=[wabs])
                    k.op(dve, lambda e: e.tensor_tensor(sgd[:], ident[:].unsqueeze(1).to_broadcast([128, 8, 128]),
                                                        sgn[:].unsqueeze(2).to_broadcast([128, 8, 128]), ALU.mult),
                         reads=[ident, sgn], writes=[sgd])

                def indexer(j):
                    sc = scL[j % 2]
                    qiTs = qiT2[j % 2]
                    L = 256 * (j + 1)
                    nch = (L + 511) // 512
                    for cc in range(nch):
                        c0 = cc * 512
                        W = min(512, L - c0)
                        pacc = k.bank(pin=True)
                        pbs = {}

                        def qi_mm(h):
                            pb = k.bank()
                            pp = (h % 2) * 64
                            k.op(pe, lambda e: e.matmul(pb[:, 0:W], qiTs[pp:pp + 64, (h // 2) * 128:(h // 2 + 1) * 128],
                                                        kiT[pp:pp + 64, c0:c0 + W], start=True, stop=True),
                                 reads=[qiTs, kiT], writes=[pb])
                            pbs[h] = pb

                        qi_mm(0)
                        qi_mm(1)
                        qi_mm(2)
                        for h in range(8):
                            if h + 3 < 8:
                                qi_mm(h + 3)
                            pb = pbs[h]
                            A = AL[ai[0] % 4]
                            ai[0] += 1
                            k.op(act, lambda e: e.activation(A[:, 0:W], pb[:, 0:W], AF.Relu, scale=wabs[:, h:h + 1]),
                                 reads=[pb, wabs], writes=[A])
                            k.op(pe, lambda e: e.matmul(pacc[:, 0:W], sgd[:, h, :], A[:, 0:W], start=(h == 0), stop=(h == 7)),
                                 reads=[sgd, A], writes=[pacc])
                        k.op(act, lambda e: e.activation(sc[:, c0:c0 + W], pacc[:, 0:W], AF.Copy), reads=[pacc], writes=[sc])
                        k.unpin(pacc)

                def thresh(j):
                    sc = scL[j % 2]
                    L = 256 * (j + 1)
                    cmask = cm2[j % 2]
                    if j == 0:
                        k.op(dve, lambda e: e.tensor_tensor(sc[:, L - 256:L], sc[:, L - 256:L], cmask[:], ALU.add),
                             reads=[sc, cmask], writes=[sc])
                        k.op(dve, lambda e: e.memset(thr, -1.0e29), writes=[st8])
                        return
                    k.op(dve, lambda e: e.tensor_reduce(st8[:, 0:1], sc[:, 0:L], AX.X, ALU.max), reads=[sc], writes=[st8])
                    k.op(dve, lambda e: e.tensor_reduce(st8[:, 1:2], sc[:, 0:L], AX.X, ALU.min), reads=[sc], writes=[st8])
                    k.op(dve, lambda e: e.tensor_tensor(sc[:, L - 256:L], sc[:, L - 256:L], cmask[:], ALU.add),
                         reads=[sc, cmask], writes=[sc])
                    k.op(dve, lambda e: e.tensor_tensor(st8[:, 2:3], st8[:, 0:1], st8[:, 1:2], ALU.subtract),
                         reads=[st8], writes=[st8])
                    k.op(dve, lambda e: e.tensor_scalar(st8[:, 5:6], st8[:, 2:3], float(2.0 ** -10), None, ALU.mult),
                         reads=[st8], writes=[st8])
                    k.op(dve, lambda e: e.tensor_scalar(st8[:, 2:3], st8[:, 2:3], float(1.0 + 2.0 ** -9), 1e-30,
                                                        ALU.mult, ALU.add), reads=[st8], writes=[st8])
                    k.op(dve, lambda e: e.tensor_tensor(st8[:, 7:8], st8[:, 1:2], st8[:, 5:6], ALU.subtract),
                         reads=[st8], writes=[st8])
                    k.op(dve, lambda e: e.tensor_scalar(Q[:], pw2[:], st8[:, 2:3], None, ALU.mult),
                         reads=[pw2, st8], writes=[Q])
                    k.op(dve, lambda e: e.tensor_scalar(Q2[:], Q[:], 2.0, None, ALU.mult), reads=[Q], writes=[Q2])
                    k.op(dve, lambda e: e.tensor_tensor(st8[:, 3:4], st8[:, 7:8], Q[:, 0:1], ALU.add),
                         reads=[st8, Q], writes=[st8])
                    for it in range(NIT):
                        k.op(dve, lambda e: e.memset(st8[:, 4:5], 0.0), writes=[st8])
                        k.op(dve, lambda e: e.tensor_scalar(msk[:, 0:L], sc[:, 0:L], st8[:, 3:4], 0.0,
                                                            ALU.is_ge, ALU.add, accum_out=st8[:, 4:5]),
                             reads=[sc, st8], writes=[st8, msk])
                        k.op(dve, lambda e: e.tensor_scalar(st8[:, 5:6], st8[:, 4:5], TOPK - 0.5,
                                                            Q2[:, it + 1:it + 2], ALU.is_ge, ALU.mult),
                             reads=[st8, Q2], writes=[st8])
                        k.op(dve, lambda e: e.scalar_tensor_tensor(st8[:, 3:4], st8[:, 3:4], Q[:, it + 1:it + 2],
                                                                   st8[:, 5:6], ALU.subtract, ALU.add),
                             reads=[st8, Q], writes=[st8])
                    k.op(dve, lambda e: e.tensor_tensor(thr, st8[:, 3:4], Q[:, NIT:NIT + 1], ALU.subtract),
                         reads=[st8, Q], writes=[st8])

                def mask(j):
                    sc = scL[j % 2]
                    L = 256 * (j + 1)
                    nkb = 2 * (j + 1)
                    if debug:
                        b_ = Buf("dbg_sc%d" % j)
                        for c0_ in range(0, L, 1024):
                            c1_ = min(L, c0_ + 1024)
                            k.dma(sp, dbg["sc"][j][:, c0_:c1_], sc[:, c0_:c1_], reads=[sc], writes=[b_], sembuf=b_)
                        k.dma(sp, dbg["thr"][j], thr, reads=[st8], writes=[b_], sembuf=b_)
                    for g0 in range(0, nkb, 8):
                        gn = min(8, nkb - g0)
                        k.op(dve, lambda e: e.tensor_scalar(msk[:, g0 * 128:(g0 + gn) * 128],
                                                            sc[:, g0 * 128:(g0 + gn) * 128], thr, None, ALU.is_ge),
                             reads=[sc, st8], writes=[msk])
                        tb = k.bank()
                        tbv = bf(tb[:])
                        for q_ in range(gn):
                            k.op(pe, lambda e: e.transpose(tbv[:, q_ * 128:(q_ + 1) * 128],
                                                           msk[:, (g0 + q_) * 128:(g0 + q_ + 1) * 128], ident[:]),
                                 reads=[msk, ident], writes=[tb])
                        k.op(act, lambda e: e.activation(Mb[:, g0:g0 + gn, :],
                                                         tbv[:, 0:gn * 128].rearrange("p (a b) -> p a b", a=gn),
                                                         AF.Identity, bias=-MBIG, scale=MBIG),
                             reads=[tb], writes=[Mb])

                def attention(j):
                    qTs = qT2[j % 2]
                    nkb = 2 * (j + 1)
                    po = [k.bank(pin=True), k.bank(pin=True)]
                    pov = [b_[:, 0:512].rearrange("p (h d) -> p h d", h=4) for b_ in po]
                    units = [(kb, hg) for kb in range(nkb) for hg in range(2)]

                    def qk(u):
                        kb, hg = units[u]
                        pl = k.bank()
                        plv = pl[:, 0:512].rearrange("p (a b) -> p a b", a=4)
                        k.op(pe, lambda e: e.matmul(plv, ident[:], Mb[:, kb, :].unsqueeze(1).to_broadcast([128, 4, 128]),
                                                    start=True, stop=False), reads=[ident, Mb], writes=[pl])
                        for pi in range(2):
                            pr = hg * 2 + pi
                            k.op(pe, lambda e: e.matmul(pl[:, pi * 256:(pi + 1) * 256], KT[:, pr, kb * 128:(kb + 1) * 128],
                                                        qTs[:, pr, :], start=False, stop=(pi == 1)),
                                 reads=[KT, qTs], writes=[pl])
                        return pl, plv

                    LA = 2
                    pend = [qk(u) for u in range(min(LA, len(units)))]
                    for u in range(len(units)):
                        if u + LA < len(units):
                            pend.append(qk(u + LA))
                        kb, hg = units[u]
                        pl, plv = pend.pop(0)
                        E = E2[u % 3]
                        k.op(act, lambda e: e.activation(E[:], plv, AF.Exp, scale=ATT_SCALE), reads=[pl], writes=[E])
                        for hh in range(4):
                            h = hg * 4 + hh
                            k.op(pe, lambda e: e.matmul(pov[hg][:, hh, 0:66], E[:, hh, :], V[:, kb, h * 66:(h + 1) * 66],
                                                        start=(kb == 0 and hh == 0), stop=(kb == nkb - 1 and hh == 3)),
                                 reads=[E, V], writes=[po[hg]])
                    for hg in range(2):
                        k.op(act, lambda e: e.activation(Osb[:, hg, :, :], pov[hg][:, :, 0:66], AF.Copy),
                             reads=[po[hg]], writes=[Osb])
                        k.unpin(po[hg])

                def finalize(j):
                    for hg in range(2):
                        k.op(dve, lambda e: e.reciprocal(rden[:, hg * 4:(hg + 1) * 4].unsqueeze(2), Osb[:, hg, :, 64:65]),
                             reads=[Osb], writes=[rden])
                        k.op(dve, lambda e: e.tensor_tensor(
                            attn_all[:, j, hg * 256:(hg + 1) * 256].rearrange("p (h d) -> p h d", h=4), Osb[:, hg, :, 0:64],
                            rden[:, hg * 4:(hg + 1) * 4].unsqueeze(2).to_broadcast([128, 4, 64]), ALU.mult),
                             reads=[Osb, rden], writes=[attn_all])
                    if debug:
                        k.op(dve, lambda e: e.tensor_copy(attnf[:], attn_all[:, j, :]), reads=[attn_all], writes=[attnf])
                        k.dma(sp, dbg["attn"][j], attnf[:], reads=[attnf])

                loads_a(0)
                loads_b(0)
                if nslots > 1:
                    loads_a(1)
                    loads_b(1)
                prep(0)
                indexer(0)
                thresh(0)
                mask(0)
                if nslots > 1:
                    prep(1)
                    indexer(1)
                for i in range(nslots):
                    if i + 2 < nslots:
                        prep(i + 2)
                    if i + 1 < nslots:
                        thresh(i + 1)
                    attention(i)
                    if i + 2 < nslots:
                        loads_a(i + 2)
                        indexer(i + 2)
                    if i + 1 < nslots:
                        mask(i + 1)
                    finalize(i)
                    if i + 2 < nslots:
                        loads_b(i + 2)
                k.barrier()

        def phase_B2():
            with ExitStack() as s3:
                if "C" in phases:
                    wload(0)
                    wload(1)
                mc2 = [k.sb("mcb%d" % i, [128, 8, 128], BF16, s3) for i in range(2)]
                sga2 = [k.sb("sgab%d" % i, [128, 8, 128], BF16, s3) for i in range(2)]
                xoL = [k.sb("xob%d" % i, [128, D], F32, s3) for i in range(2)]
                attnTL = [k.sb("attnT%d" % i, [128, 4, 128], BF16, s3) for i in range(2)]
                mTL = [k.sb("mT%d" % i, [128, 8, 128], BF16, s3) for i in range(2)]
                x1L = [k.sb("x1_%d" % i, [128, D], F32, s3) for i in range(2)]
                lgA = k.sb("lgA", [128, NS, 20], F32, s3)
                rA = k.sb("rA", [128, 8, NS], F32, s3)
                dG = k.sb("dG", [128, NS, 4], F32, s3)
                eG = k.sb("eG", [128, NS, 4], F32, s3)
                ohg1 = k.sb("ohg1", [128, NS, 4], F32, s3)
                ohgp = k.sb("ohgp", [128, NS, 4], F32, s3)
                t16A = k.sb("t16A", [128, NS, 16], F32, s3)
                eselA = k.sb("eselA", [128, NS, 4], F32, s3)
                oh1A = k.sb("oh1A", [128, NS, 4], F32, s3)
                oh2A = k.sb("oh2A", [128, NS, 4], F32, s3)
                emA = k.sb("emA", [128, NS, 4], F32, s3)
                x1d_b = Buf("x1d_b")

                def loads_m(j):
                    p = j % 2
                    k.dma(sp, mc2[p][:].rearrange("p a b -> p (a b)"), mc_d[j], writes=[mc2[p]])
                    k.dma(sp, sga2[p][:].rearrange("p a b -> p (a b)"), sga_d[j], writes=[sga2[p]])

                def loads_x(j):
                    p = j % 2
                    k.dma(sp, xoL[p][:], x_own[j * 128:(j + 1) * 128, :], writes=[xoL[p]])

                def s1(j):
                    attnT = attnTL[j % 2]
                    tb = k.bank()
                    tbv = bf(tb[:])
                    for q_ in range(4):
                        k.op(pe, lambda e: e.transpose(tbv[:, q_ * 128:(q_ + 1) * 128],
                                                       attn_all[:, j, q_ * 128:(q_ + 1) * 128], ident[:]),
                             reads=[attn_all, ident], writes=[tb])
                    k.op(act, lambda e: e.activation(attnT[:], tbv[:, 0:512].rearrange("p (a b) -> p a b", a=4), AF.Copy),
                         reads=[tb], writes=[attnT])

                def s2(j):
                    mcs, sgas = mc2[j % 2], sga2[j % 2]
                    x1, mT, attnT = x1L[j % 2], mTL[j % 2], attnTL[j % 2]
                    x1v = x1[:].rearrange("p (a b) -> p a b", a=8)
                    for hb_ in range(2):
                        py = k.bank()
                        pyv = py[:, 0:512].rearrange("p (a b) -> p a b", a=4)
                        for m in range(4):
                            mm = hb_ * 4 + m
                            for kc in range(4):
                                k.op(pe, lambda e: e.matmul(pyv[:, m, :], wao[:, kc, mm * 128:(mm + 1) * 128], attnT[:, kc, :],
                                                            start=(kc == 0), stop=(kc == 3)), reads=[wao, attnT], writes=[py])
                        k.op(dve, lambda e: e.tensor_tensor(x1v[:, hb_ * 4:(hb_ + 1) * 4, :], pyv,
                                                            sgas[:, hb_ * 4:(hb_ + 1) * 4, :], ALU.mult),
                             reads=[py, sgas], writes=[x1])
                    k.op(dve, lambda e: e.tensor_tensor(mT[:], x1v, mcs[:], ALU.add), reads=[x1, mcs], writes=[mT])

                def s3(j):
                    xo, mT = xoL[j % 2], mTL[j % 2]
                    for half in range(2):
                        px = k.bank()
                        for kc in range(8):
                            k.op(pe, lambda e: e.matmul(px[:, 0:512], mT[:, kc, :], wo[:, kc, half * 512:(half + 1) * 512],
                                                        start=(kc == 0), stop=(kc == 7)), reads=[mT, wo], writes=[px])
                        k.op(dve, lambda e: e.tensor_tensor(acc[:, j, half * 512:(half + 1) * 512], px[:, 0:512],
                                                            xo[:, half * 512:(half + 1) * 512], ALU.add),
                             reads=[px, xo], writes=[acc])
                    if debug:
                        k.dma(sp, dbg["x1"][j], acc[:, j, :], reads=[acc], sembuf=x1L[j % 2])
                    hbsT[j] = norm_pre(acc, 128, acc[:, j, :])

                def tailB(j):
                    norm_post(hbsT.pop(j), 128, gffn, h2T, h2T[:, :, j * 128:(j + 1) * 128])
                    pr_ = k.bank()
                    for kc in range(8):
                        k.op(pe, lambda e: e.matmul(pr_[:, 0:20], h2T[:, kc, j * 128:(j + 1) * 128], wr[:, kc, :],
                                                    start=(kc == 0), stop=(kc == 7)), reads=[h2T, wr], writes=[pr_])
                    k.op(dve, lambda e: e.tensor_tensor(lgA[:, j, :], pr_[:, 0:20], br[:], ALU.add),
                         reads=[pr_, br], writes=[lgA])

                def router_all():
                    n = nslots
                    G = lgA[:, 0:n, 0:4]
                    Ex = lgA[:, 0:n, 4:20].rearrange("p n (g e) -> p n g e", g=4)
                    gmax, gsum, pg, m1, m2, w1, w2 = (rA[:, i, 0:n] for i in range(7))
                    bc4 = lambda ap: ap.unsqueeze(2).to_broadcast([128, n, 4])
                    k.op(dve, lambda e: e.tensor_reduce(gmax, G, AX.X, ALU.max), reads=[lgA], writes=[rA])
                    k.op(dve, lambda e: e.tensor_tensor(dG[:, 0:n, :], G, bc4(gmax), ALU.subtract), reads=[lgA, rA], writes=[dG])
                    k.op(act, lambda e: e.activation(eG[:, 0:n, :], dG[:, 0:n, :], AF.Exp), reads=[dG], writes=[eG])
                    k.op(dve, lambda e: e.tensor_reduce(gsum, eG[:, 0:n, :], AX.X, ALU.add), reads=[eG], writes=[rA])
                    k.op(dve, lambda e: e.reciprocal(pg, gsum), reads=[rA], writes=[rA])
                    k.op(dve, lambda e: e.tensor_scalar(ohg1[:, 0:n, :], dG[:, 0:n, :], 0.0, None, ALU.is_ge), reads=[dG], writes=[ohg1])
                    k.op(dve, lambda e: e.tensor_tensor(ohgp[:, 0:n, :], ohg1[:, 0:n, :], bc4(pg), ALU.mult),
                         reads=[ohg1, rA], writes=[ohgp])
                    t4 = t16A[:, 0:n, :].rearrange("p n (g e) -> p n g e", g=4)
                    k.op(dve, lambda e: e.tensor_tensor(t4, Ex, ohg1[:, 0:n, :].unsqueeze(3).to_broadcast([128, n, 4, 4]), ALU.mult),
                         reads=[lgA, ohg1], writes=[t16A])
                    k.op(dve, lambda e: e.tensor_reduce(eselA[:, 0:n, :], t16A[:, 0:n, :].rearrange("p n (g e) -> p n e g", g=4),
                                                        AX.X, ALU.add), reads=[t16A], writes=[eselA])
                    k.op(dve, lambda e: e.tensor_reduce(m1, eselA[:, 0:n, :], AX.X, ALU.max), reads=[eselA], writes=[rA])
                    k.op(dve, lambda e: e.tensor_tensor(dG[:, 0:n, :], eselA[:, 0:n, :], bc4(m1), ALU.subtract),
                         reads=[eselA, rA], writes=[dG])
                    k.op(dve, lambda e: e.tensor_scalar(oh1A[:, 0:n, :], dG[:, 0:n, :], 0.0, None, ALU.is_ge), reads=[dG], writes=[oh1A])
                    k.op(dve, lambda e: e.scalar_tensor_tensor(emA[:, 0:n, :], oh1A[:, 0:n, :], NEG, eselA[:, 0:n, :],
                                                               ALU.mult, ALU.add), reads=[oh1A, eselA], writes=[emA])
                    k.op(dve, lambda e: e.tensor_reduce(m2, emA[:, 0:n, :], AX.X, ALU.max), reads=[emA], writes=[rA])
                    k.op(dve, lambda e: e.tensor_tensor(dG[:, 0:n, :], emA[:, 0:n, :], bc4(m2), ALU.subtract),
                         reads=[emA, rA], writes=[dG])
                    k.op(dve, lambda e: e.tensor_scalar(oh2A[:, 0:n, :], dG[:, 0:n, :], 0.0, None, ALU.is_ge), reads=[dG], writes=[oh2A])
                    k.op(dve, lambda e: e.tensor_tensor(w2, m1, m2, ALU.subtract), reads=[rA], writes=[rA])
                    k.op(act, lambda e: e.activation(w1, w2, AF.Sigmoid), reads=[rA], writes=[rA])
                    k.op(dve, lambda e: e.tensor_scalar(w2, w1, -1.0, 1.0, ALU.mult, ALU.add), reads=[rA], writes=[rA])
                    k.op(dve, lambda e: e.tensor_tensor(oh1A[:, 0:n, :], oh1A[:, 0:n, :], bc4(w1), ALU.mult),
                         reads=[oh1A, rA], writes=[oh1A])
                    k.op(dve, lambda e: e.tensor_tensor(oh2A[:, 0:n, :], oh2A[:, 0:n, :], bc4(w2), ALU.mult),
                         reads=[oh2A, rA], writes=[oh2A])
                    k.op(dve, lambda e: e.tensor_tensor(oh1A[:, 0:n, :], oh1A[:, 0:n, :], oh2A[:, 0:n, :], ALU.add),
                         reads=[oh1A, oh2A], writes=[oh1A])
                    k.op(dve, lambda e: e.tensor_tensor(
                        gw_all[:, 0:n * 16].rearrange("p (n g e) -> p n g e", g=4, e=4),
                        ohgp[:, 0:n, :].unsqueeze(3).to_broadcast([128, n, 4, 4]),
                        oh1A[:, 0:n, :].unsqueeze(2).to_broadcast([128, n, 4, 4]), ALU.mult),
                         reads=[ohgp, oh1A], writes=[gw_all])

                hbsT = {}
                for j in range(min(2, nslots)):
                    loads_m(j)
                    loads_x(j)
                for it in range(nslots + 3):
                    if 0 <= it - 3 < nslots:
                        tailB(it - 3)
                    if 0 <= it - 2 < nslots:
                        s3(it - 2)
                        if it < nslots:
                            loads_x(it)
                    if 0 <= it - 1 < nslots:
                        s2(it - 1)
                        if it + 1 < nslots:
                            loads_m(it + 1)
                    if it < nslots:
                        s1(it)
                router_all()
                k.barrier()
        def phase_C():
            with ExitStack() as s4:
                gfin = k.sb("gfin", [128, D], F32, s4)
                k.dma(sp, gfin[:], gfin_d, writes=[gfin])
                sgl2 = [k.sb("sgm%d" % i, [128, 512], F32, s4) for i in range(2)]
                hid2 = [k.sb("hid%d" % i, [128, 2, 512], BF16, s4) for i in range(2)]
                ot2 = [k.sb("ot%d" % i, [128, D], F32, s4) for i in range(4)]

                ngrp = (nslots + 3) // 4
                wload(0)
                hi = [0]

                def gu(ex, tg):
                    wg, wu = wg2[ex % 2], wu2[ex % 2]
                    ns_ = min(4, nslots - tg * 4)
                    N = ns_ * 128
                    hid = hid2[hi[0] % 2]
                    hi[0] += 1
                    for fc in range(2):
                        pg = k.bank()
                        pu = k.bank()
                        for kc in range(8):
                            k.op(pe, lambda e, kc=kc, fc=fc, pg=pg: e.matmul(
                                pg[:, 0:N], wg[:, kc, fc * 128:(fc + 1) * 128], h2T[:, kc, tg * 512:tg * 512 + N],
                                start=(kc == 0), stop=(kc == 7)), reads=[wg, h2T], writes=[pg])
                        for kc in range(8):
                            k.op(pe, lambda e, kc=kc, fc=fc, pu=pu: e.matmul(
                                pu[:, 0:N], wu[:, kc, fc * 128:(fc + 1) * 128], h2T[:, kc, tg * 512:tg * 512 + N],
                                start=(kc == 0), stop=(kc == 7)), reads=[wu, h2T], writes=[pu])
                        sgl = sgl2[fc]
                        k.op(act, lambda e, pg=pg, sgl=sgl: e.activation(sgl[:, 0:N], pg[:, 0:N], AF.Silu),
                             reads=[pg], writes=[sgl])
                        k.op(dve, lambda e, pu=pu, sgl=sgl, fc=fc, hid=hid: e.tensor_tensor(
                            hid[:, fc, 0:N], pu[:, 0:N], sgl[:, 0:N], ALU.mult), reads=[pu, sgl], writes=[hid])
                    return hid

                ss4L = [k.sb("fss%d" % i, [128, 4], F32, s4) for i in range(2)]
                rs4L = [k.sb("frs%d" % i, [128, 4], F32, s4) for i in range(2)]

                def fin_stats(tg):
                    ss4 = ss4L[tg % 2]
                    junk = hbL[0]
                    k.op(dve, lambda e: e.memset(ss4[:], 0.0), writes=[ss4])
                    for s_ in range(min(4, nslots - tg * 4)):
                        j = tg * 4 + s_
                        k.op(act, lambda e, j=j, s_=s_: e.activation(junk[:], acc[:, j, :], AF.Square,
                                                                     accum_out=ss4[:, s_:s_ + 1]),
                             reads=[acc, ss4], writes=[junk, ss4])
                    rs4 = rs4L[tg % 2]
                    k.op(act, lambda e: e.activation(rs4[:], ss4[:], AF.Sqrt, bias=EPS, scale=1.0 / D),
                         reads=[ss4], writes=[rs4])

                def fin_apply(tg):
                    rs4 = rs4L[tg % 2]
                    k.op(dve, lambda e: e.reciprocal(rs4[:], rs4[:]), reads=[rs4], writes=[rs4])
                    for s_ in range(min(4, nslots - tg * 4)):
                        j = tg * 4 + s_
                        ot = ot2[j % 4]
                        k.op(dve, lambda e, j=j, ot=ot, s_=s_: e.scalar_tensor_tensor(
                            ot[:], acc[:, j, :], rs4[:, s_:s_ + 1], gfin[:], ALU.mult, ALU.mult),
                             reads=[acc, rs4, gfin], writes=[ot])
                        k.dma(sp, out_d[j * 128:(j + 1) * 128, :], ot[:], reads=[ot], sembuf=ot)

                def down(ex, tg, hid):
                    wd = wd2[ex % 2]
                    ns_ = min(4, nslots - tg * 4)
                    for s_ in range(ns_):
                        j = tg * 4 + s_
                        for half in range(2):
                            py = k.bank()
                            for fc in range(2):
                                k.op(pe, lambda e, fc=fc, half=half, py=py, s_=s_, hid=hid: e.matmul(
                                    py[:, 0:512], hid[:, fc, s_ * 128:(s_ + 1) * 128],
                                    wd[:, fc, half * 512:(half + 1) * 512], start=(fc == 0), stop=(fc == 1)),
                                     reads=[hid, wd], writes=[py])
                            k.op(dve, lambda e, py=py, j=j, half=half, ex=ex: e.scalar_tensor_tensor(
                                acc[:, j, half * 512:(half + 1) * 512], py[:, 0:512],
                                gw_all[:, j * 16 + ex:j * 16 + ex + 1], acc[:, j, half * 512:(half + 1) * 512],
                                ALU.mult, ALU.add), reads=[py, gw_all, acc], writes=[acc])
                    if ex == 15:
                        fin_stats(tg)
                        if tg >= 1:
                            fin_apply(tg - 1)
                        if tg == ngrp - 1:
                            fin_apply(tg)

                prev = None
                for ex in range(16):
                    for tg in range(ngrp):
                        h_ = gu(ex, tg)
                        if tg == ngrp - 1 and ex + 2 < 16:
                            wload_gu(ex + 2)
                        if prev is not None:
                            down(*prev)
                        if tg == 0 and ex + 1 < 16:
                            wload_d(ex + 1)
                        prev = (ex, tg, h_)
                down(*prev)
                k.barrier()
        if "A2" in phases:
            phase_A2()
            if debug:
                dbg_A2()
        attn_all = k.sb("attn_all", [128, NS, 512], BF16)
        wao = k.sb("wao", [128, 4, D], BF16)
        wo = k.sb("wo", [128, 8, D], BF16)
        wr = k.sb("wr", [128, 8, 20], BF16)
        br = k.sb("br", [128, 20], F32)

        def load_tail_weights():
            for kc in range(4):
                k.dma(pool, wao[:, kc, :], wao_d[kc * 128:(kc + 1) * 128, :], writes=[wao])
            for kc in range(8):
                k.dma(pool, wo[:, kc, :], wo_d[kc * 128:(kc + 1) * 128, :], writes=[wo])
            k.dma(pool, wr[:], wr_d.rearrange("(k p) c -> p k c", p=128), writes=[wr])
            k.dma(sp, br[:], br_d, writes=[br])

        wloaded = set()

        def wload_gu(ex):
            if ("gu", ex) in wloaded:
                return
            wloaded.add(("gu", ex))
            p = ex % 2
            k.dma(pool, wg2[p][:], wg_d[ex].rearrange("(k p) c -> p k c", p=128), writes=[wg2[p]])
            k.dma(pool, wu2[p][:], wu_d[ex].rearrange("(k p) c -> p k c", p=128), writes=[wu2[p]])

        def wload_d(ex):
            if ("d", ex) in wloaded:
                return
            wloaded.add(("d", ex))
            p = ex % 2
            k.dma(pool, wd2[p][:], wd_d[ex].rearrange("(k p) c -> p k c", p=128), writes=[wd2[p]])

        def wload(ex):
            wload_gu(ex)
            wload_d(ex)

        with ExitStack() as skv:
            KT = k.sb("KT", [128, 4, S], BF16, skv)
            V = k.sb("V", [128, NB, 8 * 66], BF16, skv)
            kiT = k.sb("kiT", [128, S], BF16, skv)
            if "A1" not in phases:
                k.op(pool, lambda e: e.memset(V[:], 1.0), writes=[V])
            if "A1" in phases:
                phase_A1()
                if debug:
                    dbg_A1()
            if "B" in phases:
                load_tail_weights()
                phase_B()
            k.barrier()
        wg2 = [k.sb("wg%d" % i, [128, 8, 256], BF16) for i in range(2)]
        wu2 = [k.sb("wu%d" % i, [128, 8, 256], BF16) for i in range(2)]
        wd2 = [k.sb("wd%d" % i, [128, 2, D], BF16) for i in range(2)]
        acc = k.sb("acc", [128, NS, D], F32)
        h2T = k.sb("h2T", [128, 8, NS * 128], BF16)
        if "B" in phases:
            phase_B2()
            if debug:
                k.dma(sp, dbg["gw"], gw_all[:], reads=[gw_all])
        if "C" in phases:
            phase_C()
        k.barrier()
        build_nc.nins = k.nins
    return nc


def own_blocks(r):
    return [2 * j + ((j % 2) ^ r) for j in range(NS)]


def prep_core(inp, c):
    b, r = c // 2, c % 2
    x = np.asarray(inp["x"], dtype=np.float32)
    pos = np.asarray(inp["positions"]).astype(np.int32)
    blocks = own_blocks(r)
    xb = x[b]
    x_own = np.concatenate([xb[i * 128:(i + 1) * 128] for i in blocks], axis=0)
    x_halo = np.zeros((NS * 32, D), np.float32)
    for j, i in enumerate(blocks):
        if i > 0:
            x_halo[j * 32:(j + 1) * 32] = xb[i * 128 - 32:i * 128]
    pos_full = np.ascontiguousarray(pos[b].reshape(NB, 128).T)
    pos_own = np.ascontiguousarray(np.stack([pos[b][i * 128:(i + 1) * 128] for i in blocks], axis=1))
    invf = (1.0 / (np.float32(10000.0) ** (np.arange(0, 64, 2, dtype=np.float32) / np.float32(64)))).astype(np.float32)
    invf = np.ascontiguousarray(np.broadcast_to(invf[None, :], (128, 32)))
    cmask = np.zeros((128, NS, 256), np.float32)
    for j, i in enumerate(blocks):
        qidx = i * 128 + np.arange(128)[:, None]
        kidx = 256 * j + np.arange(256)[None, :]
        cmask[:, j, :] = np.where(kidx > qidx, np.float32(NEG), np.float32(0.0))

    def fm(v, nchunk):
        return np.ascontiguousarray(np.asarray(v, np.float32).reshape(nchunk, 128).T)

    w_dw = np.asarray(inp["w_dw"], np.float32)[0, :, 0, :]
    wdw = np.ascontiguousarray(w_dw.T.reshape(4, 128, 31).transpose(1, 0, 2))
    w_r = np.concatenate([np.asarray(inp["w_rg"], np.float32)[0],
                          np.asarray(inp["w_re"], np.float32)[0].reshape(D, 16)], axis=1)
    b_r = np.concatenate([np.asarray(inp["b_rg"], np.float32)[0], np.asarray(inp["b_re"], np.float32)[0].reshape(16)])
    m = {
        "x_full": np.ascontiguousarray(xb), "x_own": x_own, "x_halo": x_halo,
        "pos_full": pos_full, "pos_own": pos_own, "invf": invf, "cmask": cmask,
        "ident": np.eye(128, dtype=np.float32),
        "w_in": np.ascontiguousarray(np.asarray(inp["w_in"], np.float32)[0]),
        "wdw": wdw, "bdw": fm(inp["b_dw"][0], 4), "lng": fm(inp["ln_g"][0], 4), "lnb": fm(inp["ln_b"][0], 4),
        "gmix": fm(inp["g_mix"][0], 8), "gffn": fm(inp["g_ffn"][0], 8),
        "gfin": np.ascontiguousarray(np.broadcast_to(np.asarray(inp["g_final"], np.float32)[None, :], (128, D))),
        "b_r": np.ascontiguousarray(np.broadcast_to(b_r[None, :], (128, 20))),
        "w_r": np.ascontiguousarray(w_r),
        "w_conv_out": np.ascontiguousarray(np.asarray(inp["w_conv_out"], np.float32)[0]),
        "w_attn_out": np.ascontiguousarray(np.asarray(inp["w_attn_out"], np.float32)[0]),
        "w_o": np.ascontiguousarray(np.asarray(inp["w_o"], np.float32)[0]),
        "w_gate": np.ascontiguousarray(np.asarray(inp["w_gate"], np.float32)[0]),
        "w_up": np.ascontiguousarray(np.asarray(inp["w_up"], np.float32)[0]),
        "w_down": np.ascontiguousarray(np.asarray(inp["w_down"], np.float32)[0]),
    }
    return m


_NC_CACHE = {}


def kernel(**inputs):
    if "nc" not in _NC_CACHE:
        _NC_CACHE["nc"] = build_nc()
    nc = _NC_CACHE["nc"]
    in_maps = [prep_core(inputs, c) for c in range(8)]
    res = run_bass_kernel_spmd(nc, in_maps, core_ids=list(range(8)))
    out = np.empty((4, S, D), np.float32)
    for c in range(8):
        b, r = c // 2, c % 2
        o = np.asarray(res.results[c]["out"], dtype=np.float32)
        for j, i in enumerate(own_blocks(r)):
            out[b, i * 128:(i + 1) * 128] = o[j * 128:(j + 1) * 128]
    return out
```
